# Optimizing a Trainium2 kernel written in Bass

```python
import jax
import jax.numpy as jnp
from jax import lax
import numpy as np

D_MODEL = 1024
BATCH = 2
SEQ = 8192
DEPTH = 2

GRID_W = 64
CTX_LEN = 256
EPS = 1e-6
ROPE_BASE = 10000.0

MLA_HEADS = 8
MLA_NOPE = 64
MLA_ROPE = 32
MLA_V = 64
MLA_Q_RANK = 256
MLA_KV_RANK = 128
Q_BLOCK = 128

RET_HEADS = 4
RET_DK = 64
RET_DV = 128
RET_CHUNK = 128

GLA_HEADS = 4
GLA_DK = 64
GLA_DV = 128
GLA_GATE_RANK = 16
GLA_TAU = 16.0
GLA_CHUNK = 64

N_BRANCH = 3
BRANCH_W = 512

D_FF = 2816
N_EXPERTS = 8
TOP_K = 2
N_DENSE = (DEPTH + 1) // 2
N_MOE = DEPTH // 2

IN_SPLITS = (
    MLA_Q_RANK, MLA_KV_RANK, MLA_ROPE,
    RET_HEADS * RET_DK, RET_HEADS * RET_DK, RET_HEADS * RET_DV, RET_HEADS * RET_DV,
    GLA_HEADS * GLA_DK, GLA_HEADS * GLA_DK, GLA_HEADS * GLA_DV, GLA_HEADS * GLA_DV,
    2 * GLA_GATE_RANK,
    N_BRANCH * D_MODEL,
)
IN_TOTAL = sum(IN_SPLITS)

kernel_name = 'hybrid_mla_retnet_gla_moe_prefix_dit'


def rmsnorm(x, w):
    x32 = x.astype(jnp.float32)
    y = x32 * lax.rsqrt(jnp.mean(x32 * x32, axis=-1, keepdims=True) + EPS)
    return (y * w.astype(jnp.float32)).astype(x.dtype)


def modulate(h, shift, scale):
    return h * (1.0 + scale) + shift


def split_heads(a, n_heads):
    b, l, _ = a.shape
    return a.reshape(b, l, n_heads, -1).transpose(0, 2, 1, 3)


def merge_heads(a):
    b, h, l, d = a.shape
    return a.transpose(0, 2, 1, 3).reshape(b, l, h * d)


def rope(x, pos):
    half = x.shape[-1] // 2
    inv_freq = ROPE_BASE ** (-jnp.arange(half, dtype=jnp.float32) / half)
    ang = pos.astype(jnp.float32)[:, None] * inv_freq[None, :]
    cos, sin = jnp.cos(ang), jnp.sin(ang)
    x1 = x[..., :half].astype(jnp.float32)
    x2 = x[..., half:].astype(jnp.float32)
    return jnp.concatenate([x1 * cos - x2 * sin, x1 * sin + x2 * cos], axis=-1).astype(x.dtype)


def axial_rope(x, row, col):
    half = x.shape[-1] // 2
    return jnp.concatenate([rope(x[..., :half], row), rope(x[..., half:], col)], axis=-1)


def to_chunks(a, chunk):
    b, h, l, d = a.shape
    return a.reshape(b, h, l // chunk, chunk, d).transpose(2, 0, 1, 3, 4)


def from_chunks(a):
    n, b, h, chunk, d = a.shape
    return a.transpose(1, 2, 0, 3, 4).reshape(b, h, n * chunk, d)


def mla_qkv(cq, ckv, k_rope, q_norm, w_uq, kv_norm, w_ukv, row=None, col=None):
    q = split_heads(rmsnorm(cq, q_norm) @ w_uq, MLA_HEADS)
    kv = split_heads(rmsnorm(ckv, kv_norm) @ w_ukv, MLA_HEADS)
    q_nope, q_rope = q[..., :MLA_NOPE], q[..., MLA_NOPE:]
    k_nope, v = kv[..., :MLA_NOPE], kv[..., MLA_NOPE:]
    if row is not None:
        q_rope = axial_rope(q_rope, row, col)
        k_rope = axial_rope(k_rope, row, col)
    k_rope = jnp.broadcast_to(k_rope[:, None], k_nope.shape[:-1] + (MLA_ROPE,))
    q = jnp.concatenate([q_nope, q_rope], axis=-1)
    k = jnp.concatenate([k_nope, k_rope], axis=-1)
    return q, k, v


def attend(q, k, v):
    s = jnp.einsum('bhqd,bhkd->bhqk', q, k).astype(jnp.float32) * (q.shape[-1] ** -0.5)
    p = jax.nn.softmax(s, axis=-1).astype(v.dtype)
    return jnp.einsum('bhqk,bhkd->bhqd', p, v)


def blocked_attend(q, k, v):
    b, h, l, d = q.shape
    nb = l // Q_BLOCK
    qb = q.reshape(b, h, nb, Q_BLOCK, d).transpose(2, 0, 1, 3, 4)
    ob = lax.map(lambda qi: attend(qi, k, v), qb)
    return ob.transpose(1, 2, 0, 3, 4).reshape(b, h, l, v.shape[-1])


def retention_scan(q, k, v, log_gamma, s0):
    C = RET_CHUNK
    idx = jnp.arange(C, dtype=jnp.float32)
    lg = log_gamma.astype(jnp.float32)[:, None]
    diff = idx[:, None] - idx[None, :]
    intra = jnp.where(diff >= 0, jnp.exp(lg[:, :, None] * jnp.maximum(diff, 0.0)), 0.0)
    q_dec = jnp.exp(lg * (idx + 1.0))[:, :, None]
    k_dec = jnp.exp(lg * (C - 1.0 - idx))[:, :, None]
    chunk_dec = jnp.exp(lg * C)[:, :, None]

    def step(s, xs):
        qc, kc, vc = xs
        scores = jnp.einsum('bhid,bhjd->bhij', qc, kc) * intra
        o = jnp.einsum('bhij,bhjv->bhiv', scores, vc) + jnp.einsum('bhid,bhdv->bhiv', qc, s) * q_dec
        s = s * chunk_dec + jnp.einsum('bhjd,bhjv->bhdv', kc * k_dec, vc)
        return s.astype(jnp.float32), o.astype(jnp.float32)

    s, o = lax.scan(step, s0, (to_chunks(q, C), to_chunks(k, C), to_chunks(v, C)))
    return from_chunks(o), s


def gla_scan(q, k, v, log_a, s0):
    C = GLA_CHUNK
    lower = jnp.tril(jnp.ones((C, C), dtype=bool))[:, :, None]

    def step(s, xs):
        qc, kc, vc, ac = xs
        b = jnp.cumsum(ac.astype(jnp.float32), axis=2)
        rel = b[:, :, :, None, :] - b[:, :, None, :, :]
        decay = jnp.exp(jnp.where(lower, rel, -jnp.inf))
        scores = jnp.einsum('bhid,bhjd,bhijd->bhij', qc, kc, decay)
        o = jnp.einsum('bhij,bhjv->bhiv', scores, vc) + jnp.einsum('bhid,bhdv->bhiv', qc * jnp.exp(b), s)
        b_end = b[:, :, -1:, :]
        s = s * jnp.swapaxes(jnp.exp(b_end), 2, 3) + jnp.einsum('bhjd,bhjv->bhdv', kc * jnp.exp(b_end - b), vc)
        return s.astype(jnp.float32), o.astype(jnp.float32)

    s, o = lax.scan(step, s0, (to_chunks(q, C), to_chunks(k, C), to_chunks(v, C), to_chunks(log_a, C)))
    return from_chunks(o), s


def two_way(scan_fn, q, k, v, dec_f, dec_b, s0_f, s0_b, per_token):
    flip = lambda a: jnp.flip(a, axis=2)
    o_f, s_f = scan_fn(q, k, v, dec_f, s0_f)
    o_b, s_b = scan_fn(flip(q), flip(k), flip(v), flip(dec_b) if per_token else dec_b, s0_b)
    return o_f + flip(o_b), s_f, s_b


def gla_log_gates(gz, w_gate, b_gate):
    z_f = gz[..., :GLA_GATE_RANK] @ w_gate[0] + b_gate[0]
    z_b = gz[..., GLA_GATE_RANK:] @ w_gate[1] + b_gate[1]
    to_log_gate = lambda z: split_heads(jax.nn.log_sigmoid(z.astype(jnp.float32)) / GLA_TAU, GLA_HEADS)
    return to_log_gate(z_f), to_log_gate(z_b)


def head_norm(o, w, center):
    o = o.astype(jnp.float32)
    if center:
        o = o - jnp.mean(o, axis=-1, keepdims=True)
    o = o * lax.rsqrt(jnp.mean(o * o, axis=-1, keepdims=True) + EPS)
    return merge_heads(o) * w.astype(jnp.float32)


def mixer_block(u_c, u_x, row, col, pos, w_in, mla_q_norm, mla_w_uq, mla_kv_norm, mla_w_ukv,
                ret_decay_logit, ret_norm_w, gla_w_gate, gla_b_gate, gla_norm_w, w_branch, w_out,
                need_ctx):
    b = u_x.shape[0]
    offs = np.cumsum(IN_SPLITS)[:-1].tolist()
    (cq_c, ckv_c, kr_c, rq_c, rk_c, rv_c, rg_c, gq_c, gk_c, gv_c, gr_c, gz_c, bg_c) = jnp.split(u_c @ w_in, offs, axis=-1)
    (cq_x, ckv_x, kr_x, rq_x, rk_x, rv_x, rg_x, gq_x, gk_x, gv_x, gr_x, gz_x, bg_x) = jnp.split(u_x @ w_in, offs, axis=-1)

    qa_c, ka_c, va_c = mla_qkv(cq_c, ckv_c, kr_c, mla_q_norm, mla_w_uq, mla_kv_norm, mla_w_ukv)
    qa_x, ka_x, va_x = mla_qkv(cq_x, ckv_x, kr_x, mla_q_norm, mla_w_uq, mla_kv_norm, mla_w_ukv, row, col)
    oa_x = blocked_attend(qa_x, jnp.concatenate([ka_c, ka_x], axis=2), jnp.concatenate([va_c, va_x], axis=2))

    k_scale = RET_DK ** -0.5
    rq_c, rk_c, rv_c = split_heads(rq_c, RET_HEADS), split_heads(rk_c, RET_HEADS) * k_scale, split_heads(rv_c, RET_HEADS)
    rq_x = rope(split_heads(rq_x, RET_HEADS), pos)
    rk_x = rope(split_heads(rk_x, RET_HEADS), pos) * k_scale
    rv_x = split_heads(rv_x, RET_HEADS)
    log_gamma = jax.nn.log_sigmoid(ret_decay_logit.astype(jnp.float32))
    s0_r = jnp.zeros((b, RET_HEADS, RET_DK, RET_DV), jnp.float32)
    ob_c, sr_f, sr_b = two_way(retention_scan, rq_c, rk_c, rv_c, log_gamma[0], log_gamma[1], s0_r, s0_r, False)
    ob_x, _, _ = two_way(retention_scan, rq_x, rk_x, rv_x, log_gamma[0], log_gamma[1], sr_f, sr_b, False)

    q_scale = GLA_DK ** -0.5
    gq_c, gk_c, gv_c = split_heads(gq_c, GLA_HEADS) * q_scale, split_heads(gk_c, GLA_HEADS), split_heads(gv_c, GLA_HEADS)
    gq_x, gk_x, gv_x = split_heads(gq_x, GLA_HEADS) * q_scale, split_heads(gk_x, GLA_HEADS), split_heads(gv_x, GLA_HEADS)
    la_f_c, la_b_c = gla_log_gates(gz_c, gla_w_gate, gla_b_gate)
    la_f_x, la_b_x = gla_log_gates(gz_x, gla_w_gate, gla_b_gate)
    s0_g = jnp.zeros((b, GLA_HEADS, GLA_DK, GLA_DV), jnp.float32)
    oc_c, sg_f, sg_b = two_way(gla_scan, gq_c, gk_c, gv_c, la_f_c, la_b_c, s0_g, s0_g, True)
    oc_x, _, _ = two_way(gla_scan, gq_x, gk_x, gv_x, la_f_x, la_b_x, sg_f, sg_b, True)

    def merge(oa, ob, rg, oc, gr, bg, dtype):
        ya = merge_heads(oa)
        yb = jax.nn.silu(rg) * head_norm(ob, ret_norm_w, True)
        yc = jax.nn.silu(gr) * head_norm(oc, gla_norm_w, False)
        g_a, g_b, g_c = jnp.split(jax.nn.sigmoid(bg.astype(jnp.float32)), N_BRANCH, axis=-1)
        z = g_a * (ya @ w_branch[0]) + g_b * (yb @ w_branch[1]) + g_c * (yc @ w_branch[2])
        return (z @ w_out).astype(dtype)

    y_x = merge(oa_x, ob_x, rg_x, oc_x, gr_x, bg_x, u_x.dtype)
    y_c = merge(attend(qa_c, ka_c, va_c), ob_c, rg_c, oc_c, gr_c, bg_c, u_c.dtype) if need_ctx else None
    return y_c, y_x


def swiglu(u, w_gate, w_up, w_down):
    return (jax.nn.silu(u @ w_gate) * (u @ w_up)) @ w_down


def moe_swiglu(u, router, w_gate, w_up, w_down):
    logits = (u @ router).astype(jnp.float32)
    top_val, top_idx = lax.top_k(logits, TOP_K)
    top_w = jax.nn.softmax(top_val, axis=-1)
    gates = jnp.einsum('blk,blke->ble', top_w, jax.nn.one_hot(top_idx, N_EXPERTS, dtype=jnp.float32))
    y = jnp.zeros(u.shape, jnp.float32)
    for e in range(N_EXPERTS):
        y = y + gates[..., e:e + 1] * swiglu(u, w_gate[e], w_up[e], w_down[e])
    return y.astype(u.dtype)


def channel_mixer(u, l, ffn_w_gate, ffn_w_up, ffn_w_down, moe_router, moe_w_gate, moe_w_up, moe_w_down):
    i = l // 2
    if l % 2 == 0:
        return swiglu(u, ffn_w_gate[i], ffn_w_up[i], ffn_w_down[i])
    return moe_swiglu(u, moe_router[i], moe_w_gate[i], moe_w_up[i], moe_w_down[i])


def setup_inputs(seed: int = 0) -> dict:
    key = jax.random.key(seed)
    k = jax.random.split(key, 28)
    f32 = jnp.float32

    def w(i, shape, fan_in, gain=1.0):
        return jax.random.normal(k[i], shape, f32) * (gain * fan_in ** -0.5)

    def g(i, shape):
        return 1.0 + 0.05 * jax.random.normal(k[i], shape, f32)

    head_ids = jnp.arange(RET_HEADS, dtype=f32)
    ret_logit0 = jnp.log(2.0 ** (5.0 + head_ids) - 1.0)
    return {
        'x': jax.random.normal(k[0], (BATCH, SEQ, D_MODEL), f32),
        'c': jax.random.normal(k[1], (BATCH, D_MODEL), f32),
        'ctx': jax.random.normal(k[2], (BATCH, CTX_LEN, D_MODEL), f32),
        'c_ctx': jax.random.normal(k[3], (D_MODEL,), f32),
        'mod_w': w(4, (DEPTH, D_MODEL, 6 * D_MODEL), D_MODEL, 0.5),
        'mod_b': 0.02 * jax.random.normal(k[5], (DEPTH, 6 * D_MODEL), f32),
        'norm1_w': g(6, (DEPTH, D_MODEL)),
        'norm2_w': g(7, (DEPTH, D_MODEL)),
        'w_in': w(8, (DEPTH, D_MODEL, IN_TOTAL), D_MODEL),
        'mla_q_norm': g(9, (DEPTH, MLA_Q_RANK)),
        'mla_w_uq': w(10, (DEPTH, MLA_Q_RANK, MLA_HEADS * (MLA_NOPE + MLA_ROPE)), MLA_Q_RANK),
        'mla_kv_norm': g(11, (DEPTH, MLA_KV_RANK)),
        'mla_w_ukv': w(12, (DEPTH, MLA_KV_RANK, MLA_HEADS * (MLA_NOPE + MLA_V)), MLA_KV_RANK),
        'ret_decay_logit': ret_logit0 + 0.1 * jax.random.normal(k[13], (DEPTH, 2, RET_HEADS), f32),
        'ret_norm_w': g(14, (DEPTH, RET_HEADS * RET_DV)),
        'gla_w_gate': w(15, (DEPTH, 2, GLA_GATE_RANK, GLA_HEADS * GLA_DK), GLA_GATE_RANK),
        'gla_b_gate': 0.1 * jax.random.normal(k[16], (DEPTH, 2, GLA_HEADS * GLA_DK), f32),
        'gla_norm_w': g(17, (DEPTH, GLA_HEADS * GLA_DV)),
        'w_branch': w(18, (DEPTH, N_BRANCH, BRANCH_W, D_MODEL), BRANCH_W),
        'w_out': w(19, (DEPTH, D_MODEL, D_MODEL), D_MODEL),
        'ffn_w_gate': w(20, (N_DENSE, D_MODEL, D_FF), D_MODEL),
        'ffn_w_up': w(21, (N_DENSE, D_MODEL, D_FF), D_MODEL),
        'ffn_w_down': w(22, (N_DENSE, D_FF, D_MODEL), D_FF),
        'moe_router': w(23, (N_MOE, D_MODEL, N_EXPERTS), D_MODEL),
        'moe_w_gate': w(24, (N_MOE, N_EXPERTS, D_MODEL, D_FF), D_MODEL),
        'moe_w_up': w(25, (N_MOE, N_EXPERTS, D_MODEL, D_FF), D_MODEL),
        'moe_w_down': w(26, (N_MOE, N_EXPERTS, D_FF, D_MODEL), D_FF),
        'final_norm_w': g(27, (D_MODEL,)),
    }


def reference(x, c, ctx, c_ctx, mod_w, mod_b, norm1_w, norm2_w, w_in, mla_q_norm, mla_w_uq,
              mla_kv_norm, mla_w_ukv, ret_decay_logit, ret_norm_w, gla_w_gate, gla_b_gate,
              gla_norm_w, w_branch, w_out, ffn_w_gate, ffn_w_up, ffn_w_down, moe_router,
              moe_w_gate, moe_w_up, moe_w_down, final_norm_w):
    L = x.shape[1]
    rows = L // GRID_W
    row = jnp.repeat(jnp.arange(rows, dtype=jnp.int32), GRID_W)
    col = jnp.tile(jnp.arange(GRID_W, dtype=jnp.int32), rows)
    pos = jnp.arange(L, dtype=jnp.int32)
    for l in range(DEPTH):
        last = l == DEPTH - 1
        mod_x = jax.nn.silu(c) @ mod_w[l] + mod_b[l]
        mod_c = jax.nn.silu(c_ctx) @ mod_w[l] + mod_b[l]
        sh1x, sc1x, g1x, sh2x, sc2x, g2x = [m[:, None, :] for m in jnp.split(mod_x, 6, axis=-1)]
        sh1c, sc1c, g1c, sh2c, sc2c, g2c = jnp.split(mod_c, 6, axis=-1)

        u_x = modulate(rmsnorm(x, norm1_w[l]), sh1x, sc1x)
        u_c = modulate(rmsnorm(ctx, norm1_w[l]), sh1c, sc1c)
        y_c, y_x = mixer_block(u_c, u_x, row, col, pos, w_in[l], mla_q_norm[l], mla_w_uq[l],
                               mla_kv_norm[l], mla_w_ukv[l], ret_decay_logit[l], ret_norm_w[l],
                               gla_w_gate[l], gla_b_gate[l], gla_norm_w[l], w_branch[l], w_out[l],
                               not last)
        x = x + g1x * y_x
        v_x = modulate(rmsnorm(x, norm2_w[l]), sh2x, sc2x)
        x = x + g2x * channel_mixer(v_x, l, ffn_w_gate, ffn_w_up, ffn_w_down,
                                    moe_router, moe_w_gate, moe_w_up, moe_w_down)
        if not last:
            ctx = ctx + g1c * y_c
            v_c = modulate(rmsnorm(ctx, norm2_w[l]), sh2c, sc2c)
            ctx = ctx + g2c * channel_mixer(v_c, l, ffn_w_gate, ffn_w_up, ffn_w_down,
                                            moe_router, moe_w_gate, moe_w_up, moe_w_down)
    return rmsnorm(x, final_norm_w)
```

```python
import math
from contextlib import ExitStack
import numpy as np
import ml_dtypes
import concourse.bass as bass
import concourse.mybir as mybir
from concourse.bass_utils import run_bass_kernel_spmd

F32 = mybir.dt.float32
BF16 = mybir.dt.bfloat16
AF = mybir.ActivationFunctionType
ALU = mybir.AluOpType
AX = mybir.AxisListType

NCORES = 8
D = 1024
SEQ = 8192
CTX = 256
LAT = 2048
T = LAT + CTX
NT = T // 128
NLT = LAT // 128
DEPTH = 2
EPS = 1e-6
DFF = 2816
NFF = DFF // 128
NEXP = 8
NKEY = CTX + SEQ
NKT = NKEY // 128

PA = 0
PA_N = 480
P_RET = 480
P_GLA = P_RET + 4 * 512
P_RV = P_GLA + 4 * 256
P_GV = P_RV + 512
P_RG = P_GV + 512
P_GR = P_RG + 512
P_BG = P_GR + 512
NPACK = P_BG + 3072

XF_STATE = 0
XF_LAM = 8
NXF = 9
XB_CKV = 0
XB_KR = 8
NXB = 10

EPOCH = 30000


class Buf:
    __slots__ = ("name", "w", "r")

    def __init__(self, name=""):
        self.name = name
        self.w = None
        self.r = []


class Ring:
    def __init__(self, items):
        self.items = items
        self.i = 0

    def next(self):
        it = self.items[self.i % len(self.items)]
        self.i += 1
        return it


class K:
    def __init__(self, nc):
        self.nc = nc
        self.eng = {"pe": nc.tensor, "dve": nc.vector, "act": nc.scalar,
                    "pool": nc.gpsimd, "sp": nc.sync}
        self.sems = {}
        self.cnt = {}
        self.epoch = {e: 0 for e in self.eng}
        self.seen = {}
        for e in self.eng:
            self._new_epoch(e, first=True)
        self.dq = {}
        for q, n in (("sp", 24), ("pool", 16), ("act", 4)):
            keys = []
            for i in range(n):
                key = ("dma", q, i)
                self.sems[key] = nc.alloc_semaphore(f"d_{q}_{i}")
                self.cnt[key] = 0
                keys.append(key)
            self.dq[q] = [keys, 0]
        self.cc_key = ("cc", 0)
        self.sems[self.cc_key] = nc.alloc_semaphore("cc")
        self.cnt[self.cc_key] = 0
        self.n_ins = 0

    def _new_epoch(self, e, first=False):
        if not first:
            self.epoch[e] += 1
        key = (e, self.epoch[e])
        self.sems[key] = self.nc.alloc_semaphore(f"s_{e}_{self.epoch[e]}")
        self.cnt[key] = 0

    def _wait(self, e, toks, force_same=False):
        eng = self.eng[e]
        best = {}
        for t in toks:
            if t is None:
                continue
            key, val = t
            if key[0] == e and not force_same:
                if e in ("pe", "sp"):
                    continue
            if best.get(key, 0) < val:
                best[key] = val
        for key, val in best.items():
            if self.seen.get((e, key), 0) >= val:
                continue
            assert self.cnt[key] >= val, f"wait on unsignalled token {key} {val} > {self.cnt[key]}"
            eng.wait_ge(self.sems[key], val)
            self.seen[(e, key)] = val

    @staticmethod
    def _deps(R, W):
        toks = []
        for b in R:
            toks.append(b.w)
        for b in W:
            toks.append(b.w)
            toks.extend(b.r)
        return toks

    @staticmethod
    def _commit(tok, R, W):
        for b in R:
            b.r.append(tok)
            if len(b.r) > 24:
                b.r = b.r[-24:] if False else b.r
        for b in W:
            b.w = tok
            b.r = []

    def op(self, e, fn, *args, R=(), W=(), sig=True, **kw):
        self._wait(e, self._deps(R, W))
        ins = fn(*args, **kw)
        self.n_ins += 1
        key = (e, self.epoch[e])
        if sig:
            self.cnt[key] += 1
            ins.then_inc(self.sems[key], 1)
            tok = (key, self.cnt[key])
            if self.cnt[key] >= EPOCH:
                self._new_epoch(e)
        else:
            tok = (key, self.cnt[key] + 1)
        self._commit(tok, R, W)
        return tok

    def dma(self, q, out, in_, R=(), W=(), **kw):
        keys, idx = self.dq[q]
        key = keys[idx % len(keys)]
        self.dq[q][1] = idx + 1
        toks = self._deps(R, W)
        if self.cnt[key] > 0:
            toks.append((key, self.cnt[key]))
        self._wait(q, toks)
        ins = self.eng[q].dma_start(out=out, in_=in_, **kw)
        self.n_ins += 1
        self.cnt[key] += 16
        ins.then_inc(self.sems[key], 16)
        tok = (key, self.cnt[key])
        self._commit(tok, R, W)
        return tok

    def allgather(self, in_ap, out_ap, R=(), W=()):
        self._wait("pool", self._deps(R, W))
        ins = self.nc.gpsimd.collective_compute(
            "AllGather", ALU.bypass, replica_groups=[[0, 1, 2, 3], [4, 5, 6, 7]],
            ins=[in_ap], outs=[out_ap])
        self.n_ins += 1
        self.cnt[self.cc_key] += 1
        ins.then_inc(self.sems[self.cc_key])
        tok = (self.cc_key, self.cnt[self.cc_key])
        self._commit(tok, R, W)
        return tok

    def fence(self):
        toks = []
        for key, c in self.cnt.items():
            if c > 0:
                toks.append((key, c))
        for e in self.eng:
            self._wait(e, toks, force_same=False)

    def wait_bufs(self, e, bufs):
        toks = []
        for b in bufs:
            toks.append(b.w)
            toks.extend(b.r)
        self._wait(e, toks, force_same=True)


def build_program(nlayers=DEPTH, dbg=None):
    nc = bass.Bass("TRN2", target_bir_lowering=False)
    k = K(nc)
    dbg = dbg or {}
    uid = [0]

    def U(name):
        uid[0] += 1
        return f"{name}_{uid[0]}"

    def din(name, shape, dt=F32):
        return nc.dram_tensor(name, list(shape), dt, kind="ExternalInput").ap()

    xs_in = din("xs_in", [T, D])
    cvecT = din("cvecT", [D, 2])
    mod_w = din("mod_w", [DEPTH, D, 6 * D])
    mod_b = din("mod_b", [DEPTH, 6 * D])
    norm1_w = din("norm1_w", [DEPTH, D])
    norm2_w = din("norm2_w", [DEPTH, D])
    wp = din("wp", [DEPTH, D, NPACK])
    q_norm = din("mla_q_norm", [DEPTH, 256])
    w_uq = din("w_uq", [DEPTH, 256, 768])
    w_uq_sw = din("w_uq_sw", [DEPTH, 256, 768])
    kv_norm = din("mla_kv_norm", [DEPTH, 128])
    w_ukv = din("w_ukv", [DEPTH, 128, 1024])
    ret_logit = din("ret_decay_logit", [DEPTH, 2, 4])
    ret_norm_w = din("ret_norm_w", [DEPTH, 512])
    gwblk = din("gwblk", [DEPTH, 4, 32, 128])
    gbias = din("gbias", [DEPTH, 4, 128])
    gla_norm_w = din("gla_norm_w", [DEPTH, 512])
    w_branch = din("w_branch", [DEPTH, 3, 512, D])
    w_out = din("w_out", [DEPTH, D, D])
    ffn_wg = din("ffn_w_gate", [1, D, DFF])
    ffn_wu = din("ffn_w_up", [1, D, DFF])
    ffn_wd = din("ffn_w_down", [1, DFF, D])
    moe_router = din("moe_router", [1, D, NEXP])
    moe_wg = din("moe_w_gate", [1, NEXP, D, DFF])
    moe_wu = din("moe_w_up", [1, NEXP, D, DFF])
    moe_wd = din("moe_w_down", [1, NEXP, DFF, D])
    final_norm_w = din("final_norm_w", [D])
    c_ropeR = din("c_ropeR", [2, 128, T])
    c_ropeAq = din("c_ropeAq", [2, 32, T])
    c_ropeAk = din("c_ropeAk", [2, 32, T])
    c_rst = din("c_rst", [128, T])
    c_mask = din("c_mask", [2, 128, 128])
    c_ident = din("c_ident", [128, 128])
    c_onehot = din("c_onehot", [128, 4])

    out = nc.dram_tensor("out", [LAT, D], F32, kind="ExternalOutput").ap()
    dbg_out = {}
    for name, (shape, dt) in dbg.items():
        dbg_out[name] = nc.dram_tensor("dbg_" + name, list(shape), dt, kind="ExternalOutput").ap()

    xs = nc.dram_tensor("xs", [T, D], F32).ap()
    uT = nc.dram_tensor("uT", [8, 128, T], BF16).ap()
    modD = nc.dram_tensor("modD", [2, 6 * D], F32).ap()
    xf_in = nc.dram_tensor("xf_in", [NXF, 16, 1024], F32).ap()
    xf_out = nc.dram_tensor("xf_out", [NXF, 64, 1024], F32).ap()
    xb_in = nc.dram_tensor("xb_in", [NXB, 16, LAT], BF16).ap()
    xb_out = nc.dram_tensor("xb_out", [NXB, 64, LAT], BF16).ap()
    b_xs = [Buf(f"xs{t}") for t in range(NT)]
    b_uT = [Buf(f"uT{b}") for b in range(5)]
    b_modD = Buf("modD")
    b_xf_in = [Buf() for _ in range(NXF)]
    b_xf_out = [Buf() for _ in range(NXF)]
    b_xb_in = [Buf() for _ in range(NXB)]
    b_xb_out = [Buf() for _ in range(NXB)]
    b_out = Buf("out")

    PSA = nc.alloc_psum_tensor("psa", [128, 8, 512], F32)
    PS = []
    for i in range(8):
        PS.append((PSA[:, i, :], Buf(f"ps{i}")))

    def salloc(name, shape, dt=F32):
        return nc.alloc_sbuf_tensor(name, list(shape), dt), Buf(name)

    ident, b_ident = salloc("ident", [128, 128])
    identb, b_identb = salloc("identb", [128, 128], BF16)
    onesb, b_onesb = salloc("onesb", [128, 128], BF16)
    maskF, b_maskF = salloc("maskF", [128, 128])
    maskB, b_maskB = salloc("maskB", [128, 128])
    onehot, b_onehot = salloc("onehot", [128, 4])
    eps_t, b_eps = salloc("eps_t", [128, 1])
    one_t, b_one = salloc("one_t", [128, 1])
    modT, b_modT = salloc("modT", [128, 48, 2])
    A1, b_A1 = salloc("A1", [128, 8, 2])
    A2, b_A2 = salloc("A2", [128, 8, 2])
    rstb, b_rstb = salloc("rstb", [128, T], BF16)
    yTd = nc.dram_tensor("yTd", [3, 4, 128, T], BF16).ap()
    lsp_kt2 = nc.dram_tensor("lsp_kt2", [8, 128, T], BF16).ap()
    lsp_vth = nc.dram_tensor("lsp_vth", [8, 128, NT * 128], BF16).ap()
    lsp_eb = nc.dram_tensor("lsp_eb", [8, 128, T], BF16).ap()
    lsp_U2 = nc.dram_tensor("lsp_U2", [8, 128, NT * 128], F32).ap()
    lsp_te = nc.dram_tensor("lsp_te", [8, 128, 2 * NT], F32).ap()
    b_lsp = [Buf(f"lsp{i}") for i in range(8)]
    b_yTd = [Buf(f"yTd{i}") for i in range(3)]

    k.dma("sp", ident[:], c_ident[:, :], W=[b_ident])
    k.op("dve", nc.vector.tensor_copy, identb[:], ident[:], R=[b_ident], W=[b_identb])
    k.op("dve", nc.vector.memset, onesb[:], 1.0, W=[b_onesb])
    k.op("dve", nc.vector.memset, eps_t[:], EPS, W=[b_eps])
    k.op("dve", nc.vector.memset, one_t[:], 1.0, W=[b_one])
    k.dma("sp", maskF[:], c_mask[0], W=[b_maskF])
    k.dma("sp", maskB[:], c_mask[1], W=[b_maskB])
    k.dma("sp", onehot[:], c_onehot[:, :], W=[b_onehot])
    k.dma("pool", rstb[:], c_rst[:, :], W=[b_rstb])
    with nc.sbuf_tensor(U("zinit"), [16, 1024], F32) as zt_:
        b_zt = Buf()
        k.op("dve", nc.vector.memset, zt_[:], 0.0, W=[b_zt])
        k.dma("sp", xf_in[XF_LAM], zt_[:], R=[b_zt], W=[b_xf_in[XF_LAM]])
        k.fence()
    for t in range(NT):
        k.dma("sp", xs[t * 128:(t + 1) * 128, :], xs_in[t * 128:(t + 1) * 128, :], W=[b_xs[t]])

    BLKS = [(0, 512), (512, 512), (1024, 512), (1536, 512), (2048, 256)]

    def wview(w2d):
        return w2d.rearrange("(kc p) n -> p kc n", p=128)

    alt = [0]

    def evac_engine():
        alt[0] += 1
        return "act" if alt[0] % 2 else "dve"

    def copy_ps(e, out_ap, in_ap, R, W, scale=None):
        if e == "act":
            if scale is None:
                k.op("act", nc.scalar.copy, out_ap, in_ap, R=R, W=W)
            else:
                k.op("act", nc.scalar.mul, out_ap, in_ap, scale, R=R, W=W)
        else:
            if scale is None:
                k.op("dve", nc.vector.tensor_copy, out_ap, in_ap, R=R, W=W)
            else:
                k.op("dve", nc.vector.tensor_scalar, out_ap, in_ap, scale, None, ALU.mult, R=R, W=W)

    def rsqrt_col(es, name, src_ap, src_buf, scale, n=1, parts=128):
        t1 = es.enter_context(nc.sbuf_tensor(U(name + "_a"), [128, n], F32))
        t2 = es.enter_context(nc.sbuf_tensor(U(name + "_b"), [128, n], F32))
        b1, b2 = Buf(), Buf()
        k.op("dve", nc.vector.tensor_scalar, t1[0:parts, :], src_ap, scale, EPS, ALU.mult, ALU.add,
             R=[src_buf], W=[b1])
        k.op("act", nc.scalar.activation, t2[0:parts, :], t1[0:parts, :], AF.Sqrt, R=[b1], W=[b2])
        k.op("dve", nc.vector.reciprocal, t1[0:parts, :], t2[0:parts, :], R=[b2], W=[b1])
        return t1, b1

    def phase_mod(l):
        with ExitStack() as es:
            def sb(name, shape, dt=F32):
                return es.enter_context(nc.sbuf_tensor(U(name), list(shape), dt)), Buf(name)
            cT, b_cT = sb("cT", [128, 8, 2])
            sc, b_sc = sb("sc", [128, 8, 2])
            sg, b_sg = sb("sg", [128, 8, 2])
            modv, b_modv = sb("modv", [2, 6 * D])
            mb, b_mb = sb("mb", [2, 6 * D])
            wr = Ring([sb(f"mw{i}", [128, 8, 512]) for i in range(4)])
            k.dma("sp", cT[:], cvecT.rearrange("(kc p) r -> p kc r", p=128), W=[b_cT])
            k.op("act", nc.scalar.activation, sg[:], cT[:], AF.Sigmoid, R=[b_cT], W=[b_sg])
            k.op("dve", nc.vector.tensor_tensor, sc[:], cT[:], sg[:], ALU.mult, R=[b_cT, b_sg], W=[b_sc])
            k.dma("sp", mb[0:1, :], mod_b[l:l + 1, :], W=[b_mb])
            k.dma("sp", mb[1:2, :], mod_b[l:l + 1, :], W=[b_mb])
            psr = Ring(PS[0:2])
            for n in range(12):
                wt, b_wt = wr.next()
                k.dma("sp", wt[:], wview(mod_w[l])[:, :, n * 512:(n + 1) * 512], W=[b_wt])
                ps, b_ps = psr.next()
                for kc in range(8):
                    k.op("pe", nc.tensor.matmul, ps[0:2, :], sc[:, kc, :], wt[:, kc, :],
                         start=(kc == 0), stop=(kc == 7), R=[b_sc, b_wt], W=[b_ps], sig=(kc == 7))
                k.op("dve", nc.vector.tensor_tensor, modv[:, n * 512:(n + 1) * 512], ps[0:2, :],
                     mb[:, n * 512:(n + 1) * 512], ALU.add, R=[b_ps, b_mb], W=[b_modv])
            k.dma("sp", modD[:, :], modv[:], R=[b_modv], W=[b_modD])
            pst, b_pst = PS[2]
            for j in range(48):
                k.op("pe", nc.tensor.transpose, pst[:, 2 * j:2 * j + 2], modv[0:2, j * 128:(j + 1) * 128],
                     ident[0:2, 0:2], R=[b_modv, b_ident], W=[b_pst], sig=(j == 47))
            k.op("dve", nc.vector.tensor_copy, modT[:].rearrange("p j r -> p (j r)"), pst[:, 0:96],
                 R=[b_pst], W=[b_modT])
            nw, b_nw = sb("nw", [128, 8, 2])
            for (nsrc, joff, At, bA) in ((norm1_w, 8, A1, b_A1), (norm2_w, 32, A2, b_A2)):
                k.dma("sp", nw[:, :, 0], nsrc[l].rearrange("(kc p) -> p kc", p=128), W=[b_nw], allow_slow_non_contiguous=True)
                k.dma("sp", nw[:, :, 1], nsrc[l].rearrange("(kc p) -> p kc", p=128), W=[b_nw], allow_slow_non_contiguous=True)
                k.op("dve", nc.vector.tensor_scalar, At[:], modT[:, joff:joff + 8, :], 1.0, None, ALU.add,
                     R=[b_modT], W=[bA])
                k.op("dve", nc.vector.tensor_tensor, At[:], At[:], nw[:], ALU.mult, R=[b_nw], W=[bA])
        k.fence()

    def phase_norm(l, At, bA, shoff, tiles):
        with ExitStack() as es:
            def sb(name, shape, dt=F32):
                return es.enter_context(nc.sbuf_tensor(U(name), list(shape), dt)), Buf(name)
            xr = Ring([sb(f"nx{i}", [128, D]) for i in range(2)])
            jr = Ring([sb(f"nj{i}", [128, D]) for i in range(2)])
            xnr = Ring([sb(f"nn{i}", [128, D]) for i in range(2)])
            ur = Ring([sb(f"nu{i}", [128, 8, 128], BF16) for i in range(2)])
            ssr = Ring([sb(f"ns{i}", [128, 1]) for i in range(2)])
            psr = Ring([PS[0], PS[1], PS[2], PS[3]])
            for t in tiles:
                r = 0 if t < NLT else 1
                xt, b_xt = xr.next()
                k.dma("sp", xt[:], xs[t * 128:(t + 1) * 128, :], R=[b_xs[t]], W=[b_xt])
                jk, b_jk = jr.next()
                ss, b_ss = ssr.next()
                k.op("act", nc.scalar.activation, jk[:], xt[:], AF.Square, accum_out=ss[:, 0:1],
                     R=[b_xt], W=[b_jk, b_ss])
                rstd, b_rstd = rsqrt_col(es, f"nr{t}", ss[:, 0:1], b_ss, 1.0 / D)
                xn, b_xn = xnr.next()
                k.op("act", nc.scalar.activation, xn[:], xt[:], AF.Identity, scale=rstd[:, 0:1],
                     R=[b_xt, b_rstd], W=[b_xn])
                ut, b_ut = ur.next()
                for half in range(2):
                    ps, b_ps = psr.next()
                    for j in range(4):
                        kc = half * 4 + j
                        k.op("pe", nc.tensor.transpose, ps[:, j * 128:(j + 1) * 128],
                             xn[:, kc * 128:(kc + 1) * 128], ident[:], R=[b_xn, b_ident], W=[b_ps],
                             sig=(j == 3))
                    for j in range(4):
                        kc = half * 4 + j
                        if j % 2 == 0:
                            k.op("act", nc.scalar.activation, ut[:, kc, :], ps[:, j * 128:(j + 1) * 128],
                                 AF.Identity, scale=At[:, kc, r:r + 1], bias=modT[:, shoff + kc, r:r + 1],
                                 R=[b_ps, bA, b_modT], W=[b_ut])
                        else:
                            k.op("dve", nc.vector.tensor_scalar, ut[:, kc, :], ps[:, j * 128:(j + 1) * 128],
                                 At[:, kc, r:r + 1], modT[:, shoff + kc, r:r + 1], ALU.mult, ALU.add,
                                 R=[b_ps, bA, b_modT], W=[b_ut])
                blk = min(t // 4, 4)
                k.dma("sp", uT[:, :, t * 128:(t + 1) * 128].rearrange("kc p t -> p kc t"), ut[:],
                      R=[b_ut], W=[b_uT[blk]])
        k.fence()

    class UStream:
        def __init__(self, es, nbuf=2, tag="ub"):
            self.ring = Ring([(es.enter_context(nc.sbuf_tensor(U(f"{tag}{i}"), [128, 8, 512], BF16)), Buf())
                              for i in range(nbuf)])

        def load(self, bi):
            t0, n = BLKS[bi]
            ub, b_ub = self.ring.next()
            k.dma("sp", ub[:, :, 0:n], uT[:, :, t0:t0 + n].rearrange("kc p t -> p kc t"),
                  R=[b_uT[bi]], W=[b_ub])
            return ub, b_ub, t0, n

    def proj_fm(ps, b_ps, w, b_w, c0, m, ub, b_ub, n, prow=0):
        for kc in range(8):
            k.op("pe", nc.tensor.matmul, ps[prow:prow + m, 0:n], w[:, kc, c0:c0 + m], ub[:, kc, 0:n],
                 start=(kc == 0), stop=(kc == 7), R=[b_w, b_ub], W=[b_ps], sig=(kc == 7))

    def load_w(es, name, src3d, shape, q="pool"):
        t = es.enter_context(nc.sbuf_tensor(U(name), list(shape), BF16))
        b = Buf(name)
        k.dma(q, t[:], src3d, W=[b])
        return t, b


    LS = {}
    LG2, b_LG2 = salloc("LG2", [128, 4])
    LAMT, b_LAMT = salloc("LAMT", [128, 8])

    def phase_q(l):
        cqn, b_cqn = LS["cqn"]; ckvn, b_ckvn = LS["ckvn"]; krr, b_krr = LS["krr"]; gzT, b_gzT = LS["gzT"]
        with ExitStack() as es:
            def sb(name, shape, dt=F32):
                return es.enter_context(nc.sbuf_tensor(U(name), list(shape), dt)), Buf(name)
            wA, b_wA = load_w(es, "wA", wview(wp[l])[:, :, PA:PA + PA_N], [128, 8, PA_N])
            qnw, b_qnw = sb("qnw", [128, 2])
            kvnw, b_kvnw = sb("kvnw", [128, 1])
            k.dma("sp", qnw[:], q_norm[l].rearrange("(c p) -> p c", p=128), W=[b_qnw], allow_slow_non_contiguous=True)
            k.dma("sp", kvnw[:], kv_norm[l].rearrange("(c p) -> p c", p=128), W=[b_kvnw], allow_slow_non_contiguous=True)
            ropk, b_ropk = sb("ropk", [32, 2, T])
            k.dma("sp", ropk[:, 0, :], c_ropeAk[0], W=[b_ropk])
            k.dma("sp", ropk[:, 1, :], c_ropeAk[1], W=[b_ropk])
            us = UStream(es)
            sqr = Ring([sb(f"sq{i}", [128, 512], BF16) for i in range(3)])
            rq, b_rq = sb("rq", [128, 512])
            rq2, b_rq2 = sb("rq2", [128, 512])
            tk, b_tk = sb("tk", [32, 512])
            tk2, b_tk2 = sb("tk2", [32, 512])
            for bi in range(5):
                ub, b_ub, t0, n = us.load(bi)
                (p0, bp0), (p1, bp1), (p2, bp2), (p3, bp3) = PS[0], PS[1], PS[2], PS[3]
                proj_fm(p0, bp0, wA, b_wA, 0, 128, ub, b_ub, n)
                proj_fm(p1, bp1, wA, b_wA, 128, 128, ub, b_ub, n)
                proj_fm(p2, bp2, wA, b_wA, 256, 128, ub, b_ub, n)
                s0, bs0 = sqr.next(); s1, bs1 = sqr.next(); s2, bs2 = sqr.next()
                k.op("act", nc.scalar.activation, s0[:, 0:n], p0[:, 0:n], AF.Square, R=[bp0], W=[bs0])
                k.op("act", nc.scalar.activation, s1[:, 0:n], p1[:, 0:n], AF.Square, R=[bp1], W=[bs1])
                k.op("act", nc.scalar.activation, s2[:, 0:n], p2[:, 0:n], AF.Square, R=[bp2], W=[bs2])
                k.op("pe", nc.tensor.matmul, p3[:, 0:n], onesb[:], s0[:, 0:n], start=True, stop=False,
                     R=[b_onesb, bs0], W=[bp3], sig=False)
                k.op("pe", nc.tensor.matmul, p3[:, 0:n], onesb[:], s1[:, 0:n], start=False, stop=True,
                     R=[b_onesb, bs1], W=[bp3])
                k.op("dve", nc.vector.tensor_scalar, rq[:, 0:n], p3[:, 0:n], 1.0 / 256, EPS, ALU.mult, ALU.add,
                     R=[bp3], W=[b_rq])
                k.op("act", nc.scalar.activation, rq2[:, 0:n], rq[:, 0:n], AF.Sqrt, R=[b_rq], W=[b_rq2])
                k.op("dve", nc.vector.reciprocal, rq[:, 0:n], rq2[:, 0:n], R=[b_rq2], W=[b_rq])
                k.op("dve", nc.vector.scalar_tensor_tensor, cqn[:, 0, t0:t0 + n], p0[:, 0:n], qnw[:, 0:1], rq[:, 0:n],
                     ALU.mult, ALU.mult, R=[bp0, b_qnw, b_rq], W=[b_cqn])
                k.op("dve", nc.vector.scalar_tensor_tensor, cqn[:, 1, t0:t0 + n], p1[:, 0:n], qnw[:, 1:2], rq[:, 0:n],
                     ALU.mult, ALU.mult, R=[bp1, b_qnw, b_rq], W=[b_cqn])
                k.op("pe", nc.tensor.matmul, p3[:, 0:n], onesb[:], s2[:, 0:n], start=True, stop=True,
                     R=[b_onesb, bs2], W=[bp3])
                k.op("dve", nc.vector.tensor_scalar, rq[:, 0:n], p3[:, 0:n], 1.0 / 128, EPS, ALU.mult, ALU.add,
                     R=[bp3], W=[b_rq])
                k.op("act", nc.scalar.activation, rq2[:, 0:n], rq[:, 0:n], AF.Sqrt, R=[b_rq], W=[b_rq2])
                k.op("dve", nc.vector.reciprocal, rq[:, 0:n], rq2[:, 0:n], R=[b_rq2], W=[b_rq])
                k.op("dve", nc.vector.scalar_tensor_tensor, ckvn[:, t0:t0 + n], p2[:, 0:n], kvnw[:, 0:1], rq[:, 0:n],
                     ALU.mult, ALU.mult, R=[bp2, b_kvnw, b_rq], W=[b_ckvn])
                (p4, bp4), (p5, bp5), (p6, bp6) = PS[4], PS[5], PS[6]
                proj_fm(p4, bp4, wA, b_wA, 384, 32, ub, b_ub, n)
                proj_fm(p5, bp5, wA, b_wA, 416, 32, ub, b_ub, n)
                proj_fm(p6, bp6, wA, b_wA, 448, 32, ub, b_ub, n)
                k.op("dve", nc.vector.tensor_tensor, tk[:, 0:n], p4[0:32, 0:n], ropk[:, 0, t0:t0 + n], ALU.mult,
                     R=[bp4, b_ropk], W=[b_tk])
                k.op("dve", nc.vector.tensor_tensor, tk2[:, 0:n], p5[0:32, 0:n], ropk[:, 1, t0:t0 + n], ALU.mult,
                     R=[bp5, b_ropk], W=[b_tk2])
                k.op("dve", nc.vector.tensor_tensor, krr[:, t0:t0 + n], tk[:, 0:n], tk2[:, 0:n], ALU.add,
                     R=[b_tk, b_tk2], W=[b_krr])
                k.op("act", nc.scalar.copy, gzT[:, t0:t0 + n], p6[0:32, 0:n], R=[bp6], W=[b_gzT])
            for j in range(8):
                k.dma("sp", xb_in[XB_CKV + j], ckvn[16 * j:16 * j + 16, 0:LAT], R=[b_ckvn], W=[b_xb_in[XB_CKV + j]])
            for j in range(2):
                k.dma("sp", xb_in[XB_KR + j], krr[16 * j:16 * j + 16, 0:LAT], R=[b_krr], W=[b_xb_in[XB_KR + j]])
            for j in range(NXB):
                k.allgather(xb_in[j], xb_out[j], R=[b_xb_in[j]], W=[b_xb_out[j]])
        k.fence()

    def phase_lg(l):
        with ExitStack() as es:
            def sb(name, shape, dt=F32):
                return es.enter_context(nc.sbuf_tensor(U(name), list(shape), dt)), Buf(name)
            lt, b_lt = sb("lt", [128, 4])
            l2, b_l2 = sb("l2", [128, 4])
            k.dma("sp", lt[0:64, :], ret_logit[l, 0:1, :].partition_broadcast(64), W=[b_lt])
            k.dma("sp", lt[64:128, :], ret_logit[l, 1:2, :].partition_broadcast(64), W=[b_lt])
            k.op("act", nc.scalar.activation, l2[:], lt[:], AF.Exp, scale=-1.0, R=[b_lt], W=[b_l2])
            k.op("act", nc.scalar.activation, lt[:], l2[:], AF.Ln, bias=one_t[:, 0:1], R=[b_l2, b_one], W=[b_lt])
            k.op("dve", nc.vector.tensor_scalar, LG2[:], lt[:], -1.0, None, ALU.mult, R=[b_lt], W=[b_LG2])
        k.fence()

    def lm_cols(mix, h):
        if mix == 0:
            return P_RET + h * 512, 512
        return P_GLA + h * 256, 256

    def lm_prefetch(l, mix, h, PF):
        hm = mix * 4 + h
        base, ncol = lm_cols(mix, h)
        k.dma("pool", PF["w"][0][:, :, 0:ncol], wview(wp[l])[:, :, base:base + ncol], W=[PF["w"][1]])
        gcol = (P_RG if mix == 0 else P_GR) + h * 128
        k.dma("pool", PF["wg"][0][:], wview(wp[l])[:, :, gcol:gcol + 128], W=[PF["wg"][1]])
        k.dma("sp", PF["kt2"][0][:], lsp_kt2[hm], R=[b_lsp[hm]], W=[PF["kt2"][1]])
        k.dma("sp", PF["vth"][0][:].rearrange("p n v -> p (n v)"), lsp_vth[hm], R=[b_lsp[hm]], W=[PF["vth"][1]])
        k.dma("sp", PF["ebT"][0][:], lsp_eb[hm], R=[b_lsp[hm]], W=[PF["ebT"][1]])
        k.dma("sp", PF["U2"][0][:].rearrange("p n v -> p (n v)"), lsp_U2[hm], R=[b_lsp[hm]], W=[PF["U2"][1]])
        k.dma("sp", PF["TE"][0][:], lsp_te[hm], R=[b_lsp[hm]], W=[PF["TE"][1]])

    def linear_mixer(l, mix, h, pass_no, last, PF=None):
        hm = mix * 4 + h
        gzT, b_gzT = LS["gzT"]
        with ExitStack() as es:
            def sb(name, shape, dt=F32):
                return es.enter_context(nc.sbuf_tensor(U(name), list(shape), dt)), Buf(name)
            if mix == 0:
                base, ncol = P_RET + h * 512, 512
                cq2, cq2s, ck2, ck2s = 0, 128, 256, 384
            else:
                base, ncol = P_GLA + h * 256, 256
                cq2, ck2 = 0, 128
            if pass_no == 1:
                w, b_w = load_w(es, "lw", wview(wp[l])[:, :, base:base + ncol], [128, 8, ncol])
            else:
                w, b_w = PF["w"]
            vcol = (P_RV if mix == 0 else P_GV) + h * 128
            if pass_no == 1:
                wv, b_wv = load_w(es, "lwv", wview(wp[l])[:, :, vcol:vcol + 128], [128, 8, 128])
            if mix == 1 and pass_no == 1:
                gw, b_gw = load_w(es, "gw", gwblk[l, h], [32, 128])
                nb, b_nb = sb("nb", [128, 1])
                nb2, b_nb2 = sb("nb2", [128, 1])
                k.dma("sp", nb[:], gbias[l, h].rearrange("(p o) -> p o", o=1), W=[b_nb])
                k.op("dve", nc.vector.tensor_scalar, nb2[:], nb[:], -1.0, None, ALU.mult, R=[b_nb], W=[b_nb2])
            if pass_no == 1:
                TE, b_TOT = sb("TE", [128, 2 * NT])
                kt2, b_kt2 = sb("kt2", [128, T], BF16)
                vth, b_vth = sb("vth", [128, NT, 128], BF16)
                ebT, b_ebT = sb("ebT", [128, T], BF16)
                U2, _ = sb("U2", [128, NT, 128])
            else:
                TE, b_TOT = PF["TE"]
                kt2, b_kt2 = PF["kt2"]
                vth, b_vth = PF["vth"]
                ebT, b_ebT = PF["ebT"]
                U2, b_ld = PF["U2"]
            TOT, EEND = TE[:, 0:NT], TE[:, NT:2 * NT]
            b_EEND = b_TOT
            b_kdc = [Buf() for _ in range(NT)]
            b_vthc = [Buf() for _ in range(NT)]
            us = UStream(es)
            if pass_no == 1:
                kd, b_kd = sb("kd", [128, T], BF16)
                a2r = Ring([sb(f"a2_{i}", [128, 512]) for i in range(2)])
                csr = Ring([sb(f"cs_{i}", [128, 512]) for i in range(2)])
                B2r = Ring([sb(f"B2_{i}", [128, 512]) for i in range(2)])
                enr = Ring([sb(f"en_{i}", [128, 512]) for i in range(2)])
            else:
                qt2, b_qt2 = sb("qt2", [128, T], BF16)
                b_vthc = [b_vth for _ in range(NT)]
            t1r = Ring([sb(f"lt1{i}", [128, 512]) for i in range(2)])
            t2r = Ring([sb(f"lt2{i}", [128, 512]) for i in range(2)])
            if mix == 0:
                ropr = Ring([sb(f"rop{i}", [128, 2, 512]) for i in range(2)])

            for bi in range(5):
                ub, b_ub, t0, n = us.load(bi)
                nch = n // 128
                ch0 = t0 // 128
                if pass_no == 1:
                    a2, b_a2 = a2r.next(); cs, b_cs = csr.next(); B2, b_B2 = B2r.next()
                    enb, b_enb = enr.next()
                if pass_no == 2:
                    pass
                elif mix == 0:
                    k.op("dve", nc.vector.memset, cs[:, 0:n], 1.0, W=[b_cs])
                    k.op("act", nc.scalar.activation, a2[:, 0:n], cs[:, 0:n], AF.Identity, scale=LG2[:, h:h + 1],
                         R=[b_cs, b_LG2], W=[b_a2])
                elif pass_no == 1:
                    pz, bpz = PS[7]
                    k.op("pe", nc.tensor.matmul, pz[:, 0:n], gw[:], gzT[:, t0:t0 + n], start=True, stop=True,
                         R=[b_gw, b_gzT], W=[bpz])
                    k.op("act", nc.scalar.activation, cs[:, 0:n], pz[:, 0:n], AF.Exp, scale=-1.0,
                         bias=nb2[:, 0:1], R=[bpz, b_nb2], W=[b_cs])
                    k.op("act", nc.scalar.activation, B2[:, 0:n], cs[:, 0:n], AF.Ln, bias=one_t[:, 0:1],
                         R=[b_cs, b_one], W=[b_B2])
                    k.op("dve", nc.vector.tensor_scalar, a2[:, 0:n], B2[:, 0:n], -1.0 / 16.0, None,
                         ALU.mult, R=[b_B2], W=[b_a2])
                if pass_no == 1:
                    k.op("dve", nc.vector.tensor_tensor_scan, cs[:, 0:n], rstb[:, t0:t0 + n], a2[:, 0:n], 0.0, ALU.mult, ALU.add,
                         R=[b_rstb, b_a2], W=[b_cs])
                    k.op("dve", nc.vector.tensor_copy, B2[0:64, 0:n], cs[0:64, 0:n], R=[b_cs], W=[b_B2])
                    k.op("dve", nc.vector.tensor_tensor, B2[64:128, 0:n], a2[64:128, 0:n], cs[64:128, 0:n], ALU.subtract,
                         R=[b_a2, b_cs], W=[b_B2])
                    for c_ in range(nch):
                        k.op("dve", nc.vector.tensor_scalar, B2[64:128, c_ * 128:(c_ + 1) * 128],
                             B2[64:128, c_ * 128:(c_ + 1) * 128], cs[64:128, c_ * 128 + 127:c_ * 128 + 128], None, ALU.add,
                             R=[b_cs], W=[b_B2])
                        k.op("dve", nc.vector.tensor_copy, TOT[:, ch0 + c_:ch0 + c_ + 1], cs[:, c_ * 128 + 127:c_ * 128 + 128],
                             R=[b_cs], W=[b_TOT])
                    k.op("act", nc.scalar.activation, EEND[:, ch0:ch0 + nch], TOT[:, ch0:ch0 + nch], AF.Exp,
                         R=[b_TOT], W=[b_EEND])
                    k.op("act", nc.scalar.activation, enb[:, 0:n], B2[:, 0:n], AF.Exp, scale=-1.0, R=[b_B2], W=[b_enb])
                    k.op("act", nc.scalar.activation, ebT[:, t0:t0 + n], B2[:, 0:n], AF.Exp, R=[b_B2], W=[b_ebT])
                if mix == 0:
                    rop, b_rop = ropr.next()
                    k.dma("sp", rop[:, 0, 0:n], c_ropeR[0, :, t0:t0 + n], W=[b_rop])
                    k.dma("sp", rop[:, 1, 0:n], c_ropeR[1, :, t0:t0 + n], W=[b_rop])

                def roped(pa, bpa, pb, bpb):
                    if mix == 1:
                        return pa[:, 0:n], bpa
                    ta, bta = t1r.next()
                    tb, btb = t2r.next()
                    k.op("dve", nc.vector.tensor_tensor, ta[:, 0:n], pa[:, 0:n], rop[:, 0, 0:n], ALU.mult,
                         R=[bpa, b_rop], W=[bta])
                    k.op("dve", nc.vector.tensor_tensor, tb[:, 0:n], pb[:, 0:n], rop[:, 1, 0:n], ALU.mult,
                         R=[bpb, b_rop], W=[btb])
                    k.op("dve", nc.vector.tensor_tensor, ta[:, 0:n], ta[:, 0:n], tb[:, 0:n], ALU.add,
                         R=[btb], W=[bta])
                    return ta[:, 0:n], bta

                (pa, bpa), (pb, bpb) = (PS[0], PS[1]) if bi % 2 == 0 else (PS[2], PS[3])
                if pass_no == 1:
                    proj_fm(pa, bpa, w, b_w, ck2, 128, ub, b_ub, n)
                    if mix == 0:
                        proj_fm(pb, bpb, w, b_w, ck2s, 128, ub, b_ub, n)
                    kap, bk = roped(pa, bpa, pb, bpb)
                    k.op("dve", nc.vector.tensor_tensor, kt2[:, t0:t0 + n], kap, enb[:, 0:n], ALU.mult,
                         R=[bk, b_enb], W=[b_kt2])
                if pass_no == 2:
                    (pc, bpc), (pd, bpd) = (pa, bpa), (pb, bpb)
                    proj_fm(pc, bpc, w, b_w, cq2, 128, ub, b_ub, n)
                    if mix == 0:
                        proj_fm(pd, bpd, w, b_w, cq2s, 128, ub, b_ub, n)
                    qap, bq = roped(pc, bpc, pd, bpd)
                    k.op("dve", nc.vector.scalar_tensor_tensor, qt2[:, t0:t0 + n], qap, 0.125, ebT[:, t0:t0 + n],
                         ALU.mult, ALU.mult, R=[bq, b_ebT], W=[b_qt2])
                for c_ in (range(nch) if pass_no == 1 else []):
                    n_ = ch0 + c_
                    k.op("act", nc.scalar.activation, kd[:, n_ * 128:(n_ + 1) * 128], kt2[:, n_ * 128:(n_ + 1) * 128],
                         AF.Identity, scale=EEND[:, n_:n_ + 1], R=[b_kt2, b_EEND], W=[b_kdc[n_]])
                    pv, bpv = PS[4 + c_ % 2]
                    for kc in range(8):
                        k.op("pe", nc.tensor.matmul, pv[:, 0:128], ub[:, kc, c_ * 128:(c_ + 1) * 128], wv[:, kc, :],
                             start=(kc == 0), stop=(kc == 7), R=[b_ub, b_wv], W=[bpv], sig=(kc == 7))
                    copy_ps(evac_engine(), vth[:, n_, :], pv[:, 0:128], R=[bpv], W=[b_vthc[n_]])
            if pass_no == 1:
                k2d, _ = sb("k2d", [128, NT, 128], BF16)
            b_k2dg = [Buf() for _ in range(3)]
            b_U2g = [Buf() for _ in range(5)]
            if pass_no == 2:
                b_U2g = [b_ld]
            if pass_no == 1:
                ptb = PSA[:, 0:3, :].bitcast(BF16)
                for n_ in range(NT):
                    bk = n_ // 8
                    k.op("pe", nc.tensor.transpose, ptb[:, bk, (n_ % 8) * 128:(n_ % 8 + 1) * 128],
                         kd[:, n_ * 128:(n_ + 1) * 128], identb[:], R=[b_kdc[n_], b_identb], W=[PS[bk][1]],
                         sig=(n_ % 8 == 7 or n_ == NT - 1))
                for bk in range(3):
                    nb_ = min(8, NT - 8 * bk)
                    copy_ps(evac_engine(), k2d[:, 8 * bk:8 * bk + nb_, :],
                            ptb[:, bk, 0:nb_ * 128].rearrange("p (a c) -> p a c", c=128), R=[PS[bk][1]], W=[b_k2dg[bk]])
                for n_ in range(NT):
                    bk = 3 + n_ // 4
                    k.op("pe", nc.tensor.matmul, PSA[:, bk, (n_ % 4) * 128:(n_ % 4 + 1) * 128], k2d[:, n_, :], vth[:, n_, :],
                         start=True, stop=True, R=[b_k2dg[n_ // 8], b_vthc[n_]], W=[PS[bk][1]], sig=(n_ % 4 == 3 or n_ == NT - 1))
                for b4 in range(5):
                    nb_ = min(4, NT - 4 * b4)
                    copy_ps(evac_engine(), U2[:, 4 * b4:4 * b4 + nb_, :],
                            PSA[:, 3 + b4, 0:nb_ * 128].rearrange("p (a c) -> p a c", c=128), R=[PS[3 + b4][1]], W=[b_U2g[b4]])
            F, Bw = slice(0, 64), slice(64, 128)
            TSEQ, b_TSEQ = sb("TSEQ", [128, 17])
            ESEQ, b_ESEQ = sb("ESEQ", [128, 17])
            k.op("dve", nc.vector.memset, TSEQ[:, 0:1], 0.0, W=[b_TSEQ])
            k.op("dve", nc.vector.tensor_copy, TSEQ[F, 1:17], TOT[F, 0:NLT], R=[b_TOT], W=[b_TSEQ])
            k.op("dve", nc.vector.tensor_copy, TSEQ[Bw, 1:17], TOT[Bw, NLT - 1::-1], R=[b_TOT], W=[b_TSEQ])
            k.op("act", nc.scalar.activation, ESEQ[:], TSEQ[:], AF.Exp, R=[b_TSEQ], W=[b_ESEQ])
            k.op("dve", nc.vector.memset, ESEQ[:, 0:1], 0.0, W=[b_ESEQ])
            DS, b_DS = sb("DS", [128, 128, 17])
            US, b_US = sb("US", [128, 128, 17])
            SS, b_SS = sb("SS", [128, 128, 17])
            k.op("dve", nc.vector.tensor_copy, DS[:], ESEQ[:].unsqueeze(1).broadcast_to([128, 128, 17]),
                 R=[b_ESEQ], W=[b_DS])
            k.op("dve", nc.vector.tensor_copy, US[F, :, 1:17], U2[F, 0:NLT, :].rearrange("p n v -> p v n"),
                 R=b_U2g, W=[b_US])
            k.op("dve", nc.vector.tensor_copy, US[Bw, :, 1:17], U2[Bw, NLT - 1::-1, :].rearrange("p n v -> p v n"),
                 R=b_U2g, W=[b_US])
            St, b_St = sb("St", [128, 128])

            def run_scan():
                k.op("dve", nc.vector.tensor_tensor_scan, SS[:].rearrange("p v t -> p (v t)"),
                     DS[:].rearrange("p v t -> p (v t)"), US[:].rearrange("p v t -> p (v t)"), 0.0, ALU.mult, ALU.add,
                     R=[b_DS, b_US], W=[b_SS])

            if pass_no == 1:
                k.op("dve", nc.vector.memset, US[:, :, 0], 0.0, W=[b_US])
                run_scan()
                k.op("dve", nc.vector.tensor_copy, St[:], SS[:, :, 16], R=[b_SS], W=[b_St])
                k.dma("sp", xf_in[XF_STATE + hm].rearrange("a (b c) -> (a b) c", c=128), St[:],
                      R=[b_St], W=[b_xf_in[XF_STATE + hm]])
                ls, b_ls = sb("ls", [128, 1])
                k.op("dve", nc.vector.reduce_sum, ls[:], TOT[:, 0:NLT], AX.X, R=[b_TOT], W=[b_ls])
                k.op("act", nc.scalar.activation, LAMT[:, hm:hm + 1], ls[:], AF.Exp, R=[b_ls], W=[b_LAMT])
                k.dma("sp", lsp_kt2[hm], kt2[:], R=[b_kt2], W=[b_lsp[hm]])
                k.dma("sp", lsp_vth[hm], vth[:].rearrange("p n v -> p (n v)"), R=b_vthc, W=[b_lsp[hm]])
                k.dma("sp", lsp_eb[hm], ebT[:], R=[b_ebT], W=[b_lsp[hm]])
                k.dma("sp", lsp_U2[hm], U2[:].rearrange("p n v -> p (n v)"), R=b_U2g, W=[b_lsp[hm]])
                k.dma("sp", lsp_te[hm], TE[:], R=[b_TOT], W=[b_lsp[hm]])
                return
            FR, b_FR = sb("FR", [128, 4, 128])
            LR, b_LR = sb("LR", [128, 4, 8])
            for r in range(4):
                k.dma("sp", FR[:, r, :], xf_out[XF_STATE + hm, r * 16:(r + 1) * 16, :].rearrange("a (b c) -> (a b) c", c=128),
                      R=[b_xf_out[XF_STATE + hm]], W=[b_FR])
                k.dma("sp", LR[:, r, :], xf_out[XF_LAM, r * 16, :].rearrange("(p e) -> p e", e=8),
                      R=[b_xf_out[XF_LAM]], W=[b_LR])
            Rt, b_Rt = sb("Rt", [128, 128])
            Sin, b_Sin = sb("Sin", [128, 128])
            k.op("dve", nc.vector.memset, Sin[:], 0.0, W=[b_Sin])
            k.op("dve", nc.vector.scalar_tensor_tensor, Rt[F, :], U2[F, 16, :], EEND[F, 17:18], U2[F, 17, :],
                 ALU.mult, ALU.add, R=b_U2g + [b_EEND], W=[b_Rt])
            k.op("dve", nc.vector.scalar_tensor_tensor, Rt[Bw, :], U2[Bw, 17, :], EEND[Bw, 16:17], U2[Bw, 16, :],
                 ALU.mult, ALU.add, R=b_U2g + [b_EEND], W=[b_Rt])
            for sl, order in ((F, range(4)), (Bw, range(3, -1, -1))):
                for r in order:
                    k.op("dve", nc.vector.scalar_tensor_tensor, Sin[sl, :], Rt[sl, :], onehot[sl, r:r + 1], Sin[sl, :],
                         ALU.mult, ALU.add, R=[b_Rt, b_onehot], W=[b_Sin])
                    k.op("dve", nc.vector.scalar_tensor_tensor, Rt[sl, :], Rt[sl, :], LR[sl, r, hm:hm + 1], FR[sl, r, :],
                         ALU.mult, ALU.add, R=[b_LR, b_FR], W=[b_Rt])
            k.op("dve", nc.vector.tensor_copy, US[:, :, 0], Sin[:], R=[b_Sin], W=[b_US])
            run_scan()
            S2, b_S2 = sb("S2", [128, NT, 128], BF16)
            k.op("dve", nc.vector.tensor_copy, S2[F, 0:NLT, :], SS[F, :, 0:NLT].rearrange("p v t -> p t v"),
                 R=[b_SS], W=[b_S2])
            k.op("dve", nc.vector.tensor_copy, S2[Bw, 0:NLT, :], SS[Bw, :, NLT - 1::-1].rearrange("p v t -> p t v"),
                 R=[b_SS], W=[b_S2])
            k.op("dve", nc.vector.memset, S2[F, 16, :], 0.0, W=[b_S2])
            k.op("dve", nc.vector.memset, S2[Bw, 17, :], 0.0, W=[b_S2])
            k.op("dve", nc.vector.tensor_copy, S2[F, 17, :], U2[F, 16, :], R=b_U2g, W=[b_S2])
            k.op("dve", nc.vector.tensor_copy, S2[Bw, 16, :], U2[Bw, 17, :], R=b_U2g, W=[b_S2])
            gcol = (P_RG if mix == 0 else P_GR) + h * 128
            wg_, b_wg = PF["wg"]
            nwb, b_nwb = sb("nwb", [128, 128])
            nsrc = ret_norm_w if mix == 0 else gla_norm_w
            k.dma("sp", nwb[:], nsrc[l:l + 1, h * 128:(h + 1) * 128].partition_broadcast(128), W=[b_nwb])
            G, b_G = sb("G", [128, NT, 128], BF16)
            gs, b_gs = sb("gs", [128, 8, 128])
            nchunks = NLT if last else NT
            groups = [(g0, min(8, nchunks - g0)) for g0 in range(0, nchunks, 8)]
            ubc = None
            for gi, (g0, ng) in enumerate(groups):
                bks = [6, 7] if gi % 2 == 0 else [4, 5]
                for c_ in range(ng):
                    n_ = g0 + c_
                    bi = min(n_ // 4, 4)
                    if ubc is None or ubc[0] != bi:
                        ubc = (bi,) + tuple(us.load(bi))
                    _, ub, b_ub, t0, n = ubc
                    tt = n_ - t0 // 128
                    bk = bks[c_ // 4]
                    for kc in range(8):
                        k.op("pe", nc.tensor.matmul, PSA[:, bk, (c_ % 4) * 128:(c_ % 4 + 1) * 128],
                             ub[:, kc, tt * 128:(tt + 1) * 128], wg_[:, kc, :],
                             start=(kc == 0), stop=(kc == 7), R=[b_ub, b_wg], W=[PS[bk][1]], sig=(kc == 7))
                nbk = (ng + 3) // 4
                pgv = PSA[:, bks[0]:bks[0] + nbk, :].rearrange("p b (c v) -> p (b c) v", v=128)[:, 0:ng, :]
                k.op("act", nc.scalar.activation, gs[:, 0:ng, :], pgv, AF.Silu, R=[PS[b_][1] for b_ in bks[:nbk]], W=[b_gs])
                k.op("dve", nc.vector.tensor_tensor, G[:, g0:g0 + ng, :], gs[:, 0:ng, :],
                     nwb[:].unsqueeze(1).broadcast_to([128, ng, 128]), ALU.mult, R=[b_gs, b_nwb], W=[b_G])
            t1, b_t1 = sb("g_t1", [128, 8, 128])
            t2, b_t2 = sb("g_t2", [128, 8, 128])
            PT8, b_PT8 = sb("PT8", [128, 8, 128], BF16)
            osb, b_osb = sb("osb", [128, 8, 128])
            jk, b_jk = sb("ljk", [128, 128])
            stt, b_stt = sb("stt", [128, 6, 8])
            yn8, b_yn8 = sb("yn8", [128, 8, 128])
            yb8, b_yb8 = sb("yb8", [128, 8, 128], BF16)
            yTt, b_yT = sb("yTt", [128, T], BF16)
            b_sttc = [Buf() for _ in range(8)]
            b_osbc = [Buf() for _ in range(8)]
            b_ync = [Buf() for _ in range(8)]
            for (g0, ng) in groups:
                nbk = (ng + 3) // 4
                for c_ in range(ng):
                    c0 = (g0 + c_) * 128
                    k.op("pe", nc.tensor.matmul, PSA[:, c_ // 4, (c_ % 4) * 128:(c_ % 4 + 1) * 128],
                         kt2[0:64, c0:c0 + 128], qt2[0:64, c0:c0 + 128],
                         start=True, stop=True, R=[b_kt2, b_qt2], W=[PS[c_ // 4][1]], sig=(c_ % 4 == 3 or c_ == ng - 1))
                for c_ in range(ng):
                    c0 = (g0 + c_) * 128
                    k.op("pe", nc.tensor.matmul, PSA[:, 2 + c_ // 4, (c_ % 4) * 128:(c_ % 4 + 1) * 128],
                         kt2[64:128, c0:c0 + 128], qt2[64:128, c0:c0 + 128],
                         start=True, stop=True, R=[b_kt2, b_qt2], W=[PS[2 + c_ // 4][1]], sig=(c_ % 4 == 3 or c_ == ng - 1))
                sfv = PSA[:, 0:nbk, :].rearrange("p b (c v) -> p (b c) v", v=128)[:, 0:ng, :]
                sbv = PSA[:, 2:2 + nbk, :].rearrange("p b (c v) -> p (b c) v", v=128)[:, 0:ng, :]
                k.op("dve", nc.vector.tensor_tensor, t1[:, 0:ng, :], sfv, maskF[:].unsqueeze(1).broadcast_to([128, ng, 128]),
                     ALU.mult, R=[PS[b_][1] for b_ in range(nbk)] + [b_maskF], W=[b_t1])
                k.op("dve", nc.vector.tensor_tensor, t2[:, 0:ng, :], sbv, maskB[:].unsqueeze(1).broadcast_to([128, ng, 128]),
                     ALU.mult, R=[PS[2 + b_][1] for b_ in range(nbk)] + [b_maskB], W=[b_t2])
                k.op("dve", nc.vector.tensor_tensor, PT8[:, 0:ng, :], t1[:, 0:ng, :], t2[:, 0:ng, :], ALU.add,
                     R=[b_t1, b_t2], W=[b_PT8])
                for c_ in range(ng):
                    n_ = g0 + c_
                    c0 = n_ * 128
                    bk = 4 + c_ // 4
                    oap = PSA[:, bk, (c_ % 4) * 128:(c_ % 4 + 1) * 128]
                    k.op("pe", nc.tensor.matmul, oap, PT8[:, c_, :], vth[:, n_, :],
                         start=True, stop=False, R=[b_PT8, b_vthc[n_]], W=[PS[bk][1]], sig=False)
                    k.op("pe", nc.tensor.matmul, oap, qt2[:, c0:c0 + 128], S2[:, n_, :],
                         start=False, stop=True, R=[b_qt2, b_S2], W=[PS[bk][1]], sig=(c_ % 4 == 3 or c_ == ng - 1))
                for c_ in range(ng):
                    bk = 4 + c_ // 4
                    oap = PSA[:, bk, (c_ % 4) * 128:(c_ % 4 + 1) * 128]
                    k.op("act", nc.scalar.activation, osb[:, c_, :], oap, AF.Identity, accum_out=stt[:, 0, c_:c_ + 1],
                         R=[PS[bk][1]], W=[b_osbc[c_], b_sttc[c_]])
                    k.op("act", nc.scalar.activation, jk[:], oap, AF.Square, accum_out=stt[:, 1, c_:c_ + 1],
                         R=[PS[bk][1], b_sttc[c_]], W=[])
                k.op("dve", nc.vector.tensor_scalar, stt[:, 0:2, 0:ng], stt[:, 0:2, 0:ng], 1.0 / 128, None, ALU.mult,
                     W=[b_stt] + b_sttc[0:ng])
                if mix == 0:
                    k.op("dve", nc.vector.tensor_tensor, stt[:, 3, 0:ng], stt[:, 0, 0:ng], stt[:, 0, 0:ng], ALU.mult, R=b_sttc[0:ng], W=[b_stt])
                    k.op("dve", nc.vector.tensor_tensor, stt[:, 1, 0:ng], stt[:, 1, 0:ng], stt[:, 3, 0:ng], ALU.subtract, R=b_sttc[0:ng], W=[b_stt])
                k.op("dve", nc.vector.tensor_scalar, stt[:, 3, 0:ng], stt[:, 1, 0:ng], EPS, None, ALU.add, R=b_sttc[0:ng], W=[b_stt])
                k.op("act", nc.scalar.activation, stt[:, 4, 0:ng], stt[:, 3, 0:ng], AF.Sqrt, R=b_sttc[0:ng], W=[b_stt])
                k.op("dve", nc.vector.reciprocal, stt[:, 2, 0:ng], stt[:, 4, 0:ng], R=b_sttc[0:ng], W=[b_stt])
                for c_ in range(ng):
                    if mix == 0:
                        k.op("dve", nc.vector.tensor_scalar, yn8[:, c_, :], osb[:, c_, :], stt[:, 0, c_:c_ + 1], stt[:, 2, c_:c_ + 1],
                             ALU.subtract, ALU.mult, R=[b_osbc[c_], b_stt, b_sttc[c_]], W=[b_ync[c_]])
                    else:
                        k.op("dve", nc.vector.tensor_scalar, yn8[:, c_, :], osb[:, c_, :], stt[:, 2, c_:c_ + 1], None, ALU.mult,
                             R=[b_osbc[c_], b_stt, b_sttc[c_]], W=[b_ync[c_]])
                k.op("dve", nc.vector.tensor_tensor, yb8[:, 0:ng, :], yn8[:, 0:ng, :], G[:, g0:g0 + ng, :], ALU.mult,
                     R=b_ync[0:ng] + [b_G], W=[b_yb8])
                p6b = PSA[:, 6, :].bitcast(BF16)
                for c_ in range(ng):
                    k.op("pe", nc.tensor.transpose, p6b[:, c_ * 128:(c_ + 1) * 128], yb8[:, c_, :], identb[:],
                         R=[b_yb8, b_identb], W=[PS[6][1]], sig=(c_ == ng - 1))
                copy_ps("act", yTt[:, g0 * 128:(g0 + ng) * 128], p6b[:, 0:ng * 128], R=[PS[6][1]], W=[b_yT])
            ntok = LAT if last else T
            k.dma("sp", yTd[1 + mix, h, :, 0:ntok], yTt[:, 0:ntok], R=[b_yT], W=[b_yTd[1 + mix]])

    def phase_linear(l, pass_no, last):
        order = [(mix, h) for mix in range(2) for h in range(4)]
        if pass_no == 1:
            for (mix, h) in order:
                linear_mixer(l, mix, h, pass_no, last)
                k.fence()
        else:
            with ExitStack() as pes:
                PFs = []
                for si in range(2):
                    PF = {}
                    for nm, shp, dt in (("w", [128, 8, 512], BF16), ("wg", [128, 8, 128], BF16), ("kt2", [128, T], BF16),
                                        ("vth", [128, NT, 128], BF16), ("ebT", [128, T], BF16), ("U2", [128, NT, 128], F32),
                                        ("TE", [128, 2 * NT], F32)):
                        PF[nm] = (pes.enter_context(nc.sbuf_tensor(U(f"pf_{nm}{si}"), shp, dt)), Buf(f"pf_{nm}{si}"))
                    PFs.append(PF)
                lm_prefetch(l, order[0][0], order[0][1], PFs[0])
                for i_, (mix, h) in enumerate(order):
                    if i_ + 1 < len(order):
                        lm_prefetch(l, order[i_ + 1][0], order[i_ + 1][1], PFs[(i_ + 1) % 2])
                    linear_mixer(l, mix, h, pass_no, last, PF=PFs[i_ % 2])
                    k.fence()
            k.fence()
        if pass_no == 1:
            k.dma("sp", xf_in[XF_LAM, 0, :].rearrange("(p e) -> p e", e=8), LAMT[:], R=[b_LAMT], W=[b_xf_in[XF_LAM]])
            for j in range(NXF):
                k.allgather(xf_in[j], xf_out[j], R=[b_xf_in[j]], W=[b_xf_out[j]])
            k.fence()


    G8, b_G8 = salloc("G8", [128, NT, 8])

    def phase_attn(l, last):
        SCL = 96.0 ** -0.5
        cqn, b_cqn = LS["cqn"]; ckvn, b_ckvn = LS["ckvn"]; krr, b_krr = LS["krr"]
        with ExitStack() as es:
            def sb(name, shape, dt=F32):
                return es.enter_context(nc.sbuf_tensor(U(name), list(shape), dt)), Buf(name)
            ckvA, b_ckvA = sb("ckvA", [128, NKEY], BF16)
            k.op("pool", nc.gpsimd.tensor_copy, ckvA[:, 0:CTX], ckvn[:, LAT:T], R=[b_ckvn], W=[b_ckvA])
            for j in range(8):
                k.dma("sp", ckvA[16 * j:16 * j + 16, CTX:NKEY].rearrange("p (r t) -> p r t", r=4),
                      xb_out[XB_CKV + j].rearrange("(r p) t -> p r t", p=16),
                      R=[b_xb_out[XB_CKV + j]], W=[b_ckvA])
            KTs = [sb(f"KT{i}", [96, NKEY], BF16) for i in range(2)]
            Vs = [sb(f"V{i}", [128, NKT, 128], BF16) for i in range(2)]
            for (KT, bKT), (V, bV) in zip(KTs, Vs):
                k.dma("sp", KT[64:96, 0:CTX], krr[:, LAT:T], R=[b_krr], W=[bKT])
                for j in range(2):
                    k.dma("sp", KT[64 + 16 * j:64 + 16 * j + 16, CTX:NKEY].rearrange("p (r t) -> p r t", r=4),
                          xb_out[XB_KR + j].rearrange("(r p) t -> p r t", p=16),
                          R=[b_xb_out[XB_KR + j]], W=[bKT])
                k.op("pool", nc.gpsimd.memset, V[:, :, 64:128], 1.0, W=[bV])
            ropq_r = Ring([sb(f"ropq{i}", [96, 2, 512]) for i in range(2)])
            yat_r = Ring([sb(f"yat{i}", [64, 512], BF16) for i in range(3)])
            QTs = Ring([sb(f"QT{i}", [96, T], BF16) for i in range(2)])
            wq_r = Ring([sb(f"wq{i}", [128, 2, 96], BF16) for i in range(2)])
            wqs_r = Ring([sb(f"wqs{i}", [128, 2, 96], BF16) for i in range(2)])
            wkv_r = Ring([sb(f"wkv{i}", [128, 128], BF16) for i in range(2)])
            t1r = Ring([sb(f"at1{i}", [96, 512]) for i in range(2)])
            t2r = Ring([sb(f"at2{i}", [96, 512]) for i in range(2)])
            Pr = Ring([sb(f"P{i}", [128, 512], BF16) for i in range(4)])
            linv_r = Ring([sb(f"linv{i}", [128, 512]) for i in range(2)])
            sring = Ring([PS[0], PS[1], PS[2], PS[3]])
            oring = Ring([PS[4], PS[5]])
            qblocks = [(q0, 512, list(range(NKT))) for q0 in range(0, LAT, 512)]
            if not last:
                qblocks.append((LAT, CTX, [0, 1]))
            def build_head(h):
                wq, b_wq = wq_r.next(); wqs, b_wqs = wqs_r.next(); wkv, b_wkv = wkv_r.next()
                k.dma("pool", wq[:], w_uq[l].rearrange("(kc p) n -> p kc n", p=128)[:, :, h * 96:(h + 1) * 96], W=[b_wq])
                k.dma("pool", wqs[:], w_uq_sw[l].rearrange("(kc p) n -> p kc n", p=128)[:, :, h * 96:(h + 1) * 96], W=[b_wqs])
                k.dma("pool", wkv[:], w_ukv[l][:, h * 128:(h + 1) * 128], W=[b_wkv])
                QT, bQT = QTs.next()
                KT, bKT = KTs[h % 2]
                V, bV = Vs[h % 2]
                built[h] = (QT, bQT, KT, bKT, V, bV)
                yield
                blks = BLKS[:4] if last else BLKS
                for bi, (t0, n) in enumerate(blks):
                    (pa, bpa), (pb, bpb) = PS[6], PS[7]
                    for (pp, bpp, ww, bww) in ((pa, bpa, wq, b_wq), (pb, bpb, wqs, b_wqs)):
                        for kc in range(2):
                            k.op("pe", nc.tensor.matmul, pp[0:96, 0:n], ww[:, kc, :], cqn[:, kc, t0:t0 + n],
                                 start=(kc == 0), stop=(kc == 1), R=[bww, b_cqn], W=[bpp], sig=(kc == 1))
                    k.op("dve", nc.vector.tensor_scalar, QT[0:64, t0:t0 + n], pa[0:64, 0:n], SCL, None, ALU.mult,
                         R=[bpa], W=[bQT])
                    ta, bta = t1r.next(); tb, btb = t2r.next()
                    ropq, b_ropq = ropq_r.next()
                    k.dma("sp", ropq[64:96, 0, 0:n], c_ropeAq[0, :, t0:t0 + n], W=[b_ropq])
                    k.dma("sp", ropq[64:96, 1, 0:n], c_ropeAq[1, :, t0:t0 + n], W=[b_ropq])
                    k.op("dve", nc.vector.tensor_tensor, ta[64:96, 0:n], pa[64:96, 0:n], ropq[64:96, 0, 0:n],
                         ALU.mult, R=[bpa, b_ropq], W=[bta])
                    k.op("dve", nc.vector.tensor_tensor, tb[64:96, 0:n], pb[64:96, 0:n], ropq[64:96, 1, 0:n],
                         ALU.mult, R=[bpb, b_ropq], W=[btb])
                    k.op("dve", nc.vector.tensor_tensor, QT[64:96, t0:t0 + n], ta[64:96, 0:n], tb[64:96, 0:n],
                         ALU.add, R=[bta, btb], W=[bQT])
                    yield
                for kb in range((NKEY + 511) // 512):
                    c0 = kb * 512
                    n = min(512, NKEY - c0)
                    pk, bpk = PS[6 + kb % 2]
                    k.op("pe", nc.tensor.matmul, pk[0:64, 0:n], wkv[:, 0:64], ckvA[:, c0:c0 + n], start=True, stop=True,
                         R=[b_wkv, b_ckvA], W=[bpk])
                    copy_ps("dve", KT[0:64, c0:c0 + n], pk[0:64, 0:n], R=[bpk], W=[bKT])
                    yield
                for g0 in range(0, NKT, 8):
                    gn = min(8, NKT - g0)
                    pv, bpv = PS[7]
                    for i_ in range(gn):
                        kt = g0 + i_
                        k.op("pe", nc.tensor.matmul, pv[:, i_ * 64:(i_ + 1) * 64], ckvA[:, kt * 128:(kt + 1) * 128],
                             wkv[:, 64:128], start=True, stop=True, R=[b_ckvA, b_wkv], W=[bpv], sig=(i_ == gn - 1))
                    copy_ps("dve", V[:, g0:g0 + gn, 0:64],
                            pv[:, 0:gn * 64].rearrange("p (a b) -> p a b", b=64), R=[bpv], W=[bV])
                    yield

            built = {}
            for _ in build_head(0):
                pass
            for h in range(8):
                QT, bQT, KT, bKT, V, bV = built[h]
                gen = build_head(h + 1) if h + 1 < 8 else iter(())
                step_no = 0
                for (q0, nq, ktiles) in qblocks:
                    po, bpo = oring.next()

                    def issue_s(kt):
                        ps_, bps_ = sring.next()
                        k.op("pe", nc.tensor.matmul, ps_[:, 0:nq], KT[0:96, kt * 128:(kt + 1) * 128], QT[0:96, q0:q0 + nq],
                             start=True, stop=True, R=[bKT, bQT], W=[bps_])
                        return ps_, bps_
                    LA = 3
                    pend = [issue_s(kt_) for kt_ in ktiles[:LA]]
                    for i_, kt in enumerate(ktiles):
                        if i_ + LA < len(ktiles):
                            pend.append(issue_s(ktiles[i_ + LA]))
                        cur = pend.pop(0)
                        P, bP = Pr.next()
                        k.op("act", nc.scalar.activation, P[:, 0:nq], cur[0][:, 0:nq], AF.Exp, R=[cur[1]], W=[bP])
                        lastk = (i_ == len(ktiles) - 1)
                        k.op("pe", nc.tensor.matmul, po[:, 0:nq], V[:, kt, :], P[:, 0:nq], start=(i_ == 0), stop=lastk,
                             R=[bV, bP], W=[bpo], sig=True)
                        step_no += 1
                        if step_no % 6 == 0:
                            next(gen, None)
                    linv, b_linv = linv_r.next()
                    k.op("dve", nc.vector.reciprocal, linv[64:128, 0:nq], po[64:128, 0:nq], R=[bpo], W=[b_linv])
                    r0 = (h % 2) * 64
                    yat, b_yat = yat_r.next()
                    k.op("dve", nc.vector.tensor_tensor, yat[:, 0:nq], po[0:64, 0:nq],
                         linv[64:128, 0:nq], ALU.mult, R=[bpo, b_linv], W=[b_yat])
                    k.dma("sp", yTd[0, h // 2, r0:r0 + 64, q0:q0 + nq], yat[:, 0:nq], R=[b_yat], W=[b_yTd[0]])
                for _ in gen:
                    pass
        k.fence()

    def phase_merge(l, last):
        blks = list(enumerate(BLKS[:4] if last else BLKS))
        with ExitStack() as es:
            def sb(name, shape, dt=F32):
                return es.enter_context(nc.sbuf_tensor(U(name), list(shape), dt)), Buf(name)
            zT, b_zT = sb("zT", [128, 8, T], BF16)
            us = UStream(es)
            yb_r = Ring([sb(f"myb{i}", [128, 3, 4, 512], BF16) for i in range(2)])
            g1, b_g1 = sb("g1", [128, 2, D])
            for r in range(2):
                k.dma("sp", g1[:, r, :], modD[r:r + 1, 2 * D:3 * D].partition_broadcast(128), R=[b_modD], W=[b_g1])
            wg_r = Ring([sb(f"mwg{i}", [128, 8, 3, 128], BF16) for i in range(2)])
            wb_r = Ring([sb(f"mwb{i}", [128, 3, 4, 128], BF16) for i in range(2)])
            gj_r = Ring([sb(f"gj{i}", [128, 512]) for i in range(2)])
            za_r = Ring([sb(f"za{i}", [128, 512]) for i in range(2)])
            zt_r = Ring([sb(f"zt{i}", [128, 512]) for i in range(2)])
            pgr = Ring([PS[0], PS[1], PS[2]])
            pzr = Ring([PS[3], PS[4], PS[5]])
            for c in range(8):
                wg3, b_wg3 = wg_r.next(); wb3, b_wb3 = wb_r.next()
                for j in range(3):
                    col = P_BG + j * 1024 + c * 128
                    k.dma("pool", wg3[:, :, j, :], wview(wp[l])[:, :, col:col + 128], W=[b_wg3])
                    k.dma("pool", wb3[:, j, :, :], w_branch[l, j].rearrange("(k4 p) n -> p k4 n", p=128)[:, :, c * 128:(c + 1) * 128],
                          W=[b_wb3])
                for bi, (t0, n) in blks:
                    ub, b_ub, _, _ = us.load(bi)
                    yb3, b_yb3 = yb_r.next()
                    for j in range(3):
                        k.dma("sp", yb3[:, j, :, 0:n], yTd[j, :, :, t0:t0 + n].rearrange("k p t -> p k t"),
                              R=[b_yTd[j]], W=[b_yb3])
                    za, bza = za_r.next()
                    for j in range(3):
                        pg, bpg = pgr.next()
                        for kc in range(8):
                            k.op("pe", nc.tensor.matmul, pg[:, 0:n], wg3[:, kc, j, :], ub[:, kc, 0:n],
                                 start=(kc == 0), stop=(kc == 7), R=[b_wg3, b_ub], W=[bpg], sig=(kc == 7))
                        gj, bgj = gj_r.next()
                        k.op("act", nc.scalar.activation, gj[:, 0:n], pg[:, 0:n], AF.Sigmoid, R=[bpg], W=[bgj])
                        pz, bpz = pzr.next()
                        for k4 in range(4):
                            k.op("pe", nc.tensor.matmul, pz[:, 0:n], wb3[:, j, k4, :], yb3[:, j, k4, 0:n],
                                 start=(k4 == 0), stop=(k4 == 3), R=[b_wb3, b_yb3], W=[bpz], sig=(k4 == 3))
                        if j == 0:
                            k.op("dve", nc.vector.tensor_tensor, za[:, 0:n], pz[:, 0:n], gj[:, 0:n], ALU.mult,
                                 R=[bpz, bgj], W=[bza])
                        else:
                            zt, bzt = zt_r.next()
                            k.op("dve", nc.vector.tensor_tensor, zt[:, 0:n], pz[:, 0:n], gj[:, 0:n], ALU.mult,
                                 R=[bpz, bgj], W=[bzt])
                            if j == 1:
                                k.op("dve", nc.vector.tensor_tensor, za[:, 0:n], za[:, 0:n], zt[:, 0:n], ALU.add,
                                     R=[bzt], W=[bza])
                            else:
                                k.op("dve", nc.vector.tensor_tensor, zT[:, c, t0:t0 + n], za[:, 0:n], zt[:, 0:n], ALU.add,
                                     R=[bza, bzt], W=[b_zT])
            wo, b_wo = load_w(es, "wo", wview(w_out[l]), [128, 8, D])
            xr = Ring([sb(f"mx{i}", [128, D]) for i in range(2)])
            tmr = Ring([sb(f"mt{i}", [128, 512]) for i in range(2)])
            pyr = Ring([PS[6], PS[7]])
            tiles = list(range(NLT)) if last else list(range(NT))
            for t in tiles:
                r = 0 if t < NLT else 1
                xt, b_xt = xr.next()
                k.dma("sp", xt[:], xs[t * 128:(t + 1) * 128, :], R=[b_xs[t]], W=[b_xt])
                for half in range(2):
                    py, bpy = pyr.next()
                    for kc in range(8):
                        k.op("pe", nc.tensor.matmul, py[:, :], zT[:, kc, t * 128:(t + 1) * 128],
                             wo[:, kc, half * 512:(half + 1) * 512], start=(kc == 0), stop=(kc == 7),
                             R=[b_zT, b_wo], W=[bpy], sig=(kc == 7))
                    tm, btm = tmr.next()
                    k.op("dve", nc.vector.tensor_tensor, tm[:], py[:, :], g1[:, r, half * 512:(half + 1) * 512], ALU.mult,
                         R=[bpy, b_g1], W=[btm])
                    k.op("dve", nc.vector.tensor_tensor, xt[:, half * 512:(half + 1) * 512], xt[:, half * 512:(half + 1) * 512],
                         tm[:], ALU.add, R=[btm], W=[b_xt])
                k.dma("sp", xs[t * 128:(t + 1) * 128, :], xt[:], R=[b_xt], W=[b_xs[t]])
        k.fence()

    def phase_router(i, tiles):
        with ExitStack() as es:
            def sb(name, shape, dt=F32):
                return es.enter_context(nc.sbuf_tensor(U(name), list(shape), dt)), Buf(name)
            rw, b_rw = load_w(es, "rw", wview(moe_router[i]), [128, 8, NEXP])
            us = UStream(es)
            lg_r = Ring([sb(f"rl{i_}", [128, 8]) for i_ in range(2)])
            m8_r = Ring([sb(f"rm{i_}", [128, 8]) for i_ in range(2)])
            ex_r = Ring([sb(f"re{i_}", [128, 8]) for i_ in range(2)])
            mk_r = Ring([sb(f"rk{i_}", [128, 8]) for i_ in range(2)])
            sc_r = Ring([sb(f"rs{i_}", [128, 4]) for i_ in range(2)])
            ubc = None
            for t in tiles:
                bi = min(t // 4, 4)
                if ubc is None or ubc[0] != bi:
                    ubc = (bi,) + tuple(us.load(bi))
                _, ub, b_ub, t0, n = ubc
                tt = t - t0 // 128
                pl, bpl = PS[t % 2]
                for kc in range(8):
                    k.op("pe", nc.tensor.matmul, pl[:, 0:NEXP], ub[:, kc, tt * 128:(tt + 1) * 128], rw[:, kc, :],
                         start=(kc == 0), stop=(kc == 7), R=[b_ub, b_rw], W=[bpl], sig=(kc == 7))
                lg, blg = lg_r.next(); m8, bm8 = m8_r.next(); ex, bex = ex_r.next(); mk, bmk = mk_r.next()
                sc_, bsc = sc_r.next()
                k.op("dve", nc.vector.tensor_copy, lg[:], pl[:, 0:NEXP], R=[bpl], W=[blg])
                k.op("dve", nc.vector.max, m8[:], lg[:], R=[blg], W=[bm8])
                k.op("dve", nc.vector.tensor_scalar, mk[:], lg[:], m8[:, 1:2], None, ALU.is_ge, R=[blg, bm8], W=[bmk])
                k.op("dve", nc.vector.tensor_scalar, sc_[:, 0:1], m8[:, 0:1], -1.0, None, ALU.mult, R=[bm8], W=[bsc])
                k.op("act", nc.scalar.activation, ex[:], lg[:], AF.Exp, bias=sc_[:, 0:1], R=[blg, bsc], W=[bex])
                k.op("dve", nc.vector.tensor_tensor, ex[:], ex[:], mk[:], ALU.mult, R=[bmk], W=[bex])
                k.op("dve", nc.vector.reduce_sum, sc_[:, 1:2], ex[:], AX.X, R=[bex], W=[bsc])
                k.op("dve", nc.vector.reciprocal, sc_[:, 2:3], sc_[:, 1:2], W=[bsc])
                k.op("dve", nc.vector.tensor_scalar, G8[:, t, :], ex[:], sc_[:, 2:3], None, ALU.mult, R=[bex, bsc], W=[b_G8])
        k.fence()

    def phase_ffn(l, last):
        moe = (l % 2 == 1)
        i = l // 2
        nexp = NEXP if moe else 1
        groups = [(0, 1024), (1024, 1024)] if last else [(0, 1152), (1152, 1152)]
        for (g0, gn) in groups:
            with ExitStack() as es:
                def sb(name, shape, dt=F32):
                    return es.enter_context(nc.sbuf_tensor(U(name), list(shape), dt)), Buf(name)
                vTg, b_vTg = sb("vTg", [128, 8, gn], BF16)
                ublks = sorted(set(min(t_ // 4, 4) for t_ in range(g0 // 128, (g0 + gn) // 128)))
                k.dma("sp", vTg[:], uT[:, :, g0:g0 + gn].rearrange("kc p t -> p kc t"),
                      R=[b_uT[b_] for b_ in ublks], W=[b_vTg])
                hT, b_hT = sb("hT", [128, NFF, gn], BF16)
                acc, b_acc = sb("acc", [128, gn // 128, D])
                wd, b_wd = sb("wd", [128, NFF, D], BF16)
                wt_r = Ring([sb(f"wgu{i_}", [128, 8, 2, 256], BF16) for i_ in range(2)])
                sg_r = Ring([sb(f"sg{i_}", [128, 512]) for i_ in range(3)])
                pgr = Ring([PS[0], PS[1]])
                pur = Ring([PS[2], PS[3]])
                pyr = Ring([PS[4], PS[5], PS[6], PS[7]])
                bsz = 512 if gn % 512 == 0 else 384
                nblks = [(c0, min(bsz, gn - c0)) for c0 in range(0, gn, bsz)]
                def wsrc(e):
                    if moe:
                        return moe_wg[i, e], moe_wu[i, e], moe_wd[i, e]
                    return ffn_wg[i], ffn_wu[i], ffn_wd[i]
                tasks = [(e, fc2) for e in range(nexp) for fc2 in range(NFF // 2)]
                loaded = {}

                def issue_load(ti):
                    if ti >= len(tasks) or ti in loaded:
                        return
                    e_, fc2_ = tasks[ti]
                    Wg_, Wu_, _ = wsrc(e_)
                    wt_, b_wt_ = wt_r.next()
                    k.dma("pool", wt_[:, :, 0, :], wview(Wg_)[:, :, fc2_ * 256:(fc2_ + 1) * 256], W=[b_wt_])
                    k.dma("pool", wt_[:, :, 1, :], wview(Wu_)[:, :, fc2_ * 256:(fc2_ + 1) * 256], W=[b_wt_])
                    loaded[ti] = (wt_, b_wt_)
                issue_load(0)
                for e in range(nexp):
                    Wg, Wu, Wd = wsrc(e)
                    for fc2 in range(NFF // 2):
                        ti = e * (NFF // 2) + fc2
                        issue_load(ti)
                        issue_load(ti + 1)
                        if fc2 == 0:
                            k.dma("pool", wd[:], Wd.rearrange("(f p) n -> p f n", p=128), W=[b_wd])
                        wt, b_wt = loaded.pop(ti)
                        for sub in range(2):
                            fc = fc2 * 2 + sub
                            for (c0, n) in nblks:
                                pg, bpg = pgr.next(); pu, bpu = pur.next()
                                for kc in range(8):
                                    k.op("pe", nc.tensor.matmul, pg[:, 0:n], wt[:, kc, 0, sub * 128:(sub + 1) * 128],
                                         vTg[:, kc, c0:c0 + n], start=(kc == 0), stop=(kc == 7),
                                         R=[b_wt, b_vTg], W=[bpg], sig=(kc == 7))
                                for kc in range(8):
                                    k.op("pe", nc.tensor.matmul, pu[:, 0:n], wt[:, kc, 1, sub * 128:(sub + 1) * 128],
                                         vTg[:, kc, c0:c0 + n], start=(kc == 0), stop=(kc == 7),
                                         R=[b_wt, b_vTg], W=[bpu], sig=(kc == 7))
                                sg, bsg = sg_r.next()
                                k.op("act", nc.scalar.activation, sg[:, 0:n], pg[:, 0:n], AF.Silu, R=[bpg], W=[bsg])
                                k.op("dve", nc.vector.tensor_tensor, hT[:, fc, c0:c0 + n], sg[:, 0:n], pu[:, 0:n], ALU.mult,
                                     R=[bsg, bpu], W=[b_hT])
                    for tt in range(gn // 128):
                        t = g0 // 128 + tt
                        for half in range(2):
                            py, bpy = pyr.next()
                            for fc in range(NFF):
                                k.op("pe", nc.tensor.matmul, py[:, :], hT[:, fc, tt * 128:(tt + 1) * 128],
                                     wd[:, fc, half * 512:(half + 1) * 512], start=(fc == 0), stop=(fc == NFF - 1),
                                     R=[b_hT, b_wd], W=[bpy], sig=(fc == NFF - 1))
                            dst = acc[:, tt, half * 512:(half + 1) * 512]
                            if not moe:
                                copy_ps(evac_engine(), dst, py[:, :], R=[bpy], W=[b_acc])
                            elif e == 0:
                                k.op("dve", nc.vector.tensor_scalar, dst, py[:, :], G8[:, t, e:e + 1], None, ALU.mult,
                                     R=[bpy, b_G8], W=[b_acc])
                            else:
                                k.op("dve", nc.vector.scalar_tensor_tensor, dst, py[:, :], G8[:, t, e:e + 1], dst,
                                     ALU.mult, ALU.add, R=[bpy, b_G8], W=[b_acc])
                xr = Ring([sb(f"fx{i_}", [128, D]) for i_ in range(2)])
                jr = Ring([sb(f"fj{i_}", [128, D]) for i_ in range(2)])
                ssr = Ring([sb(f"fs{i_}", [128, 1]) for i_ in range(2)])
                g2, b_g2 = sb("g2", [128, 2, D])
                for rg_ in range(1 if last else 2):
                    k.dma("sp", g2[:, rg_, :], modD[rg_:rg_ + 1, 5 * D:6 * D].partition_broadcast(128), R=[b_modD], W=[b_g2])
                if last:
                    fnw, b_fnw = sb("fnw", [128, D])
                    k.dma("sp", fnw[:], final_norm_w.rearrange("(o d) -> o d", o=1).partition_broadcast(128), W=[b_fnw])
                for tt in range(gn // 128):
                    t = g0 // 128 + tt
                    r = 0 if t < NLT else 1
                    xt, b_xt = xr.next()
                    k.dma("sp", xt[:], xs[t * 128:(t + 1) * 128, :], R=[b_xs[t]], W=[b_xt])
                    k.op("dve", nc.vector.tensor_tensor, acc[:, tt, :], acc[:, tt, :], g2[:, r, :], ALU.mult,
                         R=[b_g2], W=[b_acc])
                    k.op("dve", nc.vector.tensor_tensor, xt[:], xt[:], acc[:, tt, :], ALU.add, R=[b_acc], W=[b_xt])
                    if not last:
                        k.dma("sp", xs[t * 128:(t + 1) * 128, :], xt[:], R=[b_xt], W=[b_xs[t]])
                    else:
                        jk, b_jk = jr.next(); ss, b_ss = ssr.next()
                        k.op("act", nc.scalar.activation, jk[:], xt[:], AF.Square, accum_out=ss[:, 0:1],
                             R=[b_xt], W=[b_jk, b_ss])
                        rstd, b_rstd = rsqrt_col(es, f"fr{t}", ss[:, 0:1], b_ss, 1.0 / D)
                        k.op("dve", nc.vector.scalar_tensor_tensor, jk[:], xt[:], rstd[:, 0:1], fnw[:], ALU.mult, ALU.mult,
                             R=[b_xt, b_rstd, b_fnw], W=[b_jk])
                        k.dma("sp", out[t * 128:(t + 1) * 128, :], jk[:], R=[b_jk], W=[b_out])
            k.fence()

    stop = dbg.get("_stop") if isinstance(dbg, dict) else None

    def tap(name, src_ap, bufs):
        if name in dbg_out:
            k.dma("sp", dbg_out[name], src_ap, R=bufs, W=[b_out])

    for l in range(nlayers):
        last = (l == DEPTH - 1)
        alltiles = list(range(NT))
        phase_mod(l)
        phase_norm(l, A1, b_A1, 0, alltiles)
        phase_lg(l)
        with ExitStack() as les:
            for nm, shp in (("cqn", [128, 2, T]), ("ckvn", [128, T]), ("krr", [32, T]), ("gzT", [32, T])):
                LS[nm] = (les.enter_context(nc.sbuf_tensor(U(nm), shp, BF16)), Buf(nm))
            phase_q(l)
            phase_linear(l, 1, last)
            phase_linear(l, 2, last)
            phase_attn(l, last)
        k.fence()
        phase_merge(l, last)
        ftiles = list(range(NLT)) if last else alltiles
        phase_norm(l, A2, b_A2, 24, ftiles)
        if l % 2 == 1:
            phase_router(l // 2, ftiles)
        phase_ffn(l, last)
    if nlayers < DEPTH:
        for t in range(NLT):
            k.dma("sp", out[t * 128:(t + 1) * 128, :], xs[t * 128:(t + 1) * 128, :], R=[b_xs[t]], W=[b_out])
    k.wait_bufs("sp", [b_out])
    return nc, k


IN_SPLITS = (256, 128, 32, 256, 256, 512, 512, 256, 256, 512, 512, 32, 3072)
OFFS = np.concatenate([[0], np.cumsum(IN_SPLITS)]).astype(int)
(O_CQ, O_CKV, O_KR, O_RQ, O_RK, O_RV, O_RG, O_GQ, O_GK, O_GV, O_GR, O_GZ, O_BG) = OFFS[:13]


def _partner(n, half):
    idx = np.arange(n)
    return np.where(idx % (2 * half) < half, idx + half, idx - half)


def _pack_w_in(w_in_l):
    cols = []
    pa = _partner(32, 8)
    cols.append(np.arange(O_CQ, O_CQ + 256))
    cols.append(np.arange(O_CKV, O_CKV + 128))
    cols.append(np.arange(O_KR, O_KR + 32))
    cols.append(O_KR + pa)
    cols.append(np.arange(O_GZ, O_GZ + 32))
    pr = _partner(64, 32)
    for h in range(4):
        q = O_RQ + h * 64 + np.arange(64)
        qs = O_RQ + h * 64 + pr
        kk = O_RK + h * 64 + np.arange(64)
        ks = O_RK + h * 64 + pr
        cols += [q, q, qs, qs, kk, kk, ks, ks]
    for h in range(4):
        q = O_GQ + h * 64 + np.arange(64)
        kk = O_GK + h * 64 + np.arange(64)
        cols += [q, q, kk, kk]
    cols.append(np.arange(O_RV, O_RV + 512))
    cols.append(np.arange(O_GV, O_GV + 512))
    cols.append(np.arange(O_RG, O_RG + 512))
    cols.append(np.arange(O_GR, O_GR + 512))
    cols.append(np.arange(O_BG, O_BG + 3072))
    cols = np.concatenate(cols)
    assert cols.shape[0] == NPACK
    return np.ascontiguousarray(w_in_l[:, cols])


def _rope_tables(pos, half, signed_rows):
    inv = 10000.0 ** (-np.arange(half, dtype=np.float32) / half)
    ang = pos.astype(np.float32)[None, :] * inv[:, None]
    cos = np.cos(ang).astype(np.float32)
    sin = np.sin(ang).astype(np.float32)
    return np.concatenate([cos, cos], 0), np.concatenate([-sin, sin], 0)


def _consts(q):
    pos = q * LAT + np.arange(LAT)
    c, s = _rope_tables(pos, 32, True)
    ropeR = np.zeros((2, 128, T), np.float32)
    ropeR[0, :, :LAT] = np.concatenate([c, c], 0)
    ropeR[1, :, :LAT] = np.concatenate([s, s], 0)
    ropeR[0, :, LAT:] = 1.0
    cr, sr = _rope_tables(pos // 64, 8, True)
    cc, sc = _rope_tables(pos % 64, 8, True)
    ak = np.zeros((2, 32, T), np.float32)
    ak[0, :, :LAT] = np.concatenate([cr, cc], 0)
    ak[1, :, :LAT] = np.concatenate([sr, sc], 0)
    ak[0, :, LAT:] = 1.0
    aq = (ak * np.float32(96.0 ** -0.5)).astype(np.float32)
    rst = np.ones((128, T), np.float32)
    rst[:, ::128] = 0.0
    j = np.arange(128)[:, None]
    i = np.arange(128)[None, :]
    mask = np.stack([(i >= j), (j >= i)]).astype(np.float32)
    onehot = np.zeros((128, 4), np.float32)
    onehot[:, q] = 1.0
    return dict(c_ropeR=ropeR, c_ropeAq=aq, c_ropeAk=ak, c_rst=rst, c_mask=mask,
                c_ident=np.eye(128, dtype=np.float32), c_onehot=onehot)


def make_in_maps(inp):
    f = lambda a: np.ascontiguousarray(np.asarray(a, dtype=np.float32))
    x, c, ctx, c_ctx = f(inp["x"]), f(inp["c"]), f(inp["ctx"]), f(inp["c_ctx"])
    w_in = f(inp["w_in"])
    wp_ = np.stack([_pack_w_in(w_in[l]) for l in range(DEPTH)])
    w_uq = f(inp["mla_w_uq"])
    pa = _partner(32, 8)
    cols = np.arange(768)
    for h in range(8):
        cols[h * 96 + 64:h * 96 + 96] = h * 96 + 64 + pa
    w_uq_sw = np.ascontiguousarray(w_uq[:, :, cols])
    gw = f(inp["gla_w_gate"])
    gb = f(inp["gla_b_gate"])
    gwblk = np.zeros((DEPTH, 4, 32, 128), np.float32)
    gbias = np.zeros((DEPTH, 4, 128), np.float32)
    for h in range(4):
        gwblk[:, h, 0:16, 0:64] = gw[:, 0, :, h * 64:(h + 1) * 64]
        gwblk[:, h, 16:32, 64:128] = gw[:, 1, :, h * 64:(h + 1) * 64]
        gbias[:, h, 0:64] = gb[:, 0, h * 64:(h + 1) * 64]
        gbias[:, h, 64:128] = gb[:, 1, h * 64:(h + 1) * 64]
    shared = dict(
        mod_w=f(inp["mod_w"]), mod_b=f(inp["mod_b"]), norm1_w=f(inp["norm1_w"]), norm2_w=f(inp["norm2_w"]),
        wp=wp_, mla_q_norm=f(inp["mla_q_norm"]), w_uq=w_uq, w_uq_sw=w_uq_sw, mla_kv_norm=f(inp["mla_kv_norm"]),
        w_ukv=f(inp["mla_w_ukv"]), ret_decay_logit=f(inp["ret_decay_logit"]), ret_norm_w=f(inp["ret_norm_w"]),
        gwblk=gwblk, gbias=gbias, gla_norm_w=f(inp["gla_norm_w"]), w_branch=f(inp["w_branch"]), w_out=f(inp["w_out"]),
        ffn_w_gate=f(inp["ffn_w_gate"]), ffn_w_up=f(inp["ffn_w_up"]), ffn_w_down=f(inp["ffn_w_down"]),
        moe_router=f(inp["moe_router"]), moe_w_gate=f(inp["moe_w_gate"]), moe_w_up=f(inp["moe_w_up"]),
        moe_w_down=f(inp["moe_w_down"]), final_norm_w=f(inp["final_norm_w"]),
    )
    maps = []
    for core in range(NCORES):
        b, q = core // 4, core % 4
        m = dict(shared)
        m["xs_in"] = np.ascontiguousarray(np.concatenate([x[b, q * LAT:(q + 1) * LAT], ctx[b]], 0))
        m["cvecT"] = np.ascontiguousarray(np.stack([c[b], c_ctx], 1))
        m.update(_consts(q))
        maps.append(m)
    return maps


_PROG = {}


def kernel(**inputs):
    if "nc" not in _PROG:
        _PROG["nc"] = build_program()[0]
    nc = _PROG["nc"]
    maps = make_in_maps(inputs)
    res = run_bass_kernel_spmd(nc, maps, core_ids=list(range(NCORES)))
    outp = np.zeros((2, SEQ, D), np.float32)
    for core in range(NCORES):
        b, q = core // 4, core % 4
        outp[b, q * LAT:(q + 1) * LAT] = res.results[core]["out"]
    return outp
```

```python
import math
from contextlib import ExitStack
import numpy as np
import ml_dtypes
import concourse.bass as bass
import concourse.mybir as mybir
from concourse.bass_utils import run_bass_kernel_spmd

F32 = mybir.dt.float32
BF16 = mybir.dt.bfloat16
AF = mybir.ActivationFunctionType
ALU = mybir.AluOpType
AX = mybir.AxisListType

NCORES = 8
D = 1024
SEQ = 8192
CTX = 256
LAT = 2048
T = LAT + CTX
NT = T // 128
NLT = LAT // 128
DEPTH = 2
EPS = 1e-6
DFF = 2816
NFF = DFF // 128
NEXP = 8
NKEY = CTX + SEQ
NKT = NKEY // 128

PA = 0
PA_N = 480
P_RET = 480
P_GLA = P_RET + 4 * 512
P_RV = P_GLA + 4 * 256
P_GV = P_RV + 512
P_RG = P_GV + 512
P_GR = P_RG + 512
P_BG = P_GR + 512
NPACK = P_BG + 3072

XF_STATE = 0
XF_LAM = 8
NXF = 9
XB_CKV = 0
XB_KR = 8
NXB = 10

EPOCH = 30000


class Buf:
    __slots__ = ("name", "w", "r")

    def __init__(self, name=""):
        self.name = name
        self.w = None
        self.r = []


class Ring:
    def __init__(self, items):
        self.items = items
        self.i = 0

    def next(self):
        it = self.items[self.i % len(self.items)]
        self.i += 1
        return it


class K:
    def __init__(self, nc):
        self.nc = nc
        self.eng = {"pe": nc.tensor, "dve": nc.vector, "act": nc.scalar,
                    "pool": nc.gpsimd, "sp": nc.sync}
        self.sems = {}
        self.cnt = {}
        self.epoch = {e: 0 for e in self.eng}
        self.seen = {}
        for e in self.eng:
            self._new_epoch(e, first=True)
        self.dq = {}
        for q, n in (("sp", 24), ("pool", 16), ("act", 4)):
            keys = []
            for i in range(n):
                key = ("dma", q, i)
                self.sems[key] = nc.alloc_semaphore(f"d_{q}_{i}")
                self.cnt[key] = 0
                keys.append(key)
            self.dq[q] = [keys, 0]
        self.cc_key = ("cc", 0)
        self.sems[self.cc_key] = nc.alloc_semaphore("cc")
        self.cnt[self.cc_key] = 0
        self.n_ins = 0

    def _new_epoch(self, e, first=False):
        if not first:
            self.epoch[e] += 1
        key = (e, self.epoch[e])
        self.sems[key] = self.nc.alloc_semaphore(f"s_{e}_{self.epoch[e]}")
        self.cnt[key] = 0

    def _wait(self, e, toks, force_same=False):
        eng = self.eng[e]
        best = {}
        for t in toks:
            if t is None:
                continue
            key, val = t
            if key[0] == e and not force_same:
                if e in ("pe", "sp"):
                    continue
            if best.get(key, 0) < val:
                best[key] = val
        for key, val in best.items():
            if self.seen.get((e, key), 0) >= val:
                continue
            assert self.cnt[key] >= val, f"wait on unsignalled token {key} {val} > {self.cnt[key]}"
            eng.wait_ge(self.sems[key], val)
            self.seen[(e, key)] = val

    @staticmethod
    def _deps(R, W):
        toks = []
        for b in R:
            toks.append(b.w)
        for b in W:
            toks.append(b.w)
            toks.extend(b.r)
        return toks

    @staticmethod
    def _commit(tok, R, W):
        for b in R:
            b.r.append(tok)
            if len(b.r) > 24:
                b.r = b.r[-24:] if False else b.r
        for b in W:
            b.w = tok
            b.r = []

    def op(self, e, fn, *args, R=(), W=(), sig=True, **kw):
        self._wait(e, self._deps(R, W))
        ins = fn(*args, **kw)
        self.n_ins += 1
        key = (e, self.epoch[e])
        if sig:
            self.cnt[key] += 1
            ins.then_inc(self.sems[key], 1)
            tok = (key, self.cnt[key])
            if self.cnt[key] >= EPOCH:
                self._new_epoch(e)
        else:
            tok = (key, self.cnt[key] + 1)
        self._commit(tok, R, W)
        return tok

    def dma(self, q, out, in_, R=(), W=(), **kw):
        keys, idx = self.dq[q]
        key = keys[idx % len(keys)]
        self.dq[q][1] = idx + 1
        toks = self._deps(R, W)
        if self.cnt[key] > 0:
            toks.append((key, self.cnt[key]))
        self._wait(q, toks)
        ins = self.eng[q].dma_start(out=out, in_=in_, **kw)
        self.n_ins += 1
        self.cnt[key] += 16
        ins.then_inc(self.sems[key], 16)
        tok = (key, self.cnt[key])
        self._commit(tok, R, W)
        return tok

    def allgather(self, in_ap, out_ap, R=(), W=()):
        self._wait("pool", self._deps(R, W))
        ins = self.nc.gpsimd.collective_compute(
            "AllGather", ALU.bypass, replica_groups=[[0, 1, 2, 3], [4, 5, 6, 7]],
            ins=[in_ap], outs=[out_ap])
        self.n_ins += 1
        self.cnt[self.cc_key] += 1
        ins.then_inc(self.sems[self.cc_key])
        tok = (self.cc_key, self.cnt[self.cc_key])
        self._commit(tok, R, W)
        return tok

    def fence(self):
        toks = []
        for key, c in self.cnt.items():
            if c > 0:
                toks.append((key, c))
        for e in self.eng:
            self._wait(e, toks, force_same=False)

    def wait_bufs(self, e, bufs):
        toks = []
        for b in bufs:
            toks.append(b.w)
            toks.extend(b.r)
        self._wait(e, toks, force_same=True)


def build_program(nlayers=DEPTH, dbg=None):
    nc = bass.Bass("TRN2", target_bir_lowering=False)
    k = K(nc)
    dbg = dbg or {}
    uid = [0]

    def U(name):
        uid[0] += 1
        return f"{name}_{uid[0]}"

    def din(name, shape, dt=F32):
        return nc.dram_tensor(name, list(shape), dt, kind="ExternalInput").ap()

    xs_in = din("xs_in", [T, D])
    cvecT = din("cvecT", [D, 2])
    mod_w = din("mod_w", [DEPTH, D, 6 * D])
    mod_b = din("mod_b", [DEPTH, 6 * D])
    norm1_w = din("norm1_w", [DEPTH, D])
    norm2_w = din("norm2_w", [DEPTH, D])
    wp = din("wp", [DEPTH, D, NPACK])
    q_norm = din("mla_q_norm", [DEPTH, 256])
    w_uq = din("w_uq", [DEPTH, 256, 768])
    w_uq_sw = din("w_uq_sw", [DEPTH, 256, 768])
    kv_norm = din("mla_kv_norm", [DEPTH, 128])
    w_ukv = din("w_ukv", [DEPTH, 128, 1024])
    ret_logit = din("ret_decay_logit", [DEPTH, 2, 4])
    ret_norm_w = din("ret_norm_w", [DEPTH, 512])
    gwblk = din("gwblk", [DEPTH, 4, 32, 128])
    gbias = din("gbias", [DEPTH, 4, 128])
    gla_norm_w = din("gla_norm_w", [DEPTH, 512])
    w_branch = din("w_branch", [DEPTH, 3, 512, D])
    w_out = din("w_out", [DEPTH, D, D])
    ffn_wg = din("ffn_w_gate", [1, D, DFF])
    ffn_wu = din("ffn_w_up", [1, D, DFF])
    ffn_wd = din("ffn_w_down", [1, DFF, D])
    moe_router = din("moe_router", [1, D, NEXP])
    moe_wg = din("moe_w_gate", [1, NEXP, D, DFF])
    moe_wu = din("moe_w_up", [1, NEXP, D, DFF])
    moe_wd = din("moe_w_down", [1, NEXP, DFF, D])
    final_norm_w = din("final_norm_w", [D])
    c_ropeR = din("c_ropeR", [2, 128, T])
    c_ropeAq = din("c_ropeAq", [2, 32, T])
    c_ropeAk = din("c_ropeAk", [2, 32, T])
    c_rst = din("c_rst", [128, T])
    c_mask = din("c_mask", [2, 128, 128])
    c_ident = din("c_ident", [128, 128])
    c_onehot = din("c_onehot", [128, 4])

    out = nc.dram_tensor("out", [LAT, D], F32, kind="ExternalOutput").ap()
    dbg_out = {}
    for name, (shape, dt) in dbg.items():
        dbg_out[name] = nc.dram_tensor("dbg_" + name, list(shape), dt, kind="ExternalOutput").ap()

    xs = nc.dram_tensor("xs", [T, D], F32).ap()
    uT = nc.dram_tensor("uT", [8, 128, T], BF16).ap()
    modD = nc.dram_tensor("modD", [2, 6 * D], F32).ap()
    xf_in = nc.dram_tensor("xf_in", [NXF, 16, 1024], F32).ap()
    xf_out = nc.dram_tensor("xf_out", [NXF, 64, 1024], F32).ap()
    xb_in = nc.dram_tensor("xb_in", [NXB, 16, LAT], BF16).ap()
    xb_out = nc.dram_tensor("xb_out", [NXB, 64, LAT], BF16).ap()
    b_xs = [Buf(f"xs{t}") for t in range(NT)]
    b_uT = [Buf(f"uT{b}") for b in range(5)]
    b_modD = Buf("modD")
    b_xf_in = [Buf() for _ in range(NXF)]
    b_xf_out = [Buf() for _ in range(NXF)]
    b_xb_in = [Buf() for _ in range(NXB)]
    b_xb_out = [Buf() for _ in range(NXB)]
    b_out = Buf("out")

    PSA = nc.alloc_psum_tensor("psa", [128, 8, 512], F32)
    PS = []
    for i in range(8):
        PS.append((PSA[:, i, :], Buf(f"ps{i}")))

    def salloc(name, shape, dt=F32):
        return nc.alloc_sbuf_tensor(name, list(shape), dt), Buf(name)

    ident, b_ident = salloc("ident", [128, 128])
    identb, b_identb = salloc("identb", [128, 128], BF16)
    onesb, b_onesb = salloc("onesb", [128, 128], BF16)
    maskF, b_maskF = salloc("maskF", [128, 128])
    maskB, b_maskB = salloc("maskB", [128, 128])
    onehot, b_onehot = salloc("onehot", [128, 4])
    eps_t, b_eps = salloc("eps_t", [128, 1])
    one_t, b_one = salloc("one_t", [128, 1])
    modT, b_modT = salloc("modT", [128, 48, 2])
    A1, b_A1 = salloc("A1", [128, 8, 2])
    A2, b_A2 = salloc("A2", [128, 8, 2])
    rstb, b_rstb = salloc("rstb", [128, T], BF16)
    yTd = nc.dram_tensor("yTd", [3, 4, 128, T], BF16).ap()
    lsp_kt2 = nc.dram_tensor("lsp_kt2", [8, 128, T], BF16).ap()
    lsp_vth = nc.dram_tensor("lsp_vth", [8, 128, NT * 128], BF16).ap()
    lsp_eb = nc.dram_tensor("lsp_eb", [8, 128, T], BF16).ap()
    lsp_U2 = nc.dram_tensor("lsp_U2", [8, 128, NT * 128], F32).ap()
    lsp_te = nc.dram_tensor("lsp_te", [8, 128, 2 * NT], F32).ap()
    b_lsp = [Buf(f"lsp{i}") for i in range(8)]
    b_yTd = [Buf(f"yTd{i}") for i in range(3)]

    k.dma("sp", ident[:], c_ident[:, :], W=[b_ident])
    k.op("dve", nc.vector.tensor_copy, identb[:], ident[:], R=[b_ident], W=[b_identb])
    k.op("dve", nc.vector.memset, onesb[:], 1.0, W=[b_onesb])
    k.op("dve", nc.vector.memset, eps_t[:], EPS, W=[b_eps])
    k.op("dve", nc.vector.memset, one_t[:], 1.0, W=[b_one])
    k.dma("sp", maskF[:], c_mask[0], W=[b_maskF])
    k.dma("sp", maskB[:], c_mask[1], W=[b_maskB])
    k.dma("sp", onehot[:], c_onehot[:, :], W=[b_onehot])
    k.dma("pool", rstb[:], c_rst[:, :], W=[b_rstb])
    with nc.sbuf_tensor(U("zinit"), [16, 1024], F32) as zt_:
        b_zt = Buf()
        k.op("dve", nc.vector.memset, zt_[:], 0.0, W=[b_zt])
        k.dma("sp", xf_in[XF_LAM], zt_[:], R=[b_zt], W=[b_xf_in[XF_LAM]])
        k.fence()

    BLKS = [(0, 512), (512, 512), (1024, 512), (1536, 512), (2048, 256)]

    def wview(w2d):
        return w2d.rearrange("(kc p) n -> p kc n", p=128)

    alt = [0]

    def evac_engine():
        alt[0] += 1
        return "act" if alt[0] % 2 else "dve"

    def copy_ps(e, out_ap, in_ap, R, W, scale=None):
        if e == "act":
            if scale is None:
                k.op("act", nc.scalar.copy, out_ap, in_ap, R=R, W=W)
            else:
                k.op("act", nc.scalar.mul, out_ap, in_ap, scale, R=R, W=W)
        else:
            if scale is None:
                k.op("dve", nc.vector.tensor_copy, out_ap, in_ap, R=R, W=W)
            else:
                k.op("dve", nc.vector.tensor_scalar, out_ap, in_ap, scale, None, ALU.mult, R=R, W=W)

    def rsqrt_col(es, name, src_ap, src_buf, scale, n=1, parts=128):
        t1 = es.enter_context(nc.sbuf_tensor(U(name + "_a"), [128, n], F32))
        t2 = es.enter_context(nc.sbuf_tensor(U(name + "_b"), [128, n], F32))
        b1, b2 = Buf(), Buf()
        k.op("dve", nc.vector.tensor_scalar, t1[0:parts, :], src_ap, scale, EPS, ALU.mult, ALU.add,
             R=[src_buf], W=[b1])
        k.op("act", nc.scalar.activation, t2[0:parts, :], t1[0:parts, :], AF.Sqrt, R=[b1], W=[b2])
        k.op("dve", nc.vector.reciprocal, t1[0:parts, :], t2[0:parts, :], R=[b2], W=[b1])
        return t1, b1

    def phase_mod(l):
        with ExitStack() as es:
            def sb(name, shape, dt=F32):
                return es.enter_context(nc.sbuf_tensor(U(name), list(shape), dt)), Buf(name)
            cT, b_cT = sb("cT", [128, 8, 2])
            sc, b_sc = sb("sc", [128, 8, 2])
            sg, b_sg = sb("sg", [128, 8, 2])
            modv, b_modv = sb("modv", [2, 6 * D])
            mb, b_mb = sb("mb", [2, 6 * D])
            wr = Ring([sb(f"mw{i}", [128, 8, 512], BF16) for i in range(4)])
            scb, b_scb = sb("scb", [128, 8, 2], BF16)
            k.dma("sp", cT[:], cvecT.rearrange("(kc p) r -> p kc r", p=128), W=[b_cT])
            k.op("act", nc.scalar.activation, sg[:], cT[:], AF.Sigmoid, R=[b_cT], W=[b_sg])
            k.op("dve", nc.vector.tensor_tensor, sc[:], cT[:], sg[:], ALU.mult, R=[b_cT, b_sg], W=[b_sc])
            k.op("dve", nc.vector.tensor_copy, scb[:], sc[:], R=[b_sc], W=[b_scb])
            k.dma("sp", mb[0:1, :], mod_b[l:l + 1, :], W=[b_mb])
            k.dma("sp", mb[1:2, :], mod_b[l:l + 1, :], W=[b_mb])
            psr = Ring(PS[0:2])
            for n in range(12):
                wt, b_wt = wr.next()
                k.dma("pool", wt[:], wview(mod_w[l])[:, :, n * 512:(n + 1) * 512], W=[b_wt])
                ps, b_ps = psr.next()
                for kc in range(8):
                    k.op("pe", nc.tensor.matmul, ps[0:2, :], scb[:, kc, :], wt[:, kc, :],
                         start=(kc == 0), stop=(kc == 7), R=[b_scb, b_wt], W=[b_ps], sig=(kc == 7))
                k.op("dve", nc.vector.tensor_tensor, modv[:, n * 512:(n + 1) * 512], ps[0:2, :],
                     mb[:, n * 512:(n + 1) * 512], ALU.add, R=[b_ps, b_mb], W=[b_modv])
            k.dma("sp", modD[:, :], modv[:], R=[b_modv], W=[b_modD])
            pst, b_pst = PS[2]
            for j in range(48):
                k.op("pe", nc.tensor.transpose, pst[:, 2 * j:2 * j + 2], modv[0:2, j * 128:(j + 1) * 128],
                     ident[0:2, 0:2], R=[b_modv, b_ident], W=[b_pst], sig=(j == 47))
            k.op("dve", nc.vector.tensor_copy, modT[:].rearrange("p j r -> p (j r)"), pst[:, 0:96],
                 R=[b_pst], W=[b_modT])
            nw, b_nw = sb("nw", [128, 8, 2])
            for (nsrc, joff, At, bA) in ((norm1_w, 8, A1, b_A1), (norm2_w, 32, A2, b_A2)):
                k.dma("sp", nw[:, :, 0], nsrc[l].rearrange("(kc p) -> p kc", p=128), W=[b_nw], allow_slow_non_contiguous=True)
                k.dma("sp", nw[:, :, 1], nsrc[l].rearrange("(kc p) -> p kc", p=128), W=[b_nw], allow_slow_non_contiguous=True)
                k.op("dve", nc.vector.tensor_scalar, At[:], modT[:, joff:joff + 8, :], 1.0, None, ALU.add,
                     R=[b_modT], W=[bA])
                k.op("dve", nc.vector.tensor_tensor, At[:], At[:], nw[:], ALU.mult, R=[b_nw], W=[bA])
        k.fence()

    def phase_norm(l, At, bA, shoff, tiles, xsrc=None):
        xsrc = xs if xsrc is None else xsrc
        with ExitStack() as es:
            def sb(name, shape, dt=F32):
                return es.enter_context(nc.sbuf_tensor(U(name), list(shape), dt)), Buf(name)
            ND = 6
            xr = Ring([sb(f"nx{i}", [128, D]) for i in range(ND)])
            jr = Ring([sb(f"nj{i}", [128, D]) for i in range(2)])
            xnr = Ring([sb(f"nn{i}", [128, D]) for i in range(ND)])
            ur = Ring([sb(f"nu{i}", [128, 8, 128], BF16) for i in range(ND)])
            ssr = Ring([sb(f"ns{i}", [128, 1]) for i in range(ND)])
            psr = Ring([PS[0], PS[1], PS[2], PS[3]])
            for t in tiles:
                r = 0 if t < NLT else 1
                xt, b_xt = xr.next()
                k.dma("sp", xt[:], xsrc[t * 128:(t + 1) * 128, :], R=[b_xs[t]], W=[b_xt])
                jk, b_jk = jr.next()
                ss, b_ss = ssr.next()
                k.op("act", nc.scalar.activation, jk[:], xt[:], AF.Square, accum_out=ss[:, 0:1],
                     R=[b_xt], W=[b_ss])
                rstd, b_rstd = rsqrt_col(es, f"nr{t}", ss[:, 0:1], b_ss, 1.0 / D)
                xn, b_xn = xnr.next()
                k.op("act", nc.scalar.activation, xn[:], xt[:], AF.Identity, scale=rstd[:, 0:1],
                     R=[b_xt, b_rstd], W=[b_xn])
                ut, b_ut = ur.next()
                for half in range(2):
                    ps, b_ps = psr.next()
                    for j in range(4):
                        kc = half * 4 + j
                        k.op("pe", nc.tensor.transpose, ps[:, j * 128:(j + 1) * 128],
                             xn[:, kc * 128:(kc + 1) * 128], ident[:], R=[b_xn, b_ident], W=[b_ps],
                             sig=(j == 3))
                    for j in range(4):
                        kc = half * 4 + j
                        if j % 2 == 0:
                            k.op("act", nc.scalar.activation, ut[:, kc, :], ps[:, j * 128:(j + 1) * 128],
                                 AF.Identity, scale=At[:, kc, r:r + 1], bias=modT[:, shoff + kc, r:r + 1],
                                 R=[b_ps, bA, b_modT], W=[b_ut])
                        else:
                            k.op("dve", nc.vector.tensor_scalar, ut[:, kc, :], ps[:, j * 128:(j + 1) * 128],
                                 At[:, kc, r:r + 1], modT[:, shoff + kc, r:r + 1], ALU.mult, ALU.add,
                                 R=[b_ps, bA, b_modT], W=[b_ut])
                blk = min(t // 4, 4)
                k.dma("sp", uT[:, :, t * 128:(t + 1) * 128].rearrange("kc p t -> p kc t"), ut[:],
                      R=[b_ut], W=[b_uT[blk]])
        k.fence()

    class UStream:
        def __init__(self, es, nbuf=2, tag="ub"):
            self.ring = Ring([(es.enter_context(nc.sbuf_tensor(U(f"{tag}{i}"), [128, 8, 512], BF16)), Buf())
                              for i in range(nbuf)])

        def load(self, bi):
            t0, n = BLKS[bi]
            ub, b_ub = self.ring.next()
            k.dma("sp", ub[:, :, 0:n], uT[:, :, t0:t0 + n].rearrange("kc p t -> p kc t"),
                  R=[b_uT[bi]], W=[b_ub])
            return ub, b_ub, t0, n

    def proj_fm(ps, b_ps, w, b_w, c0, m, ub, b_ub, n, prow=0):
        for kc in range(8):
            k.op("pe", nc.tensor.matmul, ps[prow:prow + m, 0:n], w[:, kc, c0:c0 + m], ub[:, kc, 0:n],
                 start=(kc == 0), stop=(kc == 7), R=[b_w, b_ub], W=[b_ps], sig=(kc == 7))

    def load_w(es, name, src3d, shape, q="pool"):
        t = es.enter_context(nc.sbuf_tensor(U(name), list(shape), BF16))
        b = Buf(name)
        k.dma(q, t[:], src3d, W=[b])
        return t, b


    LS = {}
    LG2, b_LG2 = salloc("LG2", [128, 4])
    LAMT, b_LAMT = salloc("LAMT", [128, 8])

    def phase_q(l):
        cqn, b_cqn = LS["cqn"]; ckvn, b_ckvn = LS["ckvn"]; krr, b_krr = LS["krr"]; gzT, b_gzT = LS["gzT"]
        with ExitStack() as es:
            def sb(name, shape, dt=F32):
                return es.enter_context(nc.sbuf_tensor(U(name), list(shape), dt)), Buf(name)
            wA, b_wA = load_w(es, "wA", wview(wp[l])[:, :, PA:PA + PA_N], [128, 8, PA_N])
            qnw, b_qnw = sb("qnw", [128, 2])
            kvnw, b_kvnw = sb("kvnw", [128, 1])
            k.dma("sp", qnw[:], q_norm[l].rearrange("(c p) -> p c", p=128), W=[b_qnw], allow_slow_non_contiguous=True)
            k.dma("sp", kvnw[:], kv_norm[l].rearrange("(c p) -> p c", p=128), W=[b_kvnw], allow_slow_non_contiguous=True)
            ropk, b_ropk = sb("ropk", [32, 2, T])
            k.dma("sp", ropk[:, 0, :], c_ropeAk[0], W=[b_ropk])
            k.dma("sp", ropk[:, 1, :], c_ropeAk[1], W=[b_ropk])
            us = UStream(es)
            sqr = Ring([sb(f"sq{i}", [128, 512], BF16) for i in range(3)])
            rq, b_rq = sb("rq", [128, 512])
            rq2, b_rq2 = sb("rq2", [128, 512])
            tk, b_tk = sb("tk", [32, 512])
            tk2, b_tk2 = sb("tk2", [32, 512])
            for bi in range(5):
                ub, b_ub, t0, n = us.load(bi)
                (p0, bp0), (p1, bp1), (p2, bp2), (p3, bp3) = PS[0], PS[1], PS[2], PS[3]
                proj_fm(p0, bp0, wA, b_wA, 0, 128, ub, b_ub, n)
                proj_fm(p1, bp1, wA, b_wA, 128, 128, ub, b_ub, n)
                proj_fm(p2, bp2, wA, b_wA, 256, 128, ub, b_ub, n)
                s0, bs0 = sqr.next(); s1, bs1 = sqr.next(); s2, bs2 = sqr.next()
                k.op("act", nc.scalar.activation, s0[:, 0:n], p0[:, 0:n], AF.Square, R=[bp0], W=[bs0])
                k.op("act", nc.scalar.activation, s1[:, 0:n], p1[:, 0:n], AF.Square, R=[bp1], W=[bs1])
                k.op("act", nc.scalar.activation, s2[:, 0:n], p2[:, 0:n], AF.Square, R=[bp2], W=[bs2])
                k.op("pe", nc.tensor.matmul, p3[:, 0:n], onesb[:], s0[:, 0:n], start=True, stop=False,
                     R=[b_onesb, bs0], W=[bp3], sig=False)
                k.op("pe", nc.tensor.matmul, p3[:, 0:n], onesb[:], s1[:, 0:n], start=False, stop=True,
                     R=[b_onesb, bs1], W=[bp3])
                k.op("dve", nc.vector.tensor_scalar, rq[:, 0:n], p3[:, 0:n], 1.0 / 256, EPS, ALU.mult, ALU.add,
                     R=[bp3], W=[b_rq])
                k.op("act", nc.scalar.activation, rq2[:, 0:n], rq[:, 0:n], AF.Sqrt, R=[b_rq], W=[b_rq2])
                k.op("dve", nc.vector.reciprocal, rq[:, 0:n], rq2[:, 0:n], R=[b_rq2], W=[b_rq])
                k.op("dve", nc.vector.scalar_tensor_tensor, cqn[:, 0, t0:t0 + n], p0[:, 0:n], qnw[:, 0:1], rq[:, 0:n],
                     ALU.mult, ALU.mult, R=[bp0, b_qnw, b_rq], W=[b_cqn])
                k.op("dve", nc.vector.scalar_tensor_tensor, cqn[:, 1, t0:t0 + n], p1[:, 0:n], qnw[:, 1:2], rq[:, 0:n],
                     ALU.mult, ALU.mult, R=[bp1, b_qnw, b_rq], W=[b_cqn])
                k.op("pe", nc.tensor.matmul, p3[:, 0:n], onesb[:], s2[:, 0:n], start=True, stop=True,
                     R=[b_onesb, bs2], W=[bp3])
                k.op("dve", nc.vector.tensor_scalar, rq[:, 0:n], p3[:, 0:n], 1.0 / 128, EPS, ALU.mult, ALU.add,
                     R=[bp3], W=[b_rq])
                k.op("act", nc.scalar.activation, rq2[:, 0:n], rq[:, 0:n], AF.Sqrt, R=[b_rq], W=[b_rq2])
                k.op("dve", nc.vector.reciprocal, rq[:, 0:n], rq2[:, 0:n], R=[b_rq2], W=[b_rq])
                k.op("dve", nc.vector.scalar_tensor_tensor, ckvn[:, t0:t0 + n], p2[:, 0:n], kvnw[:, 0:1], rq[:, 0:n],
                     ALU.mult, ALU.mult, R=[bp2, b_kvnw, b_rq], W=[b_ckvn])
                (p4, bp4), (p5, bp5), (p6, bp6) = PS[4], PS[5], PS[6]
                proj_fm(p4, bp4, wA, b_wA, 384, 32, ub, b_ub, n)
                proj_fm(p5, bp5, wA, b_wA, 416, 32, ub, b_ub, n)
                proj_fm(p6, bp6, wA, b_wA, 448, 32, ub, b_ub, n)
                k.op("dve", nc.vector.tensor_tensor, tk[:, 0:n], p4[0:32, 0:n], ropk[:, 0, t0:t0 + n], ALU.mult,
                     R=[bp4, b_ropk], W=[b_tk])
                k.op("dve", nc.vector.tensor_tensor, tk2[:, 0:n], p5[0:32, 0:n], ropk[:, 1, t0:t0 + n], ALU.mult,
                     R=[bp5, b_ropk], W=[b_tk2])
                k.op("dve", nc.vector.tensor_tensor, krr[:, t0:t0 + n], tk[:, 0:n], tk2[:, 0:n], ALU.add,
                     R=[b_tk, b_tk2], W=[b_krr])
                k.op("act", nc.scalar.copy, gzT[:, t0:t0 + n], p6[0:32, 0:n], R=[bp6], W=[b_gzT])
            for j in range(8):
                k.dma("sp", xb_in[XB_CKV + j], ckvn[16 * j:16 * j + 16, 0:LAT], R=[b_ckvn], W=[b_xb_in[XB_CKV + j]])
            for j in range(2):
                k.dma("sp", xb_in[XB_KR + j], krr[16 * j:16 * j + 16, 0:LAT], R=[b_krr], W=[b_xb_in[XB_KR + j]])
            for j in range(NXB):
                k.allgather(xb_in[j], xb_out[j], R=[b_xb_in[j]], W=[b_xb_out[j]])
        k.fence()

    def phase_lg(l):
        with ExitStack() as es:
            def sb(name, shape, dt=F32):
                return es.enter_context(nc.sbuf_tensor(U(name), list(shape), dt)), Buf(name)
            lt, b_lt = sb("lt", [128, 4])
            l2, b_l2 = sb("l2", [128, 4])
            k.dma("sp", lt[0:64, :], ret_logit[l, 0:1, :].partition_broadcast(64), W=[b_lt])
            k.dma("sp", lt[64:128, :], ret_logit[l, 1:2, :].partition_broadcast(64), W=[b_lt])
            k.op("act", nc.scalar.activation, l2[:], lt[:], AF.Exp, scale=-1.0, R=[b_lt], W=[b_l2])
            k.op("act", nc.scalar.activation, lt[:], l2[:], AF.Ln, bias=one_t[:, 0:1], R=[b_l2, b_one], W=[b_lt])
            k.op("dve", nc.vector.tensor_scalar, LG2[:], lt[:], -1.0, None, ALU.mult, R=[b_lt], W=[b_LG2])
        k.fence()

    def lm_cols(mix, h):
        if mix == 0:
            return P_RET + h * 512, 512
        return P_GLA + h * 256, 256

    def lm_prefetch(l, mix, h, PF):
        hm = mix * 4 + h
        base, ncol = lm_cols(mix, h)
        k.dma("pool", PF["w"][0][:, :, 0:ncol], wview(wp[l])[:, :, base:base + ncol], W=[PF["w"][1]])
        gcol = (P_RG if mix == 0 else P_GR) + h * 128
        k.dma("pool", PF["wg"][0][:], wview(wp[l])[:, :, gcol:gcol + 128], W=[PF["wg"][1]])
        k.dma("sp", PF["kt2"][0][:], lsp_kt2[hm], R=[b_lsp[hm]], W=[PF["kt2"][1]])
        k.dma("sp", PF["vth"][0][:].rearrange("p n v -> p (n v)"), lsp_vth[hm], R=[b_lsp[hm]], W=[PF["vth"][1]])
        k.dma("sp", PF["ebT"][0][:], lsp_eb[hm], R=[b_lsp[hm]], W=[PF["ebT"][1]])
        k.dma("sp", PF["U2"][0][:].rearrange("p n v -> p (n v)"), lsp_U2[hm], R=[b_lsp[hm]], W=[PF["U2"][1]])
        k.dma("sp", PF["TE"][0][:], lsp_te[hm], R=[b_lsp[hm]], W=[PF["TE"][1]])

    def linear_mixer(l, mix, h, pass_no, last, PF=None):
        hm = mix * 4 + h
        gzT, b_gzT = LS["gzT"]
        with ExitStack() as es:
            def sb(name, shape, dt=F32):
                return es.enter_context(nc.sbuf_tensor(U(name), list(shape), dt)), Buf(name)
            if mix == 0:
                base, ncol = P_RET + h * 512, 512
                cq2, cq2s, ck2, ck2s = 0, 128, 256, 384
            else:
                base, ncol = P_GLA + h * 256, 256
                cq2, ck2 = 0, 128
            if pass_no == 1:
                w, b_w = load_w(es, "lw", wview(wp[l])[:, :, base:base + ncol], [128, 8, ncol])
            else:
                w, b_w = PF["w"]
            vcol = (P_RV if mix == 0 else P_GV) + h * 128
            if pass_no == 1:
                wv, b_wv = load_w(es, "lwv", wview(wp[l])[:, :, vcol:vcol + 128], [128, 8, 128])
            if mix == 1 and pass_no == 1:
                gw, b_gw = load_w(es, "gw", gwblk[l, h], [32, 128])
                nb, b_nb = sb("nb", [128, 1])
                nb2, b_nb2 = sb("nb2", [128, 1])
                k.dma("sp", nb[:], gbias[l, h].rearrange("(p o) -> p o", o=1), W=[b_nb])
                k.op("dve", nc.vector.tensor_scalar, nb2[:], nb[:], -1.0, None, ALU.mult, R=[b_nb], W=[b_nb2])
            if pass_no == 1:
                TE, b_TOT = sb("TE", [128, 2 * NT])
                kt2, b_kt2 = sb("kt2", [128, T], BF16)
                vth, b_vth = sb("vth", [128, NT, 128], BF16)
                ebT, b_ebT = sb("ebT", [128, T], BF16)
                U2, _ = sb("U2", [128, NT, 128])
            else:
                TE, b_TOT = PF["TE"]
                kt2, b_kt2 = PF["kt2"]
                vth, b_vth = PF["vth"]
                ebT, b_ebT = PF["ebT"]
                U2, b_ld = PF["U2"]
            TOT, EEND = TE[:, 0:NT], TE[:, NT:2 * NT]
            b_EEND = b_TOT
            b_kdc = [Buf() for _ in range(NT)]
            b_vthc = [Buf() for _ in range(NT)]
            us = UStream(es)
            if pass_no == 1:
                kd, b_kd = sb("kd", [128, T], BF16)
                a2r = Ring([sb(f"a2_{i}", [128, 512]) for i in range(2)])
                csr = Ring([sb(f"cs_{i}", [128, 512]) for i in range(2)])
                B2r = Ring([sb(f"B2_{i}", [128, 512]) for i in range(2)])
                enr = Ring([sb(f"en_{i}", [128, 512]) for i in range(2)])
            else:
                qt2, b_qt2 = sb("qt2", [128, T], BF16)
                b_vthc = [b_vth for _ in range(NT)]
            t1r = Ring([sb(f"lt1{i}", [128, 512]) for i in range(2)])
            t2r = Ring([sb(f"lt2{i}", [128, 512]) for i in range(2)])
            if mix == 0:
                ropr = Ring([sb(f"rop{i}", [128, 2, 512]) for i in range(2)])

            for bi in range(5):
                ub, b_ub, t0, n = us.load(bi)
                nch = n // 128
                ch0 = t0 // 128
                if pass_no == 1:
                    a2, b_a2 = a2r.next(); cs, b_cs = csr.next(); B2, b_B2 = B2r.next()
                    enb, b_enb = enr.next()
                if pass_no == 2:
                    pass
                elif mix == 0:
                    k.op("dve", nc.vector.memset, cs[:, 0:n], 1.0, W=[b_cs])
                    k.op("act", nc.scalar.activation, a2[:, 0:n], cs[:, 0:n], AF.Identity, scale=LG2[:, h:h + 1],
                         R=[b_cs, b_LG2], W=[b_a2])
                elif pass_no == 1:
                    pz, bpz = PS[7]
                    k.op("pe", nc.tensor.matmul, pz[:, 0:n], gw[:], gzT[:, t0:t0 + n], start=True, stop=True,
                         R=[b_gw, b_gzT], W=[bpz])
                    k.op("act", nc.scalar.activation, cs[:, 0:n], pz[:, 0:n], AF.Exp, scale=-1.0,
                         bias=nb2[:, 0:1], R=[bpz, b_nb2], W=[b_cs])
                    k.op("act", nc.scalar.activation, B2[:, 0:n], cs[:, 0:n], AF.Ln, bias=one_t[:, 0:1],
                         R=[b_cs, b_one], W=[b_B2])
                    k.op("dve", nc.vector.tensor_scalar, a2[:, 0:n], B2[:, 0:n], -1.0 / 16.0, None,
                         ALU.mult, R=[b_B2], W=[b_a2])
                if pass_no == 1:
                    k.op("dve", nc.vector.tensor_tensor_scan, cs[:, 0:n], rstb[:, t0:t0 + n], a2[:, 0:n], 0.0, ALU.mult, ALU.add,
                         R=[b_rstb, b_a2], W=[b_cs])
                    k.op("dve", nc.vector.tensor_copy, B2[0:64, 0:n], cs[0:64, 0:n], R=[b_cs], W=[b_B2])
                    k.op("dve", nc.vector.tensor_tensor, B2[64:128, 0:n], a2[64:128, 0:n], cs[64:128, 0:n], ALU.subtract,
                         R=[b_a2, b_cs], W=[b_B2])
                    for c_ in range(nch):
                        k.op("dve", nc.vector.tensor_scalar, B2[64:128, c_ * 128:(c_ + 1) * 128],
                             B2[64:128, c_ * 128:(c_ + 1) * 128], cs[64:128, c_ * 128 + 127:c_ * 128 + 128], None, ALU.add,
                             R=[b_cs], W=[b_B2])
                        k.op("dve", nc.vector.tensor_copy, TOT[:, ch0 + c_:ch0 + c_ + 1], cs[:, c_ * 128 + 127:c_ * 128 + 128],
                             R=[b_cs], W=[b_TOT])
                    k.op("act", nc.scalar.activation, EEND[:, ch0:ch0 + nch], TOT[:, ch0:ch0 + nch], AF.Exp,
                         R=[b_TOT], W=[b_EEND])
                    k.op("act", nc.scalar.activation, enb[:, 0:n], B2[:, 0:n], AF.Exp, scale=-1.0, R=[b_B2], W=[b_enb])
                    k.op("act", nc.scalar.activation, ebT[:, t0:t0 + n], B2[:, 0:n], AF.Exp, R=[b_B2], W=[b_ebT])
                if mix == 0:
                    rop, b_rop = ropr.next()
                    k.dma("sp", rop[:, 0, 0:n], c_ropeR[0, :, t0:t0 + n], W=[b_rop])
                    k.dma("sp", rop[:, 1, 0:n], c_ropeR[1, :, t0:t0 + n], W=[b_rop])

                def roped(pa, bpa, pb, bpb):
                    if mix == 1:
                        return pa[:, 0:n], bpa
                    ta, bta = t1r.next()
                    tb, btb = t2r.next()
                    k.op("dve", nc.vector.tensor_tensor, ta[:, 0:n], pa[:, 0:n], rop[:, 0, 0:n], ALU.mult,
                         R=[bpa, b_rop], W=[bta])
                    k.op("dve", nc.vector.tensor_tensor, tb[:, 0:n], pb[:, 0:n], rop[:, 1, 0:n], ALU.mult,
                         R=[bpb, b_rop], W=[btb])
                    k.op("dve", nc.vector.tensor_tensor, ta[:, 0:n], ta[:, 0:n], tb[:, 0:n], ALU.add,
                         R=[btb], W=[bta])
                    return ta[:, 0:n], bta

                (pa, bpa), (pb, bpb) = (PS[0], PS[1]) if bi % 2 == 0 else (PS[2], PS[3])
                if pass_no == 1:
                    proj_fm(pa, bpa, w, b_w, ck2, 128, ub, b_ub, n)
                    if mix == 0:
                        proj_fm(pb, bpb, w, b_w, ck2s, 128, ub, b_ub, n)
                    kap, bk = roped(pa, bpa, pb, bpb)
                    k.op("dve", nc.vector.tensor_tensor, kt2[:, t0:t0 + n], kap, enb[:, 0:n], ALU.mult,
                         R=[bk, b_enb], W=[b_kt2])
                if pass_no == 2:
                    (pc, bpc), (pd, bpd) = (pa, bpa), (pb, bpb)
                    proj_fm(pc, bpc, w, b_w, cq2, 128, ub, b_ub, n)
                    if mix == 0:
                        proj_fm(pd, bpd, w, b_w, cq2s, 128, ub, b_ub, n)
                    qap, bq = roped(pc, bpc, pd, bpd)
                    k.op("dve", nc.vector.scalar_tensor_tensor, qt2[:, t0:t0 + n], qap, 0.125, ebT[:, t0:t0 + n],
                         ALU.mult, ALU.mult, R=[bq, b_ebT], W=[b_qt2])
                for c_ in (range(nch) if pass_no == 1 else []):
                    n_ = ch0 + c_
                    k.op("act", nc.scalar.activation, kd[:, n_ * 128:(n_ + 1) * 128], kt2[:, n_ * 128:(n_ + 1) * 128],
                         AF.Identity, scale=EEND[:, n_:n_ + 1], R=[b_kt2, b_EEND], W=[b_kdc[n_]])
                    pv, bpv = PS[4 + c_ % 2]
                    for kc in range(8):
                        k.op("pe", nc.tensor.matmul, pv[:, 0:128], ub[:, kc, c_ * 128:(c_ + 1) * 128], wv[:, kc, :],
                             start=(kc == 0), stop=(kc == 7), R=[b_ub, b_wv], W=[bpv], sig=(kc == 7))
                    copy_ps(evac_engine(), vth[:, n_, :], pv[:, 0:128], R=[bpv], W=[b_vthc[n_]])
            if pass_no == 1:
                k2d, _ = sb("k2d", [128, NT, 128], BF16)
            b_k2dg = [Buf() for _ in range(3)]
            b_U2g = [Buf() for _ in range(5)]
            if pass_no == 2:
                b_U2g = [b_ld]
            if pass_no == 1:
                ptb = PSA[:, 0:3, :].bitcast(BF16)
                for n_ in range(NT):
                    bk = n_ // 8
                    k.op("pe", nc.tensor.transpose, ptb[:, bk, (n_ % 8) * 128:(n_ % 8 + 1) * 128],
                         kd[:, n_ * 128:(n_ + 1) * 128], identb[:], R=[b_kdc[n_], b_identb], W=[PS[bk][1]],
                         sig=(n_ % 8 == 7 or n_ == NT - 1))
                for bk in range(3):
                    nb_ = min(8, NT - 8 * bk)
                    copy_ps(evac_engine(), k2d[:, 8 * bk:8 * bk + nb_, :],
                            ptb[:, bk, 0:nb_ * 128].rearrange("p (a c) -> p a c", c=128), R=[PS[bk][1]], W=[b_k2dg[bk]])
                for n_ in range(NT):
                    bk = 3 + n_ // 4
                    k.op("pe", nc.tensor.matmul, PSA[:, bk, (n_ % 4) * 128:(n_ % 4 + 1) * 128], k2d[:, n_, :], vth[:, n_, :],
                         start=True, stop=True, R=[b_k2dg[n_ // 8], b_vthc[n_]], W=[PS[bk][1]], sig=(n_ % 4 == 3 or n_ == NT - 1))
                for b4 in range(5):
                    nb_ = min(4, NT - 4 * b4)
                    copy_ps(evac_engine(), U2[:, 4 * b4:4 * b4 + nb_, :],
                            PSA[:, 3 + b4, 0:nb_ * 128].rearrange("p (a c) -> p a c", c=128), R=[PS[3 + b4][1]], W=[b_U2g[b4]])
            F, Bw = slice(0, 64), slice(64, 128)
            TSEQ, b_TSEQ = sb("TSEQ", [128, 17])
            ESEQ, b_ESEQ = sb("ESEQ", [128, 17])
            k.op("dve", nc.vector.memset, TSEQ[:, 0:1], 0.0, W=[b_TSEQ])
            k.op("dve", nc.vector.tensor_copy, TSEQ[F, 1:17], TOT[F, 0:NLT], R=[b_TOT], W=[b_TSEQ])
            k.op("dve", nc.vector.tensor_copy, TSEQ[Bw, 1:17], TOT[Bw, NLT - 1::-1], R=[b_TOT], W=[b_TSEQ])
            k.op("act", nc.scalar.activation, ESEQ[:], TSEQ[:], AF.Exp, R=[b_TSEQ], W=[b_ESEQ])
            k.op("dve", nc.vector.memset, ESEQ[:, 0:1], 0.0, W=[b_ESEQ])
            DS, b_DS = sb("DS", [128, 128, 17])
            US, b_US = sb("US", [128, 128, 17])
            SS, b_SS = sb("SS", [128, 128, 17])
            k.op("dve", nc.vector.tensor_copy, DS[:], ESEQ[:].unsqueeze(1).broadcast_to([128, 128, 17]),
                 R=[b_ESEQ], W=[b_DS])
            k.op("dve", nc.vector.tensor_copy, US[F, :, 1:17], U2[F, 0:NLT, :].rearrange("p n v -> p v n"),
                 R=b_U2g, W=[b_US])
            k.op("dve", nc.vector.tensor_copy, US[Bw, :, 1:17], U2[Bw, NLT - 1::-1, :].rearrange("p n v -> p v n"),
                 R=b_U2g, W=[b_US])
            St, b_St = sb("St", [128, 128])

            def run_scan():
                k.op("dve", nc.vector.tensor_tensor_scan, SS[:].rearrange("p v t -> p (v t)"),
                     DS[:].rearrange("p v t -> p (v t)"), US[:].rearrange("p v t -> p (v t)"), 0.0, ALU.mult, ALU.add,
                     R=[b_DS, b_US], W=[b_SS])

            if pass_no == 1:
                k.op("dve", nc.vector.memset, US[:, :, 0], 0.0, W=[b_US])
                run_scan()
                k.op("dve", nc.vector.tensor_copy, St[:], SS[:, :, 16], R=[b_SS], W=[b_St])
                k.dma("sp", xf_in[XF_STATE + hm].rearrange("a (b c) -> (a b) c", c=128), St[:],
                      R=[b_St], W=[b_xf_in[XF_STATE + hm]])
                ls, b_ls = sb("ls", [128, 1])
                k.op("dve", nc.vector.reduce_sum, ls[:], TOT[:, 0:NLT], AX.X, R=[b_TOT], W=[b_ls])
                k.op("act", nc.scalar.activation, LAMT[:, hm:hm + 1], ls[:], AF.Exp, R=[b_ls], W=[b_LAMT])
                k.dma("sp", lsp_kt2[hm], kt2[:], R=[b_kt2], W=[b_lsp[hm]])
                k.dma("sp", lsp_vth[hm], vth[:].rearrange("p n v -> p (n v)"), R=b_vthc, W=[b_lsp[hm]])
                k.dma("sp", lsp_eb[hm], ebT[:], R=[b_ebT], W=[b_lsp[hm]])
                k.dma("sp", lsp_U2[hm], U2[:].rearrange("p n v -> p (n v)"), R=b_U2g, W=[b_lsp[hm]])
                k.dma("sp", lsp_te[hm], TE[:], R=[b_TOT], W=[b_lsp[hm]])
                return
            FR, b_FR = sb("FR", [128, 4, 128])
            LR, b_LR = sb("LR", [128, 4, 8])
            for r in range(4):
                k.dma("sp", FR[:, r, :], xf_out[XF_STATE + hm, r * 16:(r + 1) * 16, :].rearrange("a (b c) -> (a b) c", c=128),
                      R=[b_xf_out[XF_STATE + hm]], W=[b_FR])
                k.dma("sp", LR[:, r, :], xf_out[XF_LAM, r * 16, :].rearrange("(p e) -> p e", e=8),
                      R=[b_xf_out[XF_LAM]], W=[b_LR])
            Rt, b_Rt = sb("Rt", [128, 128])
            Sin, b_Sin = sb("Sin", [128, 128])
            k.op("dve", nc.vector.memset, Sin[:], 0.0, W=[b_Sin])
            k.op("dve", nc.vector.scalar_tensor_tensor, Rt[F, :], U2[F, 16, :], EEND[F, 17:18], U2[F, 17, :],
                 ALU.mult, ALU.add, R=b_U2g + [b_EEND], W=[b_Rt])
            k.op("dve", nc.vector.scalar_tensor_tensor, Rt[Bw, :], U2[Bw, 17, :], EEND[Bw, 16:17], U2[Bw, 16, :],
                 ALU.mult, ALU.add, R=b_U2g + [b_EEND], W=[b_Rt])
            for sl, order in ((F, range(4)), (Bw, range(3, -1, -1))):
                for r in order:
                    k.op("dve", nc.vector.scalar_tensor_tensor, Sin[sl, :], Rt[sl, :], onehot[sl, r:r + 1], Sin[sl, :],
                         ALU.mult, ALU.add, R=[b_Rt, b_onehot], W=[b_Sin])
                    k.op("dve", nc.vector.scalar_tensor_tensor, Rt[sl, :], Rt[sl, :], LR[sl, r, hm:hm + 1], FR[sl, r, :],
                         ALU.mult, ALU.add, R=[b_LR, b_FR], W=[b_Rt])
            k.op("dve", nc.vector.tensor_copy, US[:, :, 0], Sin[:], R=[b_Sin], W=[b_US])
            run_scan()
            S2, b_S2 = sb("S2", [128, NT, 128], BF16)
            k.op("dve", nc.vector.tensor_copy, S2[F, 0:NLT, :], SS[F, :, 0:NLT].rearrange("p v t -> p t v"),
                 R=[b_SS], W=[b_S2])
            k.op("dve", nc.vector.tensor_copy, S2[Bw, 0:NLT, :], SS[Bw, :, NLT - 1::-1].rearrange("p v t -> p t v"),
                 R=[b_SS], W=[b_S2])
            k.op("dve", nc.vector.memset, S2[F, 16, :], 0.0, W=[b_S2])
            k.op("dve", nc.vector.memset, S2[Bw, 17, :], 0.0, W=[b_S2])
            k.op("dve", nc.vector.tensor_copy, S2[F, 17, :], U2[F, 16, :], R=b_U2g, W=[b_S2])
            k.op("dve", nc.vector.tensor_copy, S2[Bw, 16, :], U2[Bw, 17, :], R=b_U2g, W=[b_S2])
            gcol = (P_RG if mix == 0 else P_GR) + h * 128
            wg_, b_wg = PF["wg"]
            nwb, b_nwb = sb("nwb", [128, 128])
            nsrc = ret_norm_w if mix == 0 else gla_norm_w
            k.dma("sp", nwb[:], nsrc[l:l + 1, h * 128:(h + 1) * 128].partition_broadcast(128), W=[b_nwb])
            G, b_G = sb("G", [128, NT, 128], BF16)
            gs, b_gs = sb("gs", [128, 8, 128])
            nchunks = NLT if last else NT
            groups = [(g0, min(8, nchunks - g0)) for g0 in range(0, nchunks, 8)]
            ubc = None
            for gi, (g0, ng) in enumerate(groups):
                bks = [6, 7] if gi % 2 == 0 else [4, 5]
                for c_ in range(ng):
                    n_ = g0 + c_
                    bi = min(n_ // 4, 4)
                    if ubc is None or ubc[0] != bi:
                        ubc = (bi,) + tuple(us.load(bi))
                    _, ub, b_ub, t0, n = ubc
                    tt = n_ - t0 // 128
                    bk = bks[c_ // 4]
                    for kc in range(8):
                        k.op("pe", nc.tensor.matmul, PSA[:, bk, (c_ % 4) * 128:(c_ % 4 + 1) * 128],
                             ub[:, kc, tt * 128:(tt + 1) * 128], wg_[:, kc, :],
                             start=(kc == 0), stop=(kc == 7), R=[b_ub, b_wg], W=[PS[bk][1]], sig=(kc == 7))
                nbk = (ng + 3) // 4
                pgv = PSA[:, bks[0]:bks[0] + nbk, :].rearrange("p b (c v) -> p (b c) v", v=128)[:, 0:ng, :]
                k.op("act", nc.scalar.activation, gs[:, 0:ng, :], pgv, AF.Silu, R=[PS[b_][1] for b_ in bks[:nbk]], W=[b_gs])
                k.op("dve", nc.vector.tensor_tensor, G[:, g0:g0 + ng, :], gs[:, 0:ng, :],
                     nwb[:].unsqueeze(1).broadcast_to([128, ng, 128]), ALU.mult, R=[b_gs, b_nwb], W=[b_G])
            t1, b_t1 = sb("g_t1", [128, 8, 128])
            t2, b_t2 = sb("g_t2", [128, 8, 128])
            PT8, b_PT8 = sb("PT8", [128, 8, 128], BF16)
            osb, b_osb = sb("osb", [128, 8, 128])
            jk, b_jk = sb("ljk", [128, 128])
            stt, b_stt = sb("stt", [128, 6, 8])
            yn8, b_yn8 = sb("yn8", [128, 8, 128])
            yb8, b_yb8 = sb("yb8", [128, 8, 128], BF16)
            yTt, b_yT = sb("yTt", [128, T], BF16)
            b_sttc = [Buf() for _ in range(8)]
            b_osbc = [Buf() for _ in range(8)]
            b_ync = [Buf() for _ in range(8)]
            for (g0, ng) in groups:
                nbk = (ng + 3) // 4
                for c_ in range(ng):
                    c0 = (g0 + c_) * 128
                    k.op("pe", nc.tensor.matmul, PSA[:, c_ // 4, (c_ % 4) * 128:(c_ % 4 + 1) * 128],
                         kt2[0:64, c0:c0 + 128], qt2[0:64, c0:c0 + 128],
                         start=True, stop=True, R=[b_kt2, b_qt2], W=[PS[c_ // 4][1]], sig=(c_ % 4 == 3 or c_ == ng - 1))
                for c_ in range(ng):
                    c0 = (g0 + c_) * 128
                    k.op("pe", nc.tensor.matmul, PSA[:, 2 + c_ // 4, (c_ % 4) * 128:(c_ % 4 + 1) * 128],
                         kt2[64:128, c0:c0 + 128], qt2[64:128, c0:c0 + 128],
                         start=True, stop=True, R=[b_kt2, b_qt2], W=[PS[2 + c_ // 4][1]], sig=(c_ % 4 == 3 or c_ == ng - 1))
                sfv = PSA[:, 0:nbk, :].rearrange("p b (c v) -> p (b c) v", v=128)[:, 0:ng, :]
                sbv = PSA[:, 2:2 + nbk, :].rearrange("p b (c v) -> p (b c) v", v=128)[:, 0:ng, :]
                k.op("dve", nc.vector.tensor_tensor, t1[:, 0:ng, :], sfv, maskF[:].unsqueeze(1).broadcast_to([128, ng, 128]),
                     ALU.mult, R=[PS[b_][1] for b_ in range(nbk)] + [b_maskF], W=[b_t1])
                k.op("dve", nc.vector.tensor_tensor, t2[:, 0:ng, :], sbv, maskB[:].unsqueeze(1).broadcast_to([128, ng, 128]),
                     ALU.mult, R=[PS[2 + b_][1] for b_ in range(nbk)] + [b_maskB], W=[b_t2])
                k.op("dve", nc.vector.tensor_tensor, PT8[:, 0:ng, :], t1[:, 0:ng, :], t2[:, 0:ng, :], ALU.add,
                     R=[b_t1, b_t2], W=[b_PT8])
                for c_ in range(ng):
                    n_ = g0 + c_
                    c0 = n_ * 128
                    bk = 4 + c_ // 4
                    oap = PSA[:, bk, (c_ % 4) * 128:(c_ % 4 + 1) * 128]
                    k.op("pe", nc.tensor.matmul, oap, PT8[:, c_, :], vth[:, n_, :],
                         start=True, stop=False, R=[b_PT8, b_vthc[n_]], W=[PS[bk][1]], sig=False)
                    k.op("pe", nc.tensor.matmul, oap, qt2[:, c0:c0 + 128], S2[:, n_, :],
                         start=False, stop=True, R=[b_qt2, b_S2], W=[PS[bk][1]], sig=(c_ % 4 == 3 or c_ == ng - 1))
                for c_ in range(ng):
                    bk = 4 + c_ // 4
                    oap = PSA[:, bk, (c_ % 4) * 128:(c_ % 4 + 1) * 128]
                    k.op("act", nc.scalar.activation, osb[:, c_, :], oap, AF.Identity, accum_out=stt[:, 0, c_:c_ + 1],
                         R=[PS[bk][1]], W=[b_osbc[c_], b_sttc[c_]])
                    k.op("act", nc.scalar.activation, jk[:], oap, AF.Square, accum_out=stt[:, 1, c_:c_ + 1],
                         R=[PS[bk][1], b_sttc[c_]], W=[])
                k.op("dve", nc.vector.tensor_scalar, stt[:, 0:2, 0:ng], stt[:, 0:2, 0:ng], 1.0 / 128, None, ALU.mult,
                     W=[b_stt] + b_sttc[0:ng])
                if mix == 0:
                    k.op("dve", nc.vector.tensor_tensor, stt[:, 3, 0:ng], stt[:, 0, 0:ng], stt[:, 0, 0:ng], ALU.mult, R=b_sttc[0:ng], W=[b_stt])
                    k.op("dve", nc.vector.tensor_tensor, stt[:, 1, 0:ng], stt[:, 1, 0:ng], stt[:, 3, 0:ng], ALU.subtract, R=b_sttc[0:ng], W=[b_stt])
                k.op("dve", nc.vector.tensor_scalar, stt[:, 3, 0:ng], stt[:, 1, 0:ng], EPS, None, ALU.add, R=b_sttc[0:ng], W=[b_stt])
                k.op("act", nc.scalar.activation, stt[:, 4, 0:ng], stt[:, 3, 0:ng], AF.Sqrt, R=b_sttc[0:ng], W=[b_stt])
                k.op("dve", nc.vector.reciprocal, stt[:, 2, 0:ng], stt[:, 4, 0:ng], R=b_sttc[0:ng], W=[b_stt])
                for c_ in range(ng):
                    if mix == 0:
                        k.op("dve", nc.vector.tensor_scalar, yn8[:, c_, :], osb[:, c_, :], stt[:, 0, c_:c_ + 1], stt[:, 2, c_:c_ + 1],
                             ALU.subtract, ALU.mult, R=[b_osbc[c_], b_stt, b_sttc[c_]], W=[b_ync[c_]])
                    else:
                        k.op("dve", nc.vector.tensor_scalar, yn8[:, c_, :], osb[:, c_, :], stt[:, 2, c_:c_ + 1], None, ALU.mult,
                             R=[b_osbc[c_], b_stt, b_sttc[c_]], W=[b_ync[c_]])
                k.op("dve", nc.vector.tensor_tensor, yb8[:, 0:ng, :], yn8[:, 0:ng, :], G[:, g0:g0 + ng, :], ALU.mult,
                     R=b_ync[0:ng] + [b_G], W=[b_yb8])
                p6b = PSA[:, 6, :].bitcast(BF16)
                for c_ in range(ng):
                    k.op("pe", nc.tensor.transpose, p6b[:, c_ * 128:(c_ + 1) * 128], yb8[:, c_, :], identb[:],
                         R=[b_yb8, b_identb], W=[PS[6][1]], sig=(c_ == ng - 1))
                copy_ps("act", yTt[:, g0 * 128:(g0 + ng) * 128], p6b[:, 0:ng * 128], R=[PS[6][1]], W=[b_yT])
            ntok = LAT if last else T
            k.dma("sp", yTd[1 + mix, h, :, 0:ntok], yTt[:, 0:ntok], R=[b_yT], W=[b_yTd[1 + mix]])

    def phase_linear(l, pass_no, last):
        order = [(mix, h) for mix in range(2) for h in range(4)]
        if pass_no == 1:
            for (mix, h) in order:
                linear_mixer(l, mix, h, pass_no, last)
                k.fence()
        else:
            with ExitStack() as pes:
                PFs = []
                for si in range(2):
                    PF = {}
                    for nm, shp, dt in (("w", [128, 8, 512], BF16), ("wg", [128, 8, 128], BF16), ("kt2", [128, T], BF16),
                                        ("vth", [128, NT, 128], BF16), ("ebT", [128, T], BF16), ("U2", [128, NT, 128], F32),
                                        ("TE", [128, 2 * NT], F32)):
                        PF[nm] = (pes.enter_context(nc.sbuf_tensor(U(f"pf_{nm}{si}"), shp, dt)), Buf(f"pf_{nm}{si}"))
                    PFs.append(PF)
                lm_prefetch(l, order[0][0], order[0][1], PFs[0])
                for i_, (mix, h) in enumerate(order):
                    if i_ + 1 < len(order):
                        lm_prefetch(l, order[i_ + 1][0], order[i_ + 1][1], PFs[(i_ + 1) % 2])
                    linear_mixer(l, mix, h, pass_no, last, PF=PFs[i_ % 2])
                    k.fence()
            k.fence()
        if pass_no == 1:
            k.dma("sp", xf_in[XF_LAM, 0, :].rearrange("(p e) -> p e", e=8), LAMT[:], R=[b_LAMT], W=[b_xf_in[XF_LAM]])
            for j in range(NXF):
                k.allgather(xf_in[j], xf_out[j], R=[b_xf_in[j]], W=[b_xf_out[j]])
            k.fence()


    G8, b_G8 = salloc("G8", [128, NT, 8])

    def phase_attn(l, last):
        SCL = 96.0 ** -0.5
        cqn, b_cqn = LS["cqn"]; ckvn, b_ckvn = LS["ckvn"]; krr, b_krr = LS["krr"]
        with ExitStack() as es:
            def sb(name, shape, dt=F32):
                return es.enter_context(nc.sbuf_tensor(U(name), list(shape), dt)), Buf(name)
            ckvA, b_ckvA = sb("ckvA", [128, NKEY], BF16)
            k.op("pool", nc.gpsimd.tensor_copy, ckvA[:, 0:CTX], ckvn[:, LAT:T], R=[b_ckvn], W=[b_ckvA])
            for j in range(8):
                k.dma("sp", ckvA[16 * j:16 * j + 16, CTX:NKEY].rearrange("p (r t) -> p r t", r=4),
                      xb_out[XB_CKV + j].rearrange("(r p) t -> p r t", p=16),
                      R=[b_xb_out[XB_CKV + j]], W=[b_ckvA])
            KTs = [sb(f"KT{i}", [96, NKEY], BF16) for i in range(2)]
            Vs = [sb(f"V{i}", [128, NKT, 128], BF16) for i in range(2)]
            for (KT, bKT), (V, bV) in zip(KTs, Vs):
                k.dma("sp", KT[64:96, 0:CTX], krr[:, LAT:T], R=[b_krr], W=[bKT])
                for j in range(2):
                    k.dma("sp", KT[64 + 16 * j:64 + 16 * j + 16, CTX:NKEY].rearrange("p (r t) -> p r t", r=4),
                          xb_out[XB_KR + j].rearrange("(r p) t -> p r t", p=16),
                          R=[b_xb_out[XB_KR + j]], W=[bKT])
                k.op("pool", nc.gpsimd.memset, V[:, :, 64:128], 1.0, W=[bV])
            ropq_r = Ring([sb(f"ropq{i}", [96, 2, 512]) for i in range(2)])
            yat_r = Ring([sb(f"yat{i}", [64, 512], BF16) for i in range(3)])
            QTs = Ring([sb(f"QT{i}", [96, T], BF16) for i in range(2)])
            wq_r = Ring([sb(f"wq{i}", [128, 2, 96], BF16) for i in range(2)])
            wqs_r = Ring([sb(f"wqs{i}", [128, 2, 96], BF16) for i in range(2)])
            wkv_r = Ring([sb(f"wkv{i}", [128, 128], BF16) for i in range(2)])
            t1r = Ring([sb(f"at1{i}", [96, 512]) for i in range(2)])
            t2r = Ring([sb(f"at2{i}", [96, 512]) for i in range(2)])
            Pr = Ring([sb(f"P{i}", [128, 512], BF16) for i in range(4)])
            linv_r = Ring([sb(f"linv{i}", [128, 512]) for i in range(2)])
            sring = Ring([PS[0], PS[1], PS[2], PS[3]])
            oring = Ring([PS[4], PS[5]])
            qblocks = [(q0, 512, list(range(NKT))) for q0 in range(0, LAT, 512)]
            if not last:
                qblocks.append((LAT, CTX, [0, 1]))
            def build_head(h):
                wq, b_wq = wq_r.next(); wqs, b_wqs = wqs_r.next(); wkv, b_wkv = wkv_r.next()
                k.dma("pool", wq[:], w_uq[l].rearrange("(kc p) n -> p kc n", p=128)[:, :, h * 96:(h + 1) * 96], W=[b_wq])
                k.dma("pool", wqs[:], w_uq_sw[l].rearrange("(kc p) n -> p kc n", p=128)[:, :, h * 96:(h + 1) * 96], W=[b_wqs])
                k.dma("pool", wkv[:], w_ukv[l][:, h * 128:(h + 1) * 128], W=[b_wkv])
                QT, bQT = QTs.next()
                KT, bKT = KTs[h % 2]
                V, bV = Vs[h % 2]
                built[h] = (QT, bQT, KT, bKT, V, bV)
                yield
                blks = BLKS[:4] if last else BLKS
                for bi, (t0, n) in enumerate(blks):
                    (pa, bpa), (pb, bpb) = PS[6], PS[7]
                    for (pp, bpp, ww, bww) in ((pa, bpa, wq, b_wq), (pb, bpb, wqs, b_wqs)):
                        for kc in range(2):
                            k.op("pe", nc.tensor.matmul, pp[0:96, 0:n], ww[:, kc, :], cqn[:, kc, t0:t0 + n],
                                 start=(kc == 0), stop=(kc == 1), R=[bww, b_cqn], W=[bpp], sig=(kc == 1))
                    k.op("dve", nc.vector.tensor_scalar, QT[0:64, t0:t0 + n], pa[0:64, 0:n], SCL, None, ALU.mult,
                         R=[bpa], W=[bQT])
                    ta, bta = t1r.next(); tb, btb = t2r.next()
                    ropq, b_ropq = ropq_r.next()
                    k.dma("sp", ropq[64:96, 0, 0:n], c_ropeAq[0, :, t0:t0 + n], W=[b_ropq])
                    k.dma("sp", ropq[64:96, 1, 0:n], c_ropeAq[1, :, t0:t0 + n], W=[b_ropq])
                    k.op("dve", nc.vector.tensor_tensor, ta[64:96, 0:n], pa[64:96, 0:n], ropq[64:96, 0, 0:n],
                         ALU.mult, R=[bpa, b_ropq], W=[bta])
                    k.op("dve", nc.vector.tensor_tensor, tb[64:96, 0:n], pb[64:96, 0:n], ropq[64:96, 1, 0:n],
                         ALU.mult, R=[bpb, b_ropq], W=[btb])
                    k.op("dve", nc.vector.tensor_tensor, QT[64:96, t0:t0 + n], ta[64:96, 0:n], tb[64:96, 0:n],
                         ALU.add, R=[bta, btb], W=[bQT])
                    yield
                for kb in range((NKEY + 511) // 512):
                    c0 = kb * 512
                    n = min(512, NKEY - c0)
                    pk, bpk = PS[6 + kb % 2]
                    k.op("pe", nc.tensor.matmul, pk[0:64, 0:n], wkv[:, 0:64], ckvA[:, c0:c0 + n], start=True, stop=True,
                         R=[b_wkv, b_ckvA], W=[bpk])
                    copy_ps("dve", KT[0:64, c0:c0 + n], pk[0:64, 0:n], R=[bpk], W=[bKT])
                    yield
                for g0 in range(0, NKT, 8):
                    gn = min(8, NKT - g0)
                    pv, bpv = PS[7]
                    for i_ in range(gn):
                        kt = g0 + i_
                        k.op("pe", nc.tensor.matmul, pv[:, i_ * 64:(i_ + 1) * 64], ckvA[:, kt * 128:(kt + 1) * 128],
                             wkv[:, 64:128], start=True, stop=True, R=[b_ckvA, b_wkv], W=[bpv], sig=(i_ == gn - 1))
                    copy_ps("dve", V[:, g0:g0 + gn, 0:64],
                            pv[:, 0:gn * 64].rearrange("p (a b) -> p a b", b=64), R=[bpv], W=[bV])
                    yield

            built = {}
            for _ in build_head(0):
                pass
            for h in range(8):
                QT, bQT, KT, bKT, V, bV = built[h]
                gen = build_head(h + 1) if h + 1 < 8 else iter(())
                step_no = 0
                for (q0, nq, ktiles) in qblocks:
                    po, bpo = oring.next()

                    def issue_s(kt):
                        ps_, bps_ = sring.next()
                        k.op("pe", nc.tensor.matmul, ps_[:, 0:nq], KT[0:96, kt * 128:(kt + 1) * 128], QT[0:96, q0:q0 + nq],
                             start=True, stop=True, R=[bKT, bQT], W=[bps_])
                        return ps_, bps_
                    LA = 3
                    pend = [issue_s(kt_) for kt_ in ktiles[:LA]]
                    for i_, kt in enumerate(ktiles):
                        if i_ + LA < len(ktiles):
                            pend.append(issue_s(ktiles[i_ + LA]))
                        cur = pend.pop(0)
                        P, bP = Pr.next()
                        k.op("act", nc.scalar.activation, P[:, 0:nq], cur[0][:, 0:nq], AF.Exp, R=[cur[1]], W=[bP])
                        lastk = (i_ == len(ktiles) - 1)
                        k.op("pe", nc.tensor.matmul, po[:, 0:nq], V[:, kt, :], P[:, 0:nq], start=(i_ == 0), stop=lastk,
                             R=[bV, bP], W=[bpo], sig=True)
                        step_no += 1
                        if step_no % 6 == 0:
                            next(gen, None)
                    linv, b_linv = linv_r.next()
                    k.op("dve", nc.vector.reciprocal, linv[64:128, 0:nq], po[64:128, 0:nq], R=[bpo], W=[b_linv])
                    r0 = (h % 2) * 64
                    yat, b_yat = yat_r.next()
                    k.op("dve", nc.vector.tensor_tensor, yat[:, 0:nq], po[0:64, 0:nq],
                         linv[64:128, 0:nq], ALU.mult, R=[bpo, b_linv], W=[b_yat])
                    k.dma("sp", yTd[0, h // 2, r0:r0 + 64, q0:q0 + nq], yat[:, 0:nq], R=[b_yat], W=[b_yTd[0]])
                for _ in gen:
                    pass
        k.fence()

    def phase_merge(l, last):
        blks = list(enumerate(BLKS[:4] if last else BLKS))
        with ExitStack() as es:
            def sb(name, shape, dt=F32):
                return es.enter_context(nc.sbuf_tensor(U(name), list(shape), dt)), Buf(name)
            zT, b_zT = sb("zT", [128, 8, T], BF16)
            wo, b_wo = load_w(es, "wo", wview(w_out[l]), [128, 8, D])
            us = UStream(es)
            yb_r = Ring([sb(f"myb{i}", [128, 3, 4, 512], BF16) for i in range(2)])
            g1, b_g1 = sb("g1", [128, 2, D])
            for r in range(2):
                k.dma("sp", g1[:, r, :], modD[r:r + 1, 2 * D:3 * D].partition_broadcast(128), R=[b_modD], W=[b_g1])
            wg_r = Ring([sb(f"mwg{i}", [128, 8, 3, 128], BF16) for i in range(2)])
            wb_r = Ring([sb(f"mwb{i}", [128, 3, 4, 128], BF16) for i in range(2)])
            gj_r = Ring([sb(f"gj{i}", [128, 512]) for i in range(2)])
            za_r = Ring([sb(f"za{i}", [128, 512]) for i in range(2)])
            zt_r = Ring([sb(f"zt{i}", [128, 512]) for i in range(2)])
            pgr = Ring([PS[0], PS[1], PS[2]])
            pzr = Ring([PS[3], PS[4], PS[5]])
            for c in range(8):
                wg3, b_wg3 = wg_r.next(); wb3, b_wb3 = wb_r.next()
                for j in range(3):
                    col = P_BG + j * 1024 + c * 128
                    k.dma("pool", wg3[:, :, j, :], wview(wp[l])[:, :, col:col + 128], W=[b_wg3])
                    k.dma("pool", wb3[:, j, :, :], w_branch[l, j].rearrange("(k4 p) n -> p k4 n", p=128)[:, :, c * 128:(c + 1) * 128],
                          W=[b_wb3])
                for bi, (t0, n) in blks:
                    ub, b_ub, _, _ = us.load(bi)
                    yb3, b_yb3 = yb_r.next()
                    for j in range(3):
                        k.dma("sp", yb3[:, j, :, 0:n], yTd[j, :, :, t0:t0 + n].rearrange("k p t -> p k t"),
                              R=[b_yTd[j]], W=[b_yb3])
                    za, bza = za_r.next()
                    for j in range(3):
                        pg, bpg = pgr.next()
                        for kc in range(8):
                            k.op("pe", nc.tensor.matmul, pg[:, 0:n], wg3[:, kc, j, :], ub[:, kc, 0:n],
                                 start=(kc == 0), stop=(kc == 7), R=[b_wg3, b_ub], W=[bpg], sig=(kc == 7))
                        gj, bgj = gj_r.next()
                        k.op("act", nc.scalar.activation, gj[:, 0:n], pg[:, 0:n], AF.Sigmoid, R=[bpg], W=[bgj])
                        pz, bpz = pzr.next()
                        for k4 in range(4):
                            k.op("pe", nc.tensor.matmul, pz[:, 0:n], wb3[:, j, k4, :], yb3[:, j, k4, 0:n],
                                 start=(k4 == 0), stop=(k4 == 3), R=[b_wb3, b_yb3], W=[bpz], sig=(k4 == 3))
                        if j == 0:
                            k.op("dve", nc.vector.tensor_tensor, za[:, 0:n], pz[:, 0:n], gj[:, 0:n], ALU.mult,
                                 R=[bpz, bgj], W=[bza])
                        else:
                            zt, bzt = zt_r.next()
                            k.op("dve", nc.vector.tensor_tensor, zt[:, 0:n], pz[:, 0:n], gj[:, 0:n], ALU.mult,
                                 R=[bpz, bgj], W=[bzt])
                            if j == 1:
                                k.op("dve", nc.vector.tensor_tensor, za[:, 0:n], za[:, 0:n], zt[:, 0:n], ALU.add,
                                     R=[bzt], W=[bza])
                            else:
                                k.op("dve", nc.vector.tensor_tensor, zT[:, c, t0:t0 + n], za[:, 0:n], zt[:, 0:n], ALU.add,
                                     R=[bza, bzt], W=[b_zT])
            xr = Ring([sb(f"mx{i}", [128, D]) for i in range(2)])
            tmr = Ring([sb(f"mt{i}", [128, 512]) for i in range(2)])
            pyr = Ring([PS[6], PS[7]])
            tiles = list(range(NLT)) if last else list(range(NT))
            for t in tiles:
                r = 0 if t < NLT else 1
                xt, b_xt = xr.next()
                k.dma("sp", xt[:], (xs_in if l == 0 else xs)[t * 128:(t + 1) * 128, :], R=[b_xs[t]], W=[b_xt])
                for half in range(2):
                    py, bpy = pyr.next()
                    for kc in range(8):
                        k.op("pe", nc.tensor.matmul, py[:, :], zT[:, kc, t * 128:(t + 1) * 128],
                             wo[:, kc, half * 512:(half + 1) * 512], start=(kc == 0), stop=(kc == 7),
                             R=[b_zT, b_wo], W=[bpy], sig=(kc == 7))
                    tm, btm = tmr.next()
                    k.op("dve", nc.vector.tensor_tensor, tm[:], py[:, :], g1[:, r, half * 512:(half + 1) * 512], ALU.mult,
                         R=[bpy, b_g1], W=[btm])
                    k.op("dve", nc.vector.tensor_tensor, xt[:, half * 512:(half + 1) * 512], xt[:, half * 512:(half + 1) * 512],
                         tm[:], ALU.add, R=[btm], W=[b_xt])
                k.dma("sp", xs[t * 128:(t + 1) * 128, :], xt[:], R=[b_xt], W=[b_xs[t]])
        k.fence()

    def phase_router(i, tiles):
        with ExitStack() as es:
            def sb(name, shape, dt=F32):
                return es.enter_context(nc.sbuf_tensor(U(name), list(shape), dt)), Buf(name)
            rw, b_rw = load_w(es, "rw", wview(moe_router[i]), [128, 8, NEXP])
            us = UStream(es)
            lg_r = Ring([sb(f"rl{i_}", [128, 8]) for i_ in range(2)])
            m8_r = Ring([sb(f"rm{i_}", [128, 8]) for i_ in range(2)])
            ex_r = Ring([sb(f"re{i_}", [128, 8]) for i_ in range(2)])
            mk_r = Ring([sb(f"rk{i_}", [128, 8]) for i_ in range(2)])
            sc_r = Ring([sb(f"rs{i_}", [128, 4]) for i_ in range(2)])
            ubc = None
            for t in tiles:
                bi = min(t // 4, 4)
                if ubc is None or ubc[0] != bi:
                    ubc = (bi,) + tuple(us.load(bi))
                _, ub, b_ub, t0, n = ubc
                tt = t - t0 // 128
                pl, bpl = PS[t % 2]
                for kc in range(8):
                    k.op("pe", nc.tensor.matmul, pl[:, 0:NEXP], ub[:, kc, tt * 128:(tt + 1) * 128], rw[:, kc, :],
                         start=(kc == 0), stop=(kc == 7), R=[b_ub, b_rw], W=[bpl], sig=(kc == 7))
                lg, blg = lg_r.next(); m8, bm8 = m8_r.next(); ex, bex = ex_r.next(); mk, bmk = mk_r.next()
                sc_, bsc = sc_r.next()
                k.op("dve", nc.vector.tensor_copy, lg[:], pl[:, 0:NEXP], R=[bpl], W=[blg])
                k.op("dve", nc.vector.max, m8[:], lg[:], R=[blg], W=[bm8])
                k.op("dve", nc.vector.tensor_scalar, mk[:], lg[:], m8[:, 1:2], None, ALU.is_ge, R=[blg, bm8], W=[bmk])
                k.op("dve", nc.vector.tensor_scalar, sc_[:, 0:1], m8[:, 0:1], -1.0, None, ALU.mult, R=[bm8], W=[bsc])
                k.op("act", nc.scalar.activation, ex[:], lg[:], AF.Exp, bias=sc_[:, 0:1], R=[blg, bsc], W=[bex])
                k.op("dve", nc.vector.tensor_tensor, ex[:], ex[:], mk[:], ALU.mult, R=[bmk], W=[bex])
                k.op("dve", nc.vector.reduce_sum, sc_[:, 1:2], ex[:], AX.X, R=[bex], W=[bsc])
                k.op("dve", nc.vector.reciprocal, sc_[:, 2:3], sc_[:, 1:2], W=[bsc])
                k.op("dve", nc.vector.tensor_scalar, G8[:, t, :], ex[:], sc_[:, 2:3], None, ALU.mult, R=[bex, bsc], W=[b_G8])
        k.fence()

    def phase_ffn(l, last):
        moe = (l % 2 == 1)
        i = l // 2
        nexp = NEXP if moe else 1
        groups = [(0, 1024), (1024, 1024)] if last else [(0, 1152), (1152, 1152)]
        for (g0, gn) in groups:
            with ExitStack() as es:
                def sb(name, shape, dt=F32):
                    return es.enter_context(nc.sbuf_tensor(U(name), list(shape), dt)), Buf(name)
                vTg, b_vTg = sb("vTg", [128, 8, gn], BF16)
                ublks = sorted(set(min(t_ // 4, 4) for t_ in range(g0 // 128, (g0 + gn) // 128)))
                k.dma("sp", vTg[:], uT[:, :, g0:g0 + gn].rearrange("kc p t -> p kc t"),
                      R=[b_uT[b_] for b_ in ublks], W=[b_vTg])
                hT, b_hT = sb("hT", [128, NFF, gn], BF16)
                acc, b_acc = sb("acc", [128, gn // 128, D])
                wd, b_wd = sb("wd", [128, NFF, D], BF16)
                wt_r = Ring([sb(f"wgu{i_}", [128, 8, 2, 256], BF16) for i_ in range(2)])
                sg_r = Ring([sb(f"sg{i_}", [128, 512]) for i_ in range(3)])
                pgr = Ring([PS[0], PS[1]])
                pur = Ring([PS[2], PS[3]])
                pyr = Ring([PS[4], PS[5], PS[6], PS[7]])
                bsz = 512 if gn % 512 == 0 else 384
                nblks = [(c0, min(bsz, gn - c0)) for c0 in range(0, gn, bsz)]
                def wsrc(e):
                    if moe:
                        return moe_wg[i, e], moe_wu[i, e], moe_wd[i, e]
                    return ffn_wg[i], ffn_wu[i], ffn_wd[i]
                tasks = [(e, fc2) for e in range(nexp) for fc2 in range(NFF // 2)]
                loaded = {}

                def issue_load(ti):
                    if ti >= len(tasks) or ti in loaded:
                        return
                    e_, fc2_ = tasks[ti]
                    Wg_, Wu_, _ = wsrc(e_)
                    wt_, b_wt_ = wt_r.next()
                    k.dma("pool", wt_[:, :, 0, :], wview(Wg_)[:, :, fc2_ * 256:(fc2_ + 1) * 256], W=[b_wt_])
                    k.dma("pool", wt_[:, :, 1, :], wview(Wu_)[:, :, fc2_ * 256:(fc2_ + 1) * 256], W=[b_wt_])
                    loaded[ti] = (wt_, b_wt_)
                issue_load(0)
                for e in range(nexp):
                    Wg, Wu, Wd = wsrc(e)
                    for fc2 in range(NFF // 2):
                        ti = e * (NFF // 2) + fc2
                        issue_load(ti)
                        issue_load(ti + 1)
                        if fc2 == 0:
                            k.dma("pool", wd[:], Wd.rearrange("(f p) n -> p f n", p=128), W=[b_wd])
                        wt, b_wt = loaded.pop(ti)
                        for sub in range(2):
                            fc = fc2 * 2 + sub
                            for (c0, n) in nblks:
                                pg, bpg = pgr.next(); pu, bpu = pur.next()
                                for kc in range(8):
                                    k.op("pe", nc.tensor.matmul, pg[:, 0:n], wt[:, kc, 0, sub * 128:(sub + 1) * 128],
                                         vTg[:, kc, c0:c0 + n], start=(kc == 0), stop=(kc == 7),
                                         R=[b_wt, b_vTg], W=[bpg], sig=(kc == 7))
                                for kc in range(8):
                                    k.op("pe", nc.tensor.matmul, pu[:, 0:n], wt[:, kc, 1, sub * 128:(sub + 1) * 128],
                                         vTg[:, kc, c0:c0 + n], start=(kc == 0), stop=(kc == 7),
                                         R=[b_wt, b_vTg], W=[bpu], sig=(kc == 7))
                                sg, bsg = sg_r.next()
                                k.op("act", nc.scalar.activation, sg[:, 0:n], pg[:, 0:n], AF.Silu, R=[bpg], W=[bsg])
                                k.op("dve", nc.vector.tensor_tensor, hT[:, fc, c0:c0 + n], sg[:, 0:n], pu[:, 0:n], ALU.mult,
                                     R=[bsg, bpu], W=[b_hT])
                    for tt in range(gn // 128):
                        t = g0 // 128 + tt
                        for half in range(2):
                            py, bpy = pyr.next()
                            for fc in range(NFF):
                                k.op("pe", nc.tensor.matmul, py[:, :], hT[:, fc, tt * 128:(tt + 1) * 128],
                                     wd[:, fc, half * 512:(half + 1) * 512], start=(fc == 0), stop=(fc == NFF - 1),
                                     R=[b_hT, b_wd], W=[bpy], sig=(fc == NFF - 1))
                            dst = acc[:, tt, half * 512:(half + 1) * 512]
                            if not moe:
                                copy_ps(evac_engine(), dst, py[:, :], R=[bpy], W=[b_acc])
                            elif e == 0:
                                k.op("dve", nc.vector.tensor_scalar, dst, py[:, :], G8[:, t, e:e + 1], None, ALU.mult,
                                     R=[bpy, b_G8], W=[b_acc])
                            else:
                                k.op("dve", nc.vector.scalar_tensor_tensor, dst, py[:, :], G8[:, t, e:e + 1], dst,
                                     ALU.mult, ALU.add, R=[bpy, b_G8], W=[b_acc])
                xr = Ring([sb(f"fx{i_}", [128, D]) for i_ in range(2)])
                jr = Ring([sb(f"fj{i_}", [128, D]) for i_ in range(2)])
                ssr = Ring([sb(f"fs{i_}", [128, 1]) for i_ in range(2)])
                g2, b_g2 = sb("g2", [128, 2, D])
                for rg_ in range(1 if last else 2):
                    k.dma("sp", g2[:, rg_, :], modD[rg_:rg_ + 1, 5 * D:6 * D].partition_broadcast(128), R=[b_modD], W=[b_g2])
                if last:
                    fnw, b_fnw = sb("fnw", [128, D])
                    k.dma("sp", fnw[:], final_norm_w.rearrange("(o d) -> o d", o=1).partition_broadcast(128), W=[b_fnw])
                for tt in range(gn // 128):
                    t = g0 // 128 + tt
                    r = 0 if t < NLT else 1
                    xt, b_xt = xr.next()
                    k.dma("sp", xt[:], xs[t * 128:(t + 1) * 128, :], R=[b_xs[t]], W=[b_xt])
                    k.op("dve", nc.vector.tensor_tensor, acc[:, tt, :], acc[:, tt, :], g2[:, r, :], ALU.mult,
                         R=[b_g2], W=[b_acc])
                    k.op("dve", nc.vector.tensor_tensor, xt[:], xt[:], acc[:, tt, :], ALU.add, R=[b_acc], W=[b_xt])
                    if not last:
                        k.dma("sp", xs[t * 128:(t + 1) * 128, :], xt[:], R=[b_xt], W=[b_xs[t]])
                    else:
                        jk, b_jk = jr.next(); ss, b_ss = ssr.next()
                        k.op("act", nc.scalar.activation, jk[:], xt[:], AF.Square, accum_out=ss[:, 0:1],
                             R=[b_xt], W=[b_jk, b_ss])
                        rstd, b_rstd = rsqrt_col(es, f"fr{t}", ss[:, 0:1], b_ss, 1.0 / D)
                        k.op("dve", nc.vector.scalar_tensor_tensor, jk[:], xt[:], rstd[:, 0:1], fnw[:], ALU.mult, ALU.mult,
                             R=[b_xt, b_rstd, b_fnw], W=[b_jk])
                        k.dma("sp", out[t * 128:(t + 1) * 128, :], jk[:], R=[b_jk], W=[b_out])
            k.fence()

    stop = dbg.get("_stop") if isinstance(dbg, dict) else None

    def tap(name, src_ap, bufs):
        if name in dbg_out:
            k.dma("sp", dbg_out[name], src_ap, R=bufs, W=[b_out])

    for l in range(nlayers):
        last = (l == DEPTH - 1)
        alltiles = list(range(NT))
        phase_mod(l)
        phase_norm(l, A1, b_A1, 0, alltiles, xsrc=(xs_in if l == 0 else xs))
        phase_lg(l)
        with ExitStack() as les:
            for nm, shp in (("cqn", [128, 2, T]), ("ckvn", [128, T]), ("krr", [32, T]), ("gzT", [32, T])):
                LS[nm] = (les.enter_context(nc.sbuf_tensor(U(nm), shp, BF16)), Buf(nm))
            phase_q(l)
            phase_linear(l, 1, last)
            phase_linear(l, 2, last)
            phase_attn(l, last)
        k.fence()
        phase_merge(l, last)
        ftiles = list(range(NLT)) if last else alltiles
        phase_norm(l, A2, b_A2, 24, ftiles)
        if l % 2 == 1:
            phase_router(l // 2, ftiles)
        phase_ffn(l, last)
    if nlayers < DEPTH:
        for t in range(NLT):
            k.dma("sp", out[t * 128:(t + 1) * 128, :], xs[t * 128:(t + 1) * 128, :], R=[b_xs[t]], W=[b_out])
    k.wait_bufs("sp", [b_out])
    return nc, k


IN_SPLITS = (256, 128, 32, 256, 256, 512, 512, 256, 256, 512, 512, 32, 3072)
OFFS = np.concatenate([[0], np.cumsum(IN_SPLITS)]).astype(int)
(O_CQ, O_CKV, O_KR, O_RQ, O_RK, O_RV, O_RG, O_GQ, O_GK, O_GV, O_GR, O_GZ, O_BG) = OFFS[:13]


def _partner(n, half):
    idx = np.arange(n)
    return np.where(idx % (2 * half) < half, idx + half, idx - half)


def _pack_w_in(w_in_l):
    cols = []
    pa = _partner(32, 8)
    cols.append(np.arange(O_CQ, O_CQ + 256))
    cols.append(np.arange(O_CKV, O_CKV + 128))
    cols.append(np.arange(O_KR, O_KR + 32))
    cols.append(O_KR + pa)
    cols.append(np.arange(O_GZ, O_GZ + 32))
    pr = _partner(64, 32)
    for h in range(4):
        q = O_RQ + h * 64 + np.arange(64)
        qs = O_RQ + h * 64 + pr
        kk = O_RK + h * 64 + np.arange(64)
        ks = O_RK + h * 64 + pr
        cols += [q, q, qs, qs, kk, kk, ks, ks]
    for h in range(4):
        q = O_GQ + h * 64 + np.arange(64)
        kk = O_GK + h * 64 + np.arange(64)
        cols += [q, q, kk, kk]
    cols.append(np.arange(O_RV, O_RV + 512))
    cols.append(np.arange(O_GV, O_GV + 512))
    cols.append(np.arange(O_RG, O_RG + 512))
    cols.append(np.arange(O_GR, O_GR + 512))
    cols.append(np.arange(O_BG, O_BG + 3072))
    cols = np.concatenate(cols)
    assert cols.shape[0] == NPACK
    return np.ascontiguousarray(w_in_l[:, cols])


def _rope_tables(pos, half, signed_rows):
    inv = 10000.0 ** (-np.arange(half, dtype=np.float32) / half)
    ang = pos.astype(np.float32)[None, :] * inv[:, None]
    cos = np.cos(ang).astype(np.float32)
    sin = np.sin(ang).astype(np.float32)
    return np.concatenate([cos, cos], 0), np.concatenate([-sin, sin], 0)


def _consts(q):
    pos = q * LAT + np.arange(LAT)
    c, s = _rope_tables(pos, 32, True)
    ropeR = np.zeros((2, 128, T), np.float32)
    ropeR[0, :, :LAT] = np.concatenate([c, c], 0)
    ropeR[1, :, :LAT] = np.concatenate([s, s], 0)
    ropeR[0, :, LAT:] = 1.0
    cr, sr = _rope_tables(pos // 64, 8, True)
    cc, sc = _rope_tables(pos % 64, 8, True)
    ak = np.zeros((2, 32, T), np.float32)
    ak[0, :, :LAT] = np.concatenate([cr, cc], 0)
    ak[1, :, :LAT] = np.concatenate([sr, sc], 0)
    ak[0, :, LAT:] = 1.0
    aq = (ak * np.float32(96.0 ** -0.5)).astype(np.float32)
    rst = np.ones((128, T), np.float32)
    rst[:, ::128] = 0.0
    j = np.arange(128)[:, None]
    i = np.arange(128)[None, :]
    mask = np.stack([(i >= j), (j >= i)]).astype(np.float32)
    onehot = np.zeros((128, 4), np.float32)
    onehot[:, q] = 1.0
    return dict(c_ropeR=ropeR, c_ropeAq=aq, c_ropeAk=ak, c_rst=rst, c_mask=mask,
                c_ident=np.eye(128, dtype=np.float32), c_onehot=onehot)


def make_in_maps(inp):
    f = lambda a: np.ascontiguousarray(np.asarray(a, dtype=np.float32))
    x, c, ctx, c_ctx = f(inp["x"]), f(inp["c"]), f(inp["ctx"]), f(inp["c_ctx"])
    w_in = f(inp["w_in"])
    wp_ = np.stack([_pack_w_in(w_in[l]) for l in range(DEPTH)])
    w_uq = f(inp["mla_w_uq"])
    pa = _partner(32, 8)
    cols = np.arange(768)
    for h in range(8):
        cols[h * 96 + 64:h * 96 + 96] = h * 96 + 64 + pa
    w_uq_sw = np.ascontiguousarray(w_uq[:, :, cols])
    gw = f(inp["gla_w_gate"])
    gb = f(inp["gla_b_gate"])
    gwblk = np.zeros((DEPTH, 4, 32, 128), np.float32)
    gbias = np.zeros((DEPTH, 4, 128), np.float32)
    for h in range(4):
        gwblk[:, h, 0:16, 0:64] = gw[:, 0, :, h * 64:(h + 1) * 64]
        gwblk[:, h, 16:32, 64:128] = gw[:, 1, :, h * 64:(h + 1) * 64]
        gbias[:, h, 0:64] = gb[:, 0, h * 64:(h + 1) * 64]
        gbias[:, h, 64:128] = gb[:, 1, h * 64:(h + 1) * 64]
    shared = dict(
        mod_w=f(inp["mod_w"]), mod_b=f(inp["mod_b"]), norm1_w=f(inp["norm1_w"]), norm2_w=f(inp["norm2_w"]),
        wp=wp_, mla_q_norm=f(inp["mla_q_norm"]), w_uq=w_uq, w_uq_sw=w_uq_sw, mla_kv_norm=f(inp["mla_kv_norm"]),
        w_ukv=f(inp["mla_w_ukv"]), ret_decay_logit=f(inp["ret_decay_logit"]), ret_norm_w=f(inp["ret_norm_w"]),
        gwblk=gwblk, gbias=gbias, gla_norm_w=f(inp["gla_norm_w"]), w_branch=f(inp["w_branch"]), w_out=f(inp["w_out"]),
        ffn_w_gate=f(inp["ffn_w_gate"]), ffn_w_up=f(inp["ffn_w_up"]), ffn_w_down=f(inp["ffn_w_down"]),
        moe_router=f(inp["moe_router"]), moe_w_gate=f(inp["moe_w_gate"]), moe_w_up=f(inp["moe_w_up"]),
        moe_w_down=f(inp["moe_w_down"]), final_norm_w=f(inp["final_norm_w"]),
    )
    maps = []
    for core in range(NCORES):
        b, q = core // 4, core % 4
        m = dict(shared)
        m["xs_in"] = np.ascontiguousarray(np.concatenate([x[b, q * LAT:(q + 1) * LAT], ctx[b]], 0))
        m["cvecT"] = np.ascontiguousarray(np.stack([c[b], c_ctx], 1))
        m.update(_consts(q))
        maps.append(m)
    return maps


_PROG = {}


def kernel(**inputs):
    if "nc" not in _PROG:
        _PROG["nc"] = build_program()[0]
    nc = _PROG["nc"]
    maps = make_in_maps(inputs)
    res = run_bass_kernel_spmd(nc, maps, core_ids=list(range(NCORES)))
    outp = np.zeros((2, SEQ, D), np.float32)
    for core in range(NCORES):
        b, q = core // 4, core % 4
        outp[b, q * LAT:(q + 1) * LAT] = res.results[core]["out"]
    return outp
```

```python
import math
from contextlib import ExitStack
import numpy as np
import ml_dtypes
import concourse.bass as bass
import concourse.mybir as mybir
from concourse.bass_utils import run_bass_kernel_spmd

F32 = mybir.dt.float32
BF16 = mybir.dt.bfloat16
AF = mybir.ActivationFunctionType
ALU = mybir.AluOpType
AX = mybir.AxisListType

NCORES = 8
D = 1024
SEQ = 8192
CTX = 256
LAT = 2048
T = LAT + CTX
NT = T // 128
NLT = LAT // 128
DEPTH = 2
EPS = 1e-6
DFF = 2816
NFF = DFF // 128
NEXP = 8
NKEY = CTX + SEQ
NKT = NKEY // 128

PA = 0
PA_N = 480
P_RET = 480
P_GLA = P_RET + 4 * 512
P_RV = P_GLA + 4 * 256
P_GV = P_RV + 512
P_RG = P_GV + 512
P_GR = P_RG + 512
P_BG = P_GR + 512
NPACK = P_BG + 3072

XF_STATE = 0
XF_LAM = 8
NXF = 9
XB_CKV = 0
XB_KR = 8
NXB = 10

EPOCH = 30000


class Buf:
    __slots__ = ("name", "w", "r")

    def __init__(self, name=""):
        self.name = name
        self.w = None
        self.r = []


class Ring:
    def __init__(self, items):
        self.items = items
        self.i = 0

    def next(self):
        it = self.items[self.i % len(self.items)]
        self.i += 1
        return it


class K:
    def __init__(self, nc):
        self.nc = nc
        self.eng = {"pe": nc.tensor, "dve": nc.vector, "act": nc.scalar,
                    "pool": nc.gpsimd, "sp": nc.sync}
        self.sems = {}
        self.cnt = {}
        self.epoch = {e: 0 for e in self.eng}
        self.seen = {}
        for e in self.eng:
            self._new_epoch(e, first=True)
        self.dq = {}
        for q, n in (("sp", 24), ("pool", 16), ("act", 4)):
            keys = []
            for i in range(n):
                key = ("dma", q, i)
                self.sems[key] = nc.alloc_semaphore(f"d_{q}_{i}")
                self.cnt[key] = 0
                keys.append(key)
            self.dq[q] = [keys, 0]
        self.cc_key = ("cc", 0)
        self.sems[self.cc_key] = nc.alloc_semaphore("cc")
        self.cnt[self.cc_key] = 0
        self.n_ins = 0

    def _new_epoch(self, e, first=False):
        if not first:
            self.epoch[e] += 1
        key = (e, self.epoch[e])
        self.sems[key] = self.nc.alloc_semaphore(f"s_{e}_{self.epoch[e]}")
        self.cnt[key] = 0

    def _wait(self, e, toks, force_same=False):
        eng = self.eng[e]
        best = {}
        for t in toks:
            if t is None:
                continue
            key, val = t
            if key[0] == e and not force_same:
                if e in ("pe", "sp"):
                    continue
            if best.get(key, 0) < val:
                best[key] = val
        for key, val in best.items():
            if self.seen.get((e, key), 0) >= val:
                continue
            assert self.cnt[key] >= val, f"wait on unsignalled token {key} {val} > {self.cnt[key]}"
            eng.wait_ge(self.sems[key], val)
            self.seen[(e, key)] = val

    @staticmethod
    def _deps(R, W):
        toks = []
        for b in R:
            toks.append(b.w)
        for b in W:
            toks.append(b.w)
            toks.extend(b.r)
        return toks

    @staticmethod
    def _commit(tok, R, W):
        for b in R:
            b.r.append(tok)
            if len(b.r) > 24:
                b.r = b.r[-24:] if False else b.r
        for b in W:
            b.w = tok
            b.r = []

    def op(self, e, fn, *args, R=(), W=(), sig=True, **kw):
        self._wait(e, self._deps(R, W))
        ins = fn(*args, **kw)
        self.n_ins += 1
        key = (e, self.epoch[e])
        if sig:
            self.cnt[key] += 1
            ins.then_inc(self.sems[key], 1)
            tok = (key, self.cnt[key])
            if self.cnt[key] >= EPOCH:
                self._new_epoch(e)
        else:
            tok = (key, self.cnt[key] + 1)
        self._commit(tok, R, W)
        return tok

    def dma(self, q, out, in_, R=(), W=(), **kw):
        keys, idx = self.dq[q]
        key = keys[idx % len(keys)]
        self.dq[q][1] = idx + 1
        toks = self._deps(R, W)
        if self.cnt[key] > 0:
            toks.append((key, self.cnt[key]))
        self._wait(q, toks)
        ins = self.eng[q].dma_start(out=out, in_=in_, **kw)
        self.n_ins += 1
        self.cnt[key] += 16
        ins.then_inc(self.sems[key], 16)
        tok = (key, self.cnt[key])
        self._commit(tok, R, W)
        return tok

    def allgather(self, in_ap, out_ap, R=(), W=()):
        self._wait("pool", self._deps(R, W))
        ins = self.nc.gpsimd.collective_compute(
            "AllGather", ALU.bypass, replica_groups=[[0, 1, 2, 3], [4, 5, 6, 7]],
            ins=[in_ap], outs=[out_ap])
        self.n_ins += 1
        self.cnt[self.cc_key] += 1
        ins.then_inc(self.sems[self.cc_key])
        tok = (self.cc_key, self.cnt[self.cc_key])
        self._commit(tok, R, W)
        return tok

    def fence(self):
        toks = []
        for key, c in self.cnt.items():
            if c > 0:
                toks.append((key, c))
        for e in self.eng:
            self._wait(e, toks, force_same=False)

    def wait_bufs(self, e, bufs):
        toks = []
        for b in bufs:
            toks.append(b.w)
            toks.extend(b.r)
        self._wait(e, toks, force_same=True)


def build_program(nlayers=DEPTH, dbg=None):
    nc = bass.Bass("TRN2", target_bir_lowering=False)
    k = K(nc)
    dbg = dbg or {}
    uid = [0]

    def U(name):
        uid[0] += 1
        return f"{name}_{uid[0]}"

    def din(name, shape, dt=F32):
        return nc.dram_tensor(name, list(shape), dt, kind="ExternalInput").ap()

    xs_in = din("xs_in", [T, D])
    cvecT = din("cvecT", [D, 2])
    mod_w = din("mod_w", [DEPTH, D, 6 * D])
    mod_b = din("mod_b", [DEPTH, 6 * D])
    norm1_w = din("norm1_w", [DEPTH, D])
    norm2_w = din("norm2_w", [DEPTH, D])
    wp = din("wp", [DEPTH, D, NPACK])
    q_norm = din("mla_q_norm", [DEPTH, 256])
    w_uq = din("w_uq", [DEPTH, 256, 768])
    w_uq_sw = din("w_uq_sw", [DEPTH, 256, 768])
    kv_norm = din("mla_kv_norm", [DEPTH, 128])
    w_ukv = din("w_ukv", [DEPTH, 128, 1024])
    ret_logit = din("ret_decay_logit", [DEPTH, 2, 4])
    ret_norm_w = din("ret_norm_w", [DEPTH, 512])
    gwblk = din("gwblk", [DEPTH, 4, 32, 128])
    gbias = din("gbias", [DEPTH, 4, 128])
    gla_norm_w = din("gla_norm_w", [DEPTH, 512])
    w_branch = din("w_branch", [DEPTH, 3, 512, D])
    w_out = din("w_out", [DEPTH, D, D])
    ffn_wg = din("ffn_w_gate", [1, D, DFF])
    ffn_wu = din("ffn_w_up", [1, D, DFF])
    ffn_wd = din("ffn_w_down", [1, DFF, D])
    moe_router = din("moe_router", [1, D, NEXP])
    moe_wg = din("moe_w_gate", [1, NEXP, D, DFF])
    moe_wu = din("moe_w_up", [1, NEXP, D, DFF])
    moe_wd = din("moe_w_down", [1, NEXP, DFF, D])
    final_norm_w = din("final_norm_w", [D])
    c_ropeR = din("c_ropeR", [2, 128, T])
    c_ropeAq = din("c_ropeAq", [2, 32, T])
    c_ropeAk = din("c_ropeAk", [2, 32, T])
    c_rst = din("c_rst", [128, T])
    c_mask = din("c_mask", [2, 128, 128])
    c_ident = din("c_ident", [128, 128])
    c_onehot = din("c_onehot", [128, 4])

    out = nc.dram_tensor("out", [LAT, D], F32, kind="ExternalOutput").ap()
    dbg_out = {}
    for name, (shape, dt) in dbg.items():
        dbg_out[name] = nc.dram_tensor("dbg_" + name, list(shape), dt, kind="ExternalOutput").ap()

    xs = nc.dram_tensor("xs", [T, D], F32).ap()
    uT = nc.dram_tensor("uT", [8, 128, T], BF16).ap()
    modD = nc.dram_tensor("modD", [2, 6 * D], F32).ap()
    xf_in = nc.dram_tensor("xf_in", [NXF, 16, 1024], F32).ap()
    xf_out = nc.dram_tensor("xf_out", [NXF, 64, 1024], F32).ap()
    xb_in = nc.dram_tensor("xb_in", [NXB, 16, LAT], BF16).ap()
    xb_out = nc.dram_tensor("xb_out", [NXB, 64, LAT], BF16).ap()
    b_xs = [Buf(f"xs{t}") for t in range(NT)]
    b_uT = [Buf(f"uT{b}") for b in range(5)]
    b_modD = Buf("modD")
    b_xf_in = [Buf() for _ in range(NXF)]
    b_xf_out = [Buf() for _ in range(NXF)]
    b_xb_in = [Buf() for _ in range(NXB)]
    b_xb_out = [Buf() for _ in range(NXB)]
    b_out = Buf("out")

    PSA = nc.alloc_psum_tensor("psa", [128, 8, 512], F32)
    PS = []
    for i in range(8):
        PS.append((PSA[:, i, :], Buf(f"ps{i}")))

    def salloc(name, shape, dt=F32):
        return nc.alloc_sbuf_tensor(name, list(shape), dt), Buf(name)

    ident, b_ident = salloc("ident", [128, 128])
    identb, b_identb = salloc("identb", [128, 128], BF16)
    onesb, b_onesb = salloc("onesb", [128, 128], BF16)
    maskF, b_maskF = salloc("maskF", [128, 128])
    maskB, b_maskB = salloc("maskB", [128, 128])
    onehot, b_onehot = salloc("onehot", [128, 4])
    eps_t, b_eps = salloc("eps_t", [128, 1])
    one_t, b_one = salloc("one_t", [128, 1])
    modT, b_modT = salloc("modT", [128, 48, 2])
    A1, b_A1 = salloc("A1", [128, 8, 2])
    A2, b_A2 = salloc("A2", [128, 8, 2])
    rstb, b_rstb = salloc("rstb", [128, T], BF16)
    yTd = nc.dram_tensor("yTd", [3, 4, 128, T], BF16).ap()
    lsp_kt2 = nc.dram_tensor("lsp_kt2", [8, 128, T], BF16).ap()
    lsp_vth = nc.dram_tensor("lsp_vth", [8, 128, NT * 128], BF16).ap()
    lsp_eb = nc.dram_tensor("lsp_eb", [8, 128, T], BF16).ap()
    lsp_U2 = nc.dram_tensor("lsp_U2", [8, 128, NT * 128], F32).ap()
    lsp_te = nc.dram_tensor("lsp_te", [8, 128, 2 * NT], F32).ap()
    b_lsp = [Buf(f"lsp{i}") for i in range(8)]
    b_yTd = [Buf(f"yTd{i}") for i in range(3)]

    k.dma("sp", ident[:], c_ident[:, :], W=[b_ident])
    k.op("dve", nc.vector.tensor_copy, identb[:], ident[:], R=[b_ident], W=[b_identb])
    k.op("dve", nc.vector.memset, onesb[:], 1.0, W=[b_onesb])
    k.op("dve", nc.vector.memset, eps_t[:], EPS, W=[b_eps])
    k.op("dve", nc.vector.memset, one_t[:], 1.0, W=[b_one])
    k.dma("sp", maskF[:], c_mask[0], W=[b_maskF])
    k.dma("sp", maskB[:], c_mask[1], W=[b_maskB])
    k.dma("sp", onehot[:], c_onehot[:, :], W=[b_onehot])
    k.dma("pool", rstb[:], c_rst[:, :], W=[b_rstb])
    with nc.sbuf_tensor(U("zinit"), [16, 1024], F32) as zt_:
        b_zt = Buf()
        k.op("dve", nc.vector.memset, zt_[:], 0.0, W=[b_zt])
        k.dma("sp", xf_in[XF_LAM], zt_[:], R=[b_zt], W=[b_xf_in[XF_LAM]])
        k.fence()

    BLKS = [(0, 512), (512, 512), (1024, 512), (1536, 512), (2048, 256)]

    def wview(w2d):
        return w2d.rearrange("(kc p) n -> p kc n", p=128)

    alt = [0]

    def evac_engine():
        alt[0] += 1
        return "act" if alt[0] % 2 else "dve"

    def copy_ps(e, out_ap, in_ap, R, W, scale=None):
        if e == "act":
            if scale is None:
                k.op("act", nc.scalar.copy, out_ap, in_ap, R=R, W=W)
            else:
                k.op("act", nc.scalar.mul, out_ap, in_ap, scale, R=R, W=W)
        else:
            if scale is None:
                k.op("dve", nc.vector.tensor_copy, out_ap, in_ap, R=R, W=W)
            else:
                k.op("dve", nc.vector.tensor_scalar, out_ap, in_ap, scale, None, ALU.mult, R=R, W=W)

    def rsqrt_col(es, name, src_ap, src_buf, scale, n=1, parts=128):
        t1 = es.enter_context(nc.sbuf_tensor(U(name + "_a"), [128, n], F32))
        t2 = es.enter_context(nc.sbuf_tensor(U(name + "_b"), [128, n], F32))
        b1, b2 = Buf(), Buf()
        k.op("dve", nc.vector.tensor_scalar, t1[0:parts, :], src_ap, scale, EPS, ALU.mult, ALU.add,
             R=[src_buf], W=[b1])
        k.op("act", nc.scalar.activation, t2[0:parts, :], t1[0:parts, :], AF.Sqrt, R=[b1], W=[b2])
        k.op("dve", nc.vector.reciprocal, t1[0:parts, :], t2[0:parts, :], R=[b2], W=[b1])
        return t1, b1

    def phase_mod(l):
        with ExitStack() as es:
            def sb(name, shape, dt=F32):
                return es.enter_context(nc.sbuf_tensor(U(name), list(shape), dt)), Buf(name)
            cT, b_cT = sb("cT", [128, 8, 2])
            sc, b_sc = sb("sc", [128, 8, 2])
            sg, b_sg = sb("sg", [128, 8, 2])
            modv, b_modv = sb("modv", [2, 6 * D])
            mb, b_mb = sb("mb", [2, 6 * D])
            wr = Ring([sb(f"mw{i}", [128, 8, 512], BF16) for i in range(4)])
            scb, b_scb = sb("scb", [128, 8, 2], BF16)
            k.dma("sp", cT[:], cvecT.rearrange("(kc p) r -> p kc r", p=128), W=[b_cT])
            k.op("act", nc.scalar.activation, sg[:], cT[:], AF.Sigmoid, R=[b_cT], W=[b_sg])
            k.op("dve", nc.vector.tensor_tensor, sc[:], cT[:], sg[:], ALU.mult, R=[b_cT, b_sg], W=[b_sc])
            k.op("dve", nc.vector.tensor_copy, scb[:], sc[:], R=[b_sc], W=[b_scb])
            k.dma("sp", mb[0:1, :], mod_b[l:l + 1, :], W=[b_mb])
            k.dma("sp", mb[1:2, :], mod_b[l:l + 1, :], W=[b_mb])
            psr = Ring(PS[0:2])
            for n in range(12):
                wt, b_wt = wr.next()
                k.dma("pool", wt[:], wview(mod_w[l])[:, :, n * 512:(n + 1) * 512], W=[b_wt])
                ps, b_ps = psr.next()
                for kc in range(8):
                    k.op("pe", nc.tensor.matmul, ps[0:2, :], scb[:, kc, :], wt[:, kc, :],
                         start=(kc == 0), stop=(kc == 7), R=[b_scb, b_wt], W=[b_ps], sig=(kc == 7))
                k.op("dve", nc.vector.tensor_tensor, modv[:, n * 512:(n + 1) * 512], ps[0:2, :],
                     mb[:, n * 512:(n + 1) * 512], ALU.add, R=[b_ps, b_mb], W=[b_modv])
            k.dma("sp", modD[:, :], modv[:], R=[b_modv], W=[b_modD])
            pst, b_pst = PS[2]
            for j in range(48):
                k.op("pe", nc.tensor.transpose, pst[:, 2 * j:2 * j + 2], modv[0:2, j * 128:(j + 1) * 128],
                     ident[0:2, 0:2], R=[b_modv, b_ident], W=[b_pst], sig=(j == 47))
            k.op("dve", nc.vector.tensor_copy, modT[:].rearrange("p j r -> p (j r)"), pst[:, 0:96],
                 R=[b_pst], W=[b_modT])
            nw, b_nw = sb("nw", [128, 8, 2])
            for (nsrc, joff, At, bA) in ((norm1_w, 8, A1, b_A1), (norm2_w, 32, A2, b_A2)):
                k.dma("sp", nw[:, :, 0], nsrc[l].rearrange("(kc p) -> p kc", p=128), W=[b_nw], allow_slow_non_contiguous=True)
                k.dma("sp", nw[:, :, 1], nsrc[l].rearrange("(kc p) -> p kc", p=128), W=[b_nw], allow_slow_non_contiguous=True)
                k.op("dve", nc.vector.tensor_scalar, At[:], modT[:, joff:joff + 8, :], 1.0, None, ALU.add,
                     R=[b_modT], W=[bA])
                k.op("dve", nc.vector.tensor_tensor, At[:], At[:], nw[:], ALU.mult, R=[b_nw], W=[bA])
        k.fence()

    def phase_norm(l, At, bA, shoff, tiles, xsrc=None):
        xsrc = xs if xsrc is None else xsrc
        with ExitStack() as es:
            def sb(name, shape, dt=F32):
                return es.enter_context(nc.sbuf_tensor(U(name), list(shape), dt)), Buf(name)
            ND = 6
            xr = Ring([sb(f"nx{i}", [128, D]) for i in range(ND)])
            jr = Ring([sb(f"nj{i}", [128, D]) for i in range(2)])
            xnr = Ring([sb(f"nn{i}", [128, D]) for i in range(ND)])
            ur = Ring([sb(f"nu{i}", [128, 8, 128], BF16) for i in range(ND)])
            ssr = Ring([sb(f"ns{i}", [128, 1]) for i in range(ND)])
            psr = Ring([PS[0], PS[1], PS[2], PS[3]])
            xloads = {}

            def issue_x(i_):
                if i_ < len(tiles):
                    t_ = tiles[i_]
                    xt_, b_xt_ = xr.next()
                    k.dma("sp", xt_[:], xsrc[t_ * 128:(t_ + 1) * 128, :], R=[b_xs[t_]], W=[b_xt_])
                    xloads[i_] = (xt_, b_xt_)
            for i_ in range(ND - 1):
                issue_x(i_)
            for i_, t in enumerate(tiles):
                r = 0 if t < NLT else 1
                issue_x(i_ + ND - 1)
                xt, b_xt = xloads.pop(i_)
                jk, b_jk = jr.next()
                ss, b_ss = ssr.next()
                k.op("act", nc.scalar.activation, jk[:], xt[:], AF.Square, accum_out=ss[:, 0:1],
                     R=[b_xt], W=[b_ss])
                rstd, b_rstd = rsqrt_col(es, f"nr{t}", ss[:, 0:1], b_ss, 1.0 / D)
                xn, b_xn = xnr.next()
                k.op("act", nc.scalar.activation, xn[:], xt[:], AF.Identity, scale=rstd[:, 0:1],
                     R=[b_xt, b_rstd], W=[b_xn])
                ut, b_ut = ur.next()
                for half in range(2):
                    ps, b_ps = psr.next()
                    for j in range(4):
                        kc = half * 4 + j
                        k.op("pe", nc.tensor.transpose, ps[:, j * 128:(j + 1) * 128],
                             xn[:, kc * 128:(kc + 1) * 128], ident[:], R=[b_xn, b_ident], W=[b_ps],
                             sig=(j == 3))
                    for j in range(4):
                        kc = half * 4 + j
                        if j % 2 == 0:
                            k.op("act", nc.scalar.activation, ut[:, kc, :], ps[:, j * 128:(j + 1) * 128],
                                 AF.Identity, scale=At[:, kc, r:r + 1], bias=modT[:, shoff + kc, r:r + 1],
                                 R=[b_ps, bA, b_modT], W=[b_ut])
                        else:
                            k.op("dve", nc.vector.tensor_scalar, ut[:, kc, :], ps[:, j * 128:(j + 1) * 128],
                                 At[:, kc, r:r + 1], modT[:, shoff + kc, r:r + 1], ALU.mult, ALU.add,
                                 R=[b_ps, bA, b_modT], W=[b_ut])
                blk = min(t // 4, 4)
                k.dma("sp", uT[:, :, t * 128:(t + 1) * 128].rearrange("kc p t -> p kc t"), ut[:],
                      R=[b_ut], W=[b_uT[blk]])
        k.fence()

    class UStream:
        def __init__(self, es, nbuf=2, tag="ub"):
            self.ring = Ring([(es.enter_context(nc.sbuf_tensor(U(f"{tag}{i}"), [128, 8, 512], BF16)), Buf())
                              for i in range(nbuf)])

        def load(self, bi):
            t0, n = BLKS[bi]
            ub, b_ub = self.ring.next()
            k.dma("sp", ub[:, :, 0:n], uT[:, :, t0:t0 + n].rearrange("kc p t -> p kc t"),
                  R=[b_uT[bi]], W=[b_ub])
            return ub, b_ub, t0, n

    def proj_fm(ps, b_ps, w, b_w, c0, m, ub, b_ub, n, prow=0):
        for kc in range(8):
            k.op("pe", nc.tensor.matmul, ps[prow:prow + m, 0:n], w[:, kc, c0:c0 + m], ub[:, kc, 0:n],
                 start=(kc == 0), stop=(kc == 7), R=[b_w, b_ub], W=[b_ps], sig=(kc == 7))

    def load_w(es, name, src3d, shape, q="pool"):
        t = es.enter_context(nc.sbuf_tensor(U(name), list(shape), BF16))
        b = Buf(name)
        k.dma(q, t[:], src3d, W=[b])
        return t, b


    LS = {}
    LG2, b_LG2 = salloc("LG2", [128, 4])
    LAMT, b_LAMT = salloc("LAMT", [128, 8])

    def phase_q(l):
        cqn, b_cqn = LS["cqn"]; ckvn, b_ckvn = LS["ckvn"]; krr, b_krr = LS["krr"]; gzT, b_gzT = LS["gzT"]
        with ExitStack() as es:
            def sb(name, shape, dt=F32):
                return es.enter_context(nc.sbuf_tensor(U(name), list(shape), dt)), Buf(name)
            wA, b_wA = load_w(es, "wA", wview(wp[l])[:, :, PA:PA + PA_N], [128, 8, PA_N])
            qnw, b_qnw = sb("qnw", [128, 2])
            kvnw, b_kvnw = sb("kvnw", [128, 1])
            k.dma("sp", qnw[:], q_norm[l].rearrange("(c p) -> p c", p=128), W=[b_qnw], allow_slow_non_contiguous=True)
            k.dma("sp", kvnw[:], kv_norm[l].rearrange("(c p) -> p c", p=128), W=[b_kvnw], allow_slow_non_contiguous=True)
            ropk, b_ropk = sb("ropk", [32, 2, T])
            k.dma("sp", ropk[:, 0, :], c_ropeAk[0], W=[b_ropk])
            k.dma("sp", ropk[:, 1, :], c_ropeAk[1], W=[b_ropk])
            us = UStream(es)
            sqr = Ring([sb(f"sq{i}", [128, 512], BF16) for i in range(3)])
            rq, b_rq = sb("rq", [128, 512])
            rq2, b_rq2 = sb("rq2", [128, 512])
            tk, b_tk = sb("tk", [32, 512])
            tk2, b_tk2 = sb("tk2", [32, 512])
            for bi in range(5):
                ub, b_ub, t0, n = us.load(bi)
                (p0, bp0), (p1, bp1), (p2, bp2), (p3, bp3) = PS[0], PS[1], PS[2], PS[3]
                proj_fm(p0, bp0, wA, b_wA, 0, 128, ub, b_ub, n)
                proj_fm(p1, bp1, wA, b_wA, 128, 128, ub, b_ub, n)
                proj_fm(p2, bp2, wA, b_wA, 256, 128, ub, b_ub, n)
                s0, bs0 = sqr.next(); s1, bs1 = sqr.next(); s2, bs2 = sqr.next()
                k.op("act", nc.scalar.activation, s0[:, 0:n], p0[:, 0:n], AF.Square, R=[bp0], W=[bs0])
                k.op("act", nc.scalar.activation, s1[:, 0:n], p1[:, 0:n], AF.Square, R=[bp1], W=[bs1])
                k.op("act", nc.scalar.activation, s2[:, 0:n], p2[:, 0:n], AF.Square, R=[bp2], W=[bs2])
                k.op("pe", nc.tensor.matmul, p3[:, 0:n], onesb[:], s0[:, 0:n], start=True, stop=False,
                     R=[b_onesb, bs0], W=[bp3], sig=False)
                k.op("pe", nc.tensor.matmul, p3[:, 0:n], onesb[:], s1[:, 0:n], start=False, stop=True,
                     R=[b_onesb, bs1], W=[bp3])
                k.op("dve", nc.vector.tensor_scalar, rq[:, 0:n], p3[:, 0:n], 1.0 / 256, EPS, ALU.mult, ALU.add,
                     R=[bp3], W=[b_rq])
                k.op("act", nc.scalar.activation, rq2[:, 0:n], rq[:, 0:n], AF.Sqrt, R=[b_rq], W=[b_rq2])
                k.op("dve", nc.vector.reciprocal, rq[:, 0:n], rq2[:, 0:n], R=[b_rq2], W=[b_rq])
                k.op("dve", nc.vector.scalar_tensor_tensor, cqn[:, 0, t0:t0 + n], p0[:, 0:n], qnw[:, 0:1], rq[:, 0:n],
                     ALU.mult, ALU.mult, R=[bp0, b_qnw, b_rq], W=[b_cqn])
                k.op("dve", nc.vector.scalar_tensor_tensor, cqn[:, 1, t0:t0 + n], p1[:, 0:n], qnw[:, 1:2], rq[:, 0:n],
                     ALU.mult, ALU.mult, R=[bp1, b_qnw, b_rq], W=[b_cqn])
                k.op("pe", nc.tensor.matmul, p3[:, 0:n], onesb[:], s2[:, 0:n], start=True, stop=True,
                     R=[b_onesb, bs2], W=[bp3])
                k.op("dve", nc.vector.tensor_scalar, rq[:, 0:n], p3[:, 0:n], 1.0 / 128, EPS, ALU.mult, ALU.add,
                     R=[bp3], W=[b_rq])
                k.op("act", nc.scalar.activation, rq2[:, 0:n], rq[:, 0:n], AF.Sqrt, R=[b_rq], W=[b_rq2])
                k.op("dve", nc.vector.reciprocal, rq[:, 0:n], rq2[:, 0:n], R=[b_rq2], W=[b_rq])
                k.op("dve", nc.vector.scalar_tensor_tensor, ckvn[:, t0:t0 + n], p2[:, 0:n], kvnw[:, 0:1], rq[:, 0:n],
                     ALU.mult, ALU.mult, R=[bp2, b_kvnw, b_rq], W=[b_ckvn])
                (p4, bp4), (p5, bp5), (p6, bp6) = PS[4], PS[5], PS[6]
                proj_fm(p4, bp4, wA, b_wA, 384, 32, ub, b_ub, n)
                proj_fm(p5, bp5, wA, b_wA, 416, 32, ub, b_ub, n)
                proj_fm(p6, bp6, wA, b_wA, 448, 32, ub, b_ub, n)
                k.op("dve", nc.vector.tensor_tensor, tk[:, 0:n], p4[0:32, 0:n], ropk[:, 0, t0:t0 + n], ALU.mult,
                     R=[bp4, b_ropk], W=[b_tk])
                k.op("dve", nc.vector.tensor_tensor, tk2[:, 0:n], p5[0:32, 0:n], ropk[:, 1, t0:t0 + n], ALU.mult,
                     R=[bp5, b_ropk], W=[b_tk2])
                k.op("dve", nc.vector.tensor_tensor, krr[:, t0:t0 + n], tk[:, 0:n], tk2[:, 0:n], ALU.add,
                     R=[b_tk, b_tk2], W=[b_krr])
                k.op("act", nc.scalar.copy, gzT[:, t0:t0 + n], p6[0:32, 0:n], R=[bp6], W=[b_gzT])
            for j in range(8):
                k.dma("sp", xb_in[XB_CKV + j], ckvn[16 * j:16 * j + 16, 0:LAT], R=[b_ckvn], W=[b_xb_in[XB_CKV + j]])
            for j in range(2):
                k.dma("sp", xb_in[XB_KR + j], krr[16 * j:16 * j + 16, 0:LAT], R=[b_krr], W=[b_xb_in[XB_KR + j]])
            for j in range(NXB):
                k.allgather(xb_in[j], xb_out[j], R=[b_xb_in[j]], W=[b_xb_out[j]])
        k.fence()

    def phase_lg(l):
        with ExitStack() as es:
            def sb(name, shape, dt=F32):
                return es.enter_context(nc.sbuf_tensor(U(name), list(shape), dt)), Buf(name)
            lt, b_lt = sb("lt", [128, 4])
            l2, b_l2 = sb("l2", [128, 4])
            k.dma("sp", lt[0:64, :], ret_logit[l, 0:1, :].partition_broadcast(64), W=[b_lt])
            k.dma("sp", lt[64:128, :], ret_logit[l, 1:2, :].partition_broadcast(64), W=[b_lt])
            k.op("act", nc.scalar.activation, l2[:], lt[:], AF.Exp, scale=-1.0, R=[b_lt], W=[b_l2])
            k.op("act", nc.scalar.activation, lt[:], l2[:], AF.Ln, bias=one_t[:, 0:1], R=[b_l2, b_one], W=[b_lt])
            k.op("dve", nc.vector.tensor_scalar, LG2[:], lt[:], -1.0, None, ALU.mult, R=[b_lt], W=[b_LG2])
        k.fence()

    def lm_cols(mix, h):
        if mix == 0:
            return P_RET + h * 512, 512
        return P_GLA + h * 256, 256

    def lm_prefetch(l, mix, h, PF):
        hm = mix * 4 + h
        base, ncol = lm_cols(mix, h)
        k.dma("pool", PF["w"][0][:, :, 0:ncol], wview(wp[l])[:, :, base:base + ncol], W=[PF["w"][1]])
        gcol = (P_RG if mix == 0 else P_GR) + h * 128
        k.dma("pool", PF["wg"][0][:], wview(wp[l])[:, :, gcol:gcol + 128], W=[PF["wg"][1]])
        k.dma("sp", PF["kt2"][0][:], lsp_kt2[hm], R=[b_lsp[hm]], W=[PF["kt2"][1]])
        k.dma("sp", PF["vth"][0][:].rearrange("p n v -> p (n v)"), lsp_vth[hm], R=[b_lsp[hm]], W=[PF["vth"][1]])
        k.dma("sp", PF["ebT"][0][:], lsp_eb[hm], R=[b_lsp[hm]], W=[PF["ebT"][1]])
        k.dma("sp", PF["U2"][0][:].rearrange("p n v -> p (n v)"), lsp_U2[hm], R=[b_lsp[hm]], W=[PF["U2"][1]])
        k.dma("sp", PF["TE"][0][:], lsp_te[hm], R=[b_lsp[hm]], W=[PF["TE"][1]])

    def linear_mixer(l, mix, h, pass_no, last, PF=None, after_stage1=None):
        hm = mix * 4 + h
        gzT, b_gzT = LS["gzT"]
        with ExitStack() as es:
            def sb(name, shape, dt=F32):
                return es.enter_context(nc.sbuf_tensor(U(name), list(shape), dt)), Buf(name)
            if mix == 0:
                base, ncol = P_RET + h * 512, 512
                cq2, cq2s, ck2, ck2s = 0, 128, 256, 384
            else:
                base, ncol = P_GLA + h * 256, 256
                cq2, ck2 = 0, 128
            if pass_no == 1:
                w, b_w = load_w(es, "lw", wview(wp[l])[:, :, base:base + ncol], [128, 8, ncol])
            else:
                w, b_w = PF["w"]
            vcol = (P_RV if mix == 0 else P_GV) + h * 128
            if pass_no == 1:
                wv, b_wv = load_w(es, "lwv", wview(wp[l])[:, :, vcol:vcol + 128], [128, 8, 128])
            if mix == 1 and pass_no == 1:
                gw, b_gw = load_w(es, "gw", gwblk[l, h], [32, 128])
                nb, b_nb = sb("nb", [128, 1])
                nb2, b_nb2 = sb("nb2", [128, 1])
                k.dma("sp", nb[:], gbias[l, h].rearrange("(p o) -> p o", o=1), W=[b_nb])
                k.op("dve", nc.vector.tensor_scalar, nb2[:], nb[:], -1.0, None, ALU.mult, R=[b_nb], W=[b_nb2])
            if pass_no == 1:
                TE, b_TOT = sb("TE", [128, 2 * NT])
                kt2, b_kt2 = sb("kt2", [128, T], BF16)
                vth, b_vth = sb("vth", [128, NT, 128], BF16)
                ebT, b_ebT = sb("ebT", [128, T], BF16)
                U2, _ = sb("U2", [128, NT, 128])
            else:
                TE, b_TOT = PF["TE"]
                kt2, b_kt2 = PF["kt2"]
                vth, b_vth = PF["vth"]
                ebT, b_ebT = PF["ebT"]
                U2, b_ld = PF["U2"]
            TOT, EEND = TE[:, 0:NT], TE[:, NT:2 * NT]
            b_EEND = b_TOT
            b_kdc = [Buf() for _ in range(NT)]
            b_vthc = [Buf() for _ in range(NT)]
            us = UStream(es)
            if pass_no == 1:
                kd, b_kd = sb("kd", [128, T], BF16)
                a2r = Ring([sb(f"a2_{i}", [128, 512]) for i in range(2)])
                csr = Ring([sb(f"cs_{i}", [128, 512]) for i in range(2)])
                B2r = Ring([sb(f"B2_{i}", [128, 512]) for i in range(2)])
                enr = Ring([sb(f"en_{i}", [128, 512]) for i in range(2)])
            else:
                qt2, b_qt2 = sb("qt2", [128, T], BF16)
                b_vthc = [b_vth for _ in range(NT)]
            t1r = Ring([sb(f"lt1{i}", [128, 512]) for i in range(2)])
            t2r = Ring([sb(f"lt2{i}", [128, 512]) for i in range(2)])
            if mix == 0:
                ropr = Ring([sb(f"rop{i}", [128, 2, 512]) for i in range(2)])

            for bi in range(5):
                ub, b_ub, t0, n = us.load(bi)
                nch = n // 128
                ch0 = t0 // 128
                if pass_no == 1:
                    a2, b_a2 = a2r.next(); cs, b_cs = csr.next(); B2, b_B2 = B2r.next()
                    enb, b_enb = enr.next()
                if pass_no == 2:
                    pass
                elif mix == 0:
                    k.op("dve", nc.vector.memset, cs[:, 0:n], 1.0, W=[b_cs])
                    k.op("act", nc.scalar.activation, a2[:, 0:n], cs[:, 0:n], AF.Identity, scale=LG2[:, h:h + 1],
                         R=[b_cs, b_LG2], W=[b_a2])
                elif pass_no == 1:
                    pz, bpz = PS[7]
                    k.op("pe", nc.tensor.matmul, pz[:, 0:n], gw[:], gzT[:, t0:t0 + n], start=True, stop=True,
                         R=[b_gw, b_gzT], W=[bpz])
                    k.op("act", nc.scalar.activation, cs[:, 0:n], pz[:, 0:n], AF.Exp, scale=-1.0,
                         bias=nb2[:, 0:1], R=[bpz, b_nb2], W=[b_cs])
                    k.op("act", nc.scalar.activation, B2[:, 0:n], cs[:, 0:n], AF.Ln, bias=one_t[:, 0:1],
                         R=[b_cs, b_one], W=[b_B2])
                    k.op("dve", nc.vector.tensor_scalar, a2[:, 0:n], B2[:, 0:n], -1.0 / 16.0, None,
                         ALU.mult, R=[b_B2], W=[b_a2])
                if pass_no == 1:
                    k.op("dve", nc.vector.tensor_tensor_scan, cs[:, 0:n], rstb[:, t0:t0 + n], a2[:, 0:n], 0.0, ALU.mult, ALU.add,
                         R=[b_rstb, b_a2], W=[b_cs])
                    k.op("dve", nc.vector.tensor_copy, B2[0:64, 0:n], cs[0:64, 0:n], R=[b_cs], W=[b_B2])
                    k.op("dve", nc.vector.tensor_tensor, B2[64:128, 0:n], a2[64:128, 0:n], cs[64:128, 0:n], ALU.subtract,
                         R=[b_a2, b_cs], W=[b_B2])
                    for c_ in range(nch):
                        k.op("dve", nc.vector.tensor_scalar, B2[64:128, c_ * 128:(c_ + 1) * 128],
                             B2[64:128, c_ * 128:(c_ + 1) * 128], cs[64:128, c_ * 128 + 127:c_ * 128 + 128], None, ALU.add,
                             R=[b_cs], W=[b_B2])
                        k.op("dve", nc.vector.tensor_copy, TOT[:, ch0 + c_:ch0 + c_ + 1], cs[:, c_ * 128 + 127:c_ * 128 + 128],
                             R=[b_cs], W=[b_TOT])
                    k.op("act", nc.scalar.activation, EEND[:, ch0:ch0 + nch], TOT[:, ch0:ch0 + nch], AF.Exp,
                         R=[b_TOT], W=[b_EEND])
                    k.op("act", nc.scalar.activation, enb[:, 0:n], B2[:, 0:n], AF.Exp, scale=-1.0, R=[b_B2], W=[b_enb])
                    k.op("act", nc.scalar.activation, ebT[:, t0:t0 + n], B2[:, 0:n], AF.Exp, R=[b_B2], W=[b_ebT])
                if mix == 0:
                    rop, b_rop = ropr.next()
                    k.dma("sp", rop[:, 0, 0:n], c_ropeR[0, :, t0:t0 + n], W=[b_rop])
                    k.dma("sp", rop[:, 1, 0:n], c_ropeR[1, :, t0:t0 + n], W=[b_rop])

                def roped(pa, bpa, pb, bpb):
                    if mix == 1:
                        return pa[:, 0:n], bpa
                    ta, bta = t1r.next()
                    tb, btb = t2r.next()
                    k.op("dve", nc.vector.tensor_tensor, ta[:, 0:n], pa[:, 0:n], rop[:, 0, 0:n], ALU.mult,
                         R=[bpa, b_rop], W=[bta])
                    k.op("dve", nc.vector.tensor_tensor, tb[:, 0:n], pb[:, 0:n], rop[:, 1, 0:n], ALU.mult,
                         R=[bpb, b_rop], W=[btb])
                    k.op("dve", nc.vector.tensor_tensor, ta[:, 0:n], ta[:, 0:n], tb[:, 0:n], ALU.add,
                         R=[btb], W=[bta])
                    return ta[:, 0:n], bta

                (pa, bpa), (pb, bpb) = (PS[0], PS[1]) if bi % 2 == 0 else (PS[2], PS[3])
                if pass_no == 1:
                    proj_fm(pa, bpa, w, b_w, ck2, 128, ub, b_ub, n)
                    if mix == 0:
                        proj_fm(pb, bpb, w, b_w, ck2s, 128, ub, b_ub, n)
                    kap, bk = roped(pa, bpa, pb, bpb)
                    k.op("dve", nc.vector.tensor_tensor, kt2[:, t0:t0 + n], kap, enb[:, 0:n], ALU.mult,
                         R=[bk, b_enb], W=[b_kt2])
                if pass_no == 2:
                    (pc, bpc), (pd, bpd) = (pa, bpa), (pb, bpb)
                    proj_fm(pc, bpc, w, b_w, cq2, 128, ub, b_ub, n)
                    if mix == 0:
                        proj_fm(pd, bpd, w, b_w, cq2s, 128, ub, b_ub, n)
                    qap, bq = roped(pc, bpc, pd, bpd)
                    k.op("dve", nc.vector.scalar_tensor_tensor, qt2[:, t0:t0 + n], qap, 0.125, ebT[:, t0:t0 + n],
                         ALU.mult, ALU.mult, R=[bq, b_ebT], W=[b_qt2])
                for c_ in (range(nch) if pass_no == 1 else []):
                    n_ = ch0 + c_
                    k.op("act", nc.scalar.activation, kd[:, n_ * 128:(n_ + 1) * 128], kt2[:, n_ * 128:(n_ + 1) * 128],
                         AF.Identity, scale=EEND[:, n_:n_ + 1], R=[b_kt2, b_EEND], W=[b_kdc[n_]])
                    pv, bpv = PS[4 + c_ % 2]
                    for kc in range(8):
                        k.op("pe", nc.tensor.matmul, pv[:, 0:128], ub[:, kc, c_ * 128:(c_ + 1) * 128], wv[:, kc, :],
                             start=(kc == 0), stop=(kc == 7), R=[b_ub, b_wv], W=[bpv], sig=(kc == 7))
                    copy_ps(evac_engine(), vth[:, n_, :], pv[:, 0:128], R=[bpv], W=[b_vthc[n_]])
            if pass_no == 1:
                k.dma("sp", lsp_kt2[hm], kt2[:], R=[b_kt2], W=[b_lsp[hm]])
                k.dma("sp", lsp_vth[hm], vth[:].rearrange("p n v -> p (n v)"), R=b_vthc, W=[b_lsp[hm]])
                k.dma("sp", lsp_eb[hm], ebT[:], R=[b_ebT], W=[b_lsp[hm]])
                k.dma("sp", lsp_te[hm], TE[:], R=[b_TOT], W=[b_lsp[hm]])
            elif after_stage1 is not None:
                after_stage1()
            if pass_no == 1:
                k2d, _ = sb("k2d", [128, NT, 128], BF16)
            b_k2dg = [Buf() for _ in range(3)]
            b_U2g = [Buf() for _ in range(5)]
            if pass_no == 2:
                b_U2g = [b_ld]
            if pass_no == 1:
                ptb = PSA[:, 0:3, :].bitcast(BF16)
                for n_ in range(NT):
                    bk = n_ // 8
                    k.op("pe", nc.tensor.transpose, ptb[:, bk, (n_ % 8) * 128:(n_ % 8 + 1) * 128],
                         kd[:, n_ * 128:(n_ + 1) * 128], identb[:], R=[b_kdc[n_], b_identb], W=[PS[bk][1]],
                         sig=(n_ % 8 == 7 or n_ == NT - 1))
                for bk in range(3):
                    nb_ = min(8, NT - 8 * bk)
                    copy_ps(evac_engine(), k2d[:, 8 * bk:8 * bk + nb_, :],
                            ptb[:, bk, 0:nb_ * 128].rearrange("p (a c) -> p a c", c=128), R=[PS[bk][1]], W=[b_k2dg[bk]])
                for n_ in range(NT):
                    bk = 3 + n_ // 4
                    k.op("pe", nc.tensor.matmul, PSA[:, bk, (n_ % 4) * 128:(n_ % 4 + 1) * 128], k2d[:, n_, :], vth[:, n_, :],
                         start=True, stop=True, R=[b_k2dg[n_ // 8], b_vthc[n_]], W=[PS[bk][1]], sig=(n_ % 4 == 3 or n_ == NT - 1))
                for b4 in range(5):
                    nb_ = min(4, NT - 4 * b4)
                    copy_ps(evac_engine(), U2[:, 4 * b4:4 * b4 + nb_, :],
                            PSA[:, 3 + b4, 0:nb_ * 128].rearrange("p (a c) -> p a c", c=128), R=[PS[3 + b4][1]], W=[b_U2g[b4]])
            if pass_no == 1:
                k.dma("sp", lsp_U2[hm], U2[:].rearrange("p n v -> p (n v)"), R=b_U2g, W=[b_lsp[hm]])
            F, Bw = slice(0, 64), slice(64, 128)
            TSEQ, b_TSEQ = sb("TSEQ", [128, 17])
            ESEQ, b_ESEQ = sb("ESEQ", [128, 17])
            k.op("dve", nc.vector.memset, TSEQ[:, 0:1], 0.0, W=[b_TSEQ])
            k.op("dve", nc.vector.tensor_copy, TSEQ[F, 1:17], TOT[F, 0:NLT], R=[b_TOT], W=[b_TSEQ])
            k.op("dve", nc.vector.tensor_copy, TSEQ[Bw, 1:17], TOT[Bw, NLT - 1::-1], R=[b_TOT], W=[b_TSEQ])
            k.op("act", nc.scalar.activation, ESEQ[:], TSEQ[:], AF.Exp, R=[b_TSEQ], W=[b_ESEQ])
            k.op("dve", nc.vector.memset, ESEQ[:, 0:1], 0.0, W=[b_ESEQ])
            DS, b_DS = sb("DS", [128, 128, 17])
            US, b_US = sb("US", [128, 128, 17])
            SS, b_SS = sb("SS", [128, 128, 17])
            k.op("dve", nc.vector.tensor_copy, DS[:], ESEQ[:].unsqueeze(1).broadcast_to([128, 128, 17]),
                 R=[b_ESEQ], W=[b_DS])
            k.op("dve", nc.vector.tensor_copy, US[F, :, 1:17], U2[F, 0:NLT, :].rearrange("p n v -> p v n"),
                 R=b_U2g, W=[b_US])
            k.op("dve", nc.vector.tensor_copy, US[Bw, :, 1:17], U2[Bw, NLT - 1::-1, :].rearrange("p n v -> p v n"),
                 R=b_U2g, W=[b_US])
            St, b_St = sb("St", [128, 128])

            def run_scan():
                k.op("dve", nc.vector.tensor_tensor_scan, SS[:].rearrange("p v t -> p (v t)"),
                     DS[:].rearrange("p v t -> p (v t)"), US[:].rearrange("p v t -> p (v t)"), 0.0, ALU.mult, ALU.add,
                     R=[b_DS, b_US], W=[b_SS])

            if pass_no == 1:
                k.op("dve", nc.vector.memset, US[:, :, 0], 0.0, W=[b_US])
                run_scan()
                k.op("dve", nc.vector.tensor_copy, St[:], SS[:, :, 16], R=[b_SS], W=[b_St])
                k.dma("sp", xf_in[XF_STATE + hm].rearrange("a (b c) -> (a b) c", c=128), St[:],
                      R=[b_St], W=[b_xf_in[XF_STATE + hm]])
                ls, b_ls = sb("ls", [128, 1])
                k.op("dve", nc.vector.reduce_sum, ls[:], TOT[:, 0:NLT], AX.X, R=[b_TOT], W=[b_ls])
                k.op("act", nc.scalar.activation, LAMT[:, hm:hm + 1], ls[:], AF.Exp, R=[b_ls], W=[b_LAMT])
                return
            FR, b_FR = sb("FR", [128, 4, 128])
            LR, b_LR = sb("LR", [128, 4, 8])
            for r in range(4):
                k.dma("sp", FR[:, r, :], xf_out[XF_STATE + hm, r * 16:(r + 1) * 16, :].rearrange("a (b c) -> (a b) c", c=128),
                      R=[b_xf_out[XF_STATE + hm]], W=[b_FR])
                k.dma("sp", LR[:, r, :], xf_out[XF_LAM, r * 16, :].rearrange("(p e) -> p e", e=8),
                      R=[b_xf_out[XF_LAM]], W=[b_LR])
            Rt, b_Rt = sb("Rt", [128, 128])
            Sin, b_Sin = sb("Sin", [128, 128])
            k.op("dve", nc.vector.memset, Sin[:], 0.0, W=[b_Sin])
            k.op("dve", nc.vector.scalar_tensor_tensor, Rt[F, :], U2[F, 16, :], EEND[F, 17:18], U2[F, 17, :],
                 ALU.mult, ALU.add, R=b_U2g + [b_EEND], W=[b_Rt])
            k.op("dve", nc.vector.scalar_tensor_tensor, Rt[Bw, :], U2[Bw, 17, :], EEND[Bw, 16:17], U2[Bw, 16, :],
                 ALU.mult, ALU.add, R=b_U2g + [b_EEND], W=[b_Rt])
            for sl, order in ((F, range(4)), (Bw, range(3, -1, -1))):
                for r in order:
                    k.op("dve", nc.vector.scalar_tensor_tensor, Sin[sl, :], Rt[sl, :], onehot[sl, r:r + 1], Sin[sl, :],
                         ALU.mult, ALU.add, R=[b_Rt, b_onehot], W=[b_Sin])
                    k.op("dve", nc.vector.scalar_tensor_tensor, Rt[sl, :], Rt[sl, :], LR[sl, r, hm:hm + 1], FR[sl, r, :],
                         ALU.mult, ALU.add, R=[b_LR, b_FR], W=[b_Rt])
            k.op("dve", nc.vector.tensor_copy, US[:, :, 0], Sin[:], R=[b_Sin], W=[b_US])
            run_scan()
            S2, b_S2 = sb("S2", [128, NT, 128], BF16)
            k.op("dve", nc.vector.tensor_copy, S2[F, 0:NLT, :], SS[F, :, 0:NLT].rearrange("p v t -> p t v"),
                 R=[b_SS], W=[b_S2])
            k.op("dve", nc.vector.tensor_copy, S2[Bw, 0:NLT, :], SS[Bw, :, NLT - 1::-1].rearrange("p v t -> p t v"),
                 R=[b_SS], W=[b_S2])
            k.op("dve", nc.vector.memset, S2[F, 16, :], 0.0, W=[b_S2])
            k.op("dve", nc.vector.memset, S2[Bw, 17, :], 0.0, W=[b_S2])
            k.op("dve", nc.vector.tensor_copy, S2[F, 17, :], U2[F, 16, :], R=b_U2g, W=[b_S2])
            k.op("dve", nc.vector.tensor_copy, S2[Bw, 16, :], U2[Bw, 17, :], R=b_U2g, W=[b_S2])
            gcol = (P_RG if mix == 0 else P_GR) + h * 128
            wg_, b_wg = PF["wg"]
            nwb, b_nwb = sb("nwb", [128, 128])
            nsrc = ret_norm_w if mix == 0 else gla_norm_w
            k.dma("sp", nwb[:], nsrc[l:l + 1, h * 128:(h + 1) * 128].partition_broadcast(128), W=[b_nwb])
            G, b_G = sb("G", [128, NT, 128], BF16)
            gs, b_gs = sb("gs", [128, 8, 128])
            nchunks = NLT if last else NT
            groups = [(g0, min(8, nchunks - g0)) for g0 in range(0, nchunks, 8)]
            ubc = None
            for gi, (g0, ng) in enumerate(groups):
                bks = [6, 7] if gi % 2 == 0 else [4, 5]
                for c_ in range(ng):
                    n_ = g0 + c_
                    bi = min(n_ // 4, 4)
                    if ubc is None or ubc[0] != bi:
                        ubc = (bi,) + tuple(us.load(bi))
                    _, ub, b_ub, t0, n = ubc
                    tt = n_ - t0 // 128
                    bk = bks[c_ // 4]
                    for kc in range(8):
                        k.op("pe", nc.tensor.matmul, PSA[:, bk, (c_ % 4) * 128:(c_ % 4 + 1) * 128],
                             ub[:, kc, tt * 128:(tt + 1) * 128], wg_[:, kc, :],
                             start=(kc == 0), stop=(kc == 7), R=[b_ub, b_wg], W=[PS[bk][1]], sig=(kc == 7))
                nbk = (ng + 3) // 4
                pgv = PSA[:, bks[0]:bks[0] + nbk, :].rearrange("p b (c v) -> p (b c) v", v=128)[:, 0:ng, :]
                k.op("act", nc.scalar.activation, gs[:, 0:ng, :], pgv, AF.Silu, R=[PS[b_][1] for b_ in bks[:nbk]], W=[b_gs])
                k.op("dve", nc.vector.tensor_tensor, G[:, g0:g0 + ng, :], gs[:, 0:ng, :],
                     nwb[:].unsqueeze(1).broadcast_to([128, ng, 128]), ALU.mult, R=[b_gs, b_nwb], W=[b_G])
            t1, b_t1 = sb("g_t1", [128, 8, 128])
            t2, b_t2 = sb("g_t2", [128, 8, 128])
            PT8, b_PT8 = sb("PT8", [128, 8, 128], BF16)
            osb, b_osb = sb("osb", [128, 8, 128])
            jk, b_jk = sb("ljk", [128, 128])
            stt, b_stt = sb("stt", [128, 6, 8])
            yn8, b_yn8 = sb("yn8", [128, 8, 128])
            yb8, b_yb8 = sb("yb8", [128, 8, 128], BF16)
            yTt, b_yT = sb("yTt", [128, T], BF16)
            b_sttc = [Buf() for _ in range(8)]
            b_osbc = [Buf() for _ in range(8)]
            b_ync = [Buf() for _ in range(8)]
            for (g0, ng) in groups:
                nbk = (ng + 3) // 4
                for c_ in range(ng):
                    c0 = (g0 + c_) * 128
                    k.op("pe", nc.tensor.matmul, PSA[:, c_ // 4, (c_ % 4) * 128:(c_ % 4 + 1) * 128],
                         kt2[0:64, c0:c0 + 128], qt2[0:64, c0:c0 + 128],
                         start=True, stop=True, R=[b_kt2, b_qt2], W=[PS[c_ // 4][1]], sig=(c_ % 4 == 3 or c_ == ng - 1))
                for c_ in range(ng):
                    c0 = (g0 + c_) * 128
                    k.op("pe", nc.tensor.matmul, PSA[:, 2 + c_ // 4, (c_ % 4) * 128:(c_ % 4 + 1) * 128],
                         kt2[64:128, c0:c0 + 128], qt2[64:128, c0:c0 + 128],
                         start=True, stop=True, R=[b_kt2, b_qt2], W=[PS[2 + c_ // 4][1]], sig=(c_ % 4 == 3 or c_ == ng - 1))
                sfv = PSA[:, 0:nbk, :].rearrange("p b (c v) -> p (b c) v", v=128)[:, 0:ng, :]
                sbv = PSA[:, 2:2 + nbk, :].rearrange("p b (c v) -> p (b c) v", v=128)[:, 0:ng, :]
                k.op("dve", nc.vector.tensor_tensor, t1[:, 0:ng, :], sfv, maskF[:].unsqueeze(1).broadcast_to([128, ng, 128]),
                     ALU.mult, R=[PS[b_][1] for b_ in range(nbk)] + [b_maskF], W=[b_t1])
                k.op("dve", nc.vector.tensor_tensor, t2[:, 0:ng, :], sbv, maskB[:].unsqueeze(1).broadcast_to([128, ng, 128]),
                     ALU.mult, R=[PS[2 + b_][1] for b_ in range(nbk)] + [b_maskB], W=[b_t2])
                k.op("dve", nc.vector.tensor_tensor, PT8[:, 0:ng, :], t1[:, 0:ng, :], t2[:, 0:ng, :], ALU.add,
                     R=[b_t1, b_t2], W=[b_PT8])
                for c_ in range(ng):
                    n_ = g0 + c_
                    c0 = n_ * 128
                    bk = 4 + c_ // 4
                    oap = PSA[:, bk, (c_ % 4) * 128:(c_ % 4 + 1) * 128]
                    k.op("pe", nc.tensor.matmul, oap, PT8[:, c_, :], vth[:, n_, :],
                         start=True, stop=False, R=[b_PT8, b_vthc[n_]], W=[PS[bk][1]], sig=False)
                    k.op("pe", nc.tensor.matmul, oap, qt2[:, c0:c0 + 128], S2[:, n_, :],
                         start=False, stop=True, R=[b_qt2, b_S2], W=[PS[bk][1]], sig=(c_ % 4 == 3 or c_ == ng - 1))
                for c_ in range(ng):
                    bk = 4 + c_ // 4
                    oap = PSA[:, bk, (c_ % 4) * 128:(c_ % 4 + 1) * 128]
                    k.op("act", nc.scalar.activation, osb[:, c_, :], oap, AF.Identity, accum_out=stt[:, 0, c_:c_ + 1],
                         R=[PS[bk][1]], W=[b_osbc[c_], b_sttc[c_]])
                    k.op("act", nc.scalar.activation, jk[:], oap, AF.Square, accum_out=stt[:, 1, c_:c_ + 1],
                         R=[PS[bk][1], b_sttc[c_]], W=[])
                k.op("dve", nc.vector.tensor_scalar, stt[:, 0:2, 0:ng], stt[:, 0:2, 0:ng], 1.0 / 128, None, ALU.mult,
                     W=[b_stt] + b_sttc[0:ng])
                if mix == 0:
                    k.op("dve", nc.vector.tensor_tensor, stt[:, 3, 0:ng], stt[:, 0, 0:ng], stt[:, 0, 0:ng], ALU.mult, R=b_sttc[0:ng], W=[b_stt])
                    k.op("dve", nc.vector.tensor_tensor, stt[:, 1, 0:ng], stt[:, 1, 0:ng], stt[:, 3, 0:ng], ALU.subtract, R=b_sttc[0:ng], W=[b_stt])
                k.op("dve", nc.vector.tensor_scalar, stt[:, 3, 0:ng], stt[:, 1, 0:ng], EPS, None, ALU.add, R=b_sttc[0:ng], W=[b_stt])
                k.op("act", nc.scalar.activation, stt[:, 4, 0:ng], stt[:, 3, 0:ng], AF.Sqrt, R=b_sttc[0:ng], W=[b_stt])
                k.op("dve", nc.vector.reciprocal, stt[:, 2, 0:ng], stt[:, 4, 0:ng], R=b_sttc[0:ng], W=[b_stt])
                for c_ in range(ng):
                    if mix == 0:
                        k.op("dve", nc.vector.tensor_scalar, yn8[:, c_, :], osb[:, c_, :], stt[:, 0, c_:c_ + 1], stt[:, 2, c_:c_ + 1],
                             ALU.subtract, ALU.mult, R=[b_osbc[c_], b_stt, b_sttc[c_]], W=[b_ync[c_]])
                    else:
                        k.op("dve", nc.vector.tensor_scalar, yn8[:, c_, :], osb[:, c_, :], stt[:, 2, c_:c_ + 1], None, ALU.mult,
                             R=[b_osbc[c_], b_stt, b_sttc[c_]], W=[b_ync[c_]])
                k.op("dve", nc.vector.tensor_tensor, yb8[:, 0:ng, :], yn8[:, 0:ng, :], G[:, g0:g0 + ng, :], ALU.mult,
                     R=b_ync[0:ng] + [b_G], W=[b_yb8])
                p6b = PSA[:, 6, :].bitcast(BF16)
                for c_ in range(ng):
                    k.op("pe", nc.tensor.transpose, p6b[:, c_ * 128:(c_ + 1) * 128], yb8[:, c_, :], identb[:],
                         R=[b_yb8, b_identb], W=[PS[6][1]], sig=(c_ == ng - 1))
                copy_ps("act", yTt[:, g0 * 128:(g0 + ng) * 128], p6b[:, 0:ng * 128], R=[PS[6][1]], W=[b_yT])
            ntok = LAT if last else T
            k.dma("sp", yTd[1 + mix, h, :, 0:ntok], yTt[:, 0:ntok], R=[b_yT], W=[b_yTd[1 + mix]])

    def phase_linear(l, pass_no, last):
        order = [(mix, h) for mix in range(2) for h in range(4)]
        if pass_no == 1:
            for (mix, h) in order:
                linear_mixer(l, mix, h, pass_no, last)
                k.fence()
        else:
            with ExitStack() as pes:
                PFs = []
                for si in range(2):
                    PF = {}
                    for nm, shp, dt in (("w", [128, 8, 512], BF16), ("wg", [128, 8, 128], BF16), ("kt2", [128, T], BF16),
                                        ("vth", [128, NT, 128], BF16), ("ebT", [128, T], BF16), ("U2", [128, NT, 128], F32),
                                        ("TE", [128, 2 * NT], F32)):
                        PF[nm] = (pes.enter_context(nc.sbuf_tensor(U(f"pf_{nm}{si}"), shp, dt)), Buf(f"pf_{nm}{si}"))
                    PFs.append(PF)
                lm_prefetch(l, order[0][0], order[0][1], PFs[0])
                for i_, (mix, h) in enumerate(order):
                    cb = None
                    if i_ + 1 < len(order):
                        cb = (lambda j=i_ + 1: lm_prefetch(l, order[j][0], order[j][1], PFs[j % 2]))
                    linear_mixer(l, mix, h, pass_no, last, PF=PFs[i_ % 2], after_stage1=cb)
                    k.fence()
            k.fence()
        if pass_no == 1:
            k.dma("sp", xf_in[XF_LAM, 0, :].rearrange("(p e) -> p e", e=8), LAMT[:], R=[b_LAMT], W=[b_xf_in[XF_LAM]])
            for j in range(NXF):
                k.allgather(xf_in[j], xf_out[j], R=[b_xf_in[j]], W=[b_xf_out[j]])
            k.fence()


    G8, b_G8 = salloc("G8", [128, NT, 8])

    def phase_attn(l, last):
        SCL = 96.0 ** -0.5
        cqn, b_cqn = LS["cqn"]; ckvn, b_ckvn = LS["ckvn"]; krr, b_krr = LS["krr"]
        with ExitStack() as es:
            def sb(name, shape, dt=F32):
                return es.enter_context(nc.sbuf_tensor(U(name), list(shape), dt)), Buf(name)
            ckvA, b_ckvA = sb("ckvA", [128, NKEY], BF16)
            k.op("pool", nc.gpsimd.tensor_copy, ckvA[:, 0:CTX], ckvn[:, LAT:T], R=[b_ckvn], W=[b_ckvA])
            for j in range(8):
                k.dma("sp", ckvA[16 * j:16 * j + 16, CTX:NKEY].rearrange("p (r t) -> p r t", r=4),
                      xb_out[XB_CKV + j].rearrange("(r p) t -> p r t", p=16),
                      R=[b_xb_out[XB_CKV + j]], W=[b_ckvA])
            KTs = [sb(f"KT{i}", [96, NKEY], BF16) for i in range(2)]
            Vs = [sb(f"V{i}", [128, NKT, 128], BF16) for i in range(2)]
            for (KT, bKT), (V, bV) in zip(KTs, Vs):
                k.dma("sp", KT[64:96, 0:CTX], krr[:, LAT:T], R=[b_krr], W=[bKT])
                for j in range(2):
                    k.dma("sp", KT[64 + 16 * j:64 + 16 * j + 16, CTX:NKEY].rearrange("p (r t) -> p r t", r=4),
                          xb_out[XB_KR + j].rearrange("(r p) t -> p r t", p=16),
                          R=[b_xb_out[XB_KR + j]], W=[bKT])
                k.op("pool", nc.gpsimd.memset, V[:, :, 64:128], 1.0, W=[bV])
            ropq_r = Ring([sb(f"ropq{i}", [96, 2, 512]) for i in range(2)])
            yat_r = Ring([sb(f"yat{i}", [64, 512], BF16) for i in range(3)])
            QTs = Ring([sb(f"QT{i}", [96, T], BF16) for i in range(2)])
            wq_r = Ring([sb(f"wq{i}", [128, 2, 96], BF16) for i in range(2)])
            wqs_r = Ring([sb(f"wqs{i}", [128, 2, 96], BF16) for i in range(2)])
            wkv_r = Ring([sb(f"wkv{i}", [128, 128], BF16) for i in range(2)])
            t1r = Ring([sb(f"at1{i}", [96, 512]) for i in range(2)])
            t2r = Ring([sb(f"at2{i}", [96, 512]) for i in range(2)])
            Pr = Ring([sb(f"P{i}", [128, 512], BF16) for i in range(4)])
            linv_r = Ring([sb(f"linv{i}", [128, 512]) for i in range(2)])
            sring = Ring([PS[0], PS[1], PS[2], PS[3]])
            oring = Ring([PS[4], PS[5]])
            qblocks = [(q0, 512, list(range(NKT))) for q0 in range(0, LAT, 512)]
            if not last:
                qblocks.append((LAT, CTX, [0, 1]))
            def build_head(h):
                wq, b_wq = wq_r.next(); wqs, b_wqs = wqs_r.next(); wkv, b_wkv = wkv_r.next()
                k.dma("pool", wq[:], w_uq[l].rearrange("(kc p) n -> p kc n", p=128)[:, :, h * 96:(h + 1) * 96], W=[b_wq])
                k.dma("pool", wqs[:], w_uq_sw[l].rearrange("(kc p) n -> p kc n", p=128)[:, :, h * 96:(h + 1) * 96], W=[b_wqs])
                k.dma("pool", wkv[:], w_ukv[l][:, h * 128:(h + 1) * 128], W=[b_wkv])
                QT, bQT = QTs.next()
                KT, bKT = KTs[h % 2]
                V, bV = Vs[h % 2]
                built[h] = (QT, bQT, KT, bKT, V, bV)
                yield
                blks = BLKS[:4] if last else BLKS
                for bi, (t0, n) in enumerate(blks):
                    (pa, bpa), (pb, bpb) = PS[6], PS[7]
                    for (pp, bpp, ww, bww) in ((pa, bpa, wq, b_wq), (pb, bpb, wqs, b_wqs)):
                        for kc in range(2):
                            k.op("pe", nc.tensor.matmul, pp[0:96, 0:n], ww[:, kc, :], cqn[:, kc, t0:t0 + n],
                                 start=(kc == 0), stop=(kc == 1), R=[bww, b_cqn], W=[bpp], sig=(kc == 1))
                    k.op("dve", nc.vector.tensor_scalar, QT[0:64, t0:t0 + n], pa[0:64, 0:n], SCL, None, ALU.mult,
                         R=[bpa], W=[bQT])
                    ta, bta = t1r.next(); tb, btb = t2r.next()
                    ropq, b_ropq = ropq_r.next()
                    k.dma("sp", ropq[64:96, 0, 0:n], c_ropeAq[0, :, t0:t0 + n], W=[b_ropq])
                    k.dma("sp", ropq[64:96, 1, 0:n], c_ropeAq[1, :, t0:t0 + n], W=[b_ropq])
                    k.op("dve", nc.vector.tensor_tensor, ta[64:96, 0:n], pa[64:96, 0:n], ropq[64:96, 0, 0:n],
                         ALU.mult, R=[bpa, b_ropq], W=[bta])
                    k.op("dve", nc.vector.tensor_tensor, tb[64:96, 0:n], pb[64:96, 0:n], ropq[64:96, 1, 0:n],
                         ALU.mult, R=[bpb, b_ropq], W=[btb])
                    k.op("dve", nc.vector.tensor_tensor, QT[64:96, t0:t0 + n], ta[64:96, 0:n], tb[64:96, 0:n],
                         ALU.add, R=[bta, btb], W=[bQT])
                    yield
                for kb in range((NKEY + 511) // 512):
                    c0 = kb * 512
                    n = min(512, NKEY - c0)
                    pk, bpk = PS[6 + kb % 2]
                    k.op("pe", nc.tensor.matmul, pk[0:64, 0:n], wkv[:, 0:64], ckvA[:, c0:c0 + n], start=True, stop=True,
                         R=[b_wkv, b_ckvA], W=[bpk])
                    copy_ps("dve", KT[0:64, c0:c0 + n], pk[0:64, 0:n], R=[bpk], W=[bKT])
                    yield
                for g0 in range(0, NKT, 8):
                    gn = min(8, NKT - g0)
                    pv, bpv = PS[7]
                    for i_ in range(gn):
                        kt = g0 + i_
                        k.op("pe", nc.tensor.matmul, pv[:, i_ * 64:(i_ + 1) * 64], ckvA[:, kt * 128:(kt + 1) * 128],
                             wkv[:, 64:128], start=True, stop=True, R=[b_ckvA, b_wkv], W=[bpv], sig=(i_ == gn - 1))
                    copy_ps("dve", V[:, g0:g0 + gn, 0:64],
                            pv[:, 0:gn * 64].rearrange("p (a b) -> p a b", b=64), R=[bpv], W=[bV])
                    yield

            built = {}
            for _ in build_head(0):
                pass
            for h in range(8):
                QT, bQT, KT, bKT, V, bV = built[h]
                gen = build_head(h + 1) if h + 1 < 8 else iter(())
                step_no = 0
                for (q0, nq, ktiles) in qblocks:
                    po, bpo = oring.next()

                    def issue_s(kt):
                        ps_, bps_ = sring.next()
                        k.op("pe", nc.tensor.matmul, ps_[:, 0:nq], KT[0:96, kt * 128:(kt + 1) * 128], QT[0:96, q0:q0 + nq],
                             start=True, stop=True, R=[bKT, bQT], W=[bps_])
                        return ps_, bps_
                    LA = 3
                    pend = [issue_s(kt_) for kt_ in ktiles[:LA]]
                    for i_, kt in enumerate(ktiles):
                        if i_ + LA < len(ktiles):
                            pend.append(issue_s(ktiles[i_ + LA]))
                        cur = pend.pop(0)
                        P, bP = Pr.next()
                        k.op("act", nc.scalar.activation, P[:, 0:nq], cur[0][:, 0:nq], AF.Exp, R=[cur[1]], W=[bP])
                        lastk = (i_ == len(ktiles) - 1)
                        k.op("pe", nc.tensor.matmul, po[:, 0:nq], V[:, kt, :], P[:, 0:nq], start=(i_ == 0), stop=lastk,
                             R=[bV, bP], W=[bpo], sig=True)
                        step_no += 1
                        if step_no % 6 == 0:
                            next(gen, None)
                    linv, b_linv = linv_r.next()
                    k.op("dve", nc.vector.reciprocal, linv[64:128, 0:nq], po[64:128, 0:nq], R=[bpo], W=[b_linv])
                    r0 = (h % 2) * 64
                    yat, b_yat = yat_r.next()
                    k.op("dve", nc.vector.tensor_tensor, yat[:, 0:nq], po[0:64, 0:nq],
                         linv[64:128, 0:nq], ALU.mult, R=[bpo, b_linv], W=[b_yat])
                    k.dma("sp", yTd[0, h // 2, r0:r0 + 64, q0:q0 + nq], yat[:, 0:nq], R=[b_yat], W=[b_yTd[0]])
                for _ in gen:
                    pass
        k.fence()

    def phase_merge(l, last):
        blks = list(enumerate(BLKS[:4] if last else BLKS))
        with ExitStack() as es:
            def sb(name, shape, dt=F32):
                return es.enter_context(nc.sbuf_tensor(U(name), list(shape), dt)), Buf(name)
            zT, b_zT = sb("zT", [128, 8, T], BF16)
            wo, b_wo = load_w(es, "wo", wview(w_out[l]), [128, 8, D])
            us = UStream(es)
            yb_r = Ring([sb(f"myb{i}", [128, 3, 4, 512], BF16) for i in range(2)])
            g1, b_g1 = sb("g1", [128, 2, D])
            for r in range(2):
                k.dma("sp", g1[:, r, :], modD[r:r + 1, 2 * D:3 * D].partition_broadcast(128), R=[b_modD], W=[b_g1])
            wg_r = Ring([sb(f"mwg{i}", [128, 8, 3, 128], BF16) for i in range(2)])
            wb_r = Ring([sb(f"mwb{i}", [128, 3, 4, 128], BF16) for i in range(2)])
            gj_r = Ring([sb(f"gj{i}", [128, 512]) for i in range(2)])
            za_r = Ring([sb(f"za{i}", [128, 512]) for i in range(2)])
            zt_r = Ring([sb(f"zt{i}", [128, 512]) for i in range(2)])
            pgr = Ring([PS[0], PS[1], PS[2]])
            pzr = Ring([PS[3], PS[4], PS[5]])
            for c in range(8):
                wg3, b_wg3 = wg_r.next(); wb3, b_wb3 = wb_r.next()
                for j in range(3):
                    col = P_BG + j * 1024 + c * 128
                    k.dma("pool", wg3[:, :, j, :], wview(wp[l])[:, :, col:col + 128], W=[b_wg3])
                    k.dma("pool", wb3[:, j, :, :], w_branch[l, j].rearrange("(k4 p) n -> p k4 n", p=128)[:, :, c * 128:(c + 1) * 128],
                          W=[b_wb3])
                for bi, (t0, n) in blks:
                    ub, b_ub, _, _ = us.load(bi)
                    yb3, b_yb3 = yb_r.next()
                    for j in range(3):
                        k.dma("sp", yb3[:, j, :, 0:n], yTd[j, :, :, t0:t0 + n].rearrange("k p t -> p k t"),
                              R=[b_yTd[j]], W=[b_yb3])
                    za, bza = za_r.next()
                    for j in range(3):
                        pg, bpg = pgr.next()
                        for kc in range(8):
                            k.op("pe", nc.tensor.matmul, pg[:, 0:n], wg3[:, kc, j, :], ub[:, kc, 0:n],
                                 start=(kc == 0), stop=(kc == 7), R=[b_wg3, b_ub], W=[bpg], sig=(kc == 7))
                        gj, bgj = gj_r.next()
                        k.op("act", nc.scalar.activation, gj[:, 0:n], pg[:, 0:n], AF.Sigmoid, R=[bpg], W=[bgj])
                        pz, bpz = pzr.next()
                        for k4 in range(4):
                            k.op("pe", nc.tensor.matmul, pz[:, 0:n], wb3[:, j, k4, :], yb3[:, j, k4, 0:n],
                                 start=(k4 == 0), stop=(k4 == 3), R=[b_wb3, b_yb3], W=[bpz], sig=(k4 == 3))
                        if j == 0:
                            k.op("dve", nc.vector.tensor_tensor, za[:, 0:n], pz[:, 0:n], gj[:, 0:n], ALU.mult,
                                 R=[bpz, bgj], W=[bza])
                        else:
                            zt, bzt = zt_r.next()
                            k.op("dve", nc.vector.tensor_tensor, zt[:, 0:n], pz[:, 0:n], gj[:, 0:n], ALU.mult,
                                 R=[bpz, bgj], W=[bzt])
                            if j == 1:
                                k.op("dve", nc.vector.tensor_tensor, za[:, 0:n], za[:, 0:n], zt[:, 0:n], ALU.add,
                                     R=[bzt], W=[bza])
                            else:
                                k.op("dve", nc.vector.tensor_tensor, zT[:, c, t0:t0 + n], za[:, 0:n], zt[:, 0:n], ALU.add,
                                     R=[bza, bzt], W=[b_zT])
            xr = Ring([sb(f"mx{i}", [128, D]) for i in range(4)])
            tmr = Ring([sb(f"mt{i}", [128, 512]) for i in range(2)])
            pyr = Ring([PS[6], PS[7]])
            tiles = list(range(NLT)) if last else list(range(NT))
            xloads = {}

            def issue_x(i_):
                if i_ < len(tiles):
                    t_ = tiles[i_]
                    xt_, b_xt_ = xr.next()
                    k.dma("sp", xt_[:], (xs_in if l == 0 else xs)[t_ * 128:(t_ + 1) * 128, :], R=[b_xs[t_]], W=[b_xt_])
                    xloads[i_] = (xt_, b_xt_)
            issue_x(0); issue_x(1)
            for i_, t in enumerate(tiles):
                r = 0 if t < NLT else 1
                issue_x(i_ + 2)
                xt, b_xt = xloads.pop(i_)
                for half in range(2):
                    py, bpy = pyr.next()
                    for kc in range(8):
                        k.op("pe", nc.tensor.matmul, py[:, :], zT[:, kc, t * 128:(t + 1) * 128],
                             wo[:, kc, half * 512:(half + 1) * 512], start=(kc == 0), stop=(kc == 7),
                             R=[b_zT, b_wo], W=[bpy], sig=(kc == 7))
                    tm, btm = tmr.next()
                    k.op("dve", nc.vector.tensor_tensor, tm[:], py[:, :], g1[:, r, half * 512:(half + 1) * 512], ALU.mult,
                         R=[bpy, b_g1], W=[btm])
                    k.op("dve", nc.vector.tensor_tensor, xt[:, half * 512:(half + 1) * 512], xt[:, half * 512:(half + 1) * 512],
                         tm[:], ALU.add, R=[btm], W=[b_xt])
                k.dma("sp", xs[t * 128:(t + 1) * 128, :], xt[:], R=[b_xt], W=[b_xs[t]])
        k.fence()

    def phase_router(i, tiles):
        with ExitStack() as es:
            def sb(name, shape, dt=F32):
                return es.enter_context(nc.sbuf_tensor(U(name), list(shape), dt)), Buf(name)
            rw, b_rw = load_w(es, "rw", wview(moe_router[i]), [128, 8, NEXP])
            us = UStream(es)
            lg_r = Ring([sb(f"rl{i_}", [128, 8]) for i_ in range(2)])
            m8_r = Ring([sb(f"rm{i_}", [128, 8]) for i_ in range(2)])
            ex_r = Ring([sb(f"re{i_}", [128, 8]) for i_ in range(2)])
            mk_r = Ring([sb(f"rk{i_}", [128, 8]) for i_ in range(2)])
            sc_r = Ring([sb(f"rs{i_}", [128, 4]) for i_ in range(2)])
            ubc = None
            for t in tiles:
                bi = min(t // 4, 4)
                if ubc is None or ubc[0] != bi:
                    ubc = (bi,) + tuple(us.load(bi))
                _, ub, b_ub, t0, n = ubc
                tt = t - t0 // 128
                pl, bpl = PS[t % 2]
                for kc in range(8):
                    k.op("pe", nc.tensor.matmul, pl[:, 0:NEXP], ub[:, kc, tt * 128:(tt + 1) * 128], rw[:, kc, :],
                         start=(kc == 0), stop=(kc == 7), R=[b_ub, b_rw], W=[bpl], sig=(kc == 7))
                lg, blg = lg_r.next(); m8, bm8 = m8_r.next(); ex, bex = ex_r.next(); mk, bmk = mk_r.next()
                sc_, bsc = sc_r.next()
                k.op("dve", nc.vector.tensor_copy, lg[:], pl[:, 0:NEXP], R=[bpl], W=[blg])
                k.op("dve", nc.vector.max, m8[:], lg[:], R=[blg], W=[bm8])
                k.op("dve", nc.vector.tensor_scalar, mk[:], lg[:], m8[:, 1:2], None, ALU.is_ge, R=[blg, bm8], W=[bmk])
                k.op("dve", nc.vector.tensor_scalar, sc_[:, 0:1], m8[:, 0:1], -1.0, None, ALU.mult, R=[bm8], W=[bsc])
                k.op("act", nc.scalar.activation, ex[:], lg[:], AF.Exp, bias=sc_[:, 0:1], R=[blg, bsc], W=[bex])
                k.op("dve", nc.vector.tensor_tensor, ex[:], ex[:], mk[:], ALU.mult, R=[bmk], W=[bex])
                k.op("dve", nc.vector.reduce_sum, sc_[:, 1:2], ex[:], AX.X, R=[bex], W=[bsc])
                k.op("dve", nc.vector.reciprocal, sc_[:, 2:3], sc_[:, 1:2], W=[bsc])
                k.op("dve", nc.vector.tensor_scalar, G8[:, t, :], ex[:], sc_[:, 2:3], None, ALU.mult, R=[bex, bsc], W=[b_G8])
        k.fence()

    def phase_ffn(l, last):
        moe = (l % 2 == 1)
        i = l // 2
        nexp = NEXP if moe else 1
        groups = [(0, 1024), (1024, 1024)] if last else [(0, 1152), (1152, 1152)]
        for (g0, gn) in groups:
            with ExitStack() as es:
                def sb(name, shape, dt=F32):
                    return es.enter_context(nc.sbuf_tensor(U(name), list(shape), dt)), Buf(name)
                vTg, b_vTg = sb("vTg", [128, 8, gn], BF16)
                ublks = sorted(set(min(t_ // 4, 4) for t_ in range(g0 // 128, (g0 + gn) // 128)))
                k.dma("sp", vTg[:], uT[:, :, g0:g0 + gn].rearrange("kc p t -> p kc t"),
                      R=[b_uT[b_] for b_ in ublks], W=[b_vTg])
                hT, b_hT = sb("hT", [128, NFF, gn], BF16)
                acc, b_acc = sb("acc", [128, gn // 128, D])
                wd, b_wd = sb("wd", [128, NFF, D], BF16)
                wt_r = Ring([sb(f"wgu{i_}", [128, 8, 2, 256], BF16) for i_ in range(2)])
                sg_r = Ring([sb(f"sg{i_}", [128, 512]) for i_ in range(3)])
                pgr = Ring([PS[0], PS[1]])
                pur = Ring([PS[2], PS[3]])
                pyr = Ring([PS[4], PS[5], PS[6], PS[7]])
                bsz = 512 if gn % 512 == 0 else 384
                nblks = [(c0, min(bsz, gn - c0)) for c0 in range(0, gn, bsz)]
                def wsrc(e):
                    if moe:
                        return moe_wg[i, e], moe_wu[i, e], moe_wd[i, e]
                    return ffn_wg[i], ffn_wu[i], ffn_wd[i]
                tasks = [(e, fc2) for e in range(nexp) for fc2 in range(NFF // 2)]
                loaded = {}

                def issue_load(ti):
                    if ti >= len(tasks) or ti in loaded:
                        return
                    e_, fc2_ = tasks[ti]
                    Wg_, Wu_, _ = wsrc(e_)
                    wt_, b_wt_ = wt_r.next()
                    k.dma("pool", wt_[:, :, 0, :], wview(Wg_)[:, :, fc2_ * 256:(fc2_ + 1) * 256], W=[b_wt_])
                    k.dma("pool", wt_[:, :, 1, :], wview(Wu_)[:, :, fc2_ * 256:(fc2_ + 1) * 256], W=[b_wt_])
                    loaded[ti] = (wt_, b_wt_)
                issue_load(0)
                for e in range(nexp):
                    Wg, Wu, Wd = wsrc(e)
                    for fc2 in range(NFF // 2):
                        ti = e * (NFF // 2) + fc2
                        issue_load(ti)
                        issue_load(ti + 1)
                        if fc2 == 0:
                            k.dma("pool", wd[:], Wd.rearrange("(f p) n -> p f n", p=128), W=[b_wd])
                        wt, b_wt = loaded.pop(ti)
                        for sub in range(2):
                            fc = fc2 * 2 + sub
                            for (c0, n) in nblks:
                                pg, bpg = pgr.next(); pu, bpu = pur.next()
                                for kc in range(8):
                                    k.op("pe", nc.tensor.matmul, pg[:, 0:n], wt[:, kc, 0, sub * 128:(sub + 1) * 128],
                                         vTg[:, kc, c0:c0 + n], start=(kc == 0), stop=(kc == 7),
                                         R=[b_wt, b_vTg], W=[bpg], sig=(kc == 7))
                                for kc in range(8):
                                    k.op("pe", nc.tensor.matmul, pu[:, 0:n], wt[:, kc, 1, sub * 128:(sub + 1) * 128],
                                         vTg[:, kc, c0:c0 + n], start=(kc == 0), stop=(kc == 7),
                                         R=[b_wt, b_vTg], W=[bpu], sig=(kc == 7))
                                sg, bsg = sg_r.next()
                                k.op("act", nc.scalar.activation, sg[:, 0:n], pg[:, 0:n], AF.Silu, R=[bpg], W=[bsg])
                                k.op("dve", nc.vector.tensor_tensor, hT[:, fc, c0:c0 + n], sg[:, 0:n], pu[:, 0:n], ALU.mult,
                                     R=[bsg, bpu], W=[b_hT])
                    for tt in range(gn // 128):
                        t = g0 // 128 + tt
                        for half in range(2):
                            py, bpy = pyr.next()
                            for fc in range(NFF):
                                k.op("pe", nc.tensor.matmul, py[:, :], hT[:, fc, tt * 128:(tt + 1) * 128],
                                     wd[:, fc, half * 512:(half + 1) * 512], start=(fc == 0), stop=(fc == NFF - 1),
                                     R=[b_hT, b_wd], W=[bpy], sig=(fc == NFF - 1))
                            dst = acc[:, tt, half * 512:(half + 1) * 512]
                            if not moe:
                                copy_ps(evac_engine(), dst, py[:, :], R=[bpy], W=[b_acc])
                            elif e == 0:
                                k.op("dve", nc.vector.tensor_scalar, dst, py[:, :], G8[:, t, e:e + 1], None, ALU.mult,
                                     R=[bpy, b_G8], W=[b_acc])
                            else:
                                k.op("dve", nc.vector.scalar_tensor_tensor, dst, py[:, :], G8[:, t, e:e + 1], dst,
                                     ALU.mult, ALU.add, R=[bpy, b_G8], W=[b_acc])
                xr = Ring([sb(f"fx{i_}", [128, D]) for i_ in range(3)])
                jr = Ring([sb(f"fj{i_}", [128, D]) for i_ in range(2)])
                ssr = Ring([sb(f"fs{i_}", [128, 1]) for i_ in range(2)])
                g2, b_g2 = sb("g2", [128, 2, D])
                for rg_ in range(1 if last else 2):
                    k.dma("sp", g2[:, rg_, :], modD[rg_:rg_ + 1, 5 * D:6 * D].partition_broadcast(128), R=[b_modD], W=[b_g2])
                if last:
                    fnw, b_fnw = sb("fnw", [128, D])
                    k.dma("sp", fnw[:], final_norm_w.rearrange("(o d) -> o d", o=1).partition_broadcast(128), W=[b_fnw])
                xloads = {}

                def issue_x(tt_):
                    if tt_ < gn // 128:
                        t_ = g0 // 128 + tt_
                        xt_, b_xt_ = xr.next()
                        k.dma("sp", xt_[:], xs[t_ * 128:(t_ + 1) * 128, :], R=[b_xs[t_]], W=[b_xt_])
                        xloads[tt_] = (xt_, b_xt_)
                issue_x(0); issue_x(1)
                for tt in range(gn // 128):
                    t = g0 // 128 + tt
                    r = 0 if t < NLT else 1
                    issue_x(tt + 2)
                    xt, b_xt = xloads.pop(tt)
                    k.op("dve", nc.vector.tensor_tensor, acc[:, tt, :], acc[:, tt, :], g2[:, r, :], ALU.mult,
                         R=[b_g2], W=[b_acc])
                    k.op("dve", nc.vector.tensor_tensor, xt[:], xt[:], acc[:, tt, :], ALU.add, R=[b_acc], W=[b_xt])
                    if not last:
                        k.dma("sp", xs[t * 128:(t + 1) * 128, :], xt[:], R=[b_xt], W=[b_xs[t]])
                    else:
                        jk, b_jk = jr.next(); ss, b_ss = ssr.next()
                        k.op("act", nc.scalar.activation, jk[:], xt[:], AF.Square, accum_out=ss[:, 0:1],
                             R=[b_xt], W=[b_jk, b_ss])
                        rstd, b_rstd = rsqrt_col(es, f"fr{t}", ss[:, 0:1], b_ss, 1.0 / D)
                        k.op("dve", nc.vector.scalar_tensor_tensor, jk[:], xt[:], rstd[:, 0:1], fnw[:], ALU.mult, ALU.mult,
                             R=[b_xt, b_rstd, b_fnw], W=[b_jk])
                        k.dma("sp", out[t * 128:(t + 1) * 128, :], jk[:], R=[b_jk], W=[b_out])
            k.fence()

    stop = dbg.get("_stop") if isinstance(dbg, dict) else None

    def tap(name, src_ap, bufs):
        if name in dbg_out:
            k.dma("sp", dbg_out[name], src_ap, R=bufs, W=[b_out])

    for l in range(nlayers):
        last = (l == DEPTH - 1)
        alltiles = list(range(NT))
        phase_mod(l)
        phase_norm(l, A1, b_A1, 0, alltiles, xsrc=(xs_in if l == 0 else xs))
        phase_lg(l)
        with ExitStack() as les:
            for nm, shp in (("cqn", [128, 2, T]), ("ckvn", [128, T]), ("krr", [32, T]), ("gzT", [32, T])):
                LS[nm] = (les.enter_context(nc.sbuf_tensor(U(nm), shp, BF16)), Buf(nm))
            phase_q(l)
            phase_linear(l, 1, last)
            phase_linear(l, 2, last)
            phase_attn(l, last)
        k.fence()
        phase_merge(l, last)
        ftiles = list(range(NLT)) if last else alltiles
        phase_norm(l, A2, b_A2, 24, ftiles)
        if l % 2 == 1:
            phase_router(l // 2, ftiles)
        phase_ffn(l, last)
    if nlayers < DEPTH:
        for t in range(NLT):
            k.dma("sp", out[t * 128:(t + 1) * 128, :], xs[t * 128:(t + 1) * 128, :], R=[b_xs[t]], W=[b_out])
    k.wait_bufs("sp", [b_out])
    return nc, k


IN_SPLITS = (256, 128, 32, 256, 256, 512, 512, 256, 256, 512, 512, 32, 3072)
OFFS = np.concatenate([[0], np.cumsum(IN_SPLITS)]).astype(int)
(O_CQ, O_CKV, O_KR, O_RQ, O_RK, O_RV, O_RG, O_GQ, O_GK, O_GV, O_GR, O_GZ, O_BG) = OFFS[:13]


def _partner(n, half):
    idx = np.arange(n)
    return np.where(idx % (2 * half) < half, idx + half, idx - half)


def _pack_w_in(w_in_l):
    cols = []
    pa = _partner(32, 8)
    cols.append(np.arange(O_CQ, O_CQ + 256))
    cols.append(np.arange(O_CKV, O_CKV + 128))
    cols.append(np.arange(O_KR, O_KR + 32))
    cols.append(O_KR + pa)
    cols.append(np.arange(O_GZ, O_GZ + 32))
    pr = _partner(64, 32)
    for h in range(4):
        q = O_RQ + h * 64 + np.arange(64)
        qs = O_RQ + h * 64 + pr
        kk = O_RK + h * 64 + np.arange(64)
        ks = O_RK + h * 64 + pr
        cols += [q, q, qs, qs, kk, kk, ks, ks]
    for h in range(4):
        q = O_GQ + h * 64 + np.arange(64)
        kk = O_GK + h * 64 + np.arange(64)
        cols += [q, q, kk, kk]
    cols.append(np.arange(O_RV, O_RV + 512))
    cols.append(np.arange(O_GV, O_GV + 512))
    cols.append(np.arange(O_RG, O_RG + 512))
    cols.append(np.arange(O_GR, O_GR + 512))
    cols.append(np.arange(O_BG, O_BG + 3072))
    cols = np.concatenate(cols)
    assert cols.shape[0] == NPACK
    return np.ascontiguousarray(w_in_l[:, cols])


def _rope_tables(pos, half, signed_rows):
    inv = 10000.0 ** (-np.arange(half, dtype=np.float32) / half)
    ang = pos.astype(np.float32)[None, :] * inv[:, None]
    cos = np.cos(ang).astype(np.float32)
    sin = np.sin(ang).astype(np.float32)
    return np.concatenate([cos, cos], 0), np.concatenate([-sin, sin], 0)


def _consts(q):
    pos = q * LAT + np.arange(LAT)
    c, s = _rope_tables(pos, 32, True)
    ropeR = np.zeros((2, 128, T), np.float32)
    ropeR[0, :, :LAT] = np.concatenate([c, c], 0)
    ropeR[1, :, :LAT] = np.concatenate([s, s], 0)
    ropeR[0, :, LAT:] = 1.0
    cr, sr = _rope_tables(pos // 64, 8, True)
    cc, sc = _rope_tables(pos % 64, 8, True)
    ak = np.zeros((2, 32, T), np.float32)
    ak[0, :, :LAT] = np.concatenate([cr, cc], 0)
    ak[1, :, :LAT] = np.concatenate([sr, sc], 0)
    ak[0, :, LAT:] = 1.0
    aq = (ak * np.float32(96.0 ** -0.5)).astype(np.float32)
    rst = np.ones((128, T), np.float32)
    rst[:, ::128] = 0.0
    j = np.arange(128)[:, None]
    i = np.arange(128)[None, :]
    mask = np.stack([(i >= j), (j >= i)]).astype(np.float32)
    onehot = np.zeros((128, 4), np.float32)
    onehot[:, q] = 1.0
    return dict(c_ropeR=ropeR, c_ropeAq=aq, c_ropeAk=ak, c_rst=rst, c_mask=mask,
                c_ident=np.eye(128, dtype=np.float32), c_onehot=onehot)


def make_in_maps(inp):
    f = lambda a: np.ascontiguousarray(np.asarray(a, dtype=np.float32))
    x, c, ctx, c_ctx = f(inp["x"]), f(inp["c"]), f(inp["ctx"]), f(inp["c_ctx"])
    w_in = f(inp["w_in"])
    wp_ = np.stack([_pack_w_in(w_in[l]) for l in range(DEPTH)])
    w_uq = f(inp["mla_w_uq"])
    pa = _partner(32, 8)
    cols = np.arange(768)
    for h in range(8):
        cols[h * 96 + 64:h * 96 + 96] = h * 96 + 64 + pa
    w_uq_sw = np.ascontiguousarray(w_uq[:, :, cols])
    gw = f(inp["gla_w_gate"])
    gb = f(inp["gla_b_gate"])
    gwblk = np.zeros((DEPTH, 4, 32, 128), np.float32)
    gbias = np.zeros((DEPTH, 4, 128), np.float32)
    for h in range(4):
        gwblk[:, h, 0:16, 0:64] = gw[:, 0, :, h * 64:(h + 1) * 64]
        gwblk[:, h, 16:32, 64:128] = gw[:, 1, :, h * 64:(h + 1) * 64]
        gbias[:, h, 0:64] = gb[:, 0, h * 64:(h + 1) * 64]
        gbias[:, h, 64:128] = gb[:, 1, h * 64:(h + 1) * 64]
    shared = dict(
        mod_w=f(inp["mod_w"]), mod_b=f(inp["mod_b"]), norm1_w=f(inp["norm1_w"]), norm2_w=f(inp["norm2_w"]),
        wp=wp_, mla_q_norm=f(inp["mla_q_norm"]), w_uq=w_uq, w_uq_sw=w_uq_sw, mla_kv_norm=f(inp["mla_kv_norm"]),
        w_ukv=f(inp["mla_w_ukv"]), ret_decay_logit=f(inp["ret_decay_logit"]), ret_norm_w=f(inp["ret_norm_w"]),
        gwblk=gwblk, gbias=gbias, gla_norm_w=f(inp["gla_norm_w"]), w_branch=f(inp["w_branch"]), w_out=f(inp["w_out"]),
        ffn_w_gate=f(inp["ffn_w_gate"]), ffn_w_up=f(inp["ffn_w_up"]), ffn_w_down=f(inp["ffn_w_down"]),
        moe_router=f(inp["moe_router"]), moe_w_gate=f(inp["moe_w_gate"]), moe_w_up=f(inp["moe_w_up"]),
        moe_w_down=f(inp["moe_w_down"]), final_norm_w=f(inp["final_norm_w"]),
    )
    maps = []
    for core in range(NCORES):
        b, q = core // 4, core % 4
        m = dict(shared)
        m["xs_in"] = np.ascontiguousarray(np.concatenate([x[b, q * LAT:(q + 1) * LAT], ctx[b]], 0))
        m["cvecT"] = np.ascontiguousarray(np.stack([c[b], c_ctx], 1))
        m.update(_consts(q))
        maps.append(m)
    return maps


_PROG = {}


def kernel(**inputs):
    if "nc" not in _PROG:
        _PROG["nc"] = build_program()[0]
    nc = _PROG["nc"]
    maps = make_in_maps(inputs)
    res = run_bass_kernel_spmd(nc, maps, core_ids=list(range(NCORES)))
    outp = np.zeros((2, SEQ, D), np.float32)
    for core in range(NCORES):
        b, q = core // 4, core % 4
        outp[b, q * LAT:(q + 1) * LAT] = res.results[core]["out"]
    return outp
```

```python
import math
from contextlib import ExitStack
import numpy as np
import ml_dtypes
import concourse.bass as bass
import concourse.mybir as mybir
from concourse.bass_utils import run_bass_kernel_spmd

F32 = mybir.dt.float32
BF16 = mybir.dt.bfloat16
AF = mybir.ActivationFunctionType
ALU = mybir.AluOpType
AX = mybir.AxisListType

NCORES = 8
D = 1024
SEQ = 8192
CTX = 256
LAT = 2048
T = LAT + CTX
NT = T // 128
NLT = LAT // 128
DEPTH = 2
EPS = 1e-6
DFF = 2816
NFF = DFF // 128
NEXP = 8
NKEY = CTX + SEQ
NKT = NKEY // 128

PA = 0
PA_N = 480
P_RET = 480
P_GLA = P_RET + 4 * 512
P_RV = P_GLA + 4 * 256
P_GV = P_RV + 512
P_RG = P_GV + 512
P_GR = P_RG + 512
P_BG = P_GR + 512
NPACK = P_BG + 3072

XF_STATE = 0
XF_LAM = 8
NXF = 9
XB_CKV = 0
XB_KR = 8
NXB = 10

EPOCH = 30000


class Buf:
    __slots__ = ("name", "w", "r")

    def __init__(self, name=""):
        self.name = name
        self.w = None
        self.r = []


class Ring:
    def __init__(self, items):
        self.items = items
        self.i = 0

    def next(self):
        it = self.items[self.i % len(self.items)]
        self.i += 1
        return it


class K:
    def __init__(self, nc):
        self.nc = nc
        self.eng = {"pe": nc.tensor, "dve": nc.vector, "act": nc.scalar,
                    "pool": nc.gpsimd, "sp": nc.sync}
        self.sems = {}
        self.cnt = {}
        self.epoch = {e: 0 for e in self.eng}
        self.seen = {}
        for e in self.eng:
            self._new_epoch(e, first=True)
        self.dq = {}
        for q, n in (("sp", 24), ("pool", 16), ("act", 4)):
            keys = []
            for i in range(n):
                key = ("dma", q, i)
                self.sems[key] = nc.alloc_semaphore(f"d_{q}_{i}")
                self.cnt[key] = 0
                keys.append(key)
            self.dq[q] = [keys, 0]
        self.cc_key = ("cc", 0)
        self.sems[self.cc_key] = nc.alloc_semaphore("cc")
        self.cnt[self.cc_key] = 0
        self.n_ins = 0

    def _new_epoch(self, e, first=False):
        if not first:
            self.epoch[e] += 1
        key = (e, self.epoch[e])
        self.sems[key] = self.nc.alloc_semaphore(f"s_{e}_{self.epoch[e]}")
        self.cnt[key] = 0

    def _wait(self, e, toks, force_same=False):
        eng = self.eng[e]
        best = {}
        for t in toks:
            if t is None:
                continue
            key, val = t
            if key[0] == e and not force_same:
                if e in ("pe", "sp"):
                    continue
            if best.get(key, 0) < val:
                best[key] = val
        for key, val in best.items():
            if self.seen.get((e, key), 0) >= val:
                continue
            assert self.cnt[key] >= val, f"wait on unsignalled token {key} {val} > {self.cnt[key]}"
            eng.wait_ge(self.sems[key], val)
            self.seen[(e, key)] = val

    @staticmethod
    def _deps(R, W):
        toks = []
        for b in R:
            toks.append(b.w)
        for b in W:
            toks.append(b.w)
            toks.extend(b.r)
        return toks

    @staticmethod
    def _commit(tok, R, W):
        for b in R:
            b.r.append(tok)
            if len(b.r) > 24:
                b.r = b.r[-24:] if False else b.r
        for b in W:
            b.w = tok
            b.r = []

    def op(self, e, fn, *args, R=(), W=(), sig=True, **kw):
        self._wait(e, self._deps(R, W))
        ins = fn(*args, **kw)
        self.n_ins += 1
        key = (e, self.epoch[e])
        if sig:
            self.cnt[key] += 1
            ins.then_inc(self.sems[key], 1)
            tok = (key, self.cnt[key])
            if self.cnt[key] >= EPOCH:
                self._new_epoch(e)
        else:
            tok = (key, self.cnt[key] + 1)
        self._commit(tok, R, W)
        return tok

    def dma(self, q, out, in_, R=(), W=(), **kw):
        keys, idx = self.dq[q]
        key = keys[idx % len(keys)]
        self.dq[q][1] = idx + 1
        toks = self._deps(R, W)
        if self.cnt[key] > 0:
            toks.append((key, self.cnt[key]))
        self._wait(q, toks)
        ins = self.eng[q].dma_start(out=out, in_=in_, **kw)
        self.n_ins += 1
        self.cnt[key] += 16
        ins.then_inc(self.sems[key], 16)
        tok = (key, self.cnt[key])
        self._commit(tok, R, W)
        return tok

    def allgather(self, in_ap, out_ap, R=(), W=()):
        self._wait("pool", self._deps(R, W))
        ins = self.nc.gpsimd.collective_compute(
            "AllGather", ALU.bypass, replica_groups=[[0, 1, 2, 3], [4, 5, 6, 7]],
            ins=[in_ap], outs=[out_ap])
        self.n_ins += 1
        self.cnt[self.cc_key] += 1
        ins.then_inc(self.sems[self.cc_key])
        tok = (self.cc_key, self.cnt[self.cc_key])
        self._commit(tok, R, W)
        return tok

    def fence(self):
        toks = []
        for key, c in self.cnt.items():
            if c > 0:
                toks.append((key, c))
        for e in self.eng:
            self._wait(e, toks, force_same=False)

    def wait_bufs(self, e, bufs):
        toks = []
        for b in bufs:
            toks.append(b.w)
            toks.extend(b.r)
        self._wait(e, toks, force_same=True)


def build_program(nlayers=DEPTH, dbg=None):
    nc = bass.Bass("TRN2", target_bir_lowering=False)
    k = K(nc)
    dbg = dbg or {}
    uid = [0]

    def U(name):
        uid[0] += 1
        return f"{name}_{uid[0]}"

    def din(name, shape, dt=F32):
        return nc.dram_tensor(name, list(shape), dt, kind="ExternalInput").ap()

    xs_in = din("xs_in", [T, D])
    cvecT = din("cvecT", [D, 2])
    mod_w = din("mod_w", [DEPTH, D, 6 * D])
    mod_b = din("mod_b", [DEPTH, 6 * D])
    norm1_w = din("norm1_w", [DEPTH, D])
    norm2_w = din("norm2_w", [DEPTH, D])
    wp = din("wp", [DEPTH, D, NPACK])
    q_norm = din("mla_q_norm", [DEPTH, 256])
    w_uq = din("w_uq", [DEPTH, 256, 768])
    w_uq_sw = din("w_uq_sw", [DEPTH, 256, 768])
    kv_norm = din("mla_kv_norm", [DEPTH, 128])
    w_ukv = din("w_ukv", [DEPTH, 128, 1024])
    ret_logit = din("ret_decay_logit", [DEPTH, 2, 4])
    ret_norm_w = din("ret_norm_w", [DEPTH, 512])
    gwblk = din("gwblk", [DEPTH, 4, 32, 128])
    gbias = din("gbias", [DEPTH, 4, 128])
    gla_norm_w = din("gla_norm_w", [DEPTH, 512])
    w_branch = din("w_branch", [DEPTH, 3, 512, D])
    w_out = din("w_out", [DEPTH, D, D])
    ffn_wg = din("ffn_w_gate", [1, D, DFF])
    ffn_wu = din("ffn_w_up", [1, D, DFF])
    ffn_wd = din("ffn_w_down", [1, DFF, D])
    moe_router = din("moe_router", [1, D, NEXP])
    moe_wg = din("moe_w_gate", [1, NEXP, D, DFF])
    moe_wu = din("moe_w_up", [1, NEXP, D, DFF])
    moe_wd = din("moe_w_down", [1, NEXP, DFF, D])
    final_norm_w = din("final_norm_w", [D])
    c_ropeR = din("c_ropeR", [2, 128, T])
    c_ropeAq = din("c_ropeAq", [2, 32, T])
    c_ropeAk = din("c_ropeAk", [2, 32, T])
    c_rst = din("c_rst", [128, T])
    c_mask = din("c_mask", [2, 128, 128])
    c_ident = din("c_ident", [128, 128])
    c_onehot = din("c_onehot", [128, 4])

    out = nc.dram_tensor("out", [LAT, D], F32, kind="ExternalOutput").ap()
    dbg_out = {}
    for name, (shape, dt) in dbg.items():
        dbg_out[name] = nc.dram_tensor("dbg_" + name, list(shape), dt, kind="ExternalOutput").ap()

    xs = nc.dram_tensor("xs", [T, D], F32).ap()
    uT = nc.dram_tensor("uT", [8, 128, T], BF16).ap()
    modD = nc.dram_tensor("modD", [2, 6 * D], F32).ap()
    xf_in = nc.dram_tensor("xf_in", [NXF, 16, 1024], F32).ap()
    xf_out = nc.dram_tensor("xf_out", [NXF, 64, 1024], F32).ap()
    xb_in = nc.dram_tensor("xb_in", [NXB, 16, LAT], BF16).ap()
    xb_out = nc.dram_tensor("xb_out", [NXB, 64, LAT], BF16).ap()
    b_xs = [Buf(f"xs{t}") for t in range(NT)]
    b_uT = [Buf(f"uT{b}") for b in range(5)]
    b_modD = Buf("modD")
    b_xf_in = [Buf() for _ in range(NXF)]
    b_xf_out = [Buf() for _ in range(NXF)]
    b_xb_in = [Buf() for _ in range(NXB)]
    b_xb_out = [Buf() for _ in range(NXB)]
    b_out = Buf("out")

    PSA = nc.alloc_psum_tensor("psa", [128, 8, 512], F32)
    PS = []
    for i in range(8):
        PS.append((PSA[:, i, :], Buf(f"ps{i}")))

    def salloc(name, shape, dt=F32):
        return nc.alloc_sbuf_tensor(name, list(shape), dt), Buf(name)

    ident, b_ident = salloc("ident", [128, 128])
    identb, b_identb = salloc("identb", [128, 128], BF16)
    onesb, b_onesb = salloc("onesb", [128, 128], BF16)
    maskF, b_maskF = salloc("maskF", [128, 128])
    maskB, b_maskB = salloc("maskB", [128, 128])
    onehot, b_onehot = salloc("onehot", [128, 4])
    eps_t, b_eps = salloc("eps_t", [128, 1])
    one_t, b_one = salloc("one_t", [128, 1])
    modT, b_modT = salloc("modT", [128, 48, 2])
    A1, b_A1 = salloc("A1", [128, 8, 2])
    A2, b_A2 = salloc("A2", [128, 8, 2])
    rstb, b_rstb = salloc("rstb", [128, T], BF16)
    yTd = nc.dram_tensor("yTd", [3, 4, 128, T], BF16).ap()
    lsp_kt2 = nc.dram_tensor("lsp_kt2", [8, 128, T], BF16).ap()
    lsp_vth = nc.dram_tensor("lsp_vth", [8, 128, NT * 128], BF16).ap()
    lsp_eb = nc.dram_tensor("lsp_eb", [8, 128, T], BF16).ap()
    lsp_U2 = nc.dram_tensor("lsp_U2", [8, 128, NT * 128], F32).ap()
    lsp_te = nc.dram_tensor("lsp_te", [8, 128, 2 * NT], F32).ap()
    b_lsp = [Buf(f"lsp{i}") for i in range(8)]
    b_yTd = [Buf(f"yTd{i}") for i in range(3)]

    k.dma("sp", ident[:], c_ident[:, :], W=[b_ident])
    k.op("dve", nc.vector.tensor_copy, identb[:], ident[:], R=[b_ident], W=[b_identb])
    k.op("dve", nc.vector.memset, onesb[:], 1.0, W=[b_onesb])
    k.op("dve", nc.vector.memset, eps_t[:], EPS, W=[b_eps])
    k.op("dve", nc.vector.memset, one_t[:], 1.0, W=[b_one])
    k.dma("sp", maskF[:], c_mask[0], W=[b_maskF])
    k.dma("sp", maskB[:], c_mask[1], W=[b_maskB])
    k.dma("sp", onehot[:], c_onehot[:, :], W=[b_onehot])
    k.dma("pool", rstb[:], c_rst[:, :], W=[b_rstb])
    with nc.sbuf_tensor(U("zinit"), [16, 1024], F32) as zt_:
        b_zt = Buf()
        k.op("dve", nc.vector.memset, zt_[:], 0.0, W=[b_zt])
        k.dma("sp", xf_in[XF_LAM], zt_[:], R=[b_zt], W=[b_xf_in[XF_LAM]])
        k.fence()

    BLKS = [(0, 512), (512, 512), (1024, 512), (1536, 512), (2048, 256)]

    def wview(w2d):
        return w2d.rearrange("(kc p) n -> p kc n", p=128)

    alt = [0]

    def evac_engine():
        alt[0] += 1
        return "act" if alt[0] % 2 else "dve"

    def copy_ps(e, out_ap, in_ap, R, W, scale=None):
        if e == "act":
            if scale is None:
                k.op("act", nc.scalar.copy, out_ap, in_ap, R=R, W=W)
            else:
                k.op("act", nc.scalar.mul, out_ap, in_ap, scale, R=R, W=W)
        else:
            if scale is None:
                k.op("dve", nc.vector.tensor_copy, out_ap, in_ap, R=R, W=W)
            else:
                k.op("dve", nc.vector.tensor_scalar, out_ap, in_ap, scale, None, ALU.mult, R=R, W=W)

    def rsqrt_col(es, name, src_ap, src_buf, scale, n=1, parts=128):
        t1 = es.enter_context(nc.sbuf_tensor(U(name + "_a"), [128, n], F32))
        t2 = es.enter_context(nc.sbuf_tensor(U(name + "_b"), [128, n], F32))
        b1, b2 = Buf(), Buf()
        k.op("dve", nc.vector.tensor_scalar, t1[0:parts, :], src_ap, scale, EPS, ALU.mult, ALU.add,
             R=[src_buf], W=[b1])
        k.op("act", nc.scalar.activation, t2[0:parts, :], t1[0:parts, :], AF.Sqrt, R=[b1], W=[b2])
        k.op("dve", nc.vector.reciprocal, t1[0:parts, :], t2[0:parts, :], R=[b2], W=[b1])
        return t1, b1

    def phase_mod(l):
        with ExitStack() as es:
            def sb(name, shape, dt=F32):
                return es.enter_context(nc.sbuf_tensor(U(name), list(shape), dt)), Buf(name)
            cT, b_cT = sb("cT", [128, 8, 2])
            sc, b_sc = sb("sc", [128, 8, 2])
            sg, b_sg = sb("sg", [128, 8, 2])
            modv, b_modv = sb("modv", [2, 6 * D])
            mb, b_mb = sb("mb", [2, 6 * D])
            wr = Ring([sb(f"mw{i}", [128, 8, 512], BF16) for i in range(4)])
            scb, b_scb = sb("scb", [128, 8, 2], BF16)
            k.dma("sp", cT[:], cvecT.rearrange("(kc p) r -> p kc r", p=128), W=[b_cT])
            k.op("act", nc.scalar.activation, sg[:], cT[:], AF.Sigmoid, R=[b_cT], W=[b_sg])
            k.op("dve", nc.vector.tensor_tensor, sc[:], cT[:], sg[:], ALU.mult, R=[b_cT, b_sg], W=[b_sc])
            k.op("dve", nc.vector.tensor_copy, scb[:], sc[:], R=[b_sc], W=[b_scb])
            k.dma("sp", mb[0:1, :], mod_b[l:l + 1, :], W=[b_mb])
            k.dma("sp", mb[1:2, :], mod_b[l:l + 1, :], W=[b_mb])
            psr = Ring(PS[0:2])
            for n in range(12):
                wt, b_wt = wr.next()
                k.dma("pool", wt[:], wview(mod_w[l])[:, :, n * 512:(n + 1) * 512], W=[b_wt])
                ps, b_ps = psr.next()
                for kc in range(8):
                    k.op("pe", nc.tensor.matmul, ps[0:2, :], scb[:, kc, :], wt[:, kc, :],
                         start=(kc == 0), stop=(kc == 7), R=[b_scb, b_wt], W=[b_ps], sig=(kc == 7))
                k.op("dve", nc.vector.tensor_tensor, modv[:, n * 512:(n + 1) * 512], ps[0:2, :],
                     mb[:, n * 512:(n + 1) * 512], ALU.add, R=[b_ps, b_mb], W=[b_modv])
            k.dma("sp", modD[:, :], modv[:], R=[b_modv], W=[b_modD])
            pst, b_pst = PS[2]
            for j in range(48):
                k.op("pe", nc.tensor.transpose, pst[:, 2 * j:2 * j + 2], modv[0:2, j * 128:(j + 1) * 128],
                     ident[0:2, 0:2], R=[b_modv, b_ident], W=[b_pst], sig=(j == 47))
            k.op("dve", nc.vector.tensor_copy, modT[:].rearrange("p j r -> p (j r)"), pst[:, 0:96],
                 R=[b_pst], W=[b_modT])
            nw, b_nw = sb("nw", [128, 8, 2])
            for (nsrc, joff, At, bA) in ((norm1_w, 8, A1, b_A1), (norm2_w, 32, A2, b_A2)):
                k.dma("sp", nw[:, :, 0], nsrc[l].rearrange("(kc p) -> p kc", p=128), W=[b_nw], allow_slow_non_contiguous=True)
                k.dma("sp", nw[:, :, 1], nsrc[l].rearrange("(kc p) -> p kc", p=128), W=[b_nw], allow_slow_non_contiguous=True)
                k.op("dve", nc.vector.tensor_scalar, At[:], modT[:, joff:joff + 8, :], 1.0, None, ALU.add,
                     R=[b_modT], W=[bA])
                k.op("dve", nc.vector.tensor_tensor, At[:], At[:], nw[:], ALU.mult, R=[b_nw], W=[bA])
        k.fence()

    def phase_norm(l, At, bA, shoff, tiles, xsrc=None):
        xsrc = xs if xsrc is None else xsrc
        with ExitStack() as es:
            def sb(name, shape, dt=F32):
                return es.enter_context(nc.sbuf_tensor(U(name), list(shape), dt)), Buf(name)
            ND = 6
            xr = Ring([sb(f"nx{i}", [128, D]) for i in range(ND)])
            jr = Ring([sb(f"nj{i}", [128, D]) for i in range(2)])
            xnr = Ring([sb(f"nn{i}", [128, D]) for i in range(ND)])
            ur = Ring([sb(f"nu{i}", [128, 8, 128], BF16) for i in range(ND)])
            ssr = Ring([sb(f"ns{i}", [128, 1]) for i in range(ND)])
            psr = Ring([PS[0], PS[1], PS[2], PS[3]])
            xloads = {}

            def issue_x(i_):
                if i_ < len(tiles):
                    t_ = tiles[i_]
                    xt_, b_xt_ = xr.next()
                    k.dma("sp", xt_[:], xsrc[t_ * 128:(t_ + 1) * 128, :], R=[b_xs[t_]], W=[b_xt_])
                    xloads[i_] = (xt_, b_xt_)
            for i_ in range(ND - 1):
                issue_x(i_)
            for i_, t in enumerate(tiles):
                r = 0 if t < NLT else 1
                issue_x(i_ + ND - 1)
                xt, b_xt = xloads.pop(i_)
                jk, b_jk = jr.next()
                ss, b_ss = ssr.next()
                k.op("act", nc.scalar.activation, jk[:], xt[:], AF.Square, accum_out=ss[:, 0:1],
                     R=[b_xt], W=[b_ss])
                rstd, b_rstd = rsqrt_col(es, f"nr{t}", ss[:, 0:1], b_ss, 1.0 / D)
                xn, b_xn = xnr.next()
                k.op("act", nc.scalar.activation, xn[:], xt[:], AF.Identity, scale=rstd[:, 0:1],
                     R=[b_xt, b_rstd], W=[b_xn])
                ut, b_ut = ur.next()
                for half in range(2):
                    ps, b_ps = psr.next()
                    for j in range(4):
                        kc = half * 4 + j
                        k.op("pe", nc.tensor.transpose, ps[:, j * 128:(j + 1) * 128],
                             xn[:, kc * 128:(kc + 1) * 128], ident[:], R=[b_xn, b_ident], W=[b_ps],
                             sig=(j == 3))
                    for j in range(4):
                        kc = half * 4 + j
                        if j % 2 == 0:
                            k.op("act", nc.scalar.activation, ut[:, kc, :], ps[:, j * 128:(j + 1) * 128],
                                 AF.Identity, scale=At[:, kc, r:r + 1], bias=modT[:, shoff + kc, r:r + 1],
                                 R=[b_ps, bA, b_modT], W=[b_ut])
                        else:
                            k.op("dve", nc.vector.tensor_scalar, ut[:, kc, :], ps[:, j * 128:(j + 1) * 128],
                                 At[:, kc, r:r + 1], modT[:, shoff + kc, r:r + 1], ALU.mult, ALU.add,
                                 R=[b_ps, bA, b_modT], W=[b_ut])
                blk = min(t // 4, 4)
                k.dma("sp", uT[:, :, t * 128:(t + 1) * 128].rearrange("kc p t -> p kc t"), ut[:],
                      R=[b_ut], W=[b_uT[blk]])
        k.fence()

    class UStream:
        def __init__(self, es, nbuf=2, tag="ub"):
            self.ring = Ring([(es.enter_context(nc.sbuf_tensor(U(f"{tag}{i}"), [128, 8, 512], BF16)), Buf())
                              for i in range(nbuf)])

        def load(self, bi):
            t0, n = BLKS[bi]
            ub, b_ub = self.ring.next()
            k.dma("sp", ub[:, :, 0:n], uT[:, :, t0:t0 + n].rearrange("kc p t -> p kc t"),
                  R=[b_uT[bi]], W=[b_ub])
            return ub, b_ub, t0, n

    def proj_fm(ps, b_ps, w, b_w, c0, m, ub, b_ub, n, prow=0):
        for kc in range(8):
            k.op("pe", nc.tensor.matmul, ps[prow:prow + m, 0:n], w[:, kc, c0:c0 + m], ub[:, kc, 0:n],
                 start=(kc == 0), stop=(kc == 7), R=[b_w, b_ub], W=[b_ps], sig=(kc == 7))

    def load_w(es, name, src3d, shape, q="pool"):
        t = es.enter_context(nc.sbuf_tensor(U(name), list(shape), BF16))
        b = Buf(name)
        k.dma(q, t[:], src3d, W=[b])
        return t, b


    LS = {}
    LG2, b_LG2 = salloc("LG2", [128, 4])
    LAMT, b_LAMT = salloc("LAMT", [128, 8])

    def phase_q(l):
        cqn, b_cqn = LS["cqn"]; ckvn, b_ckvn = LS["ckvn"]; krr, b_krr = LS["krr"]; gzT, b_gzT = LS["gzT"]
        with ExitStack() as es:
            def sb(name, shape, dt=F32):
                return es.enter_context(nc.sbuf_tensor(U(name), list(shape), dt)), Buf(name)
            wA, b_wA = load_w(es, "wA", wview(wp[l])[:, :, PA:PA + PA_N], [128, 8, PA_N])
            qnw, b_qnw = sb("qnw", [128, 2])
            kvnw, b_kvnw = sb("kvnw", [128, 1])
            k.dma("sp", qnw[:], q_norm[l].rearrange("(c p) -> p c", p=128), W=[b_qnw], allow_slow_non_contiguous=True)
            k.dma("sp", kvnw[:], kv_norm[l].rearrange("(c p) -> p c", p=128), W=[b_kvnw], allow_slow_non_contiguous=True)
            ropk, b_ropk = sb("ropk", [32, 2, T])
            k.dma("sp", ropk[:, 0, :], c_ropeAk[0], W=[b_ropk])
            k.dma("sp", ropk[:, 1, :], c_ropeAk[1], W=[b_ropk])
            us = UStream(es)
            sqr = Ring([sb(f"sq{i}", [128, 512], BF16) for i in range(3)])
            rq, b_rq = sb("rq", [128, 512])
            rq2, b_rq2 = sb("rq2", [128, 512])
            tk, b_tk = sb("tk", [32, 512])
            tk2, b_tk2 = sb("tk2", [32, 512])
            for bi in range(5):
                ub, b_ub, t0, n = us.load(bi)
                (p0, bp0), (p1, bp1), (p2, bp2), (p3, bp3) = PS[0], PS[1], PS[2], PS[3]
                proj_fm(p0, bp0, wA, b_wA, 0, 128, ub, b_ub, n)
                proj_fm(p1, bp1, wA, b_wA, 128, 128, ub, b_ub, n)
                proj_fm(p2, bp2, wA, b_wA, 256, 128, ub, b_ub, n)
                s0, bs0 = sqr.next(); s1, bs1 = sqr.next(); s2, bs2 = sqr.next()
                k.op("act", nc.scalar.activation, s0[:, 0:n], p0[:, 0:n], AF.Square, R=[bp0], W=[bs0])
                k.op("act", nc.scalar.activation, s1[:, 0:n], p1[:, 0:n], AF.Square, R=[bp1], W=[bs1])
                k.op("act", nc.scalar.activation, s2[:, 0:n], p2[:, 0:n], AF.Square, R=[bp2], W=[bs2])
                k.op("pe", nc.tensor.matmul, p3[:, 0:n], onesb[:], s0[:, 0:n], start=True, stop=False,
                     R=[b_onesb, bs0], W=[bp3], sig=False)
                k.op("pe", nc.tensor.matmul, p3[:, 0:n], onesb[:], s1[:, 0:n], start=False, stop=True,
                     R=[b_onesb, bs1], W=[bp3])
                k.op("dve", nc.vector.tensor_scalar, rq[:, 0:n], p3[:, 0:n], 1.0 / 256, EPS, ALU.mult, ALU.add,
                     R=[bp3], W=[b_rq])
                k.op("act", nc.scalar.activation, rq2[:, 0:n], rq[:, 0:n], AF.Sqrt, R=[b_rq], W=[b_rq2])
                k.op("dve", nc.vector.reciprocal, rq[:, 0:n], rq2[:, 0:n], R=[b_rq2], W=[b_rq])
                k.op("dve", nc.vector.scalar_tensor_tensor, cqn[:, 0, t0:t0 + n], p0[:, 0:n], qnw[:, 0:1], rq[:, 0:n],
                     ALU.mult, ALU.mult, R=[bp0, b_qnw, b_rq], W=[b_cqn])
                k.op("dve", nc.vector.scalar_tensor_tensor, cqn[:, 1, t0:t0 + n], p1[:, 0:n], qnw[:, 1:2], rq[:, 0:n],
                     ALU.mult, ALU.mult, R=[bp1, b_qnw, b_rq], W=[b_cqn])
                k.op("pe", nc.tensor.matmul, p3[:, 0:n], onesb[:], s2[:, 0:n], start=True, stop=True,
                     R=[b_onesb, bs2], W=[bp3])
                k.op("dve", nc.vector.tensor_scalar, rq[:, 0:n], p3[:, 0:n], 1.0 / 128, EPS, ALU.mult, ALU.add,
                     R=[bp3], W=[b_rq])
                k.op("act", nc.scalar.activation, rq2[:, 0:n], rq[:, 0:n], AF.Sqrt, R=[b_rq], W=[b_rq2])
                k.op("dve", nc.vector.reciprocal, rq[:, 0:n], rq2[:, 0:n], R=[b_rq2], W=[b_rq])
                k.op("dve", nc.vector.scalar_tensor_tensor, ckvn[:, t0:t0 + n], p2[:, 0:n], kvnw[:, 0:1], rq[:, 0:n],
                     ALU.mult, ALU.mult, R=[bp2, b_kvnw, b_rq], W=[b_ckvn])
                (p4, bp4), (p5, bp5), (p6, bp6) = PS[4], PS[5], PS[6]
                proj_fm(p4, bp4, wA, b_wA, 384, 32, ub, b_ub, n)
                proj_fm(p5, bp5, wA, b_wA, 416, 32, ub, b_ub, n)
                proj_fm(p6, bp6, wA, b_wA, 448, 32, ub, b_ub, n)
                k.op("dve", nc.vector.tensor_tensor, tk[:, 0:n], p4[0:32, 0:n], ropk[:, 0, t0:t0 + n], ALU.mult,
                     R=[bp4, b_ropk], W=[b_tk])
                k.op("dve", nc.vector.tensor_tensor, tk2[:, 0:n], p5[0:32, 0:n], ropk[:, 1, t0:t0 + n], ALU.mult,
                     R=[bp5, b_ropk], W=[b_tk2])
                k.op("dve", nc.vector.tensor_tensor, krr[:, t0:t0 + n], tk[:, 0:n], tk2[:, 0:n], ALU.add,
                     R=[b_tk, b_tk2], W=[b_krr])
                k.op("act", nc.scalar.copy, gzT[:, t0:t0 + n], p6[0:32, 0:n], R=[bp6], W=[b_gzT])
            for j in range(8):
                k.dma("sp", xb_in[XB_CKV + j], ckvn[16 * j:16 * j + 16, 0:LAT], R=[b_ckvn], W=[b_xb_in[XB_CKV + j]])
            for j in range(2):
                k.dma("sp", xb_in[XB_KR + j], krr[16 * j:16 * j + 16, 0:LAT], R=[b_krr], W=[b_xb_in[XB_KR + j]])
            for j in range(NXB):
                k.allgather(xb_in[j], xb_out[j], R=[b_xb_in[j]], W=[b_xb_out[j]])
        k.fence()

    def phase_lg(l):
        with ExitStack() as es:
            def sb(name, shape, dt=F32):
                return es.enter_context(nc.sbuf_tensor(U(name), list(shape), dt)), Buf(name)
            lt, b_lt = sb("lt", [128, 4])
            l2, b_l2 = sb("l2", [128, 4])
            k.dma("sp", lt[0:64, :], ret_logit[l, 0:1, :].partition_broadcast(64), W=[b_lt])
            k.dma("sp", lt[64:128, :], ret_logit[l, 1:2, :].partition_broadcast(64), W=[b_lt])
            k.op("act", nc.scalar.activation, l2[:], lt[:], AF.Exp, scale=-1.0, R=[b_lt], W=[b_l2])
            k.op("act", nc.scalar.activation, lt[:], l2[:], AF.Ln, bias=one_t[:, 0:1], R=[b_l2, b_one], W=[b_lt])
            k.op("dve", nc.vector.tensor_scalar, LG2[:], lt[:], -1.0, None, ALU.mult, R=[b_lt], W=[b_LG2])
        k.fence()

    def lm_cols(mix, h):
        if mix == 0:
            return P_RET + h * 512, 512
        return P_GLA + h * 256, 256

    def lm_prefetch_w1(l, mix, h, PW):
        base, ncol = lm_cols(mix, h)
        k.dma("pool", PW["w"][0][:, :, 0:ncol], wview(wp[l])[:, :, base:base + ncol], W=[PW["w"][1]])
        vcol = (P_RV if mix == 0 else P_GV) + h * 128
        k.dma("pool", PW["wv"][0][:], wview(wp[l])[:, :, vcol:vcol + 128], W=[PW["wv"][1]])
        if mix == 1:
            k.dma("pool", PW["gw"][0][:], gwblk[l, h], W=[PW["gw"][1]])
            k.dma("sp", PW["nb"][0][:], gbias[l, h].rearrange("(p o) -> p o", o=1), W=[PW["nb"][1]])

    def lm_prefetch(l, mix, h, PF):
        hm = mix * 4 + h
        base, ncol = lm_cols(mix, h)
        k.dma("pool", PF["w"][0][:, :, 0:ncol], wview(wp[l])[:, :, base:base + ncol], W=[PF["w"][1]])
        gcol = (P_RG if mix == 0 else P_GR) + h * 128
        k.dma("pool", PF["wg"][0][:], wview(wp[l])[:, :, gcol:gcol + 128], W=[PF["wg"][1]])
        k.dma("sp", PF["kt2"][0][:], lsp_kt2[hm], R=[b_lsp[hm]], W=[PF["kt2"][1]])
        k.dma("sp", PF["vth"][0][:].rearrange("p n v -> p (n v)"), lsp_vth[hm], R=[b_lsp[hm]], W=[PF["vth"][1]])
        k.dma("sp", PF["ebT"][0][:], lsp_eb[hm], R=[b_lsp[hm]], W=[PF["ebT"][1]])
        k.dma("sp", PF["U2"][0][:].rearrange("p n v -> p (n v)"), lsp_U2[hm], R=[b_lsp[hm]], W=[PF["U2"][1]])
        k.dma("sp", PF["TE"][0][:], lsp_te[hm], R=[b_lsp[hm]], W=[PF["TE"][1]])

    def linear_mixer(l, mix, h, pass_no, last, PF=None, after_stage1=None):
        hm = mix * 4 + h
        gzT, b_gzT = LS["gzT"]
        with ExitStack() as es:
            def sb(name, shape, dt=F32):
                return es.enter_context(nc.sbuf_tensor(U(name), list(shape), dt)), Buf(name)
            if mix == 0:
                base, ncol = P_RET + h * 512, 512
                cq2, cq2s, ck2, ck2s = 0, 128, 256, 384
            else:
                base, ncol = P_GLA + h * 256, 256
                cq2, ck2 = 0, 128
            w, b_w = PF["w"]
            if pass_no == 1:
                wv, b_wv = PF["wv"]
            if mix == 1 and pass_no == 1:
                gw, b_gw = PF["gw"]
                nb, b_nb = PF["nb"]
                nb2, b_nb2 = sb("nb2", [128, 1])
                k.op("dve", nc.vector.tensor_scalar, nb2[:], nb[:], -1.0, None, ALU.mult, R=[b_nb], W=[b_nb2])
            if pass_no == 1:
                TE, b_TOT = sb("TE", [128, 2 * NT])
                kt2, b_kt2 = sb("kt2", [128, T], BF16)
                vth, b_vth = sb("vth", [128, NT, 128], BF16)
                ebT, b_ebT = sb("ebT", [128, T], BF16)
                U2, _ = sb("U2", [128, NT, 128])
            else:
                TE, b_TOT = PF["TE"]
                kt2, b_kt2 = PF["kt2"]
                vth, b_vth = PF["vth"]
                ebT, b_ebT = PF["ebT"]
                U2, b_ld = PF["U2"]
            TOT, EEND = TE[:, 0:NT], TE[:, NT:2 * NT]
            b_EEND = b_TOT
            b_kdc = [Buf() for _ in range(NT)]
            b_vthc = [Buf() for _ in range(NT)]
            us = UStream(es)
            if pass_no == 1:
                kd, b_kd = sb("kd", [128, T], BF16)
                a2r = Ring([sb(f"a2_{i}", [128, 512]) for i in range(2)])
                csr = Ring([sb(f"cs_{i}", [128, 512]) for i in range(2)])
                B2r = Ring([sb(f"B2_{i}", [128, 512]) for i in range(2)])
                enr = Ring([sb(f"en_{i}", [128, 512]) for i in range(2)])
            else:
                qt2, b_qt2 = sb("qt2", [128, T], BF16)
                b_vthc = [b_vth for _ in range(NT)]
            t1r = Ring([sb(f"lt1{i}", [128, 512]) for i in range(2)])
            t2r = Ring([sb(f"lt2{i}", [128, 512]) for i in range(2)])
            if mix == 0:
                ropr = Ring([sb(f"rop{i}", [128, 2, 512]) for i in range(2)])

            for bi in range(5):
                ub, b_ub, t0, n = us.load(bi)
                nch = n // 128
                ch0 = t0 // 128
                if pass_no == 1:
                    a2, b_a2 = a2r.next(); cs, b_cs = csr.next(); B2, b_B2 = B2r.next()
                    enb, b_enb = enr.next()
                if pass_no == 2:
                    pass
                elif mix == 0:
                    k.op("dve", nc.vector.memset, cs[:, 0:n], 1.0, W=[b_cs])
                    k.op("act", nc.scalar.activation, a2[:, 0:n], cs[:, 0:n], AF.Identity, scale=LG2[:, h:h + 1],
                         R=[b_cs, b_LG2], W=[b_a2])
                elif pass_no == 1:
                    pz, bpz = PS[7]
                    k.op("pe", nc.tensor.matmul, pz[:, 0:n], gw[:], gzT[:, t0:t0 + n], start=True, stop=True,
                         R=[b_gw, b_gzT], W=[bpz])
                    k.op("act", nc.scalar.activation, cs[:, 0:n], pz[:, 0:n], AF.Exp, scale=-1.0,
                         bias=nb2[:, 0:1], R=[bpz, b_nb2], W=[b_cs])
                    k.op("act", nc.scalar.activation, B2[:, 0:n], cs[:, 0:n], AF.Ln, bias=one_t[:, 0:1],
                         R=[b_cs, b_one], W=[b_B2])
                    k.op("dve", nc.vector.tensor_scalar, a2[:, 0:n], B2[:, 0:n], -1.0 / 16.0, None,
                         ALU.mult, R=[b_B2], W=[b_a2])
                if pass_no == 1:
                    k.op("dve", nc.vector.tensor_tensor_scan, cs[:, 0:n], rstb[:, t0:t0 + n], a2[:, 0:n], 0.0, ALU.mult, ALU.add,
                         R=[b_rstb, b_a2], W=[b_cs])
                    k.op("dve", nc.vector.tensor_copy, B2[0:64, 0:n], cs[0:64, 0:n], R=[b_cs], W=[b_B2])
                    k.op("dve", nc.vector.tensor_tensor, B2[64:128, 0:n], a2[64:128, 0:n], cs[64:128, 0:n], ALU.subtract,
                         R=[b_a2, b_cs], W=[b_B2])
                    for c_ in range(nch):
                        k.op("dve", nc.vector.tensor_scalar, B2[64:128, c_ * 128:(c_ + 1) * 128],
                             B2[64:128, c_ * 128:(c_ + 1) * 128], cs[64:128, c_ * 128 + 127:c_ * 128 + 128], None, ALU.add,
                             R=[b_cs], W=[b_B2])
                        k.op("dve", nc.vector.tensor_copy, TOT[:, ch0 + c_:ch0 + c_ + 1], cs[:, c_ * 128 + 127:c_ * 128 + 128],
                             R=[b_cs], W=[b_TOT])
                    k.op("act", nc.scalar.activation, EEND[:, ch0:ch0 + nch], TOT[:, ch0:ch0 + nch], AF.Exp,
                         R=[b_TOT], W=[b_EEND])
                    k.op("act", nc.scalar.activation, enb[:, 0:n], B2[:, 0:n], AF.Exp, scale=-1.0, R=[b_B2], W=[b_enb])
                    k.op("act", nc.scalar.activation, ebT[:, t0:t0 + n], B2[:, 0:n], AF.Exp, R=[b_B2], W=[b_ebT])
                if mix == 0:
                    rop, b_rop = ropr.next()
                    k.dma("sp", rop[:, 0, 0:n], c_ropeR[0, :, t0:t0 + n], W=[b_rop])
                    k.dma("sp", rop[:, 1, 0:n], c_ropeR[1, :, t0:t0 + n], W=[b_rop])

                def roped(pa, bpa, pb, bpb):
                    if mix == 1:
                        return pa[:, 0:n], bpa
                    ta, bta = t1r.next()
                    tb, btb = t2r.next()
                    k.op("dve", nc.vector.tensor_tensor, ta[:, 0:n], pa[:, 0:n], rop[:, 0, 0:n], ALU.mult,
                         R=[bpa, b_rop], W=[bta])
                    k.op("dve", nc.vector.tensor_tensor, tb[:, 0:n], pb[:, 0:n], rop[:, 1, 0:n], ALU.mult,
                         R=[bpb, b_rop], W=[btb])
                    k.op("dve", nc.vector.tensor_tensor, ta[:, 0:n], ta[:, 0:n], tb[:, 0:n], ALU.add,
                         R=[btb], W=[bta])
                    return ta[:, 0:n], bta

                (pa, bpa), (pb, bpb) = (PS[0], PS[1]) if bi % 2 == 0 else (PS[2], PS[3])
                if pass_no == 1:
                    proj_fm(pa, bpa, w, b_w, ck2, 128, ub, b_ub, n)
                    if mix == 0:
                        proj_fm(pb, bpb, w, b_w, ck2s, 128, ub, b_ub, n)
                    kap, bk = roped(pa, bpa, pb, bpb)
                    k.op("dve", nc.vector.tensor_tensor, kt2[:, t0:t0 + n], kap, enb[:, 0:n], ALU.mult,
                         R=[bk, b_enb], W=[b_kt2])
                if pass_no == 2:
                    (pc, bpc), (pd, bpd) = (pa, bpa), (pb, bpb)
                    proj_fm(pc, bpc, w, b_w, cq2, 128, ub, b_ub, n)
                    if mix == 0:
                        proj_fm(pd, bpd, w, b_w, cq2s, 128, ub, b_ub, n)
                    qap, bq = roped(pc, bpc, pd, bpd)
                    k.op("dve", nc.vector.scalar_tensor_tensor, qt2[:, t0:t0 + n], qap, 0.125, ebT[:, t0:t0 + n],
                         ALU.mult, ALU.mult, R=[bq, b_ebT], W=[b_qt2])
                for c_ in (range(nch) if pass_no == 1 else []):
                    n_ = ch0 + c_
                    k.op("act", nc.scalar.activation, kd[:, n_ * 128:(n_ + 1) * 128], kt2[:, n_ * 128:(n_ + 1) * 128],
                         AF.Identity, scale=EEND[:, n_:n_ + 1], R=[b_kt2, b_EEND], W=[b_kdc[n_]])
                    pv, bpv = PS[4 + c_ % 2]
                    for kc in range(8):
                        k.op("pe", nc.tensor.matmul, pv[:, 0:128], ub[:, kc, c_ * 128:(c_ + 1) * 128], wv[:, kc, :],
                             start=(kc == 0), stop=(kc == 7), R=[b_ub, b_wv], W=[bpv], sig=(kc == 7))
                    copy_ps(evac_engine(), vth[:, n_, :], pv[:, 0:128], R=[bpv], W=[b_vthc[n_]])
            if pass_no == 1:
                k.dma("sp", lsp_kt2[hm], kt2[:], R=[b_kt2], W=[b_lsp[hm]])
                k.dma("sp", lsp_vth[hm], vth[:].rearrange("p n v -> p (n v)"), R=b_vthc, W=[b_lsp[hm]])
                k.dma("sp", lsp_eb[hm], ebT[:], R=[b_ebT], W=[b_lsp[hm]])
                k.dma("sp", lsp_te[hm], TE[:], R=[b_TOT], W=[b_lsp[hm]])
            if pass_no == 1:
                k2d, _ = sb("k2d", [128, NT, 128], BF16)
            b_k2dg = [Buf() for _ in range(3)]
            b_U2g = [Buf() for _ in range(5)]
            if pass_no == 2:
                b_U2g = [b_ld]
            if pass_no == 1:
                ptb = PSA[:, 0:3, :].bitcast(BF16)
                for n_ in range(NT):
                    bk = n_ // 8
                    k.op("pe", nc.tensor.transpose, ptb[:, bk, (n_ % 8) * 128:(n_ % 8 + 1) * 128],
                         kd[:, n_ * 128:(n_ + 1) * 128], identb[:], R=[b_kdc[n_], b_identb], W=[PS[bk][1]],
                         sig=(n_ % 8 == 7 or n_ == NT - 1))
                for bk in range(3):
                    nb_ = min(8, NT - 8 * bk)
                    copy_ps(evac_engine(), k2d[:, 8 * bk:8 * bk + nb_, :],
                            ptb[:, bk, 0:nb_ * 128].rearrange("p (a c) -> p a c", c=128), R=[PS[bk][1]], W=[b_k2dg[bk]])
                for n_ in range(NT):
                    bk = 3 + n_ // 4
                    k.op("pe", nc.tensor.matmul, PSA[:, bk, (n_ % 4) * 128:(n_ % 4 + 1) * 128], k2d[:, n_, :], vth[:, n_, :],
                         start=True, stop=True, R=[b_k2dg[n_ // 8], b_vthc[n_]], W=[PS[bk][1]], sig=(n_ % 4 == 3 or n_ == NT - 1))
                for b4 in range(5):
                    nb_ = min(4, NT - 4 * b4)
                    copy_ps(evac_engine(), U2[:, 4 * b4:4 * b4 + nb_, :],
                            PSA[:, 3 + b4, 0:nb_ * 128].rearrange("p (a c) -> p a c", c=128), R=[PS[3 + b4][1]], W=[b_U2g[b4]])
            if pass_no == 1:
                k.dma("sp", lsp_U2[hm], U2[:].rearrange("p n v -> p (n v)"), R=b_U2g, W=[b_lsp[hm]])
            F, Bw = slice(0, 64), slice(64, 128)
            TSEQ, b_TSEQ = sb("TSEQ", [128, 17])
            ESEQ, b_ESEQ = sb("ESEQ", [128, 17])
            k.op("dve", nc.vector.memset, TSEQ[:, 0:1], 0.0, W=[b_TSEQ])
            k.op("dve", nc.vector.tensor_copy, TSEQ[F, 1:17], TOT[F, 0:NLT], R=[b_TOT], W=[b_TSEQ])
            k.op("dve", nc.vector.tensor_copy, TSEQ[Bw, 1:17], TOT[Bw, NLT - 1::-1], R=[b_TOT], W=[b_TSEQ])
            k.op("act", nc.scalar.activation, ESEQ[:], TSEQ[:], AF.Exp, R=[b_TSEQ], W=[b_ESEQ])
            k.op("dve", nc.vector.memset, ESEQ[:, 0:1], 0.0, W=[b_ESEQ])
            DS, b_DS = sb("DS", [128, 128, 17])
            US, b_US = sb("US", [128, 128, 17])
            SS, b_SS = sb("SS", [128, 128, 17])
            k.op("dve", nc.vector.tensor_copy, DS[:], ESEQ[:].unsqueeze(1).broadcast_to([128, 128, 17]),
                 R=[b_ESEQ], W=[b_DS])
            k.op("dve", nc.vector.tensor_copy, US[F, :, 1:17], U2[F, 0:NLT, :].rearrange("p n v -> p v n"),
                 R=b_U2g, W=[b_US])
            k.op("dve", nc.vector.tensor_copy, US[Bw, :, 1:17], U2[Bw, NLT - 1::-1, :].rearrange("p n v -> p v n"),
                 R=b_U2g, W=[b_US])
            St, b_St = sb("St", [128, 128])

            def run_scan():
                k.op("dve", nc.vector.tensor_tensor_scan, SS[:].rearrange("p v t -> p (v t)"),
                     DS[:].rearrange("p v t -> p (v t)"), US[:].rearrange("p v t -> p (v t)"), 0.0, ALU.mult, ALU.add,
                     R=[b_DS, b_US], W=[b_SS])

            if pass_no == 1:
                k.op("dve", nc.vector.memset, US[:, :, 0], 0.0, W=[b_US])
                run_scan()
                k.op("dve", nc.vector.tensor_copy, St[:], SS[:, :, 16], R=[b_SS], W=[b_St])
                k.dma("sp", xf_in[XF_STATE + hm].rearrange("a (b c) -> (a b) c", c=128), St[:],
                      R=[b_St], W=[b_xf_in[XF_STATE + hm]])
                ls, b_ls = sb("ls", [128, 1])
                k.op("dve", nc.vector.reduce_sum, ls[:], TOT[:, 0:NLT], AX.X, R=[b_TOT], W=[b_ls])
                k.op("act", nc.scalar.activation, LAMT[:, hm:hm + 1], ls[:], AF.Exp, R=[b_ls], W=[b_LAMT])
                return
            FR, b_FR = sb("FR", [128, 4, 128])
            LR, b_LR = sb("LR", [128, 4, 8])
            for r in range(4):
                k.dma("sp", FR[:, r, :], xf_out[XF_STATE + hm, r * 16:(r + 1) * 16, :].rearrange("a (b c) -> (a b) c", c=128),
                      R=[b_xf_out[XF_STATE + hm]], W=[b_FR])
                k.dma("sp", LR[:, r, :], xf_out[XF_LAM, r * 16, :].rearrange("(p e) -> p e", e=8),
                      R=[b_xf_out[XF_LAM]], W=[b_LR])
            Rt, b_Rt = sb("Rt", [128, 128])
            Sin, b_Sin = sb("Sin", [128, 128])
            k.op("dve", nc.vector.memset, Sin[:], 0.0, W=[b_Sin])
            k.op("dve", nc.vector.scalar_tensor_tensor, Rt[F, :], U2[F, 16, :], EEND[F, 17:18], U2[F, 17, :],
                 ALU.mult, ALU.add, R=b_U2g + [b_EEND], W=[b_Rt])
            k.op("dve", nc.vector.scalar_tensor_tensor, Rt[Bw, :], U2[Bw, 17, :], EEND[Bw, 16:17], U2[Bw, 16, :],
                 ALU.mult, ALU.add, R=b_U2g + [b_EEND], W=[b_Rt])
            for sl, order in ((F, range(4)), (Bw, range(3, -1, -1))):
                for r in order:
                    k.op("dve", nc.vector.scalar_tensor_tensor, Sin[sl, :], Rt[sl, :], onehot[sl, r:r + 1], Sin[sl, :],
                         ALU.mult, ALU.add, R=[b_Rt, b_onehot], W=[b_Sin])
                    k.op("dve", nc.vector.scalar_tensor_tensor, Rt[sl, :], Rt[sl, :], LR[sl, r, hm:hm + 1], FR[sl, r, :],
                         ALU.mult, ALU.add, R=[b_LR, b_FR], W=[b_Rt])
            k.op("dve", nc.vector.tensor_copy, US[:, :, 0], Sin[:], R=[b_Sin], W=[b_US])
            run_scan()
            S2, b_S2 = sb("S2", [128, NT, 128], BF16)
            k.op("dve", nc.vector.tensor_copy, S2[F, 0:NLT, :], SS[F, :, 0:NLT].rearrange("p v t -> p t v"),
                 R=[b_SS], W=[b_S2])
            k.op("dve", nc.vector.tensor_copy, S2[Bw, 0:NLT, :], SS[Bw, :, NLT - 1::-1].rearrange("p v t -> p t v"),
                 R=[b_SS], W=[b_S2])
            k.op("dve", nc.vector.memset, S2[F, 16, :], 0.0, W=[b_S2])
            k.op("dve", nc.vector.memset, S2[Bw, 17, :], 0.0, W=[b_S2])
            k.op("dve", nc.vector.tensor_copy, S2[F, 17, :], U2[F, 16, :], R=b_U2g, W=[b_S2])
            k.op("dve", nc.vector.tensor_copy, S2[Bw, 16, :], U2[Bw, 17, :], R=b_U2g, W=[b_S2])
            gcol = (P_RG if mix == 0 else P_GR) + h * 128
            wg_, b_wg = PF["wg"]
            nwb, b_nwb = sb("nwb", [128, 128])
            nsrc = ret_norm_w if mix == 0 else gla_norm_w
            k.dma("sp", nwb[:], nsrc[l:l + 1, h * 128:(h + 1) * 128].partition_broadcast(128), W=[b_nwb])
            G, b_G = sb("G", [128, NT, 128], BF16)
            gs, b_gs = sb("gs", [128, 8, 128])
            nchunks = NLT if last else NT
            groups = [(g0, min(8, nchunks - g0)) for g0 in range(0, nchunks, 8)]
            ubc = None
            for gi, (g0, ng) in enumerate(groups):
                bks = [6, 7] if gi % 2 == 0 else [4, 5]
                for c_ in range(ng):
                    n_ = g0 + c_
                    bi = min(n_ // 4, 4)
                    if ubc is None or ubc[0] != bi:
                        ubc = (bi,) + tuple(us.load(bi))
                    _, ub, b_ub, t0, n = ubc
                    tt = n_ - t0 // 128
                    bk = bks[c_ // 4]
                    for kc in range(8):
                        k.op("pe", nc.tensor.matmul, PSA[:, bk, (c_ % 4) * 128:(c_ % 4 + 1) * 128],
                             ub[:, kc, tt * 128:(tt + 1) * 128], wg_[:, kc, :],
                             start=(kc == 0), stop=(kc == 7), R=[b_ub, b_wg], W=[PS[bk][1]], sig=(kc == 7))
                nbk = (ng + 3) // 4
                pgv = PSA[:, bks[0]:bks[0] + nbk, :].rearrange("p b (c v) -> p (b c) v", v=128)[:, 0:ng, :]
                k.op("act", nc.scalar.activation, gs[:, 0:ng, :], pgv, AF.Silu, R=[PS[b_][1] for b_ in bks[:nbk]], W=[b_gs])
                k.op("dve", nc.vector.tensor_tensor, G[:, g0:g0 + ng, :], gs[:, 0:ng, :],
                     nwb[:].unsqueeze(1).broadcast_to([128, ng, 128]), ALU.mult, R=[b_gs, b_nwb], W=[b_G])
            if after_stage1 is not None:
                after_stage1()
            t1, b_t1 = sb("g_t1", [128, 8, 128])
            t2, b_t2 = sb("g_t2", [128, 8, 128])
            PT8, b_PT8 = sb("PT8", [128, 8, 128], BF16)
            osb, b_osb = sb("osb", [128, 8, 128])
            jk, b_jk = sb("ljk", [128, 128])
            stt, b_stt = sb("stt", [128, 6, 8])
            yn8, b_yn8 = sb("yn8", [128, 8, 128])
            yb8, b_yb8 = sb("yb8", [128, 8, 128], BF16)
            yTt, b_yT = sb("yTt", [128, T], BF16)
            b_sttc = [Buf() for _ in range(8)]
            b_osbc = [Buf() for _ in range(8)]
            b_ync = [Buf() for _ in range(8)]
            for (g0, ng) in groups:
                nbk = (ng + 3) // 4
                for c_ in range(ng):
                    c0 = (g0 + c_) * 128
                    k.op("pe", nc.tensor.matmul, PSA[:, c_ // 4, (c_ % 4) * 128:(c_ % 4 + 1) * 128],
                         kt2[0:64, c0:c0 + 128], qt2[0:64, c0:c0 + 128],
                         start=True, stop=True, R=[b_kt2, b_qt2], W=[PS[c_ // 4][1]], sig=(c_ % 4 == 3 or c_ == ng - 1))
                for c_ in range(ng):
                    c0 = (g0 + c_) * 128
                    k.op("pe", nc.tensor.matmul, PSA[:, 2 + c_ // 4, (c_ % 4) * 128:(c_ % 4 + 1) * 128],
                         kt2[64:128, c0:c0 + 128], qt2[64:128, c0:c0 + 128],
                         start=True, stop=True, R=[b_kt2, b_qt2], W=[PS[2 + c_ // 4][1]], sig=(c_ % 4 == 3 or c_ == ng - 1))
                sfv = PSA[:, 0:nbk, :].rearrange("p b (c v) -> p (b c) v", v=128)[:, 0:ng, :]
                sbv = PSA[:, 2:2 + nbk, :].rearrange("p b (c v) -> p (b c) v", v=128)[:, 0:ng, :]
                k.op("dve", nc.vector.tensor_tensor, t1[:, 0:ng, :], sfv, maskF[:].unsqueeze(1).broadcast_to([128, ng, 128]),
                     ALU.mult, R=[PS[b_][1] for b_ in range(nbk)] + [b_maskF], W=[b_t1])
                k.op("dve", nc.vector.tensor_tensor, t2[:, 0:ng, :], sbv, maskB[:].unsqueeze(1).broadcast_to([128, ng, 128]),
                     ALU.mult, R=[PS[2 + b_][1] for b_ in range(nbk)] + [b_maskB], W=[b_t2])
                k.op("dve", nc.vector.tensor_tensor, PT8[:, 0:ng, :], t1[:, 0:ng, :], t2[:, 0:ng, :], ALU.add,
                     R=[b_t1, b_t2], W=[b_PT8])
                for c_ in range(ng):
                    n_ = g0 + c_
                    c0 = n_ * 128
                    bk = 4 + c_ // 4
                    oap = PSA[:, bk, (c_ % 4) * 128:(c_ % 4 + 1) * 128]
                    k.op("pe", nc.tensor.matmul, oap, PT8[:, c_, :], vth[:, n_, :],
                         start=True, stop=False, R=[b_PT8, b_vthc[n_]], W=[PS[bk][1]], sig=False)
                    k.op("pe", nc.tensor.matmul, oap, qt2[:, c0:c0 + 128], S2[:, n_, :],
                         start=False, stop=True, R=[b_qt2, b_S2], W=[PS[bk][1]], sig=(c_ % 4 == 3 or c_ == ng - 1))
                for c_ in range(ng):
                    bk = 4 + c_ // 4
                    oap = PSA[:, bk, (c_ % 4) * 128:(c_ % 4 + 1) * 128]
                    k.op("act", nc.scalar.activation, osb[:, c_, :], oap, AF.Identity, accum_out=stt[:, 0, c_:c_ + 1],
                         R=[PS[bk][1]], W=[b_osbc[c_], b_sttc[c_]])
                    k.op("act", nc.scalar.activation, jk[:], oap, AF.Square, accum_out=stt[:, 1, c_:c_ + 1],
                         R=[PS[bk][1], b_sttc[c_]], W=[])
                k.op("dve", nc.vector.tensor_scalar, stt[:, 0:2, 0:ng], stt[:, 0:2, 0:ng], 1.0 / 128, None, ALU.mult,
                     W=[b_stt] + b_sttc[0:ng])
                if mix == 0:
                    k.op("dve", nc.vector.tensor_tensor, stt[:, 3, 0:ng], stt[:, 0, 0:ng], stt[:, 0, 0:ng], ALU.mult, R=b_sttc[0:ng], W=[b_stt])
                    k.op("dve", nc.vector.tensor_tensor, stt[:, 1, 0:ng], stt[:, 1, 0:ng], stt[:, 3, 0:ng], ALU.subtract, R=b_sttc[0:ng], W=[b_stt])
                k.op("dve", nc.vector.tensor_scalar, stt[:, 3, 0:ng], stt[:, 1, 0:ng], EPS, None, ALU.add, R=b_sttc[0:ng], W=[b_stt])
                k.op("act", nc.scalar.activation, stt[:, 4, 0:ng], stt[:, 3, 0:ng], AF.Sqrt, R=b_sttc[0:ng], W=[b_stt])
                k.op("dve", nc.vector.reciprocal, stt[:, 2, 0:ng], stt[:, 4, 0:ng], R=b_sttc[0:ng], W=[b_stt])
                for c_ in range(ng):
                    if mix == 0:
                        k.op("dve", nc.vector.tensor_scalar, yn8[:, c_, :], osb[:, c_, :], stt[:, 0, c_:c_ + 1], stt[:, 2, c_:c_ + 1],
                             ALU.subtract, ALU.mult, R=[b_osbc[c_], b_stt, b_sttc[c_]], W=[b_ync[c_]])
                    else:
                        k.op("dve", nc.vector.tensor_scalar, yn8[:, c_, :], osb[:, c_, :], stt[:, 2, c_:c_ + 1], None, ALU.mult,
                             R=[b_osbc[c_], b_stt, b_sttc[c_]], W=[b_ync[c_]])
                k.op("dve", nc.vector.tensor_tensor, yb8[:, 0:ng, :], yn8[:, 0:ng, :], G[:, g0:g0 + ng, :], ALU.mult,
                     R=b_ync[0:ng] + [b_G], W=[b_yb8])
                p6b = PSA[:, 6, :].bitcast(BF16)
                for c_ in range(ng):
                    k.op("pe", nc.tensor.transpose, p6b[:, c_ * 128:(c_ + 1) * 128], yb8[:, c_, :], identb[:],
                         R=[b_yb8, b_identb], W=[PS[6][1]], sig=(c_ == ng - 1))
                copy_ps("act", yTt[:, g0 * 128:(g0 + ng) * 128], p6b[:, 0:ng * 128], R=[PS[6][1]], W=[b_yT])
            ntok = LAT if last else T
            k.dma("sp", yTd[1 + mix, h, :, 0:ntok], yTt[:, 0:ntok], R=[b_yT], W=[b_yTd[1 + mix]])

    def phase_linear(l, pass_no, last):
        order = [(mix, h) for mix in range(2) for h in range(4)]
        if pass_no == 1:
            with ExitStack() as pes:
                PWs = []
                for si in range(2):
                    PW = {}
                    for nm, shp, dt in (("w", [128, 8, 512], BF16), ("wv", [128, 8, 128], BF16), ("gw", [32, 128], BF16),
                                        ("nb", [128, 1], F32)):
                        PW[nm] = (pes.enter_context(nc.sbuf_tensor(U(f"pw_{nm}{si}"), shp, dt)), Buf(f"pw_{nm}{si}"))
                    PWs.append(PW)
                lm_prefetch_w1(l, order[0][0], order[0][1], PWs[0])
                for i_, (mix, h) in enumerate(order):
                    if i_ + 1 < len(order):
                        lm_prefetch_w1(l, order[i_ + 1][0], order[i_ + 1][1], PWs[(i_ + 1) % 2])
                    linear_mixer(l, mix, h, pass_no, last, PF=PWs[i_ % 2])
                    k.fence()
        else:
            with ExitStack() as pes:
                PFs = []
                for si in range(2):
                    PF = {}
                    for nm, shp, dt in (("w", [128, 8, 512], BF16), ("wg", [128, 8, 128], BF16), ("kt2", [128, T], BF16),
                                        ("vth", [128, NT, 128], BF16), ("ebT", [128, T], BF16), ("U2", [128, NT, 128], F32),
                                        ("TE", [128, 2 * NT], F32)):
                        PF[nm] = (pes.enter_context(nc.sbuf_tensor(U(f"pf_{nm}{si}"), shp, dt)), Buf(f"pf_{nm}{si}"))
                    PFs.append(PF)
                lm_prefetch(l, order[0][0], order[0][1], PFs[0])
                for i_, (mix, h) in enumerate(order):
                    cb = None
                    if i_ + 1 < len(order):
                        cb = (lambda j=i_ + 1: lm_prefetch(l, order[j][0], order[j][1], PFs[j % 2]))
                    linear_mixer(l, mix, h, pass_no, last, PF=PFs[i_ % 2], after_stage1=cb)
                    k.fence()
            k.fence()
        if pass_no == 1:
            k.dma("sp", xf_in[XF_LAM, 0, :].rearrange("(p e) -> p e", e=8), LAMT[:], R=[b_LAMT], W=[b_xf_in[XF_LAM]])
            for j in range(NXF):
                k.allgather(xf_in[j], xf_out[j], R=[b_xf_in[j]], W=[b_xf_out[j]])
            k.fence()


    G8, b_G8 = salloc("G8", [128, NT, 8])

    def phase_attn(l, last):
        SCL = 96.0 ** -0.5
        cqn, b_cqn = LS["cqn"]; ckvn, b_ckvn = LS["ckvn"]; krr, b_krr = LS["krr"]
        with ExitStack() as es:
            def sb(name, shape, dt=F32):
                return es.enter_context(nc.sbuf_tensor(U(name), list(shape), dt)), Buf(name)
            ckvA, b_ckvA = sb("ckvA", [128, NKEY], BF16)
            k.op("pool", nc.gpsimd.tensor_copy, ckvA[:, 0:CTX], ckvn[:, LAT:T], R=[b_ckvn], W=[b_ckvA])
            for j in range(8):
                k.dma("sp", ckvA[16 * j:16 * j + 16, CTX:NKEY].rearrange("p (r t) -> p r t", r=4),
                      xb_out[XB_CKV + j].rearrange("(r p) t -> p r t", p=16),
                      R=[b_xb_out[XB_CKV + j]], W=[b_ckvA])
            KTs = [sb(f"KT{i}", [96, NKEY], BF16) for i in range(2)]
            Vs = [sb(f"V{i}", [128, NKT, 128], BF16) for i in range(2)]
            for (KT, bKT), (V, bV) in zip(KTs, Vs):
                k.dma("sp", KT[64:96, 0:CTX], krr[:, LAT:T], R=[b_krr], W=[bKT])
                for j in range(2):
                    k.dma("sp", KT[64 + 16 * j:64 + 16 * j + 16, CTX:NKEY].rearrange("p (r t) -> p r t", r=4),
                          xb_out[XB_KR + j].rearrange("(r p) t -> p r t", p=16),
                          R=[b_xb_out[XB_KR + j]], W=[bKT])
                k.op("pool", nc.gpsimd.memset, V[:, :, 64:128], 1.0, W=[bV])
            ropq_r = Ring([sb(f"ropq{i}", [96, 2, 512]) for i in range(2)])
            yat_r = Ring([sb(f"yat{i}", [64, 512], BF16) for i in range(3)])
            QTs = Ring([sb(f"QT{i}", [96, T], BF16) for i in range(2)])
            wq_r = Ring([sb(f"wq{i}", [128, 2, 96], BF16) for i in range(2)])
            wqs_r = Ring([sb(f"wqs{i}", [128, 2, 96], BF16) for i in range(2)])
            wkv_r = Ring([sb(f"wkv{i}", [128, 128], BF16) for i in range(2)])
            t1r = Ring([sb(f"at1{i}", [96, 512]) for i in range(2)])
            t2r = Ring([sb(f"at2{i}", [96, 512]) for i in range(2)])
            Pr = Ring([sb(f"P{i}", [128, 512], BF16) for i in range(4)])
            linv_r = Ring([sb(f"linv{i}", [128, 512]) for i in range(2)])
            sring = Ring([PS[0], PS[1], PS[2], PS[3]])
            oring = Ring([PS[4], PS[5]])
            qblocks = [(q0, 512, list(range(NKT))) for q0 in range(0, LAT, 512)]
            if not last:
                qblocks.append((LAT, CTX, [0, 1]))
            def build_head(h):
                wq, b_wq = wq_r.next(); wqs, b_wqs = wqs_r.next(); wkv, b_wkv = wkv_r.next()
                k.dma("pool", wq[:], w_uq[l].rearrange("(kc p) n -> p kc n", p=128)[:, :, h * 96:(h + 1) * 96], W=[b_wq])
                k.dma("pool", wqs[:], w_uq_sw[l].rearrange("(kc p) n -> p kc n", p=128)[:, :, h * 96:(h + 1) * 96], W=[b_wqs])
                k.dma("pool", wkv[:], w_ukv[l][:, h * 128:(h + 1) * 128], W=[b_wkv])
                QT, bQT = QTs.next()
                KT, bKT = KTs[h % 2]
                V, bV = Vs[h % 2]
                built[h] = (QT, bQT, KT, bKT, V, bV)
                yield
                blks = BLKS[:4] if last else BLKS
                for bi, (t0, n) in enumerate(blks):
                    (pa, bpa), (pb, bpb) = PS[6], PS[7]
                    for (pp, bpp, ww, bww) in ((pa, bpa, wq, b_wq), (pb, bpb, wqs, b_wqs)):
                        for kc in range(2):
                            k.op("pe", nc.tensor.matmul, pp[0:96, 0:n], ww[:, kc, :], cqn[:, kc, t0:t0 + n],
                                 start=(kc == 0), stop=(kc == 1), R=[bww, b_cqn], W=[bpp], sig=(kc == 1))
                    k.op("dve", nc.vector.tensor_scalar, QT[0:64, t0:t0 + n], pa[0:64, 0:n], SCL, None, ALU.mult,
                         R=[bpa], W=[bQT])
                    ta, bta = t1r.next(); tb, btb = t2r.next()
                    ropq, b_ropq = ropq_r.next()
                    k.dma("sp", ropq[64:96, 0, 0:n], c_ropeAq[0, :, t0:t0 + n], W=[b_ropq])
                    k.dma("sp", ropq[64:96, 1, 0:n], c_ropeAq[1, :, t0:t0 + n], W=[b_ropq])
                    k.op("dve", nc.vector.tensor_tensor, ta[64:96, 0:n], pa[64:96, 0:n], ropq[64:96, 0, 0:n],
                         ALU.mult, R=[bpa, b_ropq], W=[bta])
                    k.op("dve", nc.vector.tensor_tensor, tb[64:96, 0:n], pb[64:96, 0:n], ropq[64:96, 1, 0:n],
                         ALU.mult, R=[bpb, b_ropq], W=[btb])
                    k.op("dve", nc.vector.tensor_tensor, QT[64:96, t0:t0 + n], ta[64:96, 0:n], tb[64:96, 0:n],
                         ALU.add, R=[bta, btb], W=[bQT])
                    yield
                for kb in range((NKEY + 511) // 512):
                    c0 = kb * 512
                    n = min(512, NKEY - c0)
                    pk, bpk = PS[6 + kb % 2]
                    k.op("pe", nc.tensor.matmul, pk[0:64, 0:n], wkv[:, 0:64], ckvA[:, c0:c0 + n], start=True, stop=True,
                         R=[b_wkv, b_ckvA], W=[bpk])
                    copy_ps("dve", KT[0:64, c0:c0 + n], pk[0:64, 0:n], R=[bpk], W=[bKT])
                    yield
                for g0 in range(0, NKT, 8):
                    gn = min(8, NKT - g0)
                    pv, bpv = PS[7]
                    for i_ in range(gn):
                        kt = g0 + i_
                        k.op("pe", nc.tensor.matmul, pv[:, i_ * 64:(i_ + 1) * 64], ckvA[:, kt * 128:(kt + 1) * 128],
                             wkv[:, 64:128], start=True, stop=True, R=[b_ckvA, b_wkv], W=[bpv], sig=(i_ == gn - 1))
                    copy_ps("dve", V[:, g0:g0 + gn, 0:64],
                            pv[:, 0:gn * 64].rearrange("p (a b) -> p a b", b=64), R=[bpv], W=[bV])
                    yield

            built = {}
            for _ in build_head(0):
                pass
            for h in range(8):
                QT, bQT, KT, bKT, V, bV = built[h]
                gen = build_head(h + 1) if h + 1 < 8 else iter(())
                step_no = 0
                for (q0, nq, ktiles) in qblocks:
                    po, bpo = oring.next()

                    def issue_s(kt):
                        ps_, bps_ = sring.next()
                        k.op("pe", nc.tensor.matmul, ps_[:, 0:nq], KT[0:96, kt * 128:(kt + 1) * 128], QT[0:96, q0:q0 + nq],
                             start=True, stop=True, R=[bKT, bQT], W=[bps_])
                        return ps_, bps_
                    LA = 3
                    pend = [issue_s(kt_) for kt_ in ktiles[:LA]]
                    for i_, kt in enumerate(ktiles):
                        if i_ + LA < len(ktiles):
                            pend.append(issue_s(ktiles[i_ + LA]))
                        cur = pend.pop(0)
                        P, bP = Pr.next()
                        k.op("act", nc.scalar.activation, P[:, 0:nq], cur[0][:, 0:nq], AF.Exp, R=[cur[1]], W=[bP])
                        lastk = (i_ == len(ktiles) - 1)
                        k.op("pe", nc.tensor.matmul, po[:, 0:nq], V[:, kt, :], P[:, 0:nq], start=(i_ == 0), stop=lastk,
                             R=[bV, bP], W=[bpo], sig=True)
                        step_no += 1
                        if step_no % 6 == 0:
                            next(gen, None)
                    linv, b_linv = linv_r.next()
                    k.op("dve", nc.vector.reciprocal, linv[64:128, 0:nq], po[64:128, 0:nq], R=[bpo], W=[b_linv])
                    r0 = (h % 2) * 64
                    yat, b_yat = yat_r.next()
                    k.op("dve", nc.vector.tensor_tensor, yat[:, 0:nq], po[0:64, 0:nq],
                         linv[64:128, 0:nq], ALU.mult, R=[bpo, b_linv], W=[b_yat])
                    k.dma("sp", yTd[0, h // 2, r0:r0 + 64, q0:q0 + nq], yat[:, 0:nq], R=[b_yat], W=[b_yTd[0]])
                for _ in gen:
                    pass
        k.fence()

    def phase_merge(l, last):
        blks = list(enumerate(BLKS[:4] if last else BLKS))
        with ExitStack() as es:
            def sb(name, shape, dt=F32):
                return es.enter_context(nc.sbuf_tensor(U(name), list(shape), dt)), Buf(name)
            zT, b_zT = sb("zT", [128, 8, T], BF16)
            wo, b_wo = load_w(es, "wo", wview(w_out[l]), [128, 8, D])
            us = UStream(es)
            yb_r = Ring([sb(f"myb{i}", [128, 3, 4, 512], BF16) for i in range(2)])
            g1, b_g1 = sb("g1", [128, 2, D])
            for r in range(2):
                k.dma("sp", g1[:, r, :], modD[r:r + 1, 2 * D:3 * D].partition_broadcast(128), R=[b_modD], W=[b_g1])
            wg_r = Ring([sb(f"mwg{i}", [128, 8, 3, 128], BF16) for i in range(2)])
            wb_r = Ring([sb(f"mwb{i}", [128, 3, 4, 128], BF16) for i in range(2)])
            gj_r = Ring([sb(f"gj{i}", [128, 512]) for i in range(2)])
            za_r = Ring([sb(f"za{i}", [128, 512]) for i in range(2)])
            zt_r = Ring([sb(f"zt{i}", [128, 512]) for i in range(2)])
            pgr = Ring([PS[0], PS[1], PS[2]])
            pzr = Ring([PS[3], PS[4], PS[5]])
            for c in range(8):
                wg3, b_wg3 = wg_r.next(); wb3, b_wb3 = wb_r.next()
                for j in range(3):
                    col = P_BG + j * 1024 + c * 128
                    k.dma("pool", wg3[:, :, j, :], wview(wp[l])[:, :, col:col + 128], W=[b_wg3])
                    k.dma("pool", wb3[:, j, :, :], w_branch[l, j].rearrange("(k4 p) n -> p k4 n", p=128)[:, :, c * 128:(c + 1) * 128],
                          W=[b_wb3])
                for bi, (t0, n) in blks:
                    ub, b_ub, _, _ = us.load(bi)
                    yb3, b_yb3 = yb_r.next()
                    for j in range(3):
                        k.dma("sp", yb3[:, j, :, 0:n], yTd[j, :, :, t0:t0 + n].rearrange("k p t -> p k t"),
                              R=[b_yTd[j]], W=[b_yb3])
                    za, bza = za_r.next()
                    for j in range(3):
                        pg, bpg = pgr.next()
                        for kc in range(8):
                            k.op("pe", nc.tensor.matmul, pg[:, 0:n], wg3[:, kc, j, :], ub[:, kc, 0:n],
                                 start=(kc == 0), stop=(kc == 7), R=[b_wg3, b_ub], W=[bpg], sig=(kc == 7))
                        gj, bgj = gj_r.next()
                        k.op("act", nc.scalar.activation, gj[:, 0:n], pg[:, 0:n], AF.Sigmoid, R=[bpg], W=[bgj])
                        pz, bpz = pzr.next()
                        for k4 in range(4):
                            k.op("pe", nc.tensor.matmul, pz[:, 0:n], wb3[:, j, k4, :], yb3[:, j, k4, 0:n],
                                 start=(k4 == 0), stop=(k4 == 3), R=[b_wb3, b_yb3], W=[bpz], sig=(k4 == 3))
                        if j == 0:
                            k.op("dve", nc.vector.tensor_tensor, za[:, 0:n], pz[:, 0:n], gj[:, 0:n], ALU.mult,
                                 R=[bpz, bgj], W=[bza])
                        else:
                            zt, bzt = zt_r.next()
                            k.op("dve", nc.vector.tensor_tensor, zt[:, 0:n], pz[:, 0:n], gj[:, 0:n], ALU.mult,
                                 R=[bpz, bgj], W=[bzt])
                            if j == 1:
                                k.op("dve", nc.vector.tensor_tensor, za[:, 0:n], za[:, 0:n], zt[:, 0:n], ALU.add,
                                     R=[bzt], W=[bza])
                            else:
                                k.op("dve", nc.vector.tensor_tensor, zT[:, c, t0:t0 + n], za[:, 0:n], zt[:, 0:n], ALU.add,
                                     R=[bza, bzt], W=[b_zT])
            xr = Ring([sb(f"mx{i}", [128, D]) for i in range(4)])
            tmr = Ring([sb(f"mt{i}", [128, 512]) for i in range(2)])
            pyr = Ring([PS[6], PS[7]])
            tiles = list(range(NLT)) if last else list(range(NT))
            xloads = {}

            def issue_x(i_):
                if i_ < len(tiles):
                    t_ = tiles[i_]
                    xt_, b_xt_ = xr.next()
                    k.dma("sp", xt_[:], (xs_in if l == 0 else xs)[t_ * 128:(t_ + 1) * 128, :], R=[b_xs[t_]], W=[b_xt_])
                    xloads[i_] = (xt_, b_xt_)
            issue_x(0); issue_x(1)
            for i_, t in enumerate(tiles):
                r = 0 if t < NLT else 1
                issue_x(i_ + 2)
                xt, b_xt = xloads.pop(i_)
                for half in range(2):
                    py, bpy = pyr.next()
                    for kc in range(8):
                        k.op("pe", nc.tensor.matmul, py[:, :], zT[:, kc, t * 128:(t + 1) * 128],
                             wo[:, kc, half * 512:(half + 1) * 512], start=(kc == 0), stop=(kc == 7),
                             R=[b_zT, b_wo], W=[bpy], sig=(kc == 7))
                    tm, btm = tmr.next()
                    k.op("dve", nc.vector.tensor_tensor, tm[:], py[:, :], g1[:, r, half * 512:(half + 1) * 512], ALU.mult,
                         R=[bpy, b_g1], W=[btm])
                    k.op("dve", nc.vector.tensor_tensor, xt[:, half * 512:(half + 1) * 512], xt[:, half * 512:(half + 1) * 512],
                         tm[:], ALU.add, R=[btm], W=[b_xt])
                k.dma("sp", xs[t * 128:(t + 1) * 128, :], xt[:], R=[b_xt], W=[b_xs[t]])
        k.fence()

    def phase_router(i, tiles):
        with ExitStack() as es:
            def sb(name, shape, dt=F32):
                return es.enter_context(nc.sbuf_tensor(U(name), list(shape), dt)), Buf(name)
            rw, b_rw = load_w(es, "rw", wview(moe_router[i]), [128, 8, NEXP])
            us = UStream(es)
            lg_r = Ring([sb(f"rl{i_}", [128, 8]) for i_ in range(2)])
            m8_r = Ring([sb(f"rm{i_}", [128, 8]) for i_ in range(2)])
            ex_r = Ring([sb(f"re{i_}", [128, 8]) for i_ in range(2)])
            mk_r = Ring([sb(f"rk{i_}", [128, 8]) for i_ in range(2)])
            sc_r = Ring([sb(f"rs{i_}", [128, 4]) for i_ in range(2)])
            ubc = None
            for t in tiles:
                bi = min(t // 4, 4)
                if ubc is None or ubc[0] != bi:
                    ubc = (bi,) + tuple(us.load(bi))
                _, ub, b_ub, t0, n = ubc
                tt = t - t0 // 128
                pl, bpl = PS[t % 2]
                for kc in range(8):
                    k.op("pe", nc.tensor.matmul, pl[:, 0:NEXP], ub[:, kc, tt * 128:(tt + 1) * 128], rw[:, kc, :],
                         start=(kc == 0), stop=(kc == 7), R=[b_ub, b_rw], W=[bpl], sig=(kc == 7))
                lg, blg = lg_r.next(); m8, bm8 = m8_r.next(); ex, bex = ex_r.next(); mk, bmk = mk_r.next()
                sc_, bsc = sc_r.next()
                k.op("dve", nc.vector.tensor_copy, lg[:], pl[:, 0:NEXP], R=[bpl], W=[blg])
                k.op("dve", nc.vector.max, m8[:], lg[:], R=[blg], W=[bm8])
                k.op("dve", nc.vector.tensor_scalar, mk[:], lg[:], m8[:, 1:2], None, ALU.is_ge, R=[blg, bm8], W=[bmk])
                k.op("dve", nc.vector.tensor_scalar, sc_[:, 0:1], m8[:, 0:1], -1.0, None, ALU.mult, R=[bm8], W=[bsc])
                k.op("act", nc.scalar.activation, ex[:], lg[:], AF.Exp, bias=sc_[:, 0:1], R=[blg, bsc], W=[bex])
                k.op("dve", nc.vector.tensor_tensor, ex[:], ex[:], mk[:], ALU.mult, R=[bmk], W=[bex])
                k.op("dve", nc.vector.reduce_sum, sc_[:, 1:2], ex[:], AX.X, R=[bex], W=[bsc])
                k.op("dve", nc.vector.reciprocal, sc_[:, 2:3], sc_[:, 1:2], W=[bsc])
                k.op("dve", nc.vector.tensor_scalar, G8[:, t, :], ex[:], sc_[:, 2:3], None, ALU.mult, R=[bex, bsc], W=[b_G8])
        k.fence()

    def phase_ffn(l, last):
        moe = (l % 2 == 1)
        i = l // 2
        nexp = NEXP if moe else 1
        groups = [(0, 1024), (1024, 1024)] if last else [(0, 1152), (1152, 1152)]
        for (g0, gn) in groups:
            with ExitStack() as es:
                def sb(name, shape, dt=F32):
                    return es.enter_context(nc.sbuf_tensor(U(name), list(shape), dt)), Buf(name)
                vTg, b_vTg = sb("vTg", [128, 8, gn], BF16)
                ublks = sorted(set(min(t_ // 4, 4) for t_ in range(g0 // 128, (g0 + gn) // 128)))
                k.dma("sp", vTg[:], uT[:, :, g0:g0 + gn].rearrange("kc p t -> p kc t"),
                      R=[b_uT[b_] for b_ in ublks], W=[b_vTg])
                hT, b_hT = sb("hT", [128, NFF, gn], BF16)
                acc, b_acc = sb("acc", [128, gn // 128, D])
                wd, b_wd = sb("wd", [128, NFF, D], BF16)
                wt_r = Ring([sb(f"wgu{i_}", [128, 8, 2, 256], BF16) for i_ in range(2)])
                sg_r = Ring([sb(f"sg{i_}", [128, 512]) for i_ in range(3)])
                pgr = Ring([PS[0], PS[1]])
                pur = Ring([PS[2], PS[3]])
                pyr = Ring([PS[4], PS[5], PS[6], PS[7]])
                bsz = 512 if gn % 512 == 0 else 384
                nblks = [(c0, min(bsz, gn - c0)) for c0 in range(0, gn, bsz)]
                def wsrc(e):
                    if moe:
                        return moe_wg[i, e], moe_wu[i, e], moe_wd[i, e]
                    return ffn_wg[i], ffn_wu[i], ffn_wd[i]
                tasks = [(e, fc2) for e in range(nexp) for fc2 in range(NFF // 2)]
                loaded = {}

                def issue_load(ti):
                    if ti >= len(tasks) or ti in loaded:
                        return
                    e_, fc2_ = tasks[ti]
                    Wg_, Wu_, _ = wsrc(e_)
                    wt_, b_wt_ = wt_r.next()
                    k.dma("pool", wt_[:, :, 0, :], wview(Wg_)[:, :, fc2_ * 256:(fc2_ + 1) * 256], W=[b_wt_])
                    k.dma("pool", wt_[:, :, 1, :], wview(Wu_)[:, :, fc2_ * 256:(fc2_ + 1) * 256], W=[b_wt_])
                    loaded[ti] = (wt_, b_wt_)
                issue_load(0)
                for e in range(nexp):
                    Wg, Wu, Wd = wsrc(e)
                    for fc2 in range(NFF // 2):
                        ti = e * (NFF // 2) + fc2
                        issue_load(ti)
                        issue_load(ti + 1)
                        if fc2 == 0:
                            k.dma("pool", wd[:], Wd.rearrange("(f p) n -> p f n", p=128), W=[b_wd])
                        wt, b_wt = loaded.pop(ti)
                        for sub in range(2):
                            fc = fc2 * 2 + sub
                            for (c0, n) in nblks:
                                pg, bpg = pgr.next(); pu, bpu = pur.next()
                                for kc in range(8):
                                    k.op("pe", nc.tensor.matmul, pg[:, 0:n], wt[:, kc, 0, sub * 128:(sub + 1) * 128],
                                         vTg[:, kc, c0:c0 + n], start=(kc == 0), stop=(kc == 7),
                                         R=[b_wt, b_vTg], W=[bpg], sig=(kc == 7))
                                for kc in range(8):
                                    k.op("pe", nc.tensor.matmul, pu[:, 0:n], wt[:, kc, 1, sub * 128:(sub + 1) * 128],
                                         vTg[:, kc, c0:c0 + n], start=(kc == 0), stop=(kc == 7),
                                         R=[b_wt, b_vTg], W=[bpu], sig=(kc == 7))
                                sg, bsg = sg_r.next()
                                k.op("act", nc.scalar.activation, sg[:, 0:n], pg[:, 0:n], AF.Silu, R=[bpg], W=[bsg])
                                k.op("dve", nc.vector.tensor_tensor, hT[:, fc, c0:c0 + n], sg[:, 0:n], pu[:, 0:n], ALU.mult,
                                     R=[bsg, bpu], W=[b_hT])
                    for tt in range(gn // 128):
                        t = g0 // 128 + tt
                        for half in range(2):
                            py, bpy = pyr.next()
                            for fc in range(NFF):
                                k.op("pe", nc.tensor.matmul, py[:, :], hT[:, fc, tt * 128:(tt + 1) * 128],
                                     wd[:, fc, half * 512:(half + 1) * 512], start=(fc == 0), stop=(fc == NFF - 1),
                                     R=[b_hT, b_wd], W=[bpy], sig=(fc == NFF - 1))
                            dst = acc[:, tt, half * 512:(half + 1) * 512]
                            if not moe:
                                copy_ps(evac_engine(), dst, py[:, :], R=[bpy], W=[b_acc])
                            elif e == 0:
                                k.op("dve", nc.vector.tensor_scalar, dst, py[:, :], G8[:, t, e:e + 1], None, ALU.mult,
                                     R=[bpy, b_G8], W=[b_acc])
                            else:
                                k.op("dve", nc.vector.scalar_tensor_tensor, dst, py[:, :], G8[:, t, e:e + 1], dst,
                                     ALU.mult, ALU.add, R=[bpy, b_G8], W=[b_acc])
                xr = Ring([sb(f"fx{i_}", [128, D]) for i_ in range(3)])
                jr = Ring([sb(f"fj{i_}", [128, D]) for i_ in range(2)])
                ssr = Ring([sb(f"fs{i_}", [128, 1]) for i_ in range(2)])
                g2, b_g2 = sb("g2", [128, 2, D])
                for rg_ in range(1 if last else 2):
                    k.dma("sp", g2[:, rg_, :], modD[rg_:rg_ + 1, 5 * D:6 * D].partition_broadcast(128), R=[b_modD], W=[b_g2])
                if last:
                    fnw, b_fnw = sb("fnw", [128, D])
                    k.dma("sp", fnw[:], final_norm_w.rearrange("(o d) -> o d", o=1).partition_broadcast(128), W=[b_fnw])
                xloads = {}

                def issue_x(tt_):
                    if tt_ < gn // 128:
                        t_ = g0 // 128 + tt_
                        xt_, b_xt_ = xr.next()
                        k.dma("sp", xt_[:], xs[t_ * 128:(t_ + 1) * 128, :], R=[b_xs[t_]], W=[b_xt_])
                        xloads[tt_] = (xt_, b_xt_)
                issue_x(0); issue_x(1)
                for tt in range(gn // 128):
                    t = g0 // 128 + tt
                    r = 0 if t < NLT else 1
                    issue_x(tt + 2)
                    xt, b_xt = xloads.pop(tt)
                    k.op("dve", nc.vector.tensor_tensor, acc[:, tt, :], acc[:, tt, :], g2[:, r, :], ALU.mult,
                         R=[b_g2], W=[b_acc])
                    k.op("dve", nc.vector.tensor_tensor, xt[:], xt[:], acc[:, tt, :], ALU.add, R=[b_acc], W=[b_xt])
                    if not last:
                        k.dma("sp", xs[t * 128:(t + 1) * 128, :], xt[:], R=[b_xt], W=[b_xs[t]])
                    else:
                        jk, b_jk = jr.next(); ss, b_ss = ssr.next()
                        k.op("act", nc.scalar.activation, jk[:], xt[:], AF.Square, accum_out=ss[:, 0:1],
                             R=[b_xt], W=[b_jk, b_ss])
                        rstd, b_rstd = rsqrt_col(es, f"fr{t}", ss[:, 0:1], b_ss, 1.0 / D)
                        k.op("dve", nc.vector.scalar_tensor_tensor, jk[:], xt[:], rstd[:, 0:1], fnw[:], ALU.mult, ALU.mult,
                             R=[b_xt, b_rstd, b_fnw], W=[b_jk])
                        k.dma("sp", out[t * 128:(t + 1) * 128, :], jk[:], R=[b_jk], W=[b_out])
            k.fence()

    stop = dbg.get("_stop") if isinstance(dbg, dict) else None

    def tap(name, src_ap, bufs):
        if name in dbg_out:
            k.dma("sp", dbg_out[name], src_ap, R=bufs, W=[b_out])

    for l in range(nlayers):
        last = (l == DEPTH - 1)
        alltiles = list(range(NT))
        phase_mod(l)
        phase_norm(l, A1, b_A1, 0, alltiles, xsrc=(xs_in if l == 0 else xs))
        phase_lg(l)
        with ExitStack() as les:
            for nm, shp in (("cqn", [128, 2, T]), ("ckvn", [128, T]), ("krr", [32, T]), ("gzT", [32, T])):
                LS[nm] = (les.enter_context(nc.sbuf_tensor(U(nm), shp, BF16)), Buf(nm))
            phase_q(l)
            phase_linear(l, 1, last)
            phase_linear(l, 2, last)
            phase_attn(l, last)
        k.fence()
        phase_merge(l, last)
        ftiles = list(range(NLT)) if last else alltiles
        phase_norm(l, A2, b_A2, 24, ftiles)
        if l % 2 == 1:
            phase_router(l // 2, ftiles)
        phase_ffn(l, last)
    if nlayers < DEPTH:
        for t in range(NLT):
            k.dma("sp", out[t * 128:(t + 1) * 128, :], xs[t * 128:(t + 1) * 128, :], R=[b_xs[t]], W=[b_out])
    k.wait_bufs("sp", [b_out])
    return nc, k


IN_SPLITS = (256, 128, 32, 256, 256, 512, 512, 256, 256, 512, 512, 32, 3072)
OFFS = np.concatenate([[0], np.cumsum(IN_SPLITS)]).astype(int)
(O_CQ, O_CKV, O_KR, O_RQ, O_RK, O_RV, O_RG, O_GQ, O_GK, O_GV, O_GR, O_GZ, O_BG) = OFFS[:13]


def _partner(n, half):
    idx = np.arange(n)
    return np.where(idx % (2 * half) < half, idx + half, idx - half)


def _pack_w_in(w_in_l):
    cols = []
    pa = _partner(32, 8)
    cols.append(np.arange(O_CQ, O_CQ + 256))
    cols.append(np.arange(O_CKV, O_CKV + 128))
    cols.append(np.arange(O_KR, O_KR + 32))
    cols.append(O_KR + pa)
    cols.append(np.arange(O_GZ, O_GZ + 32))
    pr = _partner(64, 32)
    for h in range(4):
        q = O_RQ + h * 64 + np.arange(64)
        qs = O_RQ + h * 64 + pr
        kk = O_RK + h * 64 + np.arange(64)
        ks = O_RK + h * 64 + pr
        cols += [q, q, qs, qs, kk, kk, ks, ks]
    for h in range(4):
        q = O_GQ + h * 64 + np.arange(64)
        kk = O_GK + h * 64 + np.arange(64)
        cols += [q, q, kk, kk]
    cols.append(np.arange(O_RV, O_RV + 512))
    cols.append(np.arange(O_GV, O_GV + 512))
    cols.append(np.arange(O_RG, O_RG + 512))
    cols.append(np.arange(O_GR, O_GR + 512))
    cols.append(np.arange(O_BG, O_BG + 3072))
    cols = np.concatenate(cols)
    assert cols.shape[0] == NPACK
    return np.ascontiguousarray(w_in_l[:, cols])


def _rope_tables(pos, half, signed_rows):
    inv = 10000.0 ** (-np.arange(half, dtype=np.float32) / half)
    ang = pos.astype(np.float32)[None, :] * inv[:, None]
    cos = np.cos(ang).astype(np.float32)
    sin = np.sin(ang).astype(np.float32)
    return np.concatenate([cos, cos], 0), np.concatenate([-sin, sin], 0)


def _consts(q):
    pos = q * LAT + np.arange(LAT)
    c, s = _rope_tables(pos, 32, True)
    ropeR = np.zeros((2, 128, T), np.float32)
    ropeR[0, :, :LAT] = np.concatenate([c, c], 0)
    ropeR[1, :, :LAT] = np.concatenate([s, s], 0)
    ropeR[0, :, LAT:] = 1.0
    cr, sr = _rope_tables(pos // 64, 8, True)
    cc, sc = _rope_tables(pos % 64, 8, True)
    ak = np.zeros((2, 32, T), np.float32)
    ak[0, :, :LAT] = np.concatenate([cr, cc], 0)
    ak[1, :, :LAT] = np.concatenate([sr, sc], 0)
    ak[0, :, LAT:] = 1.0
    aq = (ak * np.float32(96.0 ** -0.5)).astype(np.float32)
    rst = np.ones((128, T), np.float32)
    rst[:, ::128] = 0.0
    j = np.arange(128)[:, None]
    i = np.arange(128)[None, :]
    mask = np.stack([(i >= j), (j >= i)]).astype(np.float32)
    onehot = np.zeros((128, 4), np.float32)
    onehot[:, q] = 1.0
    return dict(c_ropeR=ropeR, c_ropeAq=aq, c_ropeAk=ak, c_rst=rst, c_mask=mask,
                c_ident=np.eye(128, dtype=np.float32), c_onehot=onehot)


def make_in_maps(inp):
    f = lambda a: np.ascontiguousarray(np.asarray(a, dtype=np.float32))
    x, c, ctx, c_ctx = f(inp["x"]), f(inp["c"]), f(inp["ctx"]), f(inp["c_ctx"])
    w_in = f(inp["w_in"])
    wp_ = np.stack([_pack_w_in(w_in[l]) for l in range(DEPTH)])
    w_uq = f(inp["mla_w_uq"])
    pa = _partner(32, 8)
    cols = np.arange(768)
    for h in range(8):
        cols[h * 96 + 64:h * 96 + 96] = h * 96 + 64 + pa
    w_uq_sw = np.ascontiguousarray(w_uq[:, :, cols])
    gw = f(inp["gla_w_gate"])
    gb = f(inp["gla_b_gate"])
    gwblk = np.zeros((DEPTH, 4, 32, 128), np.float32)
    gbias = np.zeros((DEPTH, 4, 128), np.float32)
    for h in range(4):
        gwblk[:, h, 0:16, 0:64] = gw[:, 0, :, h * 64:(h + 1) * 64]
        gwblk[:, h, 16:32, 64:128] = gw[:, 1, :, h * 64:(h + 1) * 64]
        gbias[:, h, 0:64] = gb[:, 0, h * 64:(h + 1) * 64]
        gbias[:, h, 64:128] = gb[:, 1, h * 64:(h + 1) * 64]
    shared = dict(
        mod_w=f(inp["mod_w"]), mod_b=f(inp["mod_b"]), norm1_w=f(inp["norm1_w"]), norm2_w=f(inp["norm2_w"]),
        wp=wp_, mla_q_norm=f(inp["mla_q_norm"]), w_uq=w_uq, w_uq_sw=w_uq_sw, mla_kv_norm=f(inp["mla_kv_norm"]),
        w_ukv=f(inp["mla_w_ukv"]), ret_decay_logit=f(inp["ret_decay_logit"]), ret_norm_w=f(inp["ret_norm_w"]),
        gwblk=gwblk, gbias=gbias, gla_norm_w=f(inp["gla_norm_w"]), w_branch=f(inp["w_branch"]), w_out=f(inp["w_out"]),
        ffn_w_gate=f(inp["ffn_w_gate"]), ffn_w_up=f(inp["ffn_w_up"]), ffn_w_down=f(inp["ffn_w_down"]),
        moe_router=f(inp["moe_router"]), moe_w_gate=f(inp["moe_w_gate"]), moe_w_up=f(inp["moe_w_up"]),
        moe_w_down=f(inp["moe_w_down"]), final_norm_w=f(inp["final_norm_w"]),
    )
    maps = []
    for core in range(NCORES):
        b, q = core // 4, core % 4
        m = dict(shared)
        m["xs_in"] = np.ascontiguousarray(np.concatenate([x[b, q * LAT:(q + 1) * LAT], ctx[b]], 0))
        m["cvecT"] = np.ascontiguousarray(np.stack([c[b], c_ctx], 1))
        m.update(_consts(q))
        maps.append(m)
    return maps


_PROG = {}


def kernel(**inputs):
    if "nc" not in _PROG:
        _PROG["nc"] = build_program()[0]
    nc = _PROG["nc"]
    maps = make_in_maps(inputs)
    res = run_bass_kernel_spmd(nc, maps, core_ids=list(range(NCORES)))
    outp = np.zeros((2, SEQ, D), np.float32)
    for core in range(NCORES):
        b, q = core // 4, core % 4
        outp[b, q * LAT:(q + 1) * LAT] = res.results[core]["out"]
    return outp
```

```python
import math
from contextlib import ExitStack
import numpy as np
import ml_dtypes
import concourse.bass as bass
import concourse.mybir as mybir
from concourse.bass_utils import run_bass_kernel_spmd

F32 = mybir.dt.float32
BF16 = mybir.dt.bfloat16
AF = mybir.ActivationFunctionType
ALU = mybir.AluOpType
AX = mybir.AxisListType

NCORES = 8
D = 1024
SEQ = 8192
CTX = 256
LAT = 2048
T = LAT + CTX
NT = T // 128
NLT = LAT // 128
DEPTH = 2
EPS = 1e-6
DFF = 2816
NFF = DFF // 128
NEXP = 8
NKEY = CTX + SEQ
NKT = NKEY // 128

PA = 0
PA_N = 480
P_RET = 480
P_GLA = P_RET + 4 * 512
P_RV = P_GLA + 4 * 256
P_GV = P_RV + 512
P_RG = P_GV + 512
P_GR = P_RG + 512
P_BG = P_GR + 512
NPACK = P_BG + 3072

XF_STATE = 0
XF_LAM = 8
NXF = 9
XB_CKV = 0
XB_KR = 8
NXB = 10

EPOCH = 30000


class Buf:
    __slots__ = ("name", "w", "r")

    def __init__(self, name=""):
        self.name = name
        self.w = None
        self.r = []


class Ring:
    def __init__(self, items):
        self.items = items
        self.i = 0

    def next(self):
        it = self.items[self.i % len(self.items)]
        self.i += 1
        return it


class K:
    def __init__(self, nc):
        self.nc = nc
        self.eng = {"pe": nc.tensor, "dve": nc.vector, "act": nc.scalar,
                    "pool": nc.gpsimd, "sp": nc.sync}
        self.sems = {}
        self.cnt = {}
        self.epoch = {e: 0 for e in self.eng}
        self.seen = {}
        for e in self.eng:
            self._new_epoch(e, first=True)
        self.dq = {}
        for q, n in (("sp", 24), ("pool", 16), ("act", 4)):
            keys = []
            for i in range(n):
                key = ("dma", q, i)
                self.sems[key] = nc.alloc_semaphore(f"d_{q}_{i}")
                self.cnt[key] = 0
                keys.append(key)
            self.dq[q] = [keys, 0]
        self.cc_key = ("cc", 0)
        self.sems[self.cc_key] = nc.alloc_semaphore("cc")
        self.cnt[self.cc_key] = 0
        self.n_ins = 0

    def _new_epoch(self, e, first=False):
        if not first:
            self.epoch[e] += 1
        key = (e, self.epoch[e])
        self.sems[key] = self.nc.alloc_semaphore(f"s_{e}_{self.epoch[e]}")
        self.cnt[key] = 0

    def _wait(self, e, toks, force_same=False):
        eng = self.eng[e]
        best = {}
        for t in toks:
            if t is None:
                continue
            key, val = t
            if key[0] == e and not force_same:
                if e in ("pe", "sp"):
                    continue
            if best.get(key, 0) < val:
                best[key] = val
        for key, val in best.items():
            if self.seen.get((e, key), 0) >= val:
                continue
            assert self.cnt[key] >= val, f"wait on unsignalled token {key} {val} > {self.cnt[key]}"
            eng.wait_ge(self.sems[key], val)
            self.seen[(e, key)] = val

    @staticmethod
    def _deps(R, W):
        toks = []
        for b in R:
            toks.append(b.w)
        for b in W:
            toks.append(b.w)
            toks.extend(b.r)
        return toks

    @staticmethod
    def _commit(tok, R, W):
        for b in R:
            b.r.append(tok)
            if len(b.r) > 24:
                b.r = b.r[-24:] if False else b.r
        for b in W:
            b.w = tok
            b.r = []

    def op(self, e, fn, *args, R=(), W=(), sig=True, **kw):
        self._wait(e, self._deps(R, W))
        ins = fn(*args, **kw)
        self.n_ins += 1
        key = (e, self.epoch[e])
        if sig:
            self.cnt[key] += 1
            ins.then_inc(self.sems[key], 1)
            tok = (key, self.cnt[key])
            if self.cnt[key] >= EPOCH:
                self._new_epoch(e)
        else:
            tok = (key, self.cnt[key] + 1)
        self._commit(tok, R, W)
        return tok

    def dma(self, q, out, in_, R=(), W=(), **kw):
        keys, idx = self.dq[q]
        key = keys[idx % len(keys)]
        self.dq[q][1] = idx + 1
        toks = self._deps(R, W)
        if self.cnt[key] > 0:
            toks.append((key, self.cnt[key]))
        self._wait(q, toks)
        ins = self.eng[q].dma_start(out=out, in_=in_, **kw)
        self.n_ins += 1
        self.cnt[key] += 16
        ins.then_inc(self.sems[key], 16)
        tok = (key, self.cnt[key])
        self._commit(tok, R, W)
        return tok

    def allgather(self, in_ap, out_ap, R=(), W=()):
        self._wait("pool", self._deps(R, W))
        ins = self.nc.gpsimd.collective_compute(
            "AllGather", ALU.bypass, replica_groups=[[0, 1, 2, 3], [4, 5, 6, 7]],
            ins=[in_ap], outs=[out_ap])
        self.n_ins += 1
        self.cnt[self.cc_key] += 1
        ins.then_inc(self.sems[self.cc_key])
        tok = (self.cc_key, self.cnt[self.cc_key])
        self._commit(tok, R, W)
        return tok

    def fence(self):
        toks = []
        for key, c in self.cnt.items():
            if c > 0:
                toks.append((key, c))
        for e in self.eng:
            self._wait(e, toks, force_same=False)

    def wait_bufs(self, e, bufs):
        toks = []
        for b in bufs:
            toks.append(b.w)
            toks.extend(b.r)
        self._wait(e, toks, force_same=True)


def build_program(nlayers=DEPTH, dbg=None):
    nc = bass.Bass("TRN2", target_bir_lowering=False)
    k = K(nc)
    dbg = dbg or {}
    uid = [0]

    def U(name):
        uid[0] += 1
        return f"{name}_{uid[0]}"

    def din(name, shape, dt=F32):
        return nc.dram_tensor(name, list(shape), dt, kind="ExternalInput").ap()

    xs_in = din("xs_in", [T, D])
    cvecT = din("cvecT", [D, 2])
    mod_w = din("mod_w", [DEPTH, D, 6 * D])
    mod_b = din("mod_b", [DEPTH, 6 * D])
    norm1_w = din("norm1_w", [DEPTH, D])
    norm2_w = din("norm2_w", [DEPTH, D])
    wp = din("wp", [DEPTH, D, NPACK])
    q_norm = din("mla_q_norm", [DEPTH, 256])
    w_uq = din("w_uq", [DEPTH, 256, 768])
    w_uq_sw = din("w_uq_sw", [DEPTH, 256, 768])
    kv_norm = din("mla_kv_norm", [DEPTH, 128])
    w_ukv = din("w_ukv", [DEPTH, 128, 1024])
    ret_logit = din("ret_decay_logit", [DEPTH, 2, 4])
    ret_norm_w = din("ret_norm_w", [DEPTH, 512])
    gwblk = din("gwblk", [DEPTH, 4, 32, 128])
    gbias = din("gbias", [DEPTH, 4, 128])
    gla_norm_w = din("gla_norm_w", [DEPTH, 512])
    w_branch = din("w_branch", [DEPTH, 3, 512, D])
    w_out = din("w_out", [DEPTH, D, D])
    ffn_wg = din("ffn_w_gate", [1, D, DFF])
    ffn_wu = din("ffn_w_up", [1, D, DFF])
    ffn_wd = din("ffn_w_down", [1, DFF, D])
    moe_router = din("moe_router", [1, D, NEXP])
    moe_wg = din("moe_w_gate", [1, NEXP, D, DFF])
    moe_wu = din("moe_w_up", [1, NEXP, D, DFF])
    moe_wd = din("moe_w_down", [1, NEXP, DFF, D])
    final_norm_w = din("final_norm_w", [D])
    c_ropeR = din("c_ropeR", [2, 128, T])
    c_ropeAq = din("c_ropeAq", [2, 32, T])
    c_ropeAk = din("c_ropeAk", [2, 32, T])
    c_rst = din("c_rst", [128, T])
    c_mask = din("c_mask", [2, 128, 128])
    c_ident = din("c_ident", [128, 128])
    c_onehot = din("c_onehot", [128, 4])

    out = nc.dram_tensor("out", [LAT, D], F32, kind="ExternalOutput").ap()
    dbg_out = {}
    for name, (shape, dt) in dbg.items():
        dbg_out[name] = nc.dram_tensor("dbg_" + name, list(shape), dt, kind="ExternalOutput").ap()

    xs = nc.dram_tensor("xs", [T, D], F32).ap()
    uT = nc.dram_tensor("uT", [8, 128, T], BF16).ap()
    modD = nc.dram_tensor("modD", [2, 6 * D], F32).ap()
    xf_in = nc.dram_tensor("xf_in", [NXF, 16, 1024], F32).ap()
    xf_out = nc.dram_tensor("xf_out", [NXF, 64, 1024], F32).ap()
    xb_in = nc.dram_tensor("xb_in", [NXB, 16, LAT], BF16).ap()
    xb_out = nc.dram_tensor("xb_out", [NXB, 64, LAT], BF16).ap()
    b_xs = [Buf(f"xs{t}") for t in range(NT)]
    b_uT = [Buf(f"uT{b}") for b in range(5)]
    b_modD = Buf("modD")
    b_xf_in = [Buf() for _ in range(NXF)]
    b_xf_out = [Buf() for _ in range(NXF)]
    b_xb_in = [Buf() for _ in range(NXB)]
    b_xb_out = [Buf() for _ in range(NXB)]
    b_out = Buf("out")

    PSA = nc.alloc_psum_tensor("psa", [128, 8, 512], F32)
    PS = []
    for i in range(8):
        PS.append((PSA[:, i, :], Buf(f"ps{i}")))

    def salloc(name, shape, dt=F32):
        return nc.alloc_sbuf_tensor(name, list(shape), dt), Buf(name)

    ident, b_ident = salloc("ident", [128, 128])
    identb, b_identb = salloc("identb", [128, 128], BF16)
    onesb, b_onesb = salloc("onesb", [128, 128], BF16)
    maskF, b_maskF = salloc("maskF", [128, 128])
    maskB, b_maskB = salloc("maskB", [128, 128])
    onehot, b_onehot = salloc("onehot", [128, 4])
    eps_t, b_eps = salloc("eps_t", [128, 1])
    one_t, b_one = salloc("one_t", [128, 1])
    modT, b_modT = salloc("modT", [128, 48, 2])
    A1, b_A1 = salloc("A1", [128, 8, 2])
    A2, b_A2 = salloc("A2", [128, 8, 2])
    rstb, b_rstb = salloc("rstb", [128, T], BF16)
    yTd = nc.dram_tensor("yTd", [3, 4, 128, T], BF16).ap()
    lsp_kt2 = nc.dram_tensor("lsp_kt2", [8, 128, T], BF16).ap()
    lsp_vth = nc.dram_tensor("lsp_vth", [8, 128, NT * 128], BF16).ap()
    lsp_eb = nc.dram_tensor("lsp_eb", [8, 128, T], BF16).ap()
    lsp_U2 = nc.dram_tensor("lsp_U2", [8, 128, NT * 128], F32).ap()
    lsp_te = nc.dram_tensor("lsp_te", [8, 128, 2 * NT], F32).ap()
    b_lsp = [Buf(f"lsp{i}") for i in range(8)]
    b_yTd = [Buf(f"yTd{i}") for i in range(3)]

    k.dma("sp", ident[:], c_ident[:, :], W=[b_ident])
    k.op("dve", nc.vector.tensor_copy, identb[:], ident[:], R=[b_ident], W=[b_identb])
    k.op("dve", nc.vector.memset, onesb[:], 1.0, W=[b_onesb])
    k.op("dve", nc.vector.memset, eps_t[:], EPS, W=[b_eps])
    k.op("dve", nc.vector.memset, one_t[:], 1.0, W=[b_one])
    k.dma("sp", maskF[:], c_mask[0], W=[b_maskF])
    k.dma("sp", maskB[:], c_mask[1], W=[b_maskB])
    k.dma("sp", onehot[:], c_onehot[:, :], W=[b_onehot])
    k.dma("pool", rstb[:], c_rst[:, :], W=[b_rstb])
    with nc.sbuf_tensor(U("zinit"), [16, 1024], F32) as zt_:
        b_zt = Buf()
        k.op("dve", nc.vector.memset, zt_[:], 0.0, W=[b_zt])
        k.dma("sp", xf_in[XF_LAM], zt_[:], R=[b_zt], W=[b_xf_in[XF_LAM]])
        k.fence()

    BLKS = [(0, 512), (512, 512), (1024, 512), (1536, 512), (2048, 256)]

    def wview(w2d):
        return w2d.rearrange("(kc p) n -> p kc n", p=128)

    alt = [0]

    def evac_engine():
        alt[0] += 1
        return "act" if alt[0] % 2 else "dve"

    def copy_ps(e, out_ap, in_ap, R, W, scale=None):
        if e == "act":
            if scale is None:
                k.op("act", nc.scalar.copy, out_ap, in_ap, R=R, W=W)
            else:
                k.op("act", nc.scalar.mul, out_ap, in_ap, scale, R=R, W=W)
        else:
            if scale is None:
                k.op("dve", nc.vector.tensor_copy, out_ap, in_ap, R=R, W=W)
            else:
                k.op("dve", nc.vector.tensor_scalar, out_ap, in_ap, scale, None, ALU.mult, R=R, W=W)

    def rsqrt_col(es, name, src_ap, src_buf, scale, n=1, parts=128):
        t1 = es.enter_context(nc.sbuf_tensor(U(name + "_a"), [128, n], F32))
        t2 = es.enter_context(nc.sbuf_tensor(U(name + "_b"), [128, n], F32))
        b1, b2 = Buf(), Buf()
        k.op("dve", nc.vector.tensor_scalar, t1[0:parts, :], src_ap, scale, EPS, ALU.mult, ALU.add,
             R=[src_buf], W=[b1])
        k.op("act", nc.scalar.activation, t2[0:parts, :], t1[0:parts, :], AF.Sqrt, R=[b1], W=[b2])
        k.op("dve", nc.vector.reciprocal, t1[0:parts, :], t2[0:parts, :], R=[b2], W=[b1])
        return t1, b1

    def phase_mod(l):
        with ExitStack() as es:
            def sb(name, shape, dt=F32):
                return es.enter_context(nc.sbuf_tensor(U(name), list(shape), dt)), Buf(name)
            cT, b_cT = sb("cT", [128, 8, 2])
            sc, b_sc = sb("sc", [128, 8, 2])
            sg, b_sg = sb("sg", [128, 8, 2])
            modv, b_modv = sb("modv", [2, 6 * D])
            mb, b_mb = sb("mb", [2, 6 * D])
            wr = Ring([sb(f"mw{i}", [128, 8, 512], BF16) for i in range(4)])
            scb, b_scb = sb("scb", [128, 8, 2], BF16)
            k.dma("sp", cT[:], cvecT.rearrange("(kc p) r -> p kc r", p=128), W=[b_cT])
            k.op("act", nc.scalar.activation, sg[:], cT[:], AF.Sigmoid, R=[b_cT], W=[b_sg])
            k.op("dve", nc.vector.tensor_tensor, sc[:], cT[:], sg[:], ALU.mult, R=[b_cT, b_sg], W=[b_sc])
            k.op("dve", nc.vector.tensor_copy, scb[:], sc[:], R=[b_sc], W=[b_scb])
            k.dma("sp", mb[0:1, :], mod_b[l:l + 1, :], W=[b_mb])
            k.dma("sp", mb[1:2, :], mod_b[l:l + 1, :], W=[b_mb])
            psr = Ring(PS[0:2])
            for n in range(12):
                wt, b_wt = wr.next()
                k.dma("pool", wt[:], wview(mod_w[l])[:, :, n * 512:(n + 1) * 512], W=[b_wt])
                ps, b_ps = psr.next()
                for kc in range(8):
                    k.op("pe", nc.tensor.matmul, ps[0:2, :], scb[:, kc, :], wt[:, kc, :],
                         start=(kc == 0), stop=(kc == 7), R=[b_scb, b_wt], W=[b_ps], sig=(kc == 7))
                k.op("dve", nc.vector.tensor_tensor, modv[:, n * 512:(n + 1) * 512], ps[0:2, :],
                     mb[:, n * 512:(n + 1) * 512], ALU.add, R=[b_ps, b_mb], W=[b_modv])
            k.dma("sp", modD[:, :], modv[:], R=[b_modv], W=[b_modD])
            pst, b_pst = PS[2]
            for j in range(48):
                k.op("pe", nc.tensor.transpose, pst[:, 2 * j:2 * j + 2], modv[0:2, j * 128:(j + 1) * 128],
                     ident[0:2, 0:2], R=[b_modv, b_ident], W=[b_pst], sig=(j == 47))
            k.op("dve", nc.vector.tensor_copy, modT[:].rearrange("p j r -> p (j r)"), pst[:, 0:96],
                 R=[b_pst], W=[b_modT])
            nw, b_nw = sb("nw", [128, 8, 2])
            for (nsrc, joff, At, bA) in ((norm1_w, 8, A1, b_A1), (norm2_w, 32, A2, b_A2)):
                k.dma("sp", nw[:, :, 0], nsrc[l].rearrange("(kc p) -> p kc", p=128), W=[b_nw], allow_slow_non_contiguous=True)
                k.dma("sp", nw[:, :, 1], nsrc[l].rearrange("(kc p) -> p kc", p=128), W=[b_nw], allow_slow_non_contiguous=True)
                k.op("dve", nc.vector.tensor_scalar, At[:], modT[:, joff:joff + 8, :], 1.0, None, ALU.add,
                     R=[b_modT], W=[bA])
                k.op("dve", nc.vector.tensor_tensor, At[:], At[:], nw[:], ALU.mult, R=[b_nw], W=[bA])
        k.fence()

    def phase_norm(l, At, bA, shoff, tiles, xsrc=None):
        xsrc = xs if xsrc is None else xsrc
        with ExitStack() as es:
            def sb(name, shape, dt=F32):
                return es.enter_context(nc.sbuf_tensor(U(name), list(shape), dt)), Buf(name)
            ND = 6
            xr = Ring([sb(f"nx{i}", [128, D]) for i in range(ND)])
            jr = Ring([sb(f"nj{i}", [128, D]) for i in range(2)])
            xnr = Ring([sb(f"nn{i}", [128, D]) for i in range(ND)])
            ur = Ring([sb(f"nu{i}", [128, 8, 128], BF16) for i in range(ND)])
            ssr = Ring([sb(f"ns{i}", [128, 1]) for i in range(ND)])
            psr = Ring([PS[0], PS[1], PS[2], PS[3]])
            xloads = {}

            def issue_x(i_):
                if i_ < len(tiles):
                    t_ = tiles[i_]
                    xt_, b_xt_ = xr.next()
                    k.dma("sp", xt_[:], xsrc[t_ * 128:(t_ + 1) * 128, :], R=[b_xs[t_]], W=[b_xt_])
                    xloads[i_] = (xt_, b_xt_)
            for i_ in range(ND - 1):
                issue_x(i_)
            st = {}

            def front(i_):
                t = tiles[i_]
                issue_x(i_ + ND - 1)
                xt, b_xt = xloads.pop(i_)
                jk, b_jk = jr.next()
                ss, b_ss = ssr.next()
                k.op("act", nc.scalar.activation, jk[:], xt[:], AF.Square, accum_out=ss[:, 0:1],
                     R=[b_xt], W=[b_ss])
                rstd, b_rstd = rsqrt_col(es, f"nr{t}", ss[:, 0:1], b_ss, 1.0 / D)
                xn, b_xn = xnr.next()
                k.op("act", nc.scalar.activation, xn[:], xt[:], AF.Identity, scale=rstd[:, 0:1],
                     R=[b_xt, b_rstd], W=[b_xn])
                pss = []
                for half in range(2):
                    ps, b_ps = psr.next()
                    for j in range(4):
                        kc = half * 4 + j
                        k.op("pe", nc.tensor.transpose, ps[:, j * 128:(j + 1) * 128],
                             xn[:, kc * 128:(kc + 1) * 128], ident[:], R=[b_xn, b_ident], W=[b_ps],
                             sig=(j == 3))
                    pss.append((ps, b_ps))
                st[i_] = pss

            def back(i_):
                t = tiles[i_]
                r = 0 if t < NLT else 1
                pss = st.pop(i_)
                ut, b_ut = ur.next()
                for half in range(2):
                    ps, b_ps = pss[half]
                    for j in range(4):
                        kc = half * 4 + j
                        if j % 2 == 0:
                            k.op("act", nc.scalar.activation, ut[:, kc, :], ps[:, j * 128:(j + 1) * 128],
                                 AF.Identity, scale=At[:, kc, r:r + 1], bias=modT[:, shoff + kc, r:r + 1],
                                 R=[b_ps, bA, b_modT], W=[b_ut])
                        else:
                            k.op("dve", nc.vector.tensor_scalar, ut[:, kc, :], ps[:, j * 128:(j + 1) * 128],
                                 At[:, kc, r:r + 1], modT[:, shoff + kc, r:r + 1], ALU.mult, ALU.add,
                                 R=[b_ps, bA, b_modT], W=[b_ut])
                blk = min(t // 4, 4)
                k.dma("sp", uT[:, :, t * 128:(t + 1) * 128].rearrange("kc p t -> p kc t"), ut[:],
                      R=[b_ut], W=[b_uT[blk]])

            front(0)
            for i_ in range(len(tiles)):
                if i_ + 1 < len(tiles):
                    front(i_ + 1)
                back(i_)
        k.fence()

    class UStream:
        def __init__(self, es, nbuf=2, tag="ub"):
            self.ring = Ring([(es.enter_context(nc.sbuf_tensor(U(f"{tag}{i}"), [128, 8, 512], BF16)), Buf())
                              for i in range(nbuf)])

        def load(self, bi):
            t0, n = BLKS[bi]
            ub, b_ub = self.ring.next()
            k.dma("sp", ub[:, :, 0:n], uT[:, :, t0:t0 + n].rearrange("kc p t -> p kc t"),
                  R=[b_uT[bi]], W=[b_ub])
            return ub, b_ub, t0, n

    def proj_fm(ps, b_ps, w, b_w, c0, m, ub, b_ub, n, prow=0):
        for kc in range(8):
            k.op("pe", nc.tensor.matmul, ps[prow:prow + m, 0:n], w[:, kc, c0:c0 + m], ub[:, kc, 0:n],
                 start=(kc == 0), stop=(kc == 7), R=[b_w, b_ub], W=[b_ps], sig=(kc == 7))

    def load_w(es, name, src3d, shape, q="pool"):
        t = es.enter_context(nc.sbuf_tensor(U(name), list(shape), BF16))
        b = Buf(name)
        k.dma(q, t[:], src3d, W=[b])
        return t, b


    LS = {}
    LG2, b_LG2 = salloc("LG2", [128, 4])
    LAMT, b_LAMT = salloc("LAMT", [128, 8])

    def phase_q(l):
        cqn, b_cqn = LS["cqn"]; ckvn, b_ckvn = LS["ckvn"]; krr, b_krr = LS["krr"]; gzT, b_gzT = LS["gzT"]
        with ExitStack() as es:
            def sb(name, shape, dt=F32):
                return es.enter_context(nc.sbuf_tensor(U(name), list(shape), dt)), Buf(name)
            wA, b_wA = load_w(es, "wA", wview(wp[l])[:, :, PA:PA + PA_N], [128, 8, PA_N])
            qnw, b_qnw = sb("qnw", [128, 2])
            kvnw, b_kvnw = sb("kvnw", [128, 1])
            k.dma("sp", qnw[:], q_norm[l].rearrange("(c p) -> p c", p=128), W=[b_qnw], allow_slow_non_contiguous=True)
            k.dma("sp", kvnw[:], kv_norm[l].rearrange("(c p) -> p c", p=128), W=[b_kvnw], allow_slow_non_contiguous=True)
            ropk, b_ropk = sb("ropk", [32, 2, T])
            k.dma("sp", ropk[:, 0, :], c_ropeAk[0], W=[b_ropk])
            k.dma("sp", ropk[:, 1, :], c_ropeAk[1], W=[b_ropk])
            us = UStream(es)
            sqr = Ring([sb(f"sq{i}", [128, 512], BF16) for i in range(3)])
            rq, b_rq = sb("rq", [128, 512])
            rq2, b_rq2 = sb("rq2", [128, 512])
            tk, b_tk = sb("tk", [32, 512])
            tk2, b_tk2 = sb("tk2", [32, 512])
            for bi in range(5):
                ub, b_ub, t0, n = us.load(bi)
                (p0, bp0), (p1, bp1), (p2, bp2), (p3, bp3) = PS[0], PS[1], PS[2], PS[3]
                proj_fm(p0, bp0, wA, b_wA, 0, 128, ub, b_ub, n)
                proj_fm(p1, bp1, wA, b_wA, 128, 128, ub, b_ub, n)
                proj_fm(p2, bp2, wA, b_wA, 256, 128, ub, b_ub, n)
                s0, bs0 = sqr.next(); s1, bs1 = sqr.next(); s2, bs2 = sqr.next()
                k.op("act", nc.scalar.activation, s0[:, 0:n], p0[:, 0:n], AF.Square, R=[bp0], W=[bs0])
                k.op("act", nc.scalar.activation, s1[:, 0:n], p1[:, 0:n], AF.Square, R=[bp1], W=[bs1])
                k.op("act", nc.scalar.activation, s2[:, 0:n], p2[:, 0:n], AF.Square, R=[bp2], W=[bs2])
                k.op("pe", nc.tensor.matmul, p3[:, 0:n], onesb[:], s0[:, 0:n], start=True, stop=False,
                     R=[b_onesb, bs0], W=[bp3], sig=False)
                k.op("pe", nc.tensor.matmul, p3[:, 0:n], onesb[:], s1[:, 0:n], start=False, stop=True,
                     R=[b_onesb, bs1], W=[bp3])
                k.op("dve", nc.vector.tensor_scalar, rq[:, 0:n], p3[:, 0:n], 1.0 / 256, EPS, ALU.mult, ALU.add,
                     R=[bp3], W=[b_rq])
                k.op("act", nc.scalar.activation, rq2[:, 0:n], rq[:, 0:n], AF.Sqrt, R=[b_rq], W=[b_rq2])
                k.op("dve", nc.vector.reciprocal, rq[:, 0:n], rq2[:, 0:n], R=[b_rq2], W=[b_rq])
                k.op("dve", nc.vector.scalar_tensor_tensor, cqn[:, 0, t0:t0 + n], p0[:, 0:n], qnw[:, 0:1], rq[:, 0:n],
                     ALU.mult, ALU.mult, R=[bp0, b_qnw, b_rq], W=[b_cqn])
                k.op("dve", nc.vector.scalar_tensor_tensor, cqn[:, 1, t0:t0 + n], p1[:, 0:n], qnw[:, 1:2], rq[:, 0:n],
                     ALU.mult, ALU.mult, R=[bp1, b_qnw, b_rq], W=[b_cqn])
                k.op("pe", nc.tensor.matmul, p3[:, 0:n], onesb[:], s2[:, 0:n], start=True, stop=True,
                     R=[b_onesb, bs2], W=[bp3])
                k.op("dve", nc.vector.tensor_scalar, rq[:, 0:n], p3[:, 0:n], 1.0 / 128, EPS, ALU.mult, ALU.add,
                     R=[bp3], W=[b_rq])
                k.op("act", nc.scalar.activation, rq2[:, 0:n], rq[:, 0:n], AF.Sqrt, R=[b_rq], W=[b_rq2])
                k.op("dve", nc.vector.reciprocal, rq[:, 0:n], rq2[:, 0:n], R=[b_rq2], W=[b_rq])
                k.op("dve", nc.vector.scalar_tensor_tensor, ckvn[:, t0:t0 + n], p2[:, 0:n], kvnw[:, 0:1], rq[:, 0:n],
                     ALU.mult, ALU.mult, R=[bp2, b_kvnw, b_rq], W=[b_ckvn])
                (p4, bp4), (p5, bp5), (p6, bp6) = PS[4], PS[5], PS[6]
                proj_fm(p4, bp4, wA, b_wA, 384, 32, ub, b_ub, n)
                proj_fm(p5, bp5, wA, b_wA, 416, 32, ub, b_ub, n)
                proj_fm(p6, bp6, wA, b_wA, 448, 32, ub, b_ub, n)
                k.op("dve", nc.vector.tensor_tensor, tk[:, 0:n], p4[0:32, 0:n], ropk[:, 0, t0:t0 + n], ALU.mult,
                     R=[bp4, b_ropk], W=[b_tk])
                k.op("dve", nc.vector.tensor_tensor, tk2[:, 0:n], p5[0:32, 0:n], ropk[:, 1, t0:t0 + n], ALU.mult,
                     R=[bp5, b_ropk], W=[b_tk2])
                k.op("dve", nc.vector.tensor_tensor, krr[:, t0:t0 + n], tk[:, 0:n], tk2[:, 0:n], ALU.add,
                     R=[b_tk, b_tk2], W=[b_krr])
                k.op("act", nc.scalar.copy, gzT[:, t0:t0 + n], p6[0:32, 0:n], R=[bp6], W=[b_gzT])
            for j in range(8):
                k.dma("sp", xb_in[XB_CKV + j], ckvn[16 * j:16 * j + 16, 0:LAT], R=[b_ckvn], W=[b_xb_in[XB_CKV + j]])
            for j in range(2):
                k.dma("sp", xb_in[XB_KR + j], krr[16 * j:16 * j + 16, 0:LAT], R=[b_krr], W=[b_xb_in[XB_KR + j]])
            for j in range(NXB):
                k.allgather(xb_in[j], xb_out[j], R=[b_xb_in[j]], W=[b_xb_out[j]])
        k.fence()

    def phase_lg(l):
        with ExitStack() as es:
            def sb(name, shape, dt=F32):
                return es.enter_context(nc.sbuf_tensor(U(name), list(shape), dt)), Buf(name)
            lt, b_lt = sb("lt", [128, 4])
            l2, b_l2 = sb("l2", [128, 4])
            k.dma("sp", lt[0:64, :], ret_logit[l, 0:1, :].partition_broadcast(64), W=[b_lt])
            k.dma("sp", lt[64:128, :], ret_logit[l, 1:2, :].partition_broadcast(64), W=[b_lt])
            k.op("act", nc.scalar.activation, l2[:], lt[:], AF.Exp, scale=-1.0, R=[b_lt], W=[b_l2])
            k.op("act", nc.scalar.activation, lt[:], l2[:], AF.Ln, bias=one_t[:, 0:1], R=[b_l2, b_one], W=[b_lt])
            k.op("dve", nc.vector.tensor_scalar, LG2[:], lt[:], -1.0, None, ALU.mult, R=[b_lt], W=[b_LG2])
        k.fence()

    def lm_cols(mix, h):
        if mix == 0:
            return P_RET + h * 512, 512
        return P_GLA + h * 256, 256

    def lm_prefetch_w1(l, mix, h, PW):
        base, ncol = lm_cols(mix, h)
        k.dma("pool", PW["w"][0][:, :, 0:ncol], wview(wp[l])[:, :, base:base + ncol], W=[PW["w"][1]])
        vcol = (P_RV if mix == 0 else P_GV) + h * 128
        k.dma("pool", PW["wv"][0][:], wview(wp[l])[:, :, vcol:vcol + 128], W=[PW["wv"][1]])
        if mix == 1:
            k.dma("pool", PW["gw"][0][:], gwblk[l, h], W=[PW["gw"][1]])
            k.dma("sp", PW["nb"][0][:], gbias[l, h].rearrange("(p o) -> p o", o=1), W=[PW["nb"][1]])

    def lm_prefetch(l, mix, h, PF):
        hm = mix * 4 + h
        base, ncol = lm_cols(mix, h)
        k.dma("pool", PF["w"][0][:, :, 0:ncol], wview(wp[l])[:, :, base:base + ncol], W=[PF["w"][1]])
        gcol = (P_RG if mix == 0 else P_GR) + h * 128
        k.dma("pool", PF["wg"][0][:], wview(wp[l])[:, :, gcol:gcol + 128], W=[PF["wg"][1]])
        k.dma("sp", PF["kt2"][0][:], lsp_kt2[hm], R=[b_lsp[hm]], W=[PF["kt2"][1]])
        k.dma("sp", PF["vth"][0][:].rearrange("p n v -> p (n v)"), lsp_vth[hm], R=[b_lsp[hm]], W=[PF["vth"][1]])
        k.dma("sp", PF["ebT"][0][:], lsp_eb[hm], R=[b_lsp[hm]], W=[PF["ebT"][1]])
        k.dma("sp", PF["U2"][0][:].rearrange("p n v -> p (n v)"), lsp_U2[hm], R=[b_lsp[hm]], W=[PF["U2"][1]])
        k.dma("sp", PF["TE"][0][:], lsp_te[hm], R=[b_lsp[hm]], W=[PF["TE"][1]])

    def linear_mixer(l, mix, h, pass_no, last, PF=None, after_stage1=None):
        hm = mix * 4 + h
        gzT, b_gzT = LS["gzT"]
        with ExitStack() as es:
            def sb(name, shape, dt=F32):
                return es.enter_context(nc.sbuf_tensor(U(name), list(shape), dt)), Buf(name)
            if mix == 0:
                base, ncol = P_RET + h * 512, 512
                cq2, cq2s, ck2, ck2s = 0, 128, 256, 384
            else:
                base, ncol = P_GLA + h * 256, 256
                cq2, ck2 = 0, 128
            w, b_w = PF["w"]
            if pass_no == 1:
                wv, b_wv = PF["wv"]
            if mix == 1 and pass_no == 1:
                gw, b_gw = PF["gw"]
                nb, b_nb = PF["nb"]
                nb2, b_nb2 = sb("nb2", [128, 1])
                k.op("dve", nc.vector.tensor_scalar, nb2[:], nb[:], -1.0, None, ALU.mult, R=[b_nb], W=[b_nb2])
            if pass_no == 1:
                TE, b_TOT = sb("TE", [128, 2 * NT])
                kt2, b_kt2 = sb("kt2", [128, T], BF16)
                vth, b_vth = sb("vth", [128, NT, 128], BF16)
                ebT, b_ebT = sb("ebT", [128, T], BF16)
                U2, _ = sb("U2", [128, NT, 128])
            else:
                TE, b_TOT = PF["TE"]
                kt2, b_kt2 = PF["kt2"]
                vth, b_vth = PF["vth"]
                ebT, b_ebT = PF["ebT"]
                U2, b_ld = PF["U2"]
            TOT, EEND = TE[:, 0:NT], TE[:, NT:2 * NT]
            b_EEND = b_TOT
            b_kdc = [Buf() for _ in range(NT)]
            b_vthc = [Buf() for _ in range(NT)]
            us = UStream(es)
            if pass_no == 1:
                kd, b_kd = sb("kd", [128, T], BF16)
                a2r = Ring([sb(f"a2_{i}", [128, 512]) for i in range(2)])
                csr = Ring([sb(f"cs_{i}", [128, 512]) for i in range(2)])
                B2r = Ring([sb(f"B2_{i}", [128, 512]) for i in range(2)])
                enr = Ring([sb(f"en_{i}", [128, 512]) for i in range(2)])
            else:
                qt2, b_qt2 = sb("qt2", [128, T], BF16)
                b_vthc = [b_vth for _ in range(NT)]
            t1r = Ring([sb(f"lt1{i}", [128, 512]) for i in range(2)])
            t2r = Ring([sb(f"lt2{i}", [128, 512]) for i in range(2)])
            if mix == 0:
                ropr = Ring([sb(f"rop{i}", [128, 2, 512]) for i in range(2)])

            for bi in range(5):
                ub, b_ub, t0, n = us.load(bi)
                nch = n // 128
                ch0 = t0 // 128
                if pass_no == 1:
                    a2, b_a2 = a2r.next(); cs, b_cs = csr.next(); B2, b_B2 = B2r.next()
                    enb, b_enb = enr.next()
                if pass_no == 2:
                    pass
                elif mix == 0:
                    k.op("dve", nc.vector.memset, cs[:, 0:n], 1.0, W=[b_cs])
                    k.op("act", nc.scalar.activation, a2[:, 0:n], cs[:, 0:n], AF.Identity, scale=LG2[:, h:h + 1],
                         R=[b_cs, b_LG2], W=[b_a2])
                elif pass_no == 1:
                    pz, bpz = PS[7]
                    k.op("pe", nc.tensor.matmul, pz[:, 0:n], gw[:], gzT[:, t0:t0 + n], start=True, stop=True,
                         R=[b_gw, b_gzT], W=[bpz])
                    k.op("act", nc.scalar.activation, cs[:, 0:n], pz[:, 0:n], AF.Exp, scale=-1.0,
                         bias=nb2[:, 0:1], R=[bpz, b_nb2], W=[b_cs])
                    k.op("act", nc.scalar.activation, B2[:, 0:n], cs[:, 0:n], AF.Ln, bias=one_t[:, 0:1],
                         R=[b_cs, b_one], W=[b_B2])
                    k.op("dve", nc.vector.tensor_scalar, a2[:, 0:n], B2[:, 0:n], -1.0 / 16.0, None,
                         ALU.mult, R=[b_B2], W=[b_a2])
                if pass_no == 1:
                    k.op("dve", nc.vector.tensor_tensor_scan, cs[:, 0:n], rstb[:, t0:t0 + n], a2[:, 0:n], 0.0, ALU.mult, ALU.add,
                         R=[b_rstb, b_a2], W=[b_cs])
                    k.op("dve", nc.vector.tensor_copy, B2[0:64, 0:n], cs[0:64, 0:n], R=[b_cs], W=[b_B2])
                    k.op("dve", nc.vector.tensor_tensor, B2[64:128, 0:n], a2[64:128, 0:n], cs[64:128, 0:n], ALU.subtract,
                         R=[b_a2, b_cs], W=[b_B2])
                    for c_ in range(nch):
                        k.op("dve", nc.vector.tensor_scalar, B2[64:128, c_ * 128:(c_ + 1) * 128],
                             B2[64:128, c_ * 128:(c_ + 1) * 128], cs[64:128, c_ * 128 + 127:c_ * 128 + 128], None, ALU.add,
                             R=[b_cs], W=[b_B2])
                        k.op("dve", nc.vector.tensor_copy, TOT[:, ch0 + c_:ch0 + c_ + 1], cs[:, c_ * 128 + 127:c_ * 128 + 128],
                             R=[b_cs], W=[b_TOT])
                    k.op("act", nc.scalar.activation, EEND[:, ch0:ch0 + nch], TOT[:, ch0:ch0 + nch], AF.Exp,
                         R=[b_TOT], W=[b_EEND])
                    k.op("act", nc.scalar.activation, enb[:, 0:n], B2[:, 0:n], AF.Exp, scale=-1.0, R=[b_B2], W=[b_enb])
                    k.op("act", nc.scalar.activation, ebT[:, t0:t0 + n], B2[:, 0:n], AF.Exp, R=[b_B2], W=[b_ebT])
                if mix == 0:
                    rop, b_rop = ropr.next()
                    k.dma("sp", rop[:, 0, 0:n], c_ropeR[0, :, t0:t0 + n], W=[b_rop])
                    k.dma("sp", rop[:, 1, 0:n], c_ropeR[1, :, t0:t0 + n], W=[b_rop])

                def roped(pa, bpa, pb, bpb):
                    if mix == 1:
                        return pa[:, 0:n], bpa
                    ta, bta = t1r.next()
                    tb, btb = t2r.next()
                    k.op("dve", nc.vector.tensor_tensor, ta[:, 0:n], pa[:, 0:n], rop[:, 0, 0:n], ALU.mult,
                         R=[bpa, b_rop], W=[bta])
                    k.op("dve", nc.vector.tensor_tensor, tb[:, 0:n], pb[:, 0:n], rop[:, 1, 0:n], ALU.mult,
                         R=[bpb, b_rop], W=[btb])
                    k.op("dve", nc.vector.tensor_tensor, ta[:, 0:n], ta[:, 0:n], tb[:, 0:n], ALU.add,
                         R=[btb], W=[bta])
                    return ta[:, 0:n], bta

                (pa, bpa), (pb, bpb) = (PS[0], PS[1]) if bi % 2 == 0 else (PS[2], PS[3])
                if pass_no == 1:
                    proj_fm(pa, bpa, w, b_w, ck2, 128, ub, b_ub, n)
                    if mix == 0:
                        proj_fm(pb, bpb, w, b_w, ck2s, 128, ub, b_ub, n)
                    kap, bk = roped(pa, bpa, pb, bpb)
                    k.op("dve", nc.vector.tensor_tensor, kt2[:, t0:t0 + n], kap, enb[:, 0:n], ALU.mult,
                         R=[bk, b_enb], W=[b_kt2])
                if pass_no == 2:
                    (pc, bpc), (pd, bpd) = (pa, bpa), (pb, bpb)
                    proj_fm(pc, bpc, w, b_w, cq2, 128, ub, b_ub, n)
                    if mix == 0:
                        proj_fm(pd, bpd, w, b_w, cq2s, 128, ub, b_ub, n)
                    qap, bq = roped(pc, bpc, pd, bpd)
                    k.op("dve", nc.vector.scalar_tensor_tensor, qt2[:, t0:t0 + n], qap, 0.125, ebT[:, t0:t0 + n],
                         ALU.mult, ALU.mult, R=[bq, b_ebT], W=[b_qt2])
                for c_ in (range(nch) if pass_no == 1 else []):
                    n_ = ch0 + c_
                    k.op("act", nc.scalar.activation, kd[:, n_ * 128:(n_ + 1) * 128], kt2[:, n_ * 128:(n_ + 1) * 128],
                         AF.Identity, scale=EEND[:, n_:n_ + 1], R=[b_kt2, b_EEND], W=[b_kdc[n_]])
                    pv, bpv = PS[4 + c_ % 2]
                    for kc in range(8):
                        k.op("pe", nc.tensor.matmul, pv[:, 0:128], ub[:, kc, c_ * 128:(c_ + 1) * 128], wv[:, kc, :],
                             start=(kc == 0), stop=(kc == 7), R=[b_ub, b_wv], W=[bpv], sig=(kc == 7))
                    copy_ps(evac_engine(), vth[:, n_, :], pv[:, 0:128], R=[bpv], W=[b_vthc[n_]])
            if pass_no == 1:
                k.dma("sp", lsp_kt2[hm], kt2[:], R=[b_kt2], W=[b_lsp[hm]])
                k.dma("sp", lsp_vth[hm], vth[:].rearrange("p n v -> p (n v)"), R=b_vthc, W=[b_lsp[hm]])
                k.dma("sp", lsp_eb[hm], ebT[:], R=[b_ebT], W=[b_lsp[hm]])
                k.dma("sp", lsp_te[hm], TE[:], R=[b_TOT], W=[b_lsp[hm]])
            if pass_no == 1:
                k2d, _ = sb("k2d", [128, NT, 128], BF16)
            b_k2dg = [Buf() for _ in range(3)]
            b_U2g = [Buf() for _ in range(5)]
            if pass_no == 2:
                b_U2g = [b_ld]
            if pass_no == 1:
                ptb = PSA[:, 0:3, :].bitcast(BF16)
                for n_ in range(NT):
                    bk = n_ // 8
                    k.op("pe", nc.tensor.transpose, ptb[:, bk, (n_ % 8) * 128:(n_ % 8 + 1) * 128],
                         kd[:, n_ * 128:(n_ + 1) * 128], identb[:], R=[b_kdc[n_], b_identb], W=[PS[bk][1]],
                         sig=(n_ % 8 == 7 or n_ == NT - 1))
                for bk in range(3):
                    nb_ = min(8, NT - 8 * bk)
                    copy_ps(evac_engine(), k2d[:, 8 * bk:8 * bk + nb_, :],
                            ptb[:, bk, 0:nb_ * 128].rearrange("p (a c) -> p a c", c=128), R=[PS[bk][1]], W=[b_k2dg[bk]])
                for n_ in range(NT):
                    bk = 3 + n_ // 4
                    k.op("pe", nc.tensor.matmul, PSA[:, bk, (n_ % 4) * 128:(n_ % 4 + 1) * 128], k2d[:, n_, :], vth[:, n_, :],
                         start=True, stop=True, R=[b_k2dg[n_ // 8], b_vthc[n_]], W=[PS[bk][1]], sig=(n_ % 4 == 3 or n_ == NT - 1))
                for b4 in range(5):
                    nb_ = min(4, NT - 4 * b4)
                    copy_ps(evac_engine(), U2[:, 4 * b4:4 * b4 + nb_, :],
                            PSA[:, 3 + b4, 0:nb_ * 128].rearrange("p (a c) -> p a c", c=128), R=[PS[3 + b4][1]], W=[b_U2g[b4]])
            if pass_no == 1:
                k.dma("sp", lsp_U2[hm], U2[:].rearrange("p n v -> p (n v)"), R=b_U2g, W=[b_lsp[hm]])
            F, Bw = slice(0, 64), slice(64, 128)
            TSEQ, b_TSEQ = sb("TSEQ", [128, 17])
            ESEQ, b_ESEQ = sb("ESEQ", [128, 17])
            k.op("dve", nc.vector.memset, TSEQ[:, 0:1], 0.0, W=[b_TSEQ])
            k.op("dve", nc.vector.tensor_copy, TSEQ[F, 1:17], TOT[F, 0:NLT], R=[b_TOT], W=[b_TSEQ])
            k.op("dve", nc.vector.tensor_copy, TSEQ[Bw, 1:17], TOT[Bw, NLT - 1::-1], R=[b_TOT], W=[b_TSEQ])
            k.op("act", nc.scalar.activation, ESEQ[:], TSEQ[:], AF.Exp, R=[b_TSEQ], W=[b_ESEQ])
            k.op("dve", nc.vector.memset, ESEQ[:, 0:1], 0.0, W=[b_ESEQ])
            DS, b_DS = sb("DS", [128, 128, 17])
            US, b_US = sb("US", [128, 128, 17])
            SS, b_SS = sb("SS", [128, 128, 17])
            k.op("dve", nc.vector.tensor_copy, DS[:], ESEQ[:].unsqueeze(1).broadcast_to([128, 128, 17]),
                 R=[b_ESEQ], W=[b_DS])
            k.op("dve", nc.vector.tensor_copy, US[F, :, 1:17], U2[F, 0:NLT, :].rearrange("p n v -> p v n"),
                 R=b_U2g, W=[b_US])
            k.op("dve", nc.vector.tensor_copy, US[Bw, :, 1:17], U2[Bw, NLT - 1::-1, :].rearrange("p n v -> p v n"),
                 R=b_U2g, W=[b_US])
            St, b_St = sb("St", [128, 128])

            def run_scan():
                k.op("dve", nc.vector.tensor_tensor_scan, SS[:].rearrange("p v t -> p (v t)"),
                     DS[:].rearrange("p v t -> p (v t)"), US[:].rearrange("p v t -> p (v t)"), 0.0, ALU.mult, ALU.add,
                     R=[b_DS, b_US], W=[b_SS])

            if pass_no == 1:
                k.op("dve", nc.vector.memset, US[:, :, 0], 0.0, W=[b_US])
                run_scan()
                k.op("dve", nc.vector.tensor_copy, St[:], SS[:, :, 16], R=[b_SS], W=[b_St])
                k.dma("sp", xf_in[XF_STATE + hm].rearrange("a (b c) -> (a b) c", c=128), St[:],
                      R=[b_St], W=[b_xf_in[XF_STATE + hm]])
                ls, b_ls = sb("ls", [128, 1])
                k.op("dve", nc.vector.reduce_sum, ls[:], TOT[:, 0:NLT], AX.X, R=[b_TOT], W=[b_ls])
                k.op("act", nc.scalar.activation, LAMT[:, hm:hm + 1], ls[:], AF.Exp, R=[b_ls], W=[b_LAMT])
                return
            FR, b_FR = sb("FR", [128, 4, 128])
            LR, b_LR = sb("LR", [128, 4, 8])
            for r in range(4):
                k.dma("sp", FR[:, r, :], xf_out[XF_STATE + hm, r * 16:(r + 1) * 16, :].rearrange("a (b c) -> (a b) c", c=128),
                      R=[b_xf_out[XF_STATE + hm]], W=[b_FR])
                k.dma("sp", LR[:, r, :], xf_out[XF_LAM, r * 16, :].rearrange("(p e) -> p e", e=8),
                      R=[b_xf_out[XF_LAM]], W=[b_LR])
            Rt, b_Rt = sb("Rt", [128, 128])
            Sin, b_Sin = sb("Sin", [128, 128])
            k.op("dve", nc.vector.memset, Sin[:], 0.0, W=[b_Sin])
            k.op("dve", nc.vector.scalar_tensor_tensor, Rt[F, :], U2[F, 16, :], EEND[F, 17:18], U2[F, 17, :],
                 ALU.mult, ALU.add, R=b_U2g + [b_EEND], W=[b_Rt])
            k.op("dve", nc.vector.scalar_tensor_tensor, Rt[Bw, :], U2[Bw, 17, :], EEND[Bw, 16:17], U2[Bw, 16, :],
                 ALU.mult, ALU.add, R=b_U2g + [b_EEND], W=[b_Rt])
            for sl, order in ((F, range(4)), (Bw, range(3, -1, -1))):
                for r in order:
                    k.op("dve", nc.vector.scalar_tensor_tensor, Sin[sl, :], Rt[sl, :], onehot[sl, r:r + 1], Sin[sl, :],
                         ALU.mult, ALU.add, R=[b_Rt, b_onehot], W=[b_Sin])
                    k.op("dve", nc.vector.scalar_tensor_tensor, Rt[sl, :], Rt[sl, :], LR[sl, r, hm:hm + 1], FR[sl, r, :],
                         ALU.mult, ALU.add, R=[b_LR, b_FR], W=[b_Rt])
            k.op("dve", nc.vector.tensor_copy, US[:, :, 0], Sin[:], R=[b_Sin], W=[b_US])
            run_scan()
            S2, b_S2 = sb("S2", [128, NT, 128], BF16)
            k.op("dve", nc.vector.tensor_copy, S2[F, 0:NLT, :], SS[F, :, 0:NLT].rearrange("p v t -> p t v"),
                 R=[b_SS], W=[b_S2])
            k.op("dve", nc.vector.tensor_copy, S2[Bw, 0:NLT, :], SS[Bw, :, NLT - 1::-1].rearrange("p v t -> p t v"),
                 R=[b_SS], W=[b_S2])
            k.op("dve", nc.vector.memset, S2[F, 16, :], 0.0, W=[b_S2])
            k.op("dve", nc.vector.memset, S2[Bw, 17, :], 0.0, W=[b_S2])
            k.op("dve", nc.vector.tensor_copy, S2[F, 17, :], U2[F, 16, :], R=b_U2g, W=[b_S2])
            k.op("dve", nc.vector.tensor_copy, S2[Bw, 16, :], U2[Bw, 17, :], R=b_U2g, W=[b_S2])
            gcol = (P_RG if mix == 0 else P_GR) + h * 128
            wg_, b_wg = PF["wg"]
            nwb, b_nwb = sb("nwb", [128, 128])
            nsrc = ret_norm_w if mix == 0 else gla_norm_w
            k.dma("sp", nwb[:], nsrc[l:l + 1, h * 128:(h + 1) * 128].partition_broadcast(128), W=[b_nwb])
            G, b_G = sb("G", [128, NT, 128], BF16)
            gs, b_gs = sb("gs", [128, 8, 128])
            nchunks = NLT if last else NT
            groups = [(g0, min(8, nchunks - g0)) for g0 in range(0, nchunks, 8)]
            ubc = None
            for gi, (g0, ng) in enumerate(groups):
                bks = [6, 7] if gi % 2 == 0 else [4, 5]
                for c_ in range(ng):
                    n_ = g0 + c_
                    bi = min(n_ // 4, 4)
                    if ubc is None or ubc[0] != bi:
                        ubc = (bi,) + tuple(us.load(bi))
                    _, ub, b_ub, t0, n = ubc
                    tt = n_ - t0 // 128
                    bk = bks[c_ // 4]
                    for kc in range(8):
                        k.op("pe", nc.tensor.matmul, PSA[:, bk, (c_ % 4) * 128:(c_ % 4 + 1) * 128],
                             ub[:, kc, tt * 128:(tt + 1) * 128], wg_[:, kc, :],
                             start=(kc == 0), stop=(kc == 7), R=[b_ub, b_wg], W=[PS[bk][1]], sig=(kc == 7))
                nbk = (ng + 3) // 4
                pgv = PSA[:, bks[0]:bks[0] + nbk, :].rearrange("p b (c v) -> p (b c) v", v=128)[:, 0:ng, :]
                k.op("act", nc.scalar.activation, gs[:, 0:ng, :], pgv, AF.Silu, R=[PS[b_][1] for b_ in bks[:nbk]], W=[b_gs])
                k.op("dve", nc.vector.tensor_tensor, G[:, g0:g0 + ng, :], gs[:, 0:ng, :],
                     nwb[:].unsqueeze(1).broadcast_to([128, ng, 128]), ALU.mult, R=[b_gs, b_nwb], W=[b_G])
            if after_stage1 is not None:
                after_stage1()
            t1, b_t1 = sb("g_t1", [128, 8, 128])
            t2, b_t2 = sb("g_t2", [128, 8, 128])
            PT8, b_PT8 = sb("PT8", [128, 8, 128], BF16)
            osb, b_osb = sb("osb", [128, 8, 128])
            jk, b_jk = sb("ljk", [128, 128])
            stt, b_stt = sb("stt", [128, 6, 8])
            yn8, b_yn8 = sb("yn8", [128, 8, 128])
            yb8, b_yb8 = sb("yb8", [128, 8, 128], BF16)
            yTt, b_yT = sb("yTt", [128, T], BF16)
            b_sttc = [Buf() for _ in range(8)]
            b_osbc = [Buf() for _ in range(8)]
            b_ync = [Buf() for _ in range(8)]
            for (g0, ng) in groups:
                nbk = (ng + 3) // 4
                for c_ in range(ng):
                    c0 = (g0 + c_) * 128
                    k.op("pe", nc.tensor.matmul, PSA[:, c_ // 4, (c_ % 4) * 128:(c_ % 4 + 1) * 128],
                         kt2[0:64, c0:c0 + 128], qt2[0:64, c0:c0 + 128],
                         start=True, stop=True, R=[b_kt2, b_qt2], W=[PS[c_ // 4][1]], sig=(c_ % 4 == 3 or c_ == ng - 1))
                for c_ in range(ng):
                    c0 = (g0 + c_) * 128
                    k.op("pe", nc.tensor.matmul, PSA[:, 2 + c_ // 4, (c_ % 4) * 128:(c_ % 4 + 1) * 128],
                         kt2[64:128, c0:c0 + 128], qt2[64:128, c0:c0 + 128],
                         start=True, stop=True, R=[b_kt2, b_qt2], W=[PS[2 + c_ // 4][1]], sig=(c_ % 4 == 3 or c_ == ng - 1))
                sfv = PSA[:, 0:nbk, :].rearrange("p b (c v) -> p (b c) v", v=128)[:, 0:ng, :]
                sbv = PSA[:, 2:2 + nbk, :].rearrange("p b (c v) -> p (b c) v", v=128)[:, 0:ng, :]
                k.op("dve", nc.vector.tensor_tensor, t1[:, 0:ng, :], sfv, maskF[:].unsqueeze(1).broadcast_to([128, ng, 128]),
                     ALU.mult, R=[PS[b_][1] for b_ in range(nbk)] + [b_maskF], W=[b_t1])
                k.op("dve", nc.vector.tensor_tensor, t2[:, 0:ng, :], sbv, maskB[:].unsqueeze(1).broadcast_to([128, ng, 128]),
                     ALU.mult, R=[PS[2 + b_][1] for b_ in range(nbk)] + [b_maskB], W=[b_t2])
                k.op("dve", nc.vector.tensor_tensor, PT8[:, 0:ng, :], t1[:, 0:ng, :], t2[:, 0:ng, :], ALU.add,
                     R=[b_t1, b_t2], W=[b_PT8])
                for c_ in range(ng):
                    n_ = g0 + c_
                    c0 = n_ * 128
                    bk = 4 + c_ // 4
                    oap = PSA[:, bk, (c_ % 4) * 128:(c_ % 4 + 1) * 128]
                    k.op("pe", nc.tensor.matmul, oap, PT8[:, c_, :], vth[:, n_, :],
                         start=True, stop=False, R=[b_PT8, b_vthc[n_]], W=[PS[bk][1]], sig=False)
                    k.op("pe", nc.tensor.matmul, oap, qt2[:, c0:c0 + 128], S2[:, n_, :],
                         start=False, stop=True, R=[b_qt2, b_S2], W=[PS[bk][1]], sig=(c_ % 4 == 3 or c_ == ng - 1))
                for c_ in range(ng):
                    bk = 4 + c_ // 4
                    oap = PSA[:, bk, (c_ % 4) * 128:(c_ % 4 + 1) * 128]
                    k.op("act", nc.scalar.activation, osb[:, c_, :], oap, AF.Identity, accum_out=stt[:, 0, c_:c_ + 1],
                         R=[PS[bk][1]], W=[b_osbc[c_], b_sttc[c_]])
                    k.op("act", nc.scalar.activation, jk[:], oap, AF.Square, accum_out=stt[:, 1, c_:c_ + 1],
                         R=[PS[bk][1], b_sttc[c_]], W=[])
                k.op("dve", nc.vector.tensor_scalar, stt[:, 0:2, 0:ng], stt[:, 0:2, 0:ng], 1.0 / 128, None, ALU.mult,
                     W=[b_stt] + b_sttc[0:ng])
                if mix == 0:
                    k.op("dve", nc.vector.tensor_tensor, stt[:, 3, 0:ng], stt[:, 0, 0:ng], stt[:, 0, 0:ng], ALU.mult, R=b_sttc[0:ng], W=[b_stt])
                    k.op("dve", nc.vector.tensor_tensor, stt[:, 1, 0:ng], stt[:, 1, 0:ng], stt[:, 3, 0:ng], ALU.subtract, R=b_sttc[0:ng], W=[b_stt])
                k.op("dve", nc.vector.tensor_scalar, stt[:, 3, 0:ng], stt[:, 1, 0:ng], EPS, None, ALU.add, R=b_sttc[0:ng], W=[b_stt])
                k.op("act", nc.scalar.activation, stt[:, 4, 0:ng], stt[:, 3, 0:ng], AF.Sqrt, R=b_sttc[0:ng], W=[b_stt])
                k.op("dve", nc.vector.reciprocal, stt[:, 2, 0:ng], stt[:, 4, 0:ng], R=b_sttc[0:ng], W=[b_stt])
                for c_ in range(ng):
                    if mix == 0:
                        k.op("dve", nc.vector.tensor_scalar, yn8[:, c_, :], osb[:, c_, :], stt[:, 0, c_:c_ + 1], stt[:, 2, c_:c_ + 1],
                             ALU.subtract, ALU.mult, R=[b_osbc[c_], b_stt, b_sttc[c_]], W=[b_ync[c_]])
                    else:
                        k.op("dve", nc.vector.tensor_scalar, yn8[:, c_, :], osb[:, c_, :], stt[:, 2, c_:c_ + 1], None, ALU.mult,
                             R=[b_osbc[c_], b_stt, b_sttc[c_]], W=[b_ync[c_]])
                k.op("dve", nc.vector.tensor_tensor, yb8[:, 0:ng, :], yn8[:, 0:ng, :], G[:, g0:g0 + ng, :], ALU.mult,
                     R=b_ync[0:ng] + [b_G], W=[b_yb8])
                p6b = PSA[:, 6, :].bitcast(BF16)
                for c_ in range(ng):
                    k.op("pe", nc.tensor.transpose, p6b[:, c_ * 128:(c_ + 1) * 128], yb8[:, c_, :], identb[:],
                         R=[b_yb8, b_identb], W=[PS[6][1]], sig=(c_ == ng - 1))
                copy_ps("act", yTt[:, g0 * 128:(g0 + ng) * 128], p6b[:, 0:ng * 128], R=[PS[6][1]], W=[b_yT])
            ntok = LAT if last else T
            k.dma("sp", yTd[1 + mix, h, :, 0:ntok], yTt[:, 0:ntok], R=[b_yT], W=[b_yTd[1 + mix]])

    def phase_linear(l, pass_no, last):
        order = [(mix, h) for mix in range(2) for h in range(4)]
        if pass_no == 1:
            with ExitStack() as pes:
                PWs = []
                for si in range(2):
                    PW = {}
                    for nm, shp, dt in (("w", [128, 8, 512], BF16), ("wv", [128, 8, 128], BF16), ("gw", [32, 128], BF16),
                                        ("nb", [128, 1], F32)):
                        PW[nm] = (pes.enter_context(nc.sbuf_tensor(U(f"pw_{nm}{si}"), shp, dt)), Buf(f"pw_{nm}{si}"))
                    PWs.append(PW)
                lm_prefetch_w1(l, order[0][0], order[0][1], PWs[0])
                for i_, (mix, h) in enumerate(order):
                    if i_ + 1 < len(order):
                        lm_prefetch_w1(l, order[i_ + 1][0], order[i_ + 1][1], PWs[(i_ + 1) % 2])
                    linear_mixer(l, mix, h, pass_no, last, PF=PWs[i_ % 2])
                    k.fence()
        else:
            with ExitStack() as pes:
                PFs = []
                for si in range(2):
                    PF = {}
                    for nm, shp, dt in (("w", [128, 8, 512], BF16), ("wg", [128, 8, 128], BF16), ("kt2", [128, T], BF16),
                                        ("vth", [128, NT, 128], BF16), ("ebT", [128, T], BF16), ("U2", [128, NT, 128], F32),
                                        ("TE", [128, 2 * NT], F32)):
                        PF[nm] = (pes.enter_context(nc.sbuf_tensor(U(f"pf_{nm}{si}"), shp, dt)), Buf(f"pf_{nm}{si}"))
                    PFs.append(PF)
                lm_prefetch(l, order[0][0], order[0][1], PFs[0])
                for i_, (mix, h) in enumerate(order):
                    cb = None
                    if i_ + 1 < len(order):
                        cb = (lambda j=i_ + 1: lm_prefetch(l, order[j][0], order[j][1], PFs[j % 2]))
                    linear_mixer(l, mix, h, pass_no, last, PF=PFs[i_ % 2], after_stage1=cb)
                    k.fence()
            k.fence()
        if pass_no == 1:
            k.dma("sp", xf_in[XF_LAM, 0, :].rearrange("(p e) -> p e", e=8), LAMT[:], R=[b_LAMT], W=[b_xf_in[XF_LAM]])
            for j in range(NXF):
                k.allgather(xf_in[j], xf_out[j], R=[b_xf_in[j]], W=[b_xf_out[j]])
            k.fence()


    G8, b_G8 = salloc("G8", [128, NT, 8])

    def phase_attn(l, last):
        SCL = 96.0 ** -0.5
        cqn, b_cqn = LS["cqn"]; ckvn, b_ckvn = LS["ckvn"]; krr, b_krr = LS["krr"]
        with ExitStack() as es:
            def sb(name, shape, dt=F32):
                return es.enter_context(nc.sbuf_tensor(U(name), list(shape), dt)), Buf(name)
            ckvA, b_ckvA = sb("ckvA", [128, NKEY], BF16)
            k.op("pool", nc.gpsimd.tensor_copy, ckvA[:, 0:CTX], ckvn[:, LAT:T], R=[b_ckvn], W=[b_ckvA])
            for j in range(8):
                k.dma("sp", ckvA[16 * j:16 * j + 16, CTX:NKEY].rearrange("p (r t) -> p r t", r=4),
                      xb_out[XB_CKV + j].rearrange("(r p) t -> p r t", p=16),
                      R=[b_xb_out[XB_CKV + j]], W=[b_ckvA])
            KTs = [sb(f"KT{i}", [96, NKEY], BF16) for i in range(2)]
            Vs = [sb(f"V{i}", [128, NKT, 128], BF16) for i in range(2)]
            for (KT, bKT), (V, bV) in zip(KTs, Vs):
                k.dma("sp", KT[64:96, 0:CTX], krr[:, LAT:T], R=[b_krr], W=[bKT])
                for j in range(2):
                    k.dma("sp", KT[64 + 16 * j:64 + 16 * j + 16, CTX:NKEY].rearrange("p (r t) -> p r t", r=4),
                          xb_out[XB_KR + j].rearrange("(r p) t -> p r t", p=16),
                          R=[b_xb_out[XB_KR + j]], W=[bKT])
                k.op("pool", nc.gpsimd.memset, V[:, :, 64:128], 1.0, W=[bV])
            ropq_r = Ring([sb(f"ropq{i}", [96, 2, 512]) for i in range(2)])
            yat_r = Ring([sb(f"yat{i}", [64, 512], BF16) for i in range(3)])
            QTs = Ring([sb(f"QT{i}", [96, T], BF16) for i in range(2)])
            wq_r = Ring([sb(f"wq{i}", [128, 2, 96], BF16) for i in range(2)])
            wqs_r = Ring([sb(f"wqs{i}", [128, 2, 96], BF16) for i in range(2)])
            wkv_r = Ring([sb(f"wkv{i}", [128, 128], BF16) for i in range(2)])
            t1r = Ring([sb(f"at1{i}", [96, 512]) for i in range(2)])
            t2r = Ring([sb(f"at2{i}", [96, 512]) for i in range(2)])
            Pr = Ring([sb(f"P{i}", [128, 512], BF16) for i in range(4)])
            linv_r = Ring([sb(f"linv{i}", [128, 512]) for i in range(2)])
            sring = Ring([PS[0], PS[1], PS[2], PS[3]])
            oring = Ring([PS[4], PS[5]])
            qblocks = [(q0, 512, list(range(NKT))) for q0 in range(0, LAT, 512)]
            if not last:
                qblocks.append((LAT, CTX, [0, 1]))
            def build_head(h):
                wq, b_wq = wq_r.next(); wqs, b_wqs = wqs_r.next(); wkv, b_wkv = wkv_r.next()
                k.dma("pool", wq[:], w_uq[l].rearrange("(kc p) n -> p kc n", p=128)[:, :, h * 96:(h + 1) * 96], W=[b_wq])
                k.dma("pool", wqs[:], w_uq_sw[l].rearrange("(kc p) n -> p kc n", p=128)[:, :, h * 96:(h + 1) * 96], W=[b_wqs])
                k.dma("pool", wkv[:], w_ukv[l][:, h * 128:(h + 1) * 128], W=[b_wkv])
                QT, bQT = QTs.next()
                KT, bKT = KTs[h % 2]
                V, bV = Vs[h % 2]
                built[h] = (QT, bQT, KT, bKT, V, bV)
                yield
                blks = BLKS[:4] if last else BLKS
                for bi, (t0, n) in enumerate(blks):
                    (pa, bpa), (pb, bpb) = PS[6], PS[7]
                    for (pp, bpp, ww, bww) in ((pa, bpa, wq, b_wq), (pb, bpb, wqs, b_wqs)):
                        for kc in range(2):
                            k.op("pe", nc.tensor.matmul, pp[0:96, 0:n], ww[:, kc, :], cqn[:, kc, t0:t0 + n],
                                 start=(kc == 0), stop=(kc == 1), R=[bww, b_cqn], W=[bpp], sig=(kc == 1))
                    k.op("dve", nc.vector.tensor_scalar, QT[0:64, t0:t0 + n], pa[0:64, 0:n], SCL, None, ALU.mult,
                         R=[bpa], W=[bQT])
                    ta, bta = t1r.next(); tb, btb = t2r.next()
                    ropq, b_ropq = ropq_r.next()
                    k.dma("sp", ropq[64:96, 0, 0:n], c_ropeAq[0, :, t0:t0 + n], W=[b_ropq])
                    k.dma("sp", ropq[64:96, 1, 0:n], c_ropeAq[1, :, t0:t0 + n], W=[b_ropq])
                    k.op("dve", nc.vector.tensor_tensor, ta[64:96, 0:n], pa[64:96, 0:n], ropq[64:96, 0, 0:n],
                         ALU.mult, R=[bpa, b_ropq], W=[bta])
                    k.op("dve", nc.vector.tensor_tensor, tb[64:96, 0:n], pb[64:96, 0:n], ropq[64:96, 1, 0:n],
                         ALU.mult, R=[bpb, b_ropq], W=[btb])
                    k.op("dve", nc.vector.tensor_tensor, QT[64:96, t0:t0 + n], ta[64:96, 0:n], tb[64:96, 0:n],
                         ALU.add, R=[bta, btb], W=[bQT])
                    yield
                for kb in range((NKEY + 511) // 512):
                    c0 = kb * 512
                    n = min(512, NKEY - c0)
                    pk, bpk = PS[6 + kb % 2]
                    k.op("pe", nc.tensor.matmul, pk[0:64, 0:n], wkv[:, 0:64], ckvA[:, c0:c0 + n], start=True, stop=True,
                         R=[b_wkv, b_ckvA], W=[bpk])
                    copy_ps("dve", KT[0:64, c0:c0 + n], pk[0:64, 0:n], R=[bpk], W=[bKT])
                    yield
                for g0 in range(0, NKT, 8):
                    gn = min(8, NKT - g0)
                    pv, bpv = PS[7]
                    for i_ in range(gn):
                        kt = g0 + i_
                        k.op("pe", nc.tensor.matmul, pv[:, i_ * 64:(i_ + 1) * 64], ckvA[:, kt * 128:(kt + 1) * 128],
                             wkv[:, 64:128], start=True, stop=True, R=[b_ckvA, b_wkv], W=[bpv], sig=(i_ == gn - 1))
                    copy_ps("dve", V[:, g0:g0 + gn, 0:64],
                            pv[:, 0:gn * 64].rearrange("p (a b) -> p a b", b=64), R=[bpv], W=[bV])
                    yield

            built = {}
            for _ in build_head(0):
                pass
            for h in range(8):
                QT, bQT, KT, bKT, V, bV = built[h]
                gen = build_head(h + 1) if h + 1 < 8 else iter(())
                step_no = 0
                for (q0, nq, ktiles) in qblocks:
                    po, bpo = oring.next()

                    def issue_s(kt):
                        ps_, bps_ = sring.next()
                        k.op("pe", nc.tensor.matmul, ps_[:, 0:nq], KT[0:96, kt * 128:(kt + 1) * 128], QT[0:96, q0:q0 + nq],
                             start=True, stop=True, R=[bKT, bQT], W=[bps_])
                        return ps_, bps_
                    LA = 3
                    pend = [issue_s(kt_) for kt_ in ktiles[:LA]]
                    for i_, kt in enumerate(ktiles):
                        if i_ + LA < len(ktiles):
                            pend.append(issue_s(ktiles[i_ + LA]))
                        cur = pend.pop(0)
                        P, bP = Pr.next()
                        k.op("act", nc.scalar.activation, P[:, 0:nq], cur[0][:, 0:nq], AF.Exp, R=[cur[1]], W=[bP])
                        lastk = (i_ == len(ktiles) - 1)
                        k.op("pe", nc.tensor.matmul, po[:, 0:nq], V[:, kt, :], P[:, 0:nq], start=(i_ == 0), stop=lastk,
                             R=[bV, bP], W=[bpo], sig=True)
                        step_no += 1
                        if step_no % 6 == 0:
                            next(gen, None)
                    linv, b_linv = linv_r.next()
                    k.op("dve", nc.vector.reciprocal, linv[64:128, 0:nq], po[64:128, 0:nq], R=[bpo], W=[b_linv])
                    r0 = (h % 2) * 64
                    yat, b_yat = yat_r.next()
                    k.op("dve", nc.vector.tensor_tensor, yat[:, 0:nq], po[0:64, 0:nq],
                         linv[64:128, 0:nq], ALU.mult, R=[bpo, b_linv], W=[b_yat])
                    k.dma("sp", yTd[0, h // 2, r0:r0 + 64, q0:q0 + nq], yat[:, 0:nq], R=[b_yat], W=[b_yTd[0]])
                for _ in gen:
                    pass
        k.fence()

    def phase_merge(l, last):
        blks = list(enumerate(BLKS[:4] if last else BLKS))
        with ExitStack() as es:
            def sb(name, shape, dt=F32):
                return es.enter_context(nc.sbuf_tensor(U(name), list(shape), dt)), Buf(name)
            zT, b_zT = sb("zT", [128, 8, T], BF16)
            wo, b_wo = load_w(es, "wo", wview(w_out[l]), [128, 8, D])
            us = UStream(es)
            yb_r = Ring([sb(f"myb{i}", [128, 3, 4, 512], BF16) for i in range(2)])
            g1, b_g1 = sb("g1", [128, 2, D])
            for r in range(2):
                k.dma("sp", g1[:, r, :], modD[r:r + 1, 2 * D:3 * D].partition_broadcast(128), R=[b_modD], W=[b_g1])
            wg_r = Ring([sb(f"mwg{i}", [128, 8, 3, 128], BF16) for i in range(2)])
            wb_r = Ring([sb(f"mwb{i}", [128, 3, 4, 128], BF16) for i in range(2)])
            gj_r = Ring([sb(f"gj{i}", [128, 512]) for i in range(2)])
            za_r = Ring([sb(f"za{i}", [128, 512]) for i in range(2)])
            zt_r = Ring([sb(f"zt{i}", [128, 512]) for i in range(2)])
            pgr = Ring([PS[0], PS[1], PS[2]])
            pzr = Ring([PS[3], PS[4], PS[5]])
            for c in range(8):
                wg3, b_wg3 = wg_r.next(); wb3, b_wb3 = wb_r.next()
                for j in range(3):
                    col = P_BG + j * 1024 + c * 128
                    k.dma("pool", wg3[:, :, j, :], wview(wp[l])[:, :, col:col + 128], W=[b_wg3])
                    k.dma("pool", wb3[:, j, :, :], w_branch[l, j].rearrange("(k4 p) n -> p k4 n", p=128)[:, :, c * 128:(c + 1) * 128],
                          W=[b_wb3])
                for bi, (t0, n) in blks:
                    ub, b_ub, _, _ = us.load(bi)
                    yb3, b_yb3 = yb_r.next()
                    for j in range(3):
                        k.dma("sp", yb3[:, j, :, 0:n], yTd[j, :, :, t0:t0 + n].rearrange("k p t -> p k t"),
                              R=[b_yTd[j]], W=[b_yb3])
                    za, bza = za_r.next()
                    for j in range(3):
                        pg, bpg = pgr.next()
                        for kc in range(8):
                            k.op("pe", nc.tensor.matmul, pg[:, 0:n], wg3[:, kc, j, :], ub[:, kc, 0:n],
                                 start=(kc == 0), stop=(kc == 7), R=[b_wg3, b_ub], W=[bpg], sig=(kc == 7))
                        gj, bgj = gj_r.next()
                        k.op("act", nc.scalar.activation, gj[:, 0:n], pg[:, 0:n], AF.Sigmoid, R=[bpg], W=[bgj])
                        pz, bpz = pzr.next()
                        for k4 in range(4):
                            k.op("pe", nc.tensor.matmul, pz[:, 0:n], wb3[:, j, k4, :], yb3[:, j, k4, 0:n],
                                 start=(k4 == 0), stop=(k4 == 3), R=[b_wb3, b_yb3], W=[bpz], sig=(k4 == 3))
                        if j == 0:
                            k.op("dve", nc.vector.tensor_tensor, za[:, 0:n], pz[:, 0:n], gj[:, 0:n], ALU.mult,
                                 R=[bpz, bgj], W=[bza])
                        else:
                            zt, bzt = zt_r.next()
                            k.op("dve", nc.vector.tensor_tensor, zt[:, 0:n], pz[:, 0:n], gj[:, 0:n], ALU.mult,
                                 R=[bpz, bgj], W=[bzt])
                            if j == 1:
                                k.op("dve", nc.vector.tensor_tensor, za[:, 0:n], za[:, 0:n], zt[:, 0:n], ALU.add,
                                     R=[bzt], W=[bza])
                            else:
                                k.op("dve", nc.vector.tensor_tensor, zT[:, c, t0:t0 + n], za[:, 0:n], zt[:, 0:n], ALU.add,
                                     R=[bza, bzt], W=[b_zT])
            xr = Ring([sb(f"mx{i}", [128, D]) for i in range(4)])
            tmr = Ring([sb(f"mt{i}", [128, 512]) for i in range(2)])
            pyr = Ring([PS[6], PS[7]])
            tiles = list(range(NLT)) if last else list(range(NT))
            xloads = {}

            def issue_x(i_):
                if i_ < len(tiles):
                    t_ = tiles[i_]
                    xt_, b_xt_ = xr.next()
                    k.dma("sp", xt_[:], (xs_in if l == 0 else xs)[t_ * 128:(t_ + 1) * 128, :], R=[b_xs[t_]], W=[b_xt_])
                    xloads[i_] = (xt_, b_xt_)
            issue_x(0); issue_x(1)
            for i_, t in enumerate(tiles):
                r = 0 if t < NLT else 1
                issue_x(i_ + 2)
                xt, b_xt = xloads.pop(i_)
                for half in range(2):
                    py, bpy = pyr.next()
                    for kc in range(8):
                        k.op("pe", nc.tensor.matmul, py[:, :], zT[:, kc, t * 128:(t + 1) * 128],
                             wo[:, kc, half * 512:(half + 1) * 512], start=(kc == 0), stop=(kc == 7),
                             R=[b_zT, b_wo], W=[bpy], sig=(kc == 7))
                    tm, btm = tmr.next()
                    k.op("dve", nc.vector.tensor_tensor, tm[:], py[:, :], g1[:, r, half * 512:(half + 1) * 512], ALU.mult,
                         R=[bpy, b_g1], W=[btm])
                    k.op("dve", nc.vector.tensor_tensor, xt[:, half * 512:(half + 1) * 512], xt[:, half * 512:(half + 1) * 512],
                         tm[:], ALU.add, R=[btm], W=[b_xt])
                k.dma("sp", xs[t * 128:(t + 1) * 128, :], xt[:], R=[b_xt], W=[b_xs[t]])
        k.fence()

    def phase_router(i, tiles):
        with ExitStack() as es:
            def sb(name, shape, dt=F32):
                return es.enter_context(nc.sbuf_tensor(U(name), list(shape), dt)), Buf(name)
            rw, b_rw = load_w(es, "rw", wview(moe_router[i]), [128, 8, NEXP])
            us = UStream(es)
            lg_r = Ring([sb(f"rl{i_}", [128, 8]) for i_ in range(2)])
            m8_r = Ring([sb(f"rm{i_}", [128, 8]) for i_ in range(2)])
            ex_r = Ring([sb(f"re{i_}", [128, 8]) for i_ in range(2)])
            mk_r = Ring([sb(f"rk{i_}", [128, 8]) for i_ in range(2)])
            sc_r = Ring([sb(f"rs{i_}", [128, 4]) for i_ in range(2)])
            ubc = None
            for t in tiles:
                bi = min(t // 4, 4)
                if ubc is None or ubc[0] != bi:
                    ubc = (bi,) + tuple(us.load(bi))
                _, ub, b_ub, t0, n = ubc
                tt = t - t0 // 128
                pl, bpl = PS[t % 2]
                for kc in range(8):
                    k.op("pe", nc.tensor.matmul, pl[:, 0:NEXP], ub[:, kc, tt * 128:(tt + 1) * 128], rw[:, kc, :],
                         start=(kc == 0), stop=(kc == 7), R=[b_ub, b_rw], W=[bpl], sig=(kc == 7))
                lg, blg = lg_r.next(); m8, bm8 = m8_r.next(); ex, bex = ex_r.next(); mk, bmk = mk_r.next()
                sc_, bsc = sc_r.next()
                k.op("dve", nc.vector.tensor_copy, lg[:], pl[:, 0:NEXP], R=[bpl], W=[blg])
                k.op("dve", nc.vector.max, m8[:], lg[:], R=[blg], W=[bm8])
                k.op("dve", nc.vector.tensor_scalar, mk[:], lg[:], m8[:, 1:2], None, ALU.is_ge, R=[blg, bm8], W=[bmk])
                k.op("dve", nc.vector.tensor_scalar, sc_[:, 0:1], m8[:, 0:1], -1.0, None, ALU.mult, R=[bm8], W=[bsc])
                k.op("act", nc.scalar.activation, ex[:], lg[:], AF.Exp, bias=sc_[:, 0:1], R=[blg, bsc], W=[bex])
                k.op("dve", nc.vector.tensor_tensor, ex[:], ex[:], mk[:], ALU.mult, R=[bmk], W=[bex])
                k.op("dve", nc.vector.reduce_sum, sc_[:, 1:2], ex[:], AX.X, R=[bex], W=[bsc])
                k.op("dve", nc.vector.reciprocal, sc_[:, 2:3], sc_[:, 1:2], W=[bsc])
                k.op("dve", nc.vector.tensor_scalar, G8[:, t, :], ex[:], sc_[:, 2:3], None, ALU.mult, R=[bex, bsc], W=[b_G8])
        k.fence()

    def phase_ffn(l, last):
        moe = (l % 2 == 1)
        i = l // 2
        nexp = NEXP if moe else 1
        groups = [(0, 1024), (1024, 1024)] if last else [(0, 1152), (1152, 1152)]
        for (g0, gn) in groups:
            with ExitStack() as es:
                def sb(name, shape, dt=F32):
                    return es.enter_context(nc.sbuf_tensor(U(name), list(shape), dt)), Buf(name)
                vTg, b_vTg = sb("vTg", [128, 8, gn], BF16)
                ublks = sorted(set(min(t_ // 4, 4) for t_ in range(g0 // 128, (g0 + gn) // 128)))
                k.dma("sp", vTg[:], uT[:, :, g0:g0 + gn].rearrange("kc p t -> p kc t"),
                      R=[b_uT[b_] for b_ in ublks], W=[b_vTg])
                hT, b_hT = sb("hT", [128, NFF, gn], BF16)
                acc, b_acc = sb("acc", [128, gn // 128, D])
                wd, b_wd = sb("wd", [128, NFF, D], BF16)
                wt_r = Ring([sb(f"wgu{i_}", [128, 8, 2, 256], BF16) for i_ in range(2)])
                sg_r = Ring([sb(f"sg{i_}", [128, 512]) for i_ in range(3)])
                pgr = Ring([PS[0], PS[1]])
                pur = Ring([PS[2], PS[3]])
                pyr = Ring([PS[4], PS[5], PS[6], PS[7]])
                bsz = 512 if gn % 512 == 0 else 384
                nblks = [(c0, min(bsz, gn - c0)) for c0 in range(0, gn, bsz)]
                def wsrc(e):
                    if moe:
                        return moe_wg[i, e], moe_wu[i, e], moe_wd[i, e]
                    return ffn_wg[i], ffn_wu[i], ffn_wd[i]
                tasks = [(e, fc2) for e in range(nexp) for fc2 in range(NFF // 2)]
                loaded = {}

                def issue_load(ti):
                    if ti >= len(tasks) or ti in loaded:
                        return
                    e_, fc2_ = tasks[ti]
                    Wg_, Wu_, _ = wsrc(e_)
                    wt_, b_wt_ = wt_r.next()
                    k.dma("pool", wt_[:, :, 0, :], wview(Wg_)[:, :, fc2_ * 256:(fc2_ + 1) * 256], W=[b_wt_])
                    k.dma("pool", wt_[:, :, 1, :], wview(Wu_)[:, :, fc2_ * 256:(fc2_ + 1) * 256], W=[b_wt_])
                    loaded[ti] = (wt_, b_wt_)
                issue_load(0)
                for e in range(nexp):
                    Wg, Wu, Wd = wsrc(e)
                    for fc2 in range(NFF // 2):
                        ti = e * (NFF // 2) + fc2
                        issue_load(ti)
                        issue_load(ti + 1)
                        if fc2 == 0:
                            k.dma("pool", wd[:], Wd.rearrange("(f p) n -> p f n", p=128), W=[b_wd])
                        wt, b_wt = loaded.pop(ti)
                        for sub in range(2):
                            fc = fc2 * 2 + sub
                            for (c0, n) in nblks:
                                pg, bpg = pgr.next(); pu, bpu = pur.next()
                                for kc in range(8):
                                    k.op("pe", nc.tensor.matmul, pg[:, 0:n], wt[:, kc, 0, sub * 128:(sub + 1) * 128],
                                         vTg[:, kc, c0:c0 + n], start=(kc == 0), stop=(kc == 7),
                                         R=[b_wt, b_vTg], W=[bpg], sig=(kc == 7))
                                for kc in range(8):
                                    k.op("pe", nc.tensor.matmul, pu[:, 0:n], wt[:, kc, 1, sub * 128:(sub + 1) * 128],
                                         vTg[:, kc, c0:c0 + n], start=(kc == 0), stop=(kc == 7),
                                         R=[b_wt, b_vTg], W=[bpu], sig=(kc == 7))
                                sg, bsg = sg_r.next()
                                k.op("act", nc.scalar.activation, sg[:, 0:n], pg[:, 0:n], AF.Silu, R=[bpg], W=[bsg])
                                k.op("dve", nc.vector.tensor_tensor, hT[:, fc, c0:c0 + n], sg[:, 0:n], pu[:, 0:n], ALU.mult,
                                     R=[bsg, bpu], W=[b_hT])
                    for tt in range(gn // 128):
                        t = g0 // 128 + tt
                        for half in range(2):
                            py, bpy = pyr.next()
                            for fc in range(NFF):
                                k.op("pe", nc.tensor.matmul, py[:, :], hT[:, fc, tt * 128:(tt + 1) * 128],
                                     wd[:, fc, half * 512:(half + 1) * 512], start=(fc == 0), stop=(fc == NFF - 1),
                                     R=[b_hT, b_wd], W=[bpy], sig=(fc == NFF - 1))
                            dst = acc[:, tt, half * 512:(half + 1) * 512]
                            if not moe:
                                copy_ps(evac_engine(), dst, py[:, :], R=[bpy], W=[b_acc])
                            elif e == 0:
                                k.op("dve", nc.vector.tensor_scalar, dst, py[:, :], G8[:, t, e:e + 1], None, ALU.mult,
                                     R=[bpy, b_G8], W=[b_acc])
                            else:
                                k.op("dve", nc.vector.scalar_tensor_tensor, dst, py[:, :], G8[:, t, e:e + 1], dst,
                                     ALU.mult, ALU.add, R=[bpy, b_G8], W=[b_acc])
                xr = Ring([sb(f"fx{i_}", [128, D]) for i_ in range(3)])
                jr = Ring([sb(f"fj{i_}", [128, D]) for i_ in range(2)])
                ssr = Ring([sb(f"fs{i_}", [128, 1]) for i_ in range(2)])
                g2, b_g2 = sb("g2", [128, 2, D])
                for rg_ in range(1 if last else 2):
                    k.dma("sp", g2[:, rg_, :], modD[rg_:rg_ + 1, 5 * D:6 * D].partition_broadcast(128), R=[b_modD], W=[b_g2])
                if last:
                    fnw, b_fnw = sb("fnw", [128, D])
                    k.dma("sp", fnw[:], final_norm_w.rearrange("(o d) -> o d", o=1).partition_broadcast(128), W=[b_fnw])
                xloads = {}

                def issue_x(tt_):
                    if tt_ < gn // 128:
                        t_ = g0 // 128 + tt_
                        xt_, b_xt_ = xr.next()
                        k.dma("sp", xt_[:], xs[t_ * 128:(t_ + 1) * 128, :], R=[b_xs[t_]], W=[b_xt_])
                        xloads[tt_] = (xt_, b_xt_)
                issue_x(0); issue_x(1)
                for tt in range(gn // 128):
                    t = g0 // 128 + tt
                    r = 0 if t < NLT else 1
                    issue_x(tt + 2)
                    xt, b_xt = xloads.pop(tt)
                    k.op("dve", nc.vector.tensor_tensor, acc[:, tt, :], acc[:, tt, :], g2[:, r, :], ALU.mult,
                         R=[b_g2], W=[b_acc])
                    k.op("dve", nc.vector.tensor_tensor, xt[:], xt[:], acc[:, tt, :], ALU.add, R=[b_acc], W=[b_xt])
                    if not last:
                        k.dma("sp", xs[t * 128:(t + 1) * 128, :], xt[:], R=[b_xt], W=[b_xs[t]])
                    else:
                        jk, b_jk = jr.next(); ss, b_ss = ssr.next()
                        k.op("act", nc.scalar.activation, jk[:], xt[:], AF.Square, accum_out=ss[:, 0:1],
                             R=[b_xt], W=[b_jk, b_ss])
                        rstd, b_rstd = rsqrt_col(es, f"fr{t}", ss[:, 0:1], b_ss, 1.0 / D)
                        k.op("dve", nc.vector.scalar_tensor_tensor, jk[:], xt[:], rstd[:, 0:1], fnw[:], ALU.mult, ALU.mult,
                             R=[b_xt, b_rstd, b_fnw], W=[b_jk])
                        k.dma("sp", out[t * 128:(t + 1) * 128, :], jk[:], R=[b_jk], W=[b_out])
            k.fence()

    stop = dbg.get("_stop") if isinstance(dbg, dict) else None

    def tap(name, src_ap, bufs):
        if name in dbg_out:
            k.dma("sp", dbg_out[name], src_ap, R=bufs, W=[b_out])

    for l in range(nlayers):
        last = (l == DEPTH - 1)
        alltiles = list(range(NT))
        phase_mod(l)
        phase_norm(l, A1, b_A1, 0, alltiles, xsrc=(xs_in if l == 0 else xs))
        phase_lg(l)
        with ExitStack() as les:
            for nm, shp in (("cqn", [128, 2, T]), ("ckvn", [128, T]), ("krr", [32, T]), ("gzT", [32, T])):
                LS[nm] = (les.enter_context(nc.sbuf_tensor(U(nm), shp, BF16)), Buf(nm))
            phase_q(l)
            phase_linear(l, 1, last)
            phase_linear(l, 2, last)
            phase_attn(l, last)
        k.fence()
        phase_merge(l, last)
        ftiles = list(range(NLT)) if last else alltiles
        phase_norm(l, A2, b_A2, 24, ftiles)
        if l % 2 == 1:
            phase_router(l // 2, ftiles)
        phase_ffn(l, last)
    if nlayers < DEPTH:
        for t in range(NLT):
            k.dma("sp", out[t * 128:(t + 1) * 128, :], xs[t * 128:(t + 1) * 128, :], R=[b_xs[t]], W=[b_out])
    k.wait_bufs("sp", [b_out])
    return nc, k


IN_SPLITS = (256, 128, 32, 256, 256, 512, 512, 256, 256, 512, 512, 32, 3072)
OFFS = np.concatenate([[0], np.cumsum(IN_SPLITS)]).astype(int)
(O_CQ, O_CKV, O_KR, O_RQ, O_RK, O_RV, O_RG, O_GQ, O_GK, O_GV, O_GR, O_GZ, O_BG) = OFFS[:13]


def _partner(n, half):
    idx = np.arange(n)
    return np.where(idx % (2 * half) < half, idx + half, idx - half)


def _pack_w_in(w_in_l):
    cols = []
    pa = _partner(32, 8)
    cols.append(np.arange(O_CQ, O_CQ + 256))
    cols.append(np.arange(O_CKV, O_CKV + 128))
    cols.append(np.arange(O_KR, O_KR + 32))
    cols.append(O_KR + pa)
    cols.append(np.arange(O_GZ, O_GZ + 32))
    pr = _partner(64, 32)
    for h in range(4):
        q = O_RQ + h * 64 + np.arange(64)
        qs = O_RQ + h * 64 + pr
        kk = O_RK + h * 64 + np.arange(64)
        ks = O_RK + h * 64 + pr
        cols += [q, q, qs, qs, kk, kk, ks, ks]
    for h in range(4):
        q = O_GQ + h * 64 + np.arange(64)
        kk = O_GK + h * 64 + np.arange(64)
        cols += [q, q, kk, kk]
    cols.append(np.arange(O_RV, O_RV + 512))
    cols.append(np.arange(O_GV, O_GV + 512))
    cols.append(np.arange(O_RG, O_RG + 512))
    cols.append(np.arange(O_GR, O_GR + 512))
    cols.append(np.arange(O_BG, O_BG + 3072))
    cols = np.concatenate(cols)
    assert cols.shape[0] == NPACK
    return np.ascontiguousarray(w_in_l[:, cols])


def _rope_tables(pos, half, signed_rows):
    inv = 10000.0 ** (-np.arange(half, dtype=np.float32) / half)
    ang = pos.astype(np.float32)[None, :] * inv[:, None]
    cos = np.cos(ang).astype(np.float32)
    sin = np.sin(ang).astype(np.float32)
    return np.concatenate([cos, cos], 0), np.concatenate([-sin, sin], 0)


def _consts(q):
    pos = q * LAT + np.arange(LAT)
    c, s = _rope_tables(pos, 32, True)
    ropeR = np.zeros((2, 128, T), np.float32)
    ropeR[0, :, :LAT] = np.concatenate([c, c], 0)
    ropeR[1, :, :LAT] = np.concatenate([s, s], 0)
    ropeR[0, :, LAT:] = 1.0
    cr, sr = _rope_tables(pos // 64, 8, True)
    cc, sc = _rope_tables(pos % 64, 8, True)
    ak = np.zeros((2, 32, T), np.float32)
    ak[0, :, :LAT] = np.concatenate([cr, cc], 0)
    ak[1, :, :LAT] = np.concatenate([sr, sc], 0)
    ak[0, :, LAT:] = 1.0
    aq = (ak * np.float32(96.0 ** -0.5)).astype(np.float32)
    rst = np.ones((128, T), np.float32)
    rst[:, ::128] = 0.0
    j = np.arange(128)[:, None]
    i = np.arange(128)[None, :]
    mask = np.stack([(i >= j), (j >= i)]).astype(np.float32)
    onehot = np.zeros((128, 4), np.float32)
    onehot[:, q] = 1.0
    return dict(c_ropeR=ropeR, c_ropeAq=aq, c_ropeAk=ak, c_rst=rst, c_mask=mask,
                c_ident=np.eye(128, dtype=np.float32), c_onehot=onehot)


def make_in_maps(inp):
    f = lambda a: np.ascontiguousarray(np.asarray(a, dtype=np.float32))
    x, c, ctx, c_ctx = f(inp["x"]), f(inp["c"]), f(inp["ctx"]), f(inp["c_ctx"])
    w_in = f(inp["w_in"])
    wp_ = np.stack([_pack_w_in(w_in[l]) for l in range(DEPTH)])
    w_uq = f(inp["mla_w_uq"])
    pa = _partner(32, 8)
    cols = np.arange(768)
    for h in range(8):
        cols[h * 96 + 64:h * 96 + 96] = h * 96 + 64 + pa
    w_uq_sw = np.ascontiguousarray(w_uq[:, :, cols])
    gw = f(inp["gla_w_gate"])
    gb = f(inp["gla_b_gate"])
    gwblk = np.zeros((DEPTH, 4, 32, 128), np.float32)
    gbias = np.zeros((DEPTH, 4, 128), np.float32)
    for h in range(4):
        gwblk[:, h, 0:16, 0:64] = gw[:, 0, :, h * 64:(h + 1) * 64]
        gwblk[:, h, 16:32, 64:128] = gw[:, 1, :, h * 64:(h + 1) * 64]
        gbias[:, h, 0:64] = gb[:, 0, h * 64:(h + 1) * 64]
        gbias[:, h, 64:128] = gb[:, 1, h * 64:(h + 1) * 64]
    shared = dict(
        mod_w=f(inp["mod_w"]), mod_b=f(inp["mod_b"]), norm1_w=f(inp["norm1_w"]), norm2_w=f(inp["norm2_w"]),
        wp=wp_, mla_q_norm=f(inp["mla_q_norm"]), w_uq=w_uq, w_uq_sw=w_uq_sw, mla_kv_norm=f(inp["mla_kv_norm"]),
        w_ukv=f(inp["mla_w_ukv"]), ret_decay_logit=f(inp["ret_decay_logit"]), ret_norm_w=f(inp["ret_norm_w"]),
        gwblk=gwblk, gbias=gbias, gla_norm_w=f(inp["gla_norm_w"]), w_branch=f(inp["w_branch"]), w_out=f(inp["w_out"]),
        ffn_w_gate=f(inp["ffn_w_gate"]), ffn_w_up=f(inp["ffn_w_up"]), ffn_w_down=f(inp["ffn_w_down"]),
        moe_router=f(inp["moe_router"]), moe_w_gate=f(inp["moe_w_gate"]), moe_w_up=f(inp["moe_w_up"]),
        moe_w_down=f(inp["moe_w_down"]), final_norm_w=f(inp["final_norm_w"]),
    )
    maps = []
    for core in range(NCORES):
        b, q = core // 4, core % 4
        m = dict(shared)
        m["xs_in"] = np.ascontiguousarray(np.concatenate([x[b, q * LAT:(q + 1) * LAT], ctx[b]], 0))
        m["cvecT"] = np.ascontiguousarray(np.stack([c[b], c_ctx], 1))
        m.update(_consts(q))
        maps.append(m)
    return maps


_PROG = {}


def kernel(**inputs):
    if "nc" not in _PROG:
        _PROG["nc"] = build_program()[0]
    nc = _PROG["nc"]
    maps = make_in_maps(inputs)
    res = run_bass_kernel_spmd(nc, maps, core_ids=list(range(NCORES)))
    outp = np.zeros((2, SEQ, D), np.float32)
    for core in range(NCORES):
        b, q = core // 4, core % 4
        outp[b, q * LAT:(q + 1) * LAT] = res.results[core]["out"]
    return outp
```

```python
import math
from contextlib import ExitStack
import numpy as np
import ml_dtypes
import concourse.bass as bass
import concourse.mybir as mybir
from concourse.bass_utils import run_bass_kernel_spmd

F32 = mybir.dt.float32
BF16 = mybir.dt.bfloat16
AF = mybir.ActivationFunctionType
ALU = mybir.AluOpType
AX = mybir.AxisListType

NCORES = 8
D = 1024
SEQ = 8192
CTX = 256
LAT = 2048
T = LAT + CTX
NT = T // 128
NLT = LAT // 128
DEPTH = 2
EPS = 1e-6
DFF = 2816
NFF = DFF // 128
NEXP = 8
NKEY = CTX + SEQ
NKT = NKEY // 128

PA = 0
PA_N = 480
P_RET = 480
P_GLA = P_RET + 4 * 512
P_RV = P_GLA + 4 * 256
P_GV = P_RV + 512
P_RG = P_GV + 512
P_GR = P_RG + 512
P_BG = P_GR + 512
NPACK = P_BG + 3072

XF_STATE = 0
XF_LAM = 8
NXF = 9
XB_CKV = 0
XB_KR = 8
NXB = 10

EPOCH = 30000


class Buf:
    __slots__ = ("name", "w", "r")

    def __init__(self, name=""):
        self.name = name
        self.w = None
        self.r = []


class Ring:
    def __init__(self, items):
        self.items = items
        self.i = 0

    def next(self):
        it = self.items[self.i % len(self.items)]
        self.i += 1
        return it


class K:
    def __init__(self, nc):
        self.nc = nc
        self.eng = {"pe": nc.tensor, "dve": nc.vector, "act": nc.scalar,
                    "pool": nc.gpsimd, "sp": nc.sync}
        self.sems = {}
        self.cnt = {}
        self.epoch = {e: 0 for e in self.eng}
        self.seen = {}
        for e in self.eng:
            self._new_epoch(e, first=True)
        self.dq = {}
        for q, n in (("sp", 24), ("pool", 16), ("act", 4)):
            keys = []
            for i in range(n):
                key = ("dma", q, i)
                self.sems[key] = nc.alloc_semaphore(f"d_{q}_{i}")
                self.cnt[key] = 0
                keys.append(key)
            self.dq[q] = [keys, 0]
        self.cc_key = ("cc", 0)
        self.sems[self.cc_key] = nc.alloc_semaphore("cc")
        self.cnt[self.cc_key] = 0
        self.n_ins = 0

    def _new_epoch(self, e, first=False):
        if not first:
            self.epoch[e] += 1
        key = (e, self.epoch[e])
        self.sems[key] = self.nc.alloc_semaphore(f"s_{e}_{self.epoch[e]}")
        self.cnt[key] = 0

    def _wait(self, e, toks, force_same=False):
        eng = self.eng[e]
        best = {}
        for t in toks:
            if t is None:
                continue
            key, val = t
            if key[0] == e and not force_same:
                if e in ("pe", "sp"):
                    continue
            if best.get(key, 0) < val:
                best[key] = val
        for key, val in best.items():
            if self.seen.get((e, key), 0) >= val:
                continue
            assert self.cnt[key] >= val, f"wait on unsignalled token {key} {val} > {self.cnt[key]}"
            eng.wait_ge(self.sems[key], val)
            self.seen[(e, key)] = val

    @staticmethod
    def _deps(R, W):
        toks = []
        for b in R:
            toks.append(b.w)
        for b in W:
            toks.append(b.w)
            toks.extend(b.r)
        return toks

    @staticmethod
    def _commit(tok, R, W):
        for b in R:
            b.r.append(tok)
            if len(b.r) > 24:
                b.r = b.r[-24:] if False else b.r
        for b in W:
            b.w = tok
            b.r = []

    def op(self, e, fn, *args, R=(), W=(), sig=True, **kw):
        self._wait(e, self._deps(R, W))
        ins = fn(*args, **kw)
        self.n_ins += 1
        key = (e, self.epoch[e])
        if sig:
            self.cnt[key] += 1
            ins.then_inc(self.sems[key], 1)
            tok = (key, self.cnt[key])
            if self.cnt[key] >= EPOCH:
                self._new_epoch(e)
        else:
            tok = (key, self.cnt[key] + 1)
        self._commit(tok, R, W)
        return tok

    def dma(self, q, out, in_, R=(), W=(), **kw):
        keys, idx = self.dq[q]
        key = keys[idx % len(keys)]
        self.dq[q][1] = idx + 1
        toks = self._deps(R, W)
        if self.cnt[key] > 0:
            toks.append((key, self.cnt[key]))
        self._wait(q, toks)
        ins = self.eng[q].dma_start(out=out, in_=in_, **kw)
        self.n_ins += 1
        self.cnt[key] += 16
        ins.then_inc(self.sems[key], 16)
        tok = (key, self.cnt[key])
        self._commit(tok, R, W)
        return tok

    def allgather(self, in_ap, out_ap, R=(), W=()):
        self._wait("pool", self._deps(R, W))
        ins = self.nc.gpsimd.collective_compute(
            "AllGather", ALU.bypass, replica_groups=[[0, 1, 2, 3], [4, 5, 6, 7]],
            ins=[in_ap], outs=[out_ap])
        self.n_ins += 1
        self.cnt[self.cc_key] += 1
        ins.then_inc(self.sems[self.cc_key])
        tok = (self.cc_key, self.cnt[self.cc_key])
        self._commit(tok, R, W)
        return tok

    def fence(self):
        toks = []
        for key, c in self.cnt.items():
            if c > 0:
                toks.append((key, c))
        for e in self.eng:
            self._wait(e, toks, force_same=False)

    def wait_bufs(self, e, bufs):
        toks = []
        for b in bufs:
            toks.append(b.w)
            toks.extend(b.r)
        self._wait(e, toks, force_same=True)


def build_program(nlayers=DEPTH, dbg=None):
    nc = bass.Bass("TRN2", target_bir_lowering=False)
    k = K(nc)
    dbg = dbg or {}
    uid = [0]

    def U(name):
        uid[0] += 1
        return f"{name}_{uid[0]}"

    def din(name, shape, dt=F32):
        return nc.dram_tensor(name, list(shape), dt, kind="ExternalInput").ap()

    xs_in = din("xs_in", [T, D])
    cvecT = din("cvecT", [D, 2])
    mod_w = din("mod_w", [DEPTH, D, 6 * D])
    mod_b = din("mod_b", [DEPTH, 6 * D])
    norm1_w = din("norm1_w", [DEPTH, D])
    norm2_w = din("norm2_w", [DEPTH, D])
    wp = din("wp", [DEPTH, D, NPACK])
    q_norm = din("mla_q_norm", [DEPTH, 256])
    w_uq = din("w_uq", [DEPTH, 256, 768])
    w_uq_sw = din("w_uq_sw", [DEPTH, 256, 768])
    kv_norm = din("mla_kv_norm", [DEPTH, 128])
    w_ukv = din("w_ukv", [DEPTH, 128, 1024])
    ret_logit = din("ret_decay_logit", [DEPTH, 2, 4])
    ret_norm_w = din("ret_norm_w", [DEPTH, 512])
    gwblk = din("gwblk", [DEPTH, 4, 32, 128])
    gbias = din("gbias", [DEPTH, 4, 128])
    gla_norm_w = din("gla_norm_w", [DEPTH, 512])
    w_branch = din("w_branch", [DEPTH, 3, 512, D])
    w_out = din("w_out", [DEPTH, D, D])
    ffn_wg = din("ffn_w_gate", [1, D, DFF])
    ffn_wu = din("ffn_w_up", [1, D, DFF])
    ffn_wd = din("ffn_w_down", [1, DFF, D])
    moe_router = din("moe_router", [1, D, NEXP])
    moe_wg = din("moe_w_gate", [1, NEXP, D, DFF])
    moe_wu = din("moe_w_up", [1, NEXP, D, DFF])
    moe_wd = din("moe_w_down", [1, NEXP, DFF, D])
    final_norm_w = din("final_norm_w", [D])
    c_ropeR = din("c_ropeR", [2, 128, T])
    c_ropeAq = din("c_ropeAq", [2, 32, T])
    c_ropeAk = din("c_ropeAk", [2, 32, T])
    c_rst = din("c_rst", [128, T])
    c_mask = din("c_mask", [2, 128, 128])
    c_ident = din("c_ident", [128, 128])
    c_onehot = din("c_onehot", [128, 4])

    out = nc.dram_tensor("out", [LAT, D], F32, kind="ExternalOutput").ap()
    dbg_out = {}
    for name, (shape, dt) in dbg.items():
        dbg_out[name] = nc.dram_tensor("dbg_" + name, list(shape), dt, kind="ExternalOutput").ap()

    xs = nc.dram_tensor("xs", [T, D], F32).ap()
    uT = nc.dram_tensor("uT", [8, 128, T], BF16).ap()
    modD = nc.dram_tensor("modD", [2, 6 * D], F32).ap()
    xf_in = nc.dram_tensor("xf_in", [NXF, 16, 1024], F32).ap()
    xf_out = nc.dram_tensor("xf_out", [NXF, 64, 1024], F32).ap()
    xb_in = nc.dram_tensor("xb_in", [NXB, 16, LAT], BF16).ap()
    xb_out = nc.dram_tensor("xb_out", [NXB, 64, LAT], BF16).ap()
    b_xs = [Buf(f"xs{t}") for t in range(NT)]
    b_uT = [Buf(f"uT{b}") for b in range(5)]
    b_modD = Buf("modD")
    b_xf_in = [Buf() for _ in range(NXF)]
    b_xf_out = [Buf() for _ in range(NXF)]
    b_xb_in = [Buf() for _ in range(NXB)]
    b_xb_out = [Buf() for _ in range(NXB)]
    b_out = Buf("out")

    PSA = nc.alloc_psum_tensor("psa", [128, 8, 512], F32)
    PS = []
    for i in range(8):
        PS.append((PSA[:, i, :], Buf(f"ps{i}")))

    def salloc(name, shape, dt=F32):
        return nc.alloc_sbuf_tensor(name, list(shape), dt), Buf(name)

    ident, b_ident = salloc("ident", [128, 128])
    identb, b_identb = salloc("identb", [128, 128], BF16)
    onesb, b_onesb = salloc("onesb", [128, 128], BF16)
    maskF, b_maskF = salloc("maskF", [128, 128])
    maskB, b_maskB = salloc("maskB", [128, 128])
    onehot, b_onehot = salloc("onehot", [128, 4])
    eps_t, b_eps = salloc("eps_t", [128, 1])
    one_t, b_one = salloc("one_t", [128, 1])
    modT, b_modT = salloc("modT", [128, 48, 2])
    A1, b_A1 = salloc("A1", [128, 8, 2])
    A2, b_A2 = salloc("A2", [128, 8, 2])
    rstb, b_rstb = salloc("rstb", [128, T], BF16)
    yTd = nc.dram_tensor("yTd", [3, 4, 128, T], BF16).ap()
    lsp_kt2 = nc.dram_tensor("lsp_kt2", [8, 128, T], BF16).ap()
    lsp_vth = nc.dram_tensor("lsp_vth", [8, 128, NT * 128], BF16).ap()
    lsp_eb = nc.dram_tensor("lsp_eb", [8, 128, T], BF16).ap()
    lsp_U2 = nc.dram_tensor("lsp_U2", [8, 128, NT * 128], F32).ap()
    lsp_te = nc.dram_tensor("lsp_te", [8, 128, 2 * NT], F32).ap()
    b_lsp = [Buf(f"lsp{i}") for i in range(8)]
    b_yTd = [Buf(f"yTd{i}") for i in range(3)]

    k.dma("sp", ident[:], c_ident[:, :], W=[b_ident])
    k.op("dve", nc.vector.tensor_copy, identb[:], ident[:], R=[b_ident], W=[b_identb])
    k.op("dve", nc.vector.memset, onesb[:], 1.0, W=[b_onesb])
    k.op("dve", nc.vector.memset, eps_t[:], EPS, W=[b_eps])
    k.op("dve", nc.vector.memset, one_t[:], 1.0, W=[b_one])
    k.dma("sp", maskF[:], c_mask[0], W=[b_maskF])
    k.dma("sp", maskB[:], c_mask[1], W=[b_maskB])
    k.dma("sp", onehot[:], c_onehot[:, :], W=[b_onehot])
    k.dma("pool", rstb[:], c_rst[:, :], W=[b_rstb])
    with nc.sbuf_tensor(U("zinit"), [16, 1024], F32) as zt_:
        b_zt = Buf()
        k.op("dve", nc.vector.memset, zt_[:], 0.0, W=[b_zt])
        k.dma("sp", xf_in[XF_LAM], zt_[:], R=[b_zt], W=[b_xf_in[XF_LAM]])
        k.fence()

    BLKS = [(0, 512), (512, 512), (1024, 512), (1536, 512), (2048, 256)]

    def wview(w2d):
        return w2d.rearrange("(kc p) n -> p kc n", p=128)

    alt = [0]

    def evac_engine():
        alt[0] += 1
        return "act" if alt[0] % 2 else "dve"

    def copy_ps(e, out_ap, in_ap, R, W, scale=None):
        if e == "act":
            if scale is None:
                k.op("act", nc.scalar.copy, out_ap, in_ap, R=R, W=W)
            else:
                k.op("act", nc.scalar.mul, out_ap, in_ap, scale, R=R, W=W)
        else:
            if scale is None:
                k.op("dve", nc.vector.tensor_copy, out_ap, in_ap, R=R, W=W)
            else:
                k.op("dve", nc.vector.tensor_scalar, out_ap, in_ap, scale, None, ALU.mult, R=R, W=W)

    def rsqrt_col(es, name, src_ap, src_buf, scale, n=1, parts=128):
        t1 = es.enter_context(nc.sbuf_tensor(U(name + "_a"), [128, n], F32))
        t2 = es.enter_context(nc.sbuf_tensor(U(name + "_b"), [128, n], F32))
        b1, b2 = Buf(), Buf()
        k.op("dve", nc.vector.tensor_scalar, t1[0:parts, :], src_ap, scale, EPS, ALU.mult, ALU.add,
             R=[src_buf], W=[b1])
        k.op("act", nc.scalar.activation, t2[0:parts, :], t1[0:parts, :], AF.Sqrt, R=[b1], W=[b2])
        k.op("dve", nc.vector.reciprocal, t1[0:parts, :], t2[0:parts, :], R=[b2], W=[b1])
        return t1, b1

    def phase_mod(l):
        with ExitStack() as es:
            def sb(name, shape, dt=F32):
                return es.enter_context(nc.sbuf_tensor(U(name), list(shape), dt)), Buf(name)
            cT, b_cT = sb("cT", [128, 8, 2])
            sc, b_sc = sb("sc", [128, 8, 2])
            sg, b_sg = sb("sg", [128, 8, 2])
            modv, b_modv = sb("modv", [2, 6 * D])
            mb, b_mb = sb("mb", [2, 6 * D])
            wr = Ring([sb(f"mw{i}", [128, 8, 512], BF16) for i in range(4)])
            scb, b_scb = sb("scb", [128, 8, 2], BF16)
            k.dma("sp", cT[:], cvecT.rearrange("(kc p) r -> p kc r", p=128), W=[b_cT])
            k.op("act", nc.scalar.activation, sg[:], cT[:], AF.Sigmoid, R=[b_cT], W=[b_sg])
            k.op("dve", nc.vector.tensor_tensor, sc[:], cT[:], sg[:], ALU.mult, R=[b_cT, b_sg], W=[b_sc])
            k.op("dve", nc.vector.tensor_copy, scb[:], sc[:], R=[b_sc], W=[b_scb])
            k.dma("sp", mb[0:1, :], mod_b[l:l + 1, :], W=[b_mb])
            k.dma("sp", mb[1:2, :], mod_b[l:l + 1, :], W=[b_mb])
            psr = Ring(PS[0:2])
            for n in range(12):
                wt, b_wt = wr.next()
                k.dma("pool", wt[:], wview(mod_w[l])[:, :, n * 512:(n + 1) * 512], W=[b_wt])
                ps, b_ps = psr.next()
                for kc in range(8):
                    k.op("pe", nc.tensor.matmul, ps[0:2, :], scb[:, kc, :], wt[:, kc, :],
                         start=(kc == 0), stop=(kc == 7), R=[b_scb, b_wt], W=[b_ps], sig=(kc == 7))
                k.op("dve", nc.vector.tensor_tensor, modv[:, n * 512:(n + 1) * 512], ps[0:2, :],
                     mb[:, n * 512:(n + 1) * 512], ALU.add, R=[b_ps, b_mb], W=[b_modv])
            k.dma("sp", modD[:, :], modv[:], R=[b_modv], W=[b_modD])
            pst, b_pst = PS[2]
            for j in range(48):
                k.op("pe", nc.tensor.transpose, pst[:, 2 * j:2 * j + 2], modv[0:2, j * 128:(j + 1) * 128],
                     ident[0:2, 0:2], R=[b_modv, b_ident], W=[b_pst], sig=(j == 47))
            k.op("dve", nc.vector.tensor_copy, modT[:].rearrange("p j r -> p (j r)"), pst[:, 0:96],
                 R=[b_pst], W=[b_modT])
            nw, b_nw = sb("nw", [128, 8, 2])
            for (nsrc, joff, At, bA) in ((norm1_w, 8, A1, b_A1), (norm2_w, 32, A2, b_A2)):
                k.dma("sp", nw[:, :, 0], nsrc[l].rearrange("(kc p) -> p kc", p=128), W=[b_nw], allow_slow_non_contiguous=True)
                k.dma("sp", nw[:, :, 1], nsrc[l].rearrange("(kc p) -> p kc", p=128), W=[b_nw], allow_slow_non_contiguous=True)
                k.op("dve", nc.vector.tensor_scalar, At[:], modT[:, joff:joff + 8, :], 1.0, None, ALU.add,
                     R=[b_modT], W=[bA])
                k.op("dve", nc.vector.tensor_tensor, At[:], At[:], nw[:], ALU.mult, R=[b_nw], W=[bA])
        k.fence()

    def phase_norm(l, At, bA, shoff, tiles, xsrc=None):
        xsrc = xs if xsrc is None else xsrc
        with ExitStack() as es:
            def sb(name, shape, dt=F32):
                return es.enter_context(nc.sbuf_tensor(U(name), list(shape), dt)), Buf(name)
            ND = 6
            xr = Ring([sb(f"nx{i}", [128, D]) for i in range(ND)])
            jr = Ring([sb(f"nj{i}", [128, D]) for i in range(2)])
            xnr = Ring([sb(f"nn{i}", [128, D]) for i in range(ND)])
            ur = Ring([sb(f"nu{i}", [128, 8, 128], BF16) for i in range(ND)])
            ssr = Ring([sb(f"ns{i}", [128, 1]) for i in range(ND)])
            psr = Ring([PS[0], PS[1], PS[2], PS[3]])
            xloads = {}

            def issue_x(i_):
                if i_ < len(tiles):
                    t_ = tiles[i_]
                    xt_, b_xt_ = xr.next()
                    k.dma("sp", xt_[:], xsrc[t_ * 128:(t_ + 1) * 128, :], R=[b_xs[t_]], W=[b_xt_])
                    xloads[i_] = (xt_, b_xt_)
            for i_ in range(ND - 1):
                issue_x(i_)
            st = {}

            def front(i_):
                t = tiles[i_]
                issue_x(i_ + ND - 1)
                xt, b_xt = xloads.pop(i_)
                jk, b_jk = jr.next()
                ss, b_ss = ssr.next()
                k.op("act", nc.scalar.activation, jk[:], xt[:], AF.Square, accum_out=ss[:, 0:1],
                     R=[b_xt], W=[b_ss])
                rstd, b_rstd = rsqrt_col(es, f"nr{t}", ss[:, 0:1], b_ss, 1.0 / D)
                xn, b_xn = xnr.next()
                k.op("dve", nc.vector.tensor_scalar, xn[:], xt[:], rstd[:, 0:1], None, ALU.mult,
                     R=[b_xt, b_rstd], W=[b_xn])
                pss = []
                for half in range(2):
                    ps, b_ps = psr.next()
                    for j in range(4):
                        kc = half * 4 + j
                        k.op("pe", nc.tensor.transpose, ps[:, j * 128:(j + 1) * 128],
                             xn[:, kc * 128:(kc + 1) * 128], ident[:], R=[b_xn, b_ident], W=[b_ps],
                             sig=(j == 3))
                    pss.append((ps, b_ps))
                st[i_] = pss

            def back(i_):
                t = tiles[i_]
                r = 0 if t < NLT else 1
                pss = st.pop(i_)
                ut, b_ut = ur.next()
                for half in range(2):
                    ps, b_ps = pss[half]
                    for j in range(4):
                        kc = half * 4 + j
                        if j % 2 == 0:
                            k.op("act", nc.scalar.activation, ut[:, kc, :], ps[:, j * 128:(j + 1) * 128],
                                 AF.Identity, scale=At[:, kc, r:r + 1], bias=modT[:, shoff + kc, r:r + 1],
                                 R=[b_ps, bA, b_modT], W=[b_ut])
                        else:
                            k.op("dve", nc.vector.tensor_scalar, ut[:, kc, :], ps[:, j * 128:(j + 1) * 128],
                                 At[:, kc, r:r + 1], modT[:, shoff + kc, r:r + 1], ALU.mult, ALU.add,
                                 R=[b_ps, bA, b_modT], W=[b_ut])
                blk = min(t // 4, 4)
                k.dma("sp", uT[:, :, t * 128:(t + 1) * 128].rearrange("kc p t -> p kc t"), ut[:],
                      R=[b_ut], W=[b_uT[blk]])

            front(0)
            for i_ in range(len(tiles)):
                if i_ + 1 < len(tiles):
                    front(i_ + 1)
                back(i_)
        k.fence()

    class UStream:
        def __init__(self, es, nbuf=2, tag="ub"):
            self.ring = Ring([(es.enter_context(nc.sbuf_tensor(U(f"{tag}{i}"), [128, 8, 512], BF16)), Buf())
                              for i in range(nbuf)])

        def load(self, bi):
            t0, n = BLKS[bi]
            ub, b_ub = self.ring.next()
            k.dma("sp", ub[:, :, 0:n], uT[:, :, t0:t0 + n].rearrange("kc p t -> p kc t"),
                  R=[b_uT[bi]], W=[b_ub])
            return ub, b_ub, t0, n

    def proj_fm(ps, b_ps, w, b_w, c0, m, ub, b_ub, n, prow=0):
        for kc in range(8):
            k.op("pe", nc.tensor.matmul, ps[prow:prow + m, 0:n], w[:, kc, c0:c0 + m], ub[:, kc, 0:n],
                 start=(kc == 0), stop=(kc == 7), R=[b_w, b_ub], W=[b_ps], sig=(kc == 7))

    def load_w(es, name, src3d, shape, q="pool"):
        t = es.enter_context(nc.sbuf_tensor(U(name), list(shape), BF16))
        b = Buf(name)
        k.dma(q, t[:], src3d, W=[b])
        return t, b


    LS = {}
    LG2, b_LG2 = salloc("LG2", [128, 4])
    LAMT, b_LAMT = salloc("LAMT", [128, 8])

    def phase_q(l):
        cqn, b_cqn = LS["cqn"]; ckvn, b_ckvn = LS["ckvn"]; krr, b_krr = LS["krr"]; gzT, b_gzT = LS["gzT"]
        with ExitStack() as es:
            def sb(name, shape, dt=F32):
                return es.enter_context(nc.sbuf_tensor(U(name), list(shape), dt)), Buf(name)
            wA, b_wA = load_w(es, "wA", wview(wp[l])[:, :, PA:PA + PA_N], [128, 8, PA_N])
            qnw, b_qnw = sb("qnw", [128, 2])
            kvnw, b_kvnw = sb("kvnw", [128, 1])
            k.dma("sp", qnw[:], q_norm[l].rearrange("(c p) -> p c", p=128), W=[b_qnw], allow_slow_non_contiguous=True)
            k.dma("sp", kvnw[:], kv_norm[l].rearrange("(c p) -> p c", p=128), W=[b_kvnw], allow_slow_non_contiguous=True)
            ropk, b_ropk = sb("ropk", [32, 2, T])
            k.dma("sp", ropk[:, 0, :], c_ropeAk[0], W=[b_ropk])
            k.dma("sp", ropk[:, 1, :], c_ropeAk[1], W=[b_ropk])
            us = UStream(es)
            sqr = Ring([sb(f"sq{i}", [128, 512], BF16) for i in range(3)])
            rq, b_rq = sb("rq", [128, 512])
            rq2, b_rq2 = sb("rq2", [128, 512])
            tk, b_tk = sb("tk", [32, 512])
            tk2, b_tk2 = sb("tk2", [32, 512])
            for bi in range(5):
                ub, b_ub, t0, n = us.load(bi)
                (p0, bp0), (p1, bp1), (p2, bp2), (p3, bp3) = PS[0], PS[1], PS[2], PS[3]
                proj_fm(p0, bp0, wA, b_wA, 0, 128, ub, b_ub, n)
                proj_fm(p1, bp1, wA, b_wA, 128, 128, ub, b_ub, n)
                proj_fm(p2, bp2, wA, b_wA, 256, 128, ub, b_ub, n)
                s0, bs0 = sqr.next(); s1, bs1 = sqr.next(); s2, bs2 = sqr.next()
                k.op("act", nc.scalar.activation, s0[:, 0:n], p0[:, 0:n], AF.Square, R=[bp0], W=[bs0])
                k.op("act", nc.scalar.activation, s1[:, 0:n], p1[:, 0:n], AF.Square, R=[bp1], W=[bs1])
                k.op("act", nc.scalar.activation, s2[:, 0:n], p2[:, 0:n], AF.Square, R=[bp2], W=[bs2])
                k.op("pe", nc.tensor.matmul, p3[:, 0:n], onesb[:], s0[:, 0:n], start=True, stop=False,
                     R=[b_onesb, bs0], W=[bp3], sig=False)
                k.op("pe", nc.tensor.matmul, p3[:, 0:n], onesb[:], s1[:, 0:n], start=False, stop=True,
                     R=[b_onesb, bs1], W=[bp3])
                k.op("dve", nc.vector.tensor_scalar, rq[:, 0:n], p3[:, 0:n], 1.0 / 256, EPS, ALU.mult, ALU.add,
                     R=[bp3], W=[b_rq])
                k.op("act", nc.scalar.activation, rq2[:, 0:n], rq[:, 0:n], AF.Sqrt, R=[b_rq], W=[b_rq2])
                k.op("dve", nc.vector.reciprocal, rq[:, 0:n], rq2[:, 0:n], R=[b_rq2], W=[b_rq])
                k.op("dve", nc.vector.scalar_tensor_tensor, cqn[:, 0, t0:t0 + n], p0[:, 0:n], qnw[:, 0:1], rq[:, 0:n],
                     ALU.mult, ALU.mult, R=[bp0, b_qnw, b_rq], W=[b_cqn])
                k.op("dve", nc.vector.scalar_tensor_tensor, cqn[:, 1, t0:t0 + n], p1[:, 0:n], qnw[:, 1:2], rq[:, 0:n],
                     ALU.mult, ALU.mult, R=[bp1, b_qnw, b_rq], W=[b_cqn])
                k.op("pe", nc.tensor.matmul, p3[:, 0:n], onesb[:], s2[:, 0:n], start=True, stop=True,
                     R=[b_onesb, bs2], W=[bp3])
                k.op("dve", nc.vector.tensor_scalar, rq[:, 0:n], p3[:, 0:n], 1.0 / 128, EPS, ALU.mult, ALU.add,
                     R=[bp3], W=[b_rq])
                k.op("act", nc.scalar.activation, rq2[:, 0:n], rq[:, 0:n], AF.Sqrt, R=[b_rq], W=[b_rq2])
                k.op("dve", nc.vector.reciprocal, rq[:, 0:n], rq2[:, 0:n], R=[b_rq2], W=[b_rq])
                k.op("dve", nc.vector.scalar_tensor_tensor, ckvn[:, t0:t0 + n], p2[:, 0:n], kvnw[:, 0:1], rq[:, 0:n],
                     ALU.mult, ALU.mult, R=[bp2, b_kvnw, b_rq], W=[b_ckvn])
                (p4, bp4), (p5, bp5), (p6, bp6) = PS[4], PS[5], PS[6]
                proj_fm(p4, bp4, wA, b_wA, 384, 32, ub, b_ub, n)
                proj_fm(p5, bp5, wA, b_wA, 416, 32, ub, b_ub, n)
                proj_fm(p6, bp6, wA, b_wA, 448, 32, ub, b_ub, n)
                k.op("dve", nc.vector.tensor_tensor, tk[:, 0:n], p4[0:32, 0:n], ropk[:, 0, t0:t0 + n], ALU.mult,
                     R=[bp4, b_ropk], W=[b_tk])
                k.op("dve", nc.vector.tensor_tensor, tk2[:, 0:n], p5[0:32, 0:n], ropk[:, 1, t0:t0 + n], ALU.mult,
                     R=[bp5, b_ropk], W=[b_tk2])
                k.op("dve", nc.vector.tensor_tensor, krr[:, t0:t0 + n], tk[:, 0:n], tk2[:, 0:n], ALU.add,
                     R=[b_tk, b_tk2], W=[b_krr])
                k.op("act", nc.scalar.copy, gzT[:, t0:t0 + n], p6[0:32, 0:n], R=[bp6], W=[b_gzT])
            for j in range(8):
                k.dma("sp", xb_in[XB_CKV + j], ckvn[16 * j:16 * j + 16, 0:LAT], R=[b_ckvn], W=[b_xb_in[XB_CKV + j]])
            for j in range(2):
                k.dma("sp", xb_in[XB_KR + j], krr[16 * j:16 * j + 16, 0:LAT], R=[b_krr], W=[b_xb_in[XB_KR + j]])
            for j in range(NXB):
                k.allgather(xb_in[j], xb_out[j], R=[b_xb_in[j]], W=[b_xb_out[j]])
        k.fence()

    def phase_lg(l):
        with ExitStack() as es:
            def sb(name, shape, dt=F32):
                return es.enter_context(nc.sbuf_tensor(U(name), list(shape), dt)), Buf(name)
            lt, b_lt = sb("lt", [128, 4])
            l2, b_l2 = sb("l2", [128, 4])
            k.dma("sp", lt[0:64, :], ret_logit[l, 0:1, :].partition_broadcast(64), W=[b_lt])
            k.dma("sp", lt[64:128, :], ret_logit[l, 1:2, :].partition_broadcast(64), W=[b_lt])
            k.op("act", nc.scalar.activation, l2[:], lt[:], AF.Exp, scale=-1.0, R=[b_lt], W=[b_l2])
            k.op("act", nc.scalar.activation, lt[:], l2[:], AF.Ln, bias=one_t[:, 0:1], R=[b_l2, b_one], W=[b_lt])
            k.op("dve", nc.vector.tensor_scalar, LG2[:], lt[:], -1.0, None, ALU.mult, R=[b_lt], W=[b_LG2])
        k.fence()

    def lm_cols(mix, h):
        if mix == 0:
            return P_RET + h * 512, 512
        return P_GLA + h * 256, 256

    def lm_prefetch_w1(l, mix, h, PW):
        base, ncol = lm_cols(mix, h)
        k.dma("pool", PW["w"][0][:, :, 0:ncol], wview(wp[l])[:, :, base:base + ncol], W=[PW["w"][1]])
        vcol = (P_RV if mix == 0 else P_GV) + h * 128
        k.dma("pool", PW["wv"][0][:], wview(wp[l])[:, :, vcol:vcol + 128], W=[PW["wv"][1]])
        if mix == 1:
            k.dma("pool", PW["gw"][0][:], gwblk[l, h], W=[PW["gw"][1]])
            k.dma("sp", PW["nb"][0][:], gbias[l, h].rearrange("(p o) -> p o", o=1), W=[PW["nb"][1]])

    def lm_prefetch(l, mix, h, PF):
        hm = mix * 4 + h
        base, ncol = lm_cols(mix, h)
        k.dma("pool", PF["w"][0][:, :, 0:ncol], wview(wp[l])[:, :, base:base + ncol], W=[PF["w"][1]])
        gcol = (P_RG if mix == 0 else P_GR) + h * 128
        k.dma("pool", PF["wg"][0][:], wview(wp[l])[:, :, gcol:gcol + 128], W=[PF["wg"][1]])
        k.dma("sp", PF["kt2"][0][:], lsp_kt2[hm], R=[b_lsp[hm]], W=[PF["kt2"][1]])
        k.dma("sp", PF["vth"][0][:].rearrange("p n v -> p (n v)"), lsp_vth[hm], R=[b_lsp[hm]], W=[PF["vth"][1]])
        k.dma("sp", PF["ebT"][0][:], lsp_eb[hm], R=[b_lsp[hm]], W=[PF["ebT"][1]])
        k.dma("sp", PF["U2"][0][:].rearrange("p n v -> p (n v)"), lsp_U2[hm], R=[b_lsp[hm]], W=[PF["U2"][1]])
        k.dma("sp", PF["TE"][0][:], lsp_te[hm], R=[b_lsp[hm]], W=[PF["TE"][1]])

    def linear_mixer(l, mix, h, pass_no, last, PF=None, after_stage1=None):
        hm = mix * 4 + h
        gzT, b_gzT = LS["gzT"]
        with ExitStack() as es:
            def sb(name, shape, dt=F32):
                return es.enter_context(nc.sbuf_tensor(U(name), list(shape), dt)), Buf(name)
            if mix == 0:
                base, ncol = P_RET + h * 512, 512
                cq2, cq2s, ck2, ck2s = 0, 128, 256, 384
            else:
                base, ncol = P_GLA + h * 256, 256
                cq2, ck2 = 0, 128
            w, b_w = PF["w"]
            if pass_no == 1:
                wv, b_wv = PF["wv"]
            if mix == 1 and pass_no == 1:
                gw, b_gw = PF["gw"]
                nb, b_nb = PF["nb"]
                nb2, b_nb2 = sb("nb2", [128, 1])
                k.op("dve", nc.vector.tensor_scalar, nb2[:], nb[:], -1.0, None, ALU.mult, R=[b_nb], W=[b_nb2])
            if pass_no == 1:
                TE, b_TOT = sb("TE", [128, 2 * NT])
                kt2, b_kt2 = sb("kt2", [128, T], BF16)
                vth, b_vth = sb("vth", [128, NT, 128], BF16)
                ebT, b_ebT = sb("ebT", [128, T], BF16)
                U2, _ = sb("U2", [128, NT, 128])
            else:
                TE, b_TOT = PF["TE"]
                kt2, b_kt2 = PF["kt2"]
                vth, b_vth = PF["vth"]
                ebT, b_ebT = PF["ebT"]
                U2, b_ld = PF["U2"]
            TOT, EEND = TE[:, 0:NT], TE[:, NT:2 * NT]
            b_EEND = b_TOT
            b_kdc = [Buf() for _ in range(NT)]
            b_vthc = [Buf() for _ in range(NT)]
            us = UStream(es)
            if pass_no == 1:
                kd, b_kd = sb("kd", [128, T], BF16)
                a2r = Ring([sb(f"a2_{i}", [128, 512]) for i in range(2)])
                csr = Ring([sb(f"cs_{i}", [128, 512]) for i in range(2)])
                B2r = Ring([sb(f"B2_{i}", [128, 512]) for i in range(2)])
                enr = Ring([sb(f"en_{i}", [128, 512]) for i in range(2)])
            else:
                qt2, b_qt2 = sb("qt2", [128, T], BF16)
                b_vthc = [b_vth for _ in range(NT)]
            t1r = Ring([sb(f"lt1{i}", [128, 512]) for i in range(2)])
            t2r = Ring([sb(f"lt2{i}", [128, 512]) for i in range(2)])
            if mix == 0:
                ropr = Ring([sb(f"rop{i}", [128, 2, 512]) for i in range(2)])

            for bi in range(5):
                ub, b_ub, t0, n = us.load(bi)
                nch = n // 128
                ch0 = t0 // 128
                if pass_no == 1:
                    a2, b_a2 = a2r.next(); cs, b_cs = csr.next(); B2, b_B2 = B2r.next()
                    enb, b_enb = enr.next()
                if pass_no == 2:
                    pass
                elif mix == 0:
                    k.op("dve", nc.vector.memset, cs[:, 0:n], 1.0, W=[b_cs])
                    k.op("act", nc.scalar.activation, a2[:, 0:n], cs[:, 0:n], AF.Identity, scale=LG2[:, h:h + 1],
                         R=[b_cs, b_LG2], W=[b_a2])
                elif pass_no == 1:
                    pz, bpz = PS[7]
                    k.op("pe", nc.tensor.matmul, pz[:, 0:n], gw[:], gzT[:, t0:t0 + n], start=True, stop=True,
                         R=[b_gw, b_gzT], W=[bpz])
                    k.op("act", nc.scalar.activation, cs[:, 0:n], pz[:, 0:n], AF.Exp, scale=-1.0,
                         bias=nb2[:, 0:1], R=[bpz, b_nb2], W=[b_cs])
                    k.op("act", nc.scalar.activation, B2[:, 0:n], cs[:, 0:n], AF.Ln, bias=one_t[:, 0:1],
                         R=[b_cs, b_one], W=[b_B2])
                    k.op("dve", nc.vector.tensor_scalar, a2[:, 0:n], B2[:, 0:n], -1.0 / 16.0, None,
                         ALU.mult, R=[b_B2], W=[b_a2])
                if pass_no == 1:
                    k.op("dve", nc.vector.tensor_tensor_scan, cs[:, 0:n], rstb[:, t0:t0 + n], a2[:, 0:n], 0.0, ALU.mult, ALU.add,
                         R=[b_rstb, b_a2], W=[b_cs])
                    k.op("dve", nc.vector.tensor_copy, B2[0:64, 0:n], cs[0:64, 0:n], R=[b_cs], W=[b_B2])
                    k.op("dve", nc.vector.tensor_tensor, B2[64:128, 0:n], a2[64:128, 0:n], cs[64:128, 0:n], ALU.subtract,
                         R=[b_a2, b_cs], W=[b_B2])
                    for c_ in range(nch):
                        k.op("dve", nc.vector.tensor_scalar, B2[64:128, c_ * 128:(c_ + 1) * 128],
                             B2[64:128, c_ * 128:(c_ + 1) * 128], cs[64:128, c_ * 128 + 127:c_ * 128 + 128], None, ALU.add,
                             R=[b_cs], W=[b_B2])
                        k.op("dve", nc.vector.tensor_copy, TOT[:, ch0 + c_:ch0 + c_ + 1], cs[:, c_ * 128 + 127:c_ * 128 + 128],
                             R=[b_cs], W=[b_TOT])
                    k.op("act", nc.scalar.activation, EEND[:, ch0:ch0 + nch], TOT[:, ch0:ch0 + nch], AF.Exp,
                         R=[b_TOT], W=[b_EEND])
                    k.op("act", nc.scalar.activation, enb[:, 0:n], B2[:, 0:n], AF.Exp, scale=-1.0, R=[b_B2], W=[b_enb])
                    k.op("act", nc.scalar.activation, ebT[:, t0:t0 + n], B2[:, 0:n], AF.Exp, R=[b_B2], W=[b_ebT])
                if mix == 0:
                    rop, b_rop = ropr.next()
                    k.dma("sp", rop[:, 0, 0:n], c_ropeR[0, :, t0:t0 + n], W=[b_rop])
                    k.dma("sp", rop[:, 1, 0:n], c_ropeR[1, :, t0:t0 + n], W=[b_rop])

                def roped(pa, bpa, pb, bpb):
                    if mix == 1:
                        return pa[:, 0:n], bpa
                    ta, bta = t1r.next()
                    tb, btb = t2r.next()
                    k.op("dve", nc.vector.tensor_tensor, ta[:, 0:n], pa[:, 0:n], rop[:, 0, 0:n], ALU.mult,
                         R=[bpa, b_rop], W=[bta])
                    k.op("dve", nc.vector.tensor_tensor, tb[:, 0:n], pb[:, 0:n], rop[:, 1, 0:n], ALU.mult,
                         R=[bpb, b_rop], W=[btb])
                    k.op("dve", nc.vector.tensor_tensor, ta[:, 0:n], ta[:, 0:n], tb[:, 0:n], ALU.add,
                         R=[btb], W=[bta])
                    return ta[:, 0:n], bta

                (pa, bpa), (pb, bpb) = (PS[0], PS[1]) if bi % 2 == 0 else (PS[2], PS[3])
                if pass_no == 1:
                    proj_fm(pa, bpa, w, b_w, ck2, 128, ub, b_ub, n)
                    if mix == 0:
                        proj_fm(pb, bpb, w, b_w, ck2s, 128, ub, b_ub, n)
                    kap, bk = roped(pa, bpa, pb, bpb)
                    k.op("dve", nc.vector.tensor_tensor, kt2[:, t0:t0 + n], kap, enb[:, 0:n], ALU.mult,
                         R=[bk, b_enb], W=[b_kt2])
                if pass_no == 2:
                    (pc, bpc), (pd, bpd) = (pa, bpa), (pb, bpb)
                    proj_fm(pc, bpc, w, b_w, cq2, 128, ub, b_ub, n)
                    if mix == 0:
                        proj_fm(pd, bpd, w, b_w, cq2s, 128, ub, b_ub, n)
                    qap, bq = roped(pc, bpc, pd, bpd)
                    k.op("dve", nc.vector.scalar_tensor_tensor, qt2[:, t0:t0 + n], qap, 0.125, ebT[:, t0:t0 + n],
                         ALU.mult, ALU.mult, R=[bq, b_ebT], W=[b_qt2])
                for c_ in (range(nch) if pass_no == 1 else []):
                    n_ = ch0 + c_
                    k.op("act", nc.scalar.activation, kd[:, n_ * 128:(n_ + 1) * 128], kt2[:, n_ * 128:(n_ + 1) * 128],
                         AF.Identity, scale=EEND[:, n_:n_ + 1], R=[b_kt2, b_EEND], W=[b_kdc[n_]])
                    pv, bpv = PS[4 + c_ % 2]
                    for kc in range(8):
                        k.op("pe", nc.tensor.matmul, pv[:, 0:128], ub[:, kc, c_ * 128:(c_ + 1) * 128], wv[:, kc, :],
                             start=(kc == 0), stop=(kc == 7), R=[b_ub, b_wv], W=[bpv], sig=(kc == 7))
                    copy_ps(evac_engine(), vth[:, n_, :], pv[:, 0:128], R=[bpv], W=[b_vthc[n_]])
            if pass_no == 1:
                k.dma("sp", lsp_kt2[hm], kt2[:], R=[b_kt2], W=[b_lsp[hm]])
                k.dma("sp", lsp_vth[hm], vth[:].rearrange("p n v -> p (n v)"), R=b_vthc, W=[b_lsp[hm]])
                k.dma("sp", lsp_eb[hm], ebT[:], R=[b_ebT], W=[b_lsp[hm]])
                k.dma("sp", lsp_te[hm], TE[:], R=[b_TOT], W=[b_lsp[hm]])
            if pass_no == 1:
                k2d, _ = sb("k2d", [128, NT, 128], BF16)
            b_k2dg = [Buf() for _ in range(3)]
            b_U2g = [Buf() for _ in range(5)]
            if pass_no == 2:
                b_U2g = [b_ld]
            if pass_no == 1:
                ptb = PSA[:, 0:3, :].bitcast(BF16)
                for n_ in range(NT):
                    bk = n_ // 8
                    k.op("pe", nc.tensor.transpose, ptb[:, bk, (n_ % 8) * 128:(n_ % 8 + 1) * 128],
                         kd[:, n_ * 128:(n_ + 1) * 128], identb[:], R=[b_kdc[n_], b_identb], W=[PS[bk][1]],
                         sig=(n_ % 8 == 7 or n_ == NT - 1))
                for bk in range(3):
                    nb_ = min(8, NT - 8 * bk)
                    copy_ps(evac_engine(), k2d[:, 8 * bk:8 * bk + nb_, :],
                            ptb[:, bk, 0:nb_ * 128].rearrange("p (a c) -> p a c", c=128), R=[PS[bk][1]], W=[b_k2dg[bk]])
                for n_ in range(NT):
                    bk = 3 + n_ // 4
                    k.op("pe", nc.tensor.matmul, PSA[:, bk, (n_ % 4) * 128:(n_ % 4 + 1) * 128], k2d[:, n_, :], vth[:, n_, :],
                         start=True, stop=True, R=[b_k2dg[n_ // 8], b_vthc[n_]], W=[PS[bk][1]], sig=(n_ % 4 == 3 or n_ == NT - 1))
                for b4 in range(5):
                    nb_ = min(4, NT - 4 * b4)
                    copy_ps(evac_engine(), U2[:, 4 * b4:4 * b4 + nb_, :],
                            PSA[:, 3 + b4, 0:nb_ * 128].rearrange("p (a c) -> p a c", c=128), R=[PS[3 + b4][1]], W=[b_U2g[b4]])
            if pass_no == 1:
                k.dma("sp", lsp_U2[hm], U2[:].rearrange("p n v -> p (n v)"), R=b_U2g, W=[b_lsp[hm]])
            F, Bw = slice(0, 64), slice(64, 128)
            TSEQ, b_TSEQ = sb("TSEQ", [128, 17])
            ESEQ, b_ESEQ = sb("ESEQ", [128, 17])
            k.op("dve", nc.vector.memset, TSEQ[:, 0:1], 0.0, W=[b_TSEQ])
            k.op("dve", nc.vector.tensor_copy, TSEQ[F, 1:17], TOT[F, 0:NLT], R=[b_TOT], W=[b_TSEQ])
            k.op("dve", nc.vector.tensor_copy, TSEQ[Bw, 1:17], TOT[Bw, NLT - 1::-1], R=[b_TOT], W=[b_TSEQ])
            k.op("act", nc.scalar.activation, ESEQ[:], TSEQ[:], AF.Exp, R=[b_TSEQ], W=[b_ESEQ])
            k.op("dve", nc.vector.memset, ESEQ[:, 0:1], 0.0, W=[b_ESEQ])
            DS, b_DS = sb("DS", [128, 128, 17])
            US, b_US = sb("US", [128, 128, 17])
            SS, b_SS = sb("SS", [128, 128, 17])
            k.op("dve", nc.vector.tensor_copy, DS[:], ESEQ[:].unsqueeze(1).broadcast_to([128, 128, 17]),
                 R=[b_ESEQ], W=[b_DS])
            k.op("dve", nc.vector.tensor_copy, US[F, :, 1:17], U2[F, 0:NLT, :].rearrange("p n v -> p v n"),
                 R=b_U2g, W=[b_US])
            b_USb = Buf("USb")
            k.op("act", nc.scalar.copy, US[Bw, :, 1:17], U2[Bw, NLT - 1::-1, :].rearrange("p n v -> p v n"),
                 R=b_U2g, W=[b_USb])
            St, b_St = sb("St", [128, 128])

            def run_scan():
                k.op("dve", nc.vector.tensor_tensor_scan, SS[:].rearrange("p v t -> p (v t)"),
                     DS[:].rearrange("p v t -> p (v t)"), US[:].rearrange("p v t -> p (v t)"), 0.0, ALU.mult, ALU.add,
                     R=[b_DS, b_US, b_USb], W=[b_SS])

            if pass_no == 1:
                k.op("dve", nc.vector.memset, US[:, :, 0], 0.0, W=[b_US])
                run_scan()
                k.op("dve", nc.vector.tensor_copy, St[:], SS[:, :, 16], R=[b_SS], W=[b_St])
                k.dma("sp", xf_in[XF_STATE + hm].rearrange("a (b c) -> (a b) c", c=128), St[:],
                      R=[b_St], W=[b_xf_in[XF_STATE + hm]])
                ls, b_ls = sb("ls", [128, 1])
                k.op("dve", nc.vector.reduce_sum, ls[:], TOT[:, 0:NLT], AX.X, R=[b_TOT], W=[b_ls])
                k.op("act", nc.scalar.activation, LAMT[:, hm:hm + 1], ls[:], AF.Exp, R=[b_ls], W=[b_LAMT])
                return
            FR, b_FR = sb("FR", [128, 4, 128])
            LR, b_LR = sb("LR", [128, 4, 8])
            for r in range(4):
                k.dma("sp", FR[:, r, :], xf_out[XF_STATE + hm, r * 16:(r + 1) * 16, :].rearrange("a (b c) -> (a b) c", c=128),
                      R=[b_xf_out[XF_STATE + hm]], W=[b_FR])
                k.dma("sp", LR[:, r, :], xf_out[XF_LAM, r * 16, :].rearrange("(p e) -> p e", e=8),
                      R=[b_xf_out[XF_LAM]], W=[b_LR])
            Rt, b_Rt = sb("Rt", [128, 128])
            Sin, b_Sin = sb("Sin", [128, 128])
            k.op("dve", nc.vector.memset, Sin[:], 0.0, W=[b_Sin])
            k.op("dve", nc.vector.scalar_tensor_tensor, Rt[F, :], U2[F, 16, :], EEND[F, 17:18], U2[F, 17, :],
                 ALU.mult, ALU.add, R=b_U2g + [b_EEND], W=[b_Rt])
            k.op("dve", nc.vector.scalar_tensor_tensor, Rt[Bw, :], U2[Bw, 17, :], EEND[Bw, 16:17], U2[Bw, 16, :],
                 ALU.mult, ALU.add, R=b_U2g + [b_EEND], W=[b_Rt])
            for sl, order in ((F, range(4)), (Bw, range(3, -1, -1))):
                for r in order:
                    k.op("dve", nc.vector.scalar_tensor_tensor, Sin[sl, :], Rt[sl, :], onehot[sl, r:r + 1], Sin[sl, :],
                         ALU.mult, ALU.add, R=[b_Rt, b_onehot], W=[b_Sin])
                    k.op("dve", nc.vector.scalar_tensor_tensor, Rt[sl, :], Rt[sl, :], LR[sl, r, hm:hm + 1], FR[sl, r, :],
                         ALU.mult, ALU.add, R=[b_LR, b_FR], W=[b_Rt])
            k.op("dve", nc.vector.tensor_copy, US[:, :, 0], Sin[:], R=[b_Sin], W=[b_US])
            run_scan()
            S2, b_S2 = sb("S2", [128, NT, 128], BF16)
            b_S2b = Buf("S2b")
            k.op("dve", nc.vector.tensor_copy, S2[F, 0:NLT, :], SS[F, :, 0:NLT].rearrange("p v t -> p t v"),
                 R=[b_SS], W=[b_S2])
            k.op("act", nc.scalar.copy, S2[Bw, 0:NLT, :], SS[Bw, :, NLT - 1::-1].rearrange("p v t -> p t v"),
                 R=[b_SS], W=[b_S2b])
            k.op("dve", nc.vector.memset, S2[F, 16, :], 0.0, W=[b_S2])
            k.op("dve", nc.vector.memset, S2[Bw, 17, :], 0.0, W=[b_S2])
            k.op("dve", nc.vector.tensor_copy, S2[F, 17, :], U2[F, 16, :], R=b_U2g, W=[b_S2])
            k.op("dve", nc.vector.tensor_copy, S2[Bw, 16, :], U2[Bw, 17, :], R=b_U2g, W=[b_S2])
            gcol = (P_RG if mix == 0 else P_GR) + h * 128
            wg_, b_wg = PF["wg"]
            nwb, b_nwb = sb("nwb", [128, 128])
            nsrc = ret_norm_w if mix == 0 else gla_norm_w
            k.dma("sp", nwb[:], nsrc[l:l + 1, h * 128:(h + 1) * 128].partition_broadcast(128), W=[b_nwb])
            G, b_G = sb("G", [128, NT, 128], BF16)
            gs, b_gs = sb("gs", [128, 8, 128])
            nchunks = NLT if last else NT
            groups = [(g0, min(8, nchunks - g0)) for g0 in range(0, nchunks, 8)]
            ubc = None
            for gi, (g0, ng) in enumerate(groups):
                bks = [6, 7] if gi % 2 == 0 else [4, 5]
                for c_ in range(ng):
                    n_ = g0 + c_
                    bi = min(n_ // 4, 4)
                    if ubc is None or ubc[0] != bi:
                        ubc = (bi,) + tuple(us.load(bi))
                    _, ub, b_ub, t0, n = ubc
                    tt = n_ - t0 // 128
                    bk = bks[c_ // 4]
                    for kc in range(8):
                        k.op("pe", nc.tensor.matmul, PSA[:, bk, (c_ % 4) * 128:(c_ % 4 + 1) * 128],
                             ub[:, kc, tt * 128:(tt + 1) * 128], wg_[:, kc, :],
                             start=(kc == 0), stop=(kc == 7), R=[b_ub, b_wg], W=[PS[bk][1]], sig=(kc == 7))
                nbk = (ng + 3) // 4
                pgv = PSA[:, bks[0]:bks[0] + nbk, :].rearrange("p b (c v) -> p (b c) v", v=128)[:, 0:ng, :]
                k.op("act", nc.scalar.activation, gs[:, 0:ng, :], pgv, AF.Silu, R=[PS[b_][1] for b_ in bks[:nbk]], W=[b_gs])
                k.op("dve", nc.vector.tensor_tensor, G[:, g0:g0 + ng, :], gs[:, 0:ng, :],
                     nwb[:].unsqueeze(1).broadcast_to([128, ng, 128]), ALU.mult, R=[b_gs, b_nwb], W=[b_G])
            if after_stage1 is not None:
                after_stage1()
            t1, b_t1 = sb("g_t1", [128, 8, 128])
            t2, b_t2 = sb("g_t2", [128, 8, 128])
            PT8, b_PT8 = sb("PT8", [128, 8, 128], BF16)
            osb, b_osb = sb("osb", [128, 8, 128])
            jk, b_jk = sb("ljk", [128, 128])
            stt, b_stt = sb("stt", [128, 6, 8])
            yn8, b_yn8 = sb("yn8", [128, 8, 128])
            yb8, b_yb8 = sb("yb8", [128, 8, 128], BF16)
            yTt, b_yT = sb("yTt", [128, T], BF16)
            b_sttc = [Buf() for _ in range(8)]
            b_osbc = [Buf() for _ in range(8)]
            b_ync = [Buf() for _ in range(8)]
            for (g0, ng) in groups:
                nbk = (ng + 3) // 4
                for c_ in range(ng):
                    c0 = (g0 + c_) * 128
                    k.op("pe", nc.tensor.matmul, PSA[:, c_ // 4, (c_ % 4) * 128:(c_ % 4 + 1) * 128],
                         kt2[0:64, c0:c0 + 128], qt2[0:64, c0:c0 + 128],
                         start=True, stop=True, R=[b_kt2, b_qt2], W=[PS[c_ // 4][1]], sig=(c_ % 4 == 3 or c_ == ng - 1))
                for c_ in range(ng):
                    c0 = (g0 + c_) * 128
                    k.op("pe", nc.tensor.matmul, PSA[:, 2 + c_ // 4, (c_ % 4) * 128:(c_ % 4 + 1) * 128],
                         kt2[64:128, c0:c0 + 128], qt2[64:128, c0:c0 + 128],
                         start=True, stop=True, R=[b_kt2, b_qt2], W=[PS[2 + c_ // 4][1]], sig=(c_ % 4 == 3 or c_ == ng - 1))
                sfv = PSA[:, 0:nbk, :].rearrange("p b (c v) -> p (b c) v", v=128)[:, 0:ng, :]
                sbv = PSA[:, 2:2 + nbk, :].rearrange("p b (c v) -> p (b c) v", v=128)[:, 0:ng, :]
                k.op("dve", nc.vector.tensor_tensor, t1[:, 0:ng, :], sfv, maskF[:].unsqueeze(1).broadcast_to([128, ng, 128]),
                     ALU.mult, R=[PS[b_][1] for b_ in range(nbk)] + [b_maskF], W=[b_t1])
                k.op("dve", nc.vector.tensor_tensor, t2[:, 0:ng, :], sbv, maskB[:].unsqueeze(1).broadcast_to([128, ng, 128]),
                     ALU.mult, R=[PS[2 + b_][1] for b_ in range(nbk)] + [b_maskB], W=[b_t2])
                k.op("dve", nc.vector.tensor_tensor, PT8[:, 0:ng, :], t1[:, 0:ng, :], t2[:, 0:ng, :], ALU.add,
                     R=[b_t1, b_t2], W=[b_PT8])
                for c_ in range(ng):
                    n_ = g0 + c_
                    c0 = n_ * 128
                    bk = 4 + c_ // 4
                    oap = PSA[:, bk, (c_ % 4) * 128:(c_ % 4 + 1) * 128]
                    k.op("pe", nc.tensor.matmul, oap, PT8[:, c_, :], vth[:, n_, :],
                         start=True, stop=False, R=[b_PT8, b_vthc[n_]], W=[PS[bk][1]], sig=False)
                    k.op("pe", nc.tensor.matmul, oap, qt2[:, c0:c0 + 128], S2[:, n_, :],
                         start=False, stop=True, R=[b_qt2, b_S2, b_S2b], W=[PS[bk][1]], sig=(c_ % 4 == 3 or c_ == ng - 1))
                for c_ in range(ng):
                    bk = 4 + c_ // 4
                    oap = PSA[:, bk, (c_ % 4) * 128:(c_ % 4 + 1) * 128]
                    k.op("act", nc.scalar.activation, osb[:, c_, :], oap, AF.Identity, accum_out=stt[:, 0, c_:c_ + 1],
                         R=[PS[bk][1]], W=[b_osbc[c_], b_sttc[c_]])
                    k.op("act", nc.scalar.activation, jk[:], oap, AF.Square, accum_out=stt[:, 1, c_:c_ + 1],
                         R=[PS[bk][1], b_sttc[c_]], W=[])
                k.op("dve", nc.vector.tensor_scalar, stt[:, 0:2, 0:ng], stt[:, 0:2, 0:ng], 1.0 / 128, None, ALU.mult,
                     W=[b_stt] + b_sttc[0:ng])
                if mix == 0:
                    k.op("dve", nc.vector.tensor_tensor, stt[:, 3, 0:ng], stt[:, 0, 0:ng], stt[:, 0, 0:ng], ALU.mult, R=b_sttc[0:ng], W=[b_stt])
                    k.op("dve", nc.vector.tensor_tensor, stt[:, 1, 0:ng], stt[:, 1, 0:ng], stt[:, 3, 0:ng], ALU.subtract, R=b_sttc[0:ng], W=[b_stt])
                k.op("dve", nc.vector.tensor_scalar, stt[:, 3, 0:ng], stt[:, 1, 0:ng], EPS, None, ALU.add, R=b_sttc[0:ng], W=[b_stt])
                k.op("act", nc.scalar.activation, stt[:, 4, 0:ng], stt[:, 3, 0:ng], AF.Sqrt, R=b_sttc[0:ng], W=[b_stt])
                k.op("dve", nc.vector.reciprocal, stt[:, 2, 0:ng], stt[:, 4, 0:ng], R=b_sttc[0:ng], W=[b_stt])
                for c_ in range(ng):
                    if mix == 0:
                        k.op("dve", nc.vector.tensor_scalar, yn8[:, c_, :], osb[:, c_, :], stt[:, 0, c_:c_ + 1], stt[:, 2, c_:c_ + 1],
                             ALU.subtract, ALU.mult, R=[b_osbc[c_], b_stt, b_sttc[c_]], W=[b_ync[c_]])
                    else:
                        k.op("dve", nc.vector.tensor_scalar, yn8[:, c_, :], osb[:, c_, :], stt[:, 2, c_:c_ + 1], None, ALU.mult,
                             R=[b_osbc[c_], b_stt, b_sttc[c_]], W=[b_ync[c_]])
                k.op("dve", nc.vector.tensor_tensor, yb8[:, 0:ng, :], yn8[:, 0:ng, :], G[:, g0:g0 + ng, :], ALU.mult,
                     R=b_ync[0:ng] + [b_G], W=[b_yb8])
                p6b = PSA[:, 6, :].bitcast(BF16)
                for c_ in range(ng):
                    k.op("pe", nc.tensor.transpose, p6b[:, c_ * 128:(c_ + 1) * 128], yb8[:, c_, :], identb[:],
                         R=[b_yb8, b_identb], W=[PS[6][1]], sig=(c_ == ng - 1))
                copy_ps("act", yTt[:, g0 * 128:(g0 + ng) * 128], p6b[:, 0:ng * 128], R=[PS[6][1]], W=[b_yT])
            ntok = LAT if last else T
            k.dma("sp", yTd[1 + mix, h, :, 0:ntok], yTt[:, 0:ntok], R=[b_yT], W=[b_yTd[1 + mix]])

    def phase_linear(l, pass_no, last):
        order = [(mix, h) for mix in range(2) for h in range(4)]
        if pass_no == 1:
            with ExitStack() as pes:
                PWs = []
                for si in range(2):
                    PW = {}
                    for nm, shp, dt in (("w", [128, 8, 512], BF16), ("wv", [128, 8, 128], BF16), ("gw", [32, 128], BF16),
                                        ("nb", [128, 1], F32)):
                        PW[nm] = (pes.enter_context(nc.sbuf_tensor(U(f"pw_{nm}{si}"), shp, dt)), Buf(f"pw_{nm}{si}"))
                    PWs.append(PW)
                lm_prefetch_w1(l, order[0][0], order[0][1], PWs[0])
                for i_, (mix, h) in enumerate(order):
                    if i_ + 1 < len(order):
                        lm_prefetch_w1(l, order[i_ + 1][0], order[i_ + 1][1], PWs[(i_ + 1) % 2])
                    linear_mixer(l, mix, h, pass_no, last, PF=PWs[i_ % 2])
                    k.fence()
        else:
            with ExitStack() as pes:
                PFs = []
                for si in range(2):
                    PF = {}
                    for nm, shp, dt in (("w", [128, 8, 512], BF16), ("wg", [128, 8, 128], BF16), ("kt2", [128, T], BF16),
                                        ("vth", [128, NT, 128], BF16), ("ebT", [128, T], BF16), ("U2", [128, NT, 128], F32),
                                        ("TE", [128, 2 * NT], F32)):
                        PF[nm] = (pes.enter_context(nc.sbuf_tensor(U(f"pf_{nm}{si}"), shp, dt)), Buf(f"pf_{nm}{si}"))
                    PFs.append(PF)
                lm_prefetch(l, order[0][0], order[0][1], PFs[0])
                for i_, (mix, h) in enumerate(order):
                    cb = None
                    if i_ + 1 < len(order):
                        cb = (lambda j=i_ + 1: lm_prefetch(l, order[j][0], order[j][1], PFs[j % 2]))
                    linear_mixer(l, mix, h, pass_no, last, PF=PFs[i_ % 2], after_stage1=cb)
                    k.fence()
            k.fence()
        if pass_no == 1:
            k.dma("sp", xf_in[XF_LAM, 0, :].rearrange("(p e) -> p e", e=8), LAMT[:], R=[b_LAMT], W=[b_xf_in[XF_LAM]])
            for j in range(NXF):
                k.allgather(xf_in[j], xf_out[j], R=[b_xf_in[j]], W=[b_xf_out[j]])
            k.fence()


    G8, b_G8 = salloc("G8", [128, NT, 8])

    def phase_attn(l, last):
        SCL = 96.0 ** -0.5
        cqn, b_cqn = LS["cqn"]; ckvn, b_ckvn = LS["ckvn"]; krr, b_krr = LS["krr"]
        with ExitStack() as es:
            def sb(name, shape, dt=F32):
                return es.enter_context(nc.sbuf_tensor(U(name), list(shape), dt)), Buf(name)
            ckvA, b_ckvA = sb("ckvA", [128, NKEY], BF16)
            k.op("pool", nc.gpsimd.tensor_copy, ckvA[:, 0:CTX], ckvn[:, LAT:T], R=[b_ckvn], W=[b_ckvA])
            for j in range(8):
                k.dma("sp", ckvA[16 * j:16 * j + 16, CTX:NKEY].rearrange("p (r t) -> p r t", r=4),
                      xb_out[XB_CKV + j].rearrange("(r p) t -> p r t", p=16),
                      R=[b_xb_out[XB_CKV + j]], W=[b_ckvA])
            KTs = [sb(f"KT{i}", [96, NKEY], BF16) for i in range(2)]
            Vs = [sb(f"V{i}", [128, NKT, 128], BF16) for i in range(2)]
            for (KT, bKT), (V, bV) in zip(KTs, Vs):
                k.dma("sp", KT[64:96, 0:CTX], krr[:, LAT:T], R=[b_krr], W=[bKT])
                for j in range(2):
                    k.dma("sp", KT[64 + 16 * j:64 + 16 * j + 16, CTX:NKEY].rearrange("p (r t) -> p r t", r=4),
                          xb_out[XB_KR + j].rearrange("(r p) t -> p r t", p=16),
                          R=[b_xb_out[XB_KR + j]], W=[bKT])
                k.op("pool", nc.gpsimd.memset, V[:, :, 64:128], 1.0, W=[bV])
            ropq_r = Ring([sb(f"ropq{i}", [96, 2, 512]) for i in range(2)])
            yat_r = Ring([sb(f"yat{i}", [64, 512], BF16) for i in range(3)])
            QTs = Ring([sb(f"QT{i}", [96, T], BF16) for i in range(2)])
            wq_r = Ring([sb(f"wq{i}", [128, 2, 96], BF16) for i in range(2)])
            wqs_r = Ring([sb(f"wqs{i}", [128, 2, 96], BF16) for i in range(2)])
            wkv_r = Ring([sb(f"wkv{i}", [128, 128], BF16) for i in range(2)])
            t1r = Ring([sb(f"at1{i}", [96, 512]) for i in range(2)])
            t2r = Ring([sb(f"at2{i}", [96, 512]) for i in range(2)])
            Pr = Ring([sb(f"P{i}", [128, 512], BF16) for i in range(4)])
            linv_r = Ring([sb(f"linv{i}", [128, 512]) for i in range(2)])
            sring = Ring([PS[0], PS[1], PS[2], PS[3]])
            oring = Ring([PS[4], PS[5]])
            qblocks = [(q0, 512, list(range(NKT))) for q0 in range(0, LAT, 512)]
            if not last:
                qblocks.append((LAT, CTX, [0, 1]))
            def build_head(h):
                wq, b_wq = wq_r.next(); wqs, b_wqs = wqs_r.next(); wkv, b_wkv = wkv_r.next()
                k.dma("pool", wq[:], w_uq[l].rearrange("(kc p) n -> p kc n", p=128)[:, :, h * 96:(h + 1) * 96], W=[b_wq])
                k.dma("pool", wqs[:], w_uq_sw[l].rearrange("(kc p) n -> p kc n", p=128)[:, :, h * 96:(h + 1) * 96], W=[b_wqs])
                k.dma("pool", wkv[:], w_ukv[l][:, h * 128:(h + 1) * 128], W=[b_wkv])
                QT, bQT = QTs.next()
                KT, bKT = KTs[h % 2]
                V, bV = Vs[h % 2]
                built[h] = (QT, bQT, KT, bKT, V, bV)
                yield
                blks = BLKS[:4] if last else BLKS
                for bi, (t0, n) in enumerate(blks):
                    (pa, bpa), (pb, bpb) = PS[6], PS[7]
                    for (pp, bpp, ww, bww) in ((pa, bpa, wq, b_wq), (pb, bpb, wqs, b_wqs)):
                        for kc in range(2):
                            k.op("pe", nc.tensor.matmul, pp[0:96, 0:n], ww[:, kc, :], cqn[:, kc, t0:t0 + n],
                                 start=(kc == 0), stop=(kc == 1), R=[bww, b_cqn], W=[bpp], sig=(kc == 1))
                    k.op("dve", nc.vector.tensor_scalar, QT[0:64, t0:t0 + n], pa[0:64, 0:n], SCL, None, ALU.mult,
                         R=[bpa], W=[bQT])
                    ta, bta = t1r.next(); tb, btb = t2r.next()
                    ropq, b_ropq = ropq_r.next()
                    k.dma("sp", ropq[64:96, 0, 0:n], c_ropeAq[0, :, t0:t0 + n], W=[b_ropq])
                    k.dma("sp", ropq[64:96, 1, 0:n], c_ropeAq[1, :, t0:t0 + n], W=[b_ropq])
                    k.op("dve", nc.vector.tensor_tensor, ta[64:96, 0:n], pa[64:96, 0:n], ropq[64:96, 0, 0:n],
                         ALU.mult, R=[bpa, b_ropq], W=[bta])
                    k.op("dve", nc.vector.tensor_tensor, tb[64:96, 0:n], pb[64:96, 0:n], ropq[64:96, 1, 0:n],
                         ALU.mult, R=[bpb, b_ropq], W=[btb])
                    k.op("dve", nc.vector.tensor_tensor, QT[64:96, t0:t0 + n], ta[64:96, 0:n], tb[64:96, 0:n],
                         ALU.add, R=[bta, btb], W=[bQT])
                    yield
                for kb in range((NKEY + 511) // 512):
                    c0 = kb * 512
                    n = min(512, NKEY - c0)
                    pk, bpk = PS[6 + kb % 2]
                    k.op("pe", nc.tensor.matmul, pk[0:64, 0:n], wkv[:, 0:64], ckvA[:, c0:c0 + n], start=True, stop=True,
                         R=[b_wkv, b_ckvA], W=[bpk])
                    copy_ps("dve", KT[0:64, c0:c0 + n], pk[0:64, 0:n], R=[bpk], W=[bKT])
                    yield
                for g0 in range(0, NKT, 8):
                    gn = min(8, NKT - g0)
                    pv, bpv = PS[7]
                    for i_ in range(gn):
                        kt = g0 + i_
                        k.op("pe", nc.tensor.matmul, pv[:, i_ * 64:(i_ + 1) * 64], ckvA[:, kt * 128:(kt + 1) * 128],
                             wkv[:, 64:128], start=True, stop=True, R=[b_ckvA, b_wkv], W=[bpv], sig=(i_ == gn - 1))
                    copy_ps("dve", V[:, g0:g0 + gn, 0:64],
                            pv[:, 0:gn * 64].rearrange("p (a b) -> p a b", b=64), R=[bpv], W=[bV])
                    yield

            built = {}
            for _ in build_head(0):
                pass
            for h in range(8):
                QT, bQT, KT, bKT, V, bV = built[h]
                gen = build_head(h + 1) if h + 1 < 8 else iter(())
                step_no = 0
                for (q0, nq, ktiles) in qblocks:
                    po, bpo = oring.next()

                    def issue_s(kt):
                        ps_, bps_ = sring.next()
                        k.op("pe", nc.tensor.matmul, ps_[:, 0:nq], KT[0:96, kt * 128:(kt + 1) * 128], QT[0:96, q0:q0 + nq],
                             start=True, stop=True, R=[bKT, bQT], W=[bps_])
                        return ps_, bps_
                    LA = 3
                    pend = [issue_s(kt_) for kt_ in ktiles[:LA]]
                    for i_, kt in enumerate(ktiles):
                        if i_ + LA < len(ktiles):
                            pend.append(issue_s(ktiles[i_ + LA]))
                        cur = pend.pop(0)
                        P, bP = Pr.next()
                        k.op("act", nc.scalar.activation, P[:, 0:nq], cur[0][:, 0:nq], AF.Exp, R=[cur[1]], W=[bP])
                        lastk = (i_ == len(ktiles) - 1)
                        k.op("pe", nc.tensor.matmul, po[:, 0:nq], V[:, kt, :], P[:, 0:nq], start=(i_ == 0), stop=lastk,
                             R=[bV, bP], W=[bpo], sig=True)
                        step_no += 1
                        if step_no % 6 == 0:
                            next(gen, None)
                    linv, b_linv = linv_r.next()
                    k.op("dve", nc.vector.reciprocal, linv[64:128, 0:nq], po[64:128, 0:nq], R=[bpo], W=[b_linv])
                    r0 = (h % 2) * 64
                    yat, b_yat = yat_r.next()
                    k.op("dve", nc.vector.tensor_tensor, yat[:, 0:nq], po[0:64, 0:nq],
                         linv[64:128, 0:nq], ALU.mult, R=[bpo, b_linv], W=[b_yat])
                    k.dma("sp", yTd[0, h // 2, r0:r0 + 64, q0:q0 + nq], yat[:, 0:nq], R=[b_yat], W=[b_yTd[0]])
                for _ in gen:
                    pass
        k.fence()

    def phase_merge(l, last):
        blks = list(enumerate(BLKS[:4] if last else BLKS))
        with ExitStack() as es:
            def sb(name, shape, dt=F32):
                return es.enter_context(nc.sbuf_tensor(U(name), list(shape), dt)), Buf(name)
            zT, b_zT = sb("zT", [128, 8, T], BF16)
            wo, b_wo = load_w(es, "wo", wview(w_out[l]), [128, 8, D])
            us = UStream(es)
            yb_r = Ring([sb(f"myb{i}", [128, 3, 4, 512], BF16) for i in range(2)])
            g1, b_g1 = sb("g1", [128, 2, D])
            for r in range(2):
                k.dma("sp", g1[:, r, :], modD[r:r + 1, 2 * D:3 * D].partition_broadcast(128), R=[b_modD], W=[b_g1])
            wg_r = Ring([sb(f"mwg{i}", [128, 8, 3, 128], BF16) for i in range(2)])
            wb_r = Ring([sb(f"mwb{i}", [128, 3, 4, 128], BF16) for i in range(2)])
            gj_r = Ring([sb(f"gj{i}", [128, 512]) for i in range(2)])
            za_r = Ring([sb(f"za{i}", [128, 512]) for i in range(2)])
            zt_r = Ring([sb(f"zt{i}", [128, 512]) for i in range(2)])
            pgr = Ring([PS[0], PS[1], PS[2]])
            pzr = Ring([PS[3], PS[4], PS[5]])
            for c in range(8):
                wg3, b_wg3 = wg_r.next(); wb3, b_wb3 = wb_r.next()
                for j in range(3):
                    col = P_BG + j * 1024 + c * 128
                    k.dma("pool", wg3[:, :, j, :], wview(wp[l])[:, :, col:col + 128], W=[b_wg3])
                    k.dma("pool", wb3[:, j, :, :], w_branch[l, j].rearrange("(k4 p) n -> p k4 n", p=128)[:, :, c * 128:(c + 1) * 128],
                          W=[b_wb3])
                for bi, (t0, n) in blks:
                    ub, b_ub, _, _ = us.load(bi)
                    yb3, b_yb3 = yb_r.next()
                    for j in range(3):
                        k.dma("sp", yb3[:, j, :, 0:n], yTd[j, :, :, t0:t0 + n].rearrange("k p t -> p k t"),
                              R=[b_yTd[j]], W=[b_yb3])
                    za, bza = za_r.next()
                    for j in range(3):
                        pg, bpg = pgr.next()
                        for kc in range(8):
                            k.op("pe", nc.tensor.matmul, pg[:, 0:n], wg3[:, kc, j, :], ub[:, kc, 0:n],
                                 start=(kc == 0), stop=(kc == 7), R=[b_wg3, b_ub], W=[bpg], sig=(kc == 7))
                        gj, bgj = gj_r.next()
                        k.op("act", nc.scalar.activation, gj[:, 0:n], pg[:, 0:n], AF.Sigmoid, R=[bpg], W=[bgj])
                        pz, bpz = pzr.next()
                        for k4 in range(4):
                            k.op("pe", nc.tensor.matmul, pz[:, 0:n], wb3[:, j, k4, :], yb3[:, j, k4, 0:n],
                                 start=(k4 == 0), stop=(k4 == 3), R=[b_wb3, b_yb3], W=[bpz], sig=(k4 == 3))
                        if j == 0:
                            k.op("dve", nc.vector.tensor_tensor, za[:, 0:n], pz[:, 0:n], gj[:, 0:n], ALU.mult,
                                 R=[bpz, bgj], W=[bza])
                        else:
                            zt, bzt = zt_r.next()
                            k.op("dve", nc.vector.tensor_tensor, zt[:, 0:n], pz[:, 0:n], gj[:, 0:n], ALU.mult,
                                 R=[bpz, bgj], W=[bzt])
                            if j == 1:
                                k.op("dve", nc.vector.tensor_tensor, za[:, 0:n], za[:, 0:n], zt[:, 0:n], ALU.add,
                                     R=[bzt], W=[bza])
                            else:
                                k.op("dve", nc.vector.tensor_tensor, zT[:, c, t0:t0 + n], za[:, 0:n], zt[:, 0:n], ALU.add,
                                     R=[bza, bzt], W=[b_zT])
            xr = Ring([sb(f"mx{i}", [128, D]) for i in range(4)])
            tmr = Ring([sb(f"mt{i}", [128, 512]) for i in range(2)])
            pyr = Ring([PS[6], PS[7]])
            tiles = list(range(NLT)) if last else list(range(NT))
            xloads = {}

            def issue_x(i_):
                if i_ < len(tiles):
                    t_ = tiles[i_]
                    xt_, b_xt_ = xr.next()
                    k.dma("sp", xt_[:], (xs_in if l == 0 else xs)[t_ * 128:(t_ + 1) * 128, :], R=[b_xs[t_]], W=[b_xt_])
                    xloads[i_] = (xt_, b_xt_)
            issue_x(0); issue_x(1)
            for i_, t in enumerate(tiles):
                r = 0 if t < NLT else 1
                issue_x(i_ + 2)
                xt, b_xt = xloads.pop(i_)
                for half in range(2):
                    py, bpy = pyr.next()
                    for kc in range(8):
                        k.op("pe", nc.tensor.matmul, py[:, :], zT[:, kc, t * 128:(t + 1) * 128],
                             wo[:, kc, half * 512:(half + 1) * 512], start=(kc == 0), stop=(kc == 7),
                             R=[b_zT, b_wo], W=[bpy], sig=(kc == 7))
                    tm, btm = tmr.next()
                    k.op("dve", nc.vector.tensor_tensor, tm[:], py[:, :], g1[:, r, half * 512:(half + 1) * 512], ALU.mult,
                         R=[bpy, b_g1], W=[btm])
                    k.op("dve", nc.vector.tensor_tensor, xt[:, half * 512:(half + 1) * 512], xt[:, half * 512:(half + 1) * 512],
                         tm[:], ALU.add, R=[btm], W=[b_xt])
                k.dma("sp", xs[t * 128:(t + 1) * 128, :], xt[:], R=[b_xt], W=[b_xs[t]])
        k.fence()

    def phase_router(i, tiles):
        with ExitStack() as es:
            def sb(name, shape, dt=F32):
                return es.enter_context(nc.sbuf_tensor(U(name), list(shape), dt)), Buf(name)
            rw, b_rw = load_w(es, "rw", wview(moe_router[i]), [128, 8, NEXP])
            us = UStream(es)
            lg_r = Ring([sb(f"rl{i_}", [128, 8]) for i_ in range(2)])
            m8_r = Ring([sb(f"rm{i_}", [128, 8]) for i_ in range(2)])
            ex_r = Ring([sb(f"re{i_}", [128, 8]) for i_ in range(2)])
            mk_r = Ring([sb(f"rk{i_}", [128, 8]) for i_ in range(2)])
            sc_r = Ring([sb(f"rs{i_}", [128, 4]) for i_ in range(2)])
            ubc = None
            for t in tiles:
                bi = min(t // 4, 4)
                if ubc is None or ubc[0] != bi:
                    ubc = (bi,) + tuple(us.load(bi))
                _, ub, b_ub, t0, n = ubc
                tt = t - t0 // 128
                pl, bpl = PS[t % 2]
                for kc in range(8):
                    k.op("pe", nc.tensor.matmul, pl[:, 0:NEXP], ub[:, kc, tt * 128:(tt + 1) * 128], rw[:, kc, :],
                         start=(kc == 0), stop=(kc == 7), R=[b_ub, b_rw], W=[bpl], sig=(kc == 7))
                lg, blg = lg_r.next(); m8, bm8 = m8_r.next(); ex, bex = ex_r.next(); mk, bmk = mk_r.next()
                sc_, bsc = sc_r.next()
                k.op("dve", nc.vector.tensor_copy, lg[:], pl[:, 0:NEXP], R=[bpl], W=[blg])
                k.op("dve", nc.vector.max, m8[:], lg[:], R=[blg], W=[bm8])
                k.op("dve", nc.vector.tensor_scalar, mk[:], lg[:], m8[:, 1:2], None, ALU.is_ge, R=[blg, bm8], W=[bmk])
                k.op("dve", nc.vector.tensor_scalar, sc_[:, 0:1], m8[:, 0:1], -1.0, None, ALU.mult, R=[bm8], W=[bsc])
                k.op("act", nc.scalar.activation, ex[:], lg[:], AF.Exp, bias=sc_[:, 0:1], R=[blg, bsc], W=[bex])
                k.op("dve", nc.vector.tensor_tensor, ex[:], ex[:], mk[:], ALU.mult, R=[bmk], W=[bex])
                k.op("dve", nc.vector.reduce_sum, sc_[:, 1:2], ex[:], AX.X, R=[bex], W=[bsc])
                k.op("dve", nc.vector.reciprocal, sc_[:, 2:3], sc_[:, 1:2], W=[bsc])
                k.op("dve", nc.vector.tensor_scalar, G8[:, t, :], ex[:], sc_[:, 2:3], None, ALU.mult, R=[bex, bsc], W=[b_G8])
        k.fence()

    def phase_ffn(l, last):
        moe = (l % 2 == 1)
        i = l // 2
        nexp = NEXP if moe else 1
        groups = [(0, 1024), (1024, 1024)] if last else [(0, 1152), (1152, 1152)]
        for (g0, gn) in groups:
            with ExitStack() as es:
                def sb(name, shape, dt=F32):
                    return es.enter_context(nc.sbuf_tensor(U(name), list(shape), dt)), Buf(name)
                vTg, b_vTg = sb("vTg", [128, 8, gn], BF16)
                ublks = sorted(set(min(t_ // 4, 4) for t_ in range(g0 // 128, (g0 + gn) // 128)))
                k.dma("sp", vTg[:], uT[:, :, g0:g0 + gn].rearrange("kc p t -> p kc t"),
                      R=[b_uT[b_] for b_ in ublks], W=[b_vTg])
                hT, b_hT = sb("hT", [128, NFF, gn], BF16)
                acc, b_acc = sb("acc", [128, gn // 128, D])
                wd, b_wd = sb("wd", [128, NFF, D], BF16)
                wt_r = Ring([sb(f"wgu{i_}", [128, 8, 2, 256], BF16) for i_ in range(2)])
                sg_r = Ring([sb(f"sg{i_}", [128, 512]) for i_ in range(3)])
                pgr = Ring([PS[0], PS[1]])
                pur = Ring([PS[2], PS[3]])
                pyr = Ring([PS[4], PS[5], PS[6], PS[7]])
                bsz = 512 if gn % 512 == 0 else 384
                nblks = [(c0, min(bsz, gn - c0)) for c0 in range(0, gn, bsz)]
                def wsrc(e):
                    if moe:
                        return moe_wg[i, e], moe_wu[i, e], moe_wd[i, e]
                    return ffn_wg[i], ffn_wu[i], ffn_wd[i]
                tasks = [(e, fc2) for e in range(nexp) for fc2 in range(NFF // 2)]
                loaded = {}

                def issue_load(ti):
                    if ti >= len(tasks) or ti in loaded:
                        return
                    e_, fc2_ = tasks[ti]
                    Wg_, Wu_, _ = wsrc(e_)
                    wt_, b_wt_ = wt_r.next()
                    k.dma("pool", wt_[:, :, 0, :], wview(Wg_)[:, :, fc2_ * 256:(fc2_ + 1) * 256], W=[b_wt_])
                    k.dma("pool", wt_[:, :, 1, :], wview(Wu_)[:, :, fc2_ * 256:(fc2_ + 1) * 256], W=[b_wt_])
                    loaded[ti] = (wt_, b_wt_)
                issue_load(0)
                for e in range(nexp):
                    Wg, Wu, Wd = wsrc(e)
                    for fc2 in range(NFF // 2):
                        ti = e * (NFF // 2) + fc2
                        issue_load(ti)
                        issue_load(ti + 1)
                        if fc2 == 0:
                            k.dma("pool", wd[:], Wd.rearrange("(f p) n -> p f n", p=128), W=[b_wd])
                        wt, b_wt = loaded.pop(ti)
                        for sub in range(2):
                            fc = fc2 * 2 + sub
                            for (c0, n) in nblks:
                                pg, bpg = pgr.next(); pu, bpu = pur.next()
                                for kc in range(8):
                                    k.op("pe", nc.tensor.matmul, pg[:, 0:n], wt[:, kc, 0, sub * 128:(sub + 1) * 128],
                                         vTg[:, kc, c0:c0 + n], start=(kc == 0), stop=(kc == 7),
                                         R=[b_wt, b_vTg], W=[bpg], sig=(kc == 7))
                                for kc in range(8):
                                    k.op("pe", nc.tensor.matmul, pu[:, 0:n], wt[:, kc, 1, sub * 128:(sub + 1) * 128],
                                         vTg[:, kc, c0:c0 + n], start=(kc == 0), stop=(kc == 7),
                                         R=[b_wt, b_vTg], W=[bpu], sig=(kc == 7))
                                sg, bsg = sg_r.next()
                                k.op("act", nc.scalar.activation, sg[:, 0:n], pg[:, 0:n], AF.Silu, R=[bpg], W=[bsg])
                                k.op("dve", nc.vector.tensor_tensor, hT[:, fc, c0:c0 + n], sg[:, 0:n], pu[:, 0:n], ALU.mult,
                                     R=[bsg, bpu], W=[b_hT])
                    for tt in range(gn // 128):
                        t = g0 // 128 + tt
                        for half in range(2):
                            py, bpy = pyr.next()
                            for fc in range(NFF):
                                k.op("pe", nc.tensor.matmul, py[:, :], hT[:, fc, tt * 128:(tt + 1) * 128],
                                     wd[:, fc, half * 512:(half + 1) * 512], start=(fc == 0), stop=(fc == NFF - 1),
                                     R=[b_hT, b_wd], W=[bpy], sig=(fc == NFF - 1))
                            dst = acc[:, tt, half * 512:(half + 1) * 512]
                            if not moe:
                                copy_ps(evac_engine(), dst, py[:, :], R=[bpy], W=[b_acc])
                            elif e == 0:
                                k.op("dve", nc.vector.tensor_scalar, dst, py[:, :], G8[:, t, e:e + 1], None, ALU.mult,
                                     R=[bpy, b_G8], W=[b_acc])
                            else:
                                k.op("dve", nc.vector.scalar_tensor_tensor, dst, py[:, :], G8[:, t, e:e + 1], dst,
                                     ALU.mult, ALU.add, R=[bpy, b_G8], W=[b_acc])
                xr = Ring([sb(f"fx{i_}", [128, D]) for i_ in range(3)])
                jr = Ring([sb(f"fj{i_}", [128, D]) for i_ in range(2)])
                ssr = Ring([sb(f"fs{i_}", [128, 1]) for i_ in range(2)])
                g2, b_g2 = sb("g2", [128, 2, D])
                for rg_ in range(1 if last else 2):
                    k.dma("sp", g2[:, rg_, :], modD[rg_:rg_ + 1, 5 * D:6 * D].partition_broadcast(128), R=[b_modD], W=[b_g2])
                if last:
                    fnw, b_fnw = sb("fnw", [128, D])
                    k.dma("sp", fnw[:], final_norm_w.rearrange("(o d) -> o d", o=1).partition_broadcast(128), W=[b_fnw])
                xloads = {}

                def issue_x(tt_):
                    if tt_ < gn // 128:
                        t_ = g0 // 128 + tt_
                        xt_, b_xt_ = xr.next()
                        k.dma("sp", xt_[:], xs[t_ * 128:(t_ + 1) * 128, :], R=[b_xs[t_]], W=[b_xt_])
                        xloads[tt_] = (xt_, b_xt_)
                issue_x(0); issue_x(1)
                for tt in range(gn // 128):
                    t = g0 // 128 + tt
                    r = 0 if t < NLT else 1
                    issue_x(tt + 2)
                    xt, b_xt = xloads.pop(tt)
                    k.op("dve", nc.vector.tensor_tensor, acc[:, tt, :], acc[:, tt, :], g2[:, r, :], ALU.mult,
                         R=[b_g2], W=[b_acc])
                    k.op("dve", nc.vector.tensor_tensor, xt[:], xt[:], acc[:, tt, :], ALU.add, R=[b_acc], W=[b_xt])
                    if not last:
                        k.dma("sp", xs[t * 128:(t + 1) * 128, :], xt[:], R=[b_xt], W=[b_xs[t]])
                    else:
                        jk, b_jk = jr.next(); ss, b_ss = ssr.next()
                        k.op("act", nc.scalar.activation, jk[:], xt[:], AF.Square, accum_out=ss[:, 0:1],
                             R=[b_xt], W=[b_jk, b_ss])
                        rstd, b_rstd = rsqrt_col(es, f"fr{t}", ss[:, 0:1], b_ss, 1.0 / D)
                        k.op("dve", nc.vector.scalar_tensor_tensor, jk[:], xt[:], rstd[:, 0:1], fnw[:], ALU.mult, ALU.mult,
                             R=[b_xt, b_rstd, b_fnw], W=[b_jk])
                        k.dma("sp", out[t * 128:(t + 1) * 128, :], jk[:], R=[b_jk], W=[b_out])
            k.fence()

    stop = dbg.get("_stop") if isinstance(dbg, dict) else None

    def tap(name, src_ap, bufs):
        if name in dbg_out:
            k.dma("sp", dbg_out[name], src_ap, R=bufs, W=[b_out])

    for l in range(nlayers):
        last = (l == DEPTH - 1)
        alltiles = list(range(NT))
        phase_mod(l)
        phase_norm(l, A1, b_A1, 0, alltiles, xsrc=(xs_in if l == 0 else xs))
        phase_lg(l)
        with ExitStack() as les:
            for nm, shp in (("cqn", [128, 2, T]), ("ckvn", [128, T]), ("krr", [32, T]), ("gzT", [32, T])):
                LS[nm] = (les.enter_context(nc.sbuf_tensor(U(nm), shp, BF16)), Buf(nm))
            phase_q(l)
            phase_linear(l, 1, last)
            phase_linear(l, 2, last)
            phase_attn(l, last)
        k.fence()
        phase_merge(l, last)
        ftiles = list(range(NLT)) if last else alltiles
        phase_norm(l, A2, b_A2, 24, ftiles)
        if l % 2 == 1:
            phase_router(l // 2, ftiles)
        phase_ffn(l, last)
    if nlayers < DEPTH:
        for t in range(NLT):
            k.dma("sp", out[t * 128:(t + 1) * 128, :], xs[t * 128:(t + 1) * 128, :], R=[b_xs[t]], W=[b_out])
    k.wait_bufs("sp", [b_out])
    return nc, k


IN_SPLITS = (256, 128, 32, 256, 256, 512, 512, 256, 256, 512, 512, 32, 3072)
OFFS = np.concatenate([[0], np.cumsum(IN_SPLITS)]).astype(int)
(O_CQ, O_CKV, O_KR, O_RQ, O_RK, O_RV, O_RG, O_GQ, O_GK, O_GV, O_GR, O_GZ, O_BG) = OFFS[:13]


def _partner(n, half):
    idx = np.arange(n)
    return np.where(idx % (2 * half) < half, idx + half, idx - half)


def _pack_w_in(w_in_l):
    cols = []
    pa = _partner(32, 8)
    cols.append(np.arange(O_CQ, O_CQ + 256))
    cols.append(np.arange(O_CKV, O_CKV + 128))
    cols.append(np.arange(O_KR, O_KR + 32))
    cols.append(O_KR + pa)
    cols.append(np.arange(O_GZ, O_GZ + 32))
    pr = _partner(64, 32)
    for h in range(4):
        q = O_RQ + h * 64 + np.arange(64)
        qs = O_RQ + h * 64 + pr
        kk = O_RK + h * 64 + np.arange(64)
        ks = O_RK + h * 64 + pr
        cols += [q, q, qs, qs, kk, kk, ks, ks]
    for h in range(4):
        q = O_GQ + h * 64 + np.arange(64)
        kk = O_GK + h * 64 + np.arange(64)
        cols += [q, q, kk, kk]
    cols.append(np.arange(O_RV, O_RV + 512))
    cols.append(np.arange(O_GV, O_GV + 512))
    cols.append(np.arange(O_RG, O_RG + 512))
    cols.append(np.arange(O_GR, O_GR + 512))
    cols.append(np.arange(O_BG, O_BG + 3072))
    cols = np.concatenate(cols)
    assert cols.shape[0] == NPACK
    return np.ascontiguousarray(w_in_l[:, cols])


def _rope_tables(pos, half, signed_rows):
    inv = 10000.0 ** (-np.arange(half, dtype=np.float32) / half)
    ang = pos.astype(np.float32)[None, :] * inv[:, None]
    cos = np.cos(ang).astype(np.float32)
    sin = np.sin(ang).astype(np.float32)
    return np.concatenate([cos, cos], 0), np.concatenate([-sin, sin], 0)


def _consts(q):
    pos = q * LAT + np.arange(LAT)
    c, s = _rope_tables(pos, 32, True)
    ropeR = np.zeros((2, 128, T), np.float32)
    ropeR[0, :, :LAT] = np.concatenate([c, c], 0)
    ropeR[1, :, :LAT] = np.concatenate([s, s], 0)
    ropeR[0, :, LAT:] = 1.0
    cr, sr = _rope_tables(pos // 64, 8, True)
    cc, sc = _rope_tables(pos % 64, 8, True)
    ak = np.zeros((2, 32, T), np.float32)
    ak[0, :, :LAT] = np.concatenate([cr, cc], 0)
    ak[1, :, :LAT] = np.concatenate([sr, sc], 0)
    ak[0, :, LAT:] = 1.0
    aq = (ak * np.float32(96.0 ** -0.5)).astype(np.float32)
    rst = np.ones((128, T), np.float32)
    rst[:, ::128] = 0.0
    j = np.arange(128)[:, None]
    i = np.arange(128)[None, :]
    mask = np.stack([(i >= j), (j >= i)]).astype(np.float32)
    onehot = np.zeros((128, 4), np.float32)
    onehot[:, q] = 1.0
    return dict(c_ropeR=ropeR, c_ropeAq=aq, c_ropeAk=ak, c_rst=rst, c_mask=mask,
                c_ident=np.eye(128, dtype=np.float32), c_onehot=onehot)


def make_in_maps(inp):
    f = lambda a: np.ascontiguousarray(np.asarray(a, dtype=np.float32))
    x, c, ctx, c_ctx = f(inp["x"]), f(inp["c"]), f(inp["ctx"]), f(inp["c_ctx"])
    w_in = f(inp["w_in"])
    wp_ = np.stack([_pack_w_in(w_in[l]) for l in range(DEPTH)])
    w_uq = f(inp["mla_w_uq"])
    pa = _partner(32, 8)
    cols = np.arange(768)
    for h in range(8):
        cols[h * 96 + 64:h * 96 + 96] = h * 96 + 64 + pa
    w_uq_sw = np.ascontiguousarray(w_uq[:, :, cols])
    gw = f(inp["gla_w_gate"])
    gb = f(inp["gla_b_gate"])
    gwblk = np.zeros((DEPTH, 4, 32, 128), np.float32)
    gbias = np.zeros((DEPTH, 4, 128), np.float32)
    for h in range(4):
        gwblk[:, h, 0:16, 0:64] = gw[:, 0, :, h * 64:(h + 1) * 64]
        gwblk[:, h, 16:32, 64:128] = gw[:, 1, :, h * 64:(h + 1) * 64]
        gbias[:, h, 0:64] = gb[:, 0, h * 64:(h + 1) * 64]
        gbias[:, h, 64:128] = gb[:, 1, h * 64:(h + 1) * 64]
    shared = dict(
        mod_w=f(inp["mod_w"]), mod_b=f(inp["mod_b"]), norm1_w=f(inp["norm1_w"]), norm2_w=f(inp["norm2_w"]),
        wp=wp_, mla_q_norm=f(inp["mla_q_norm"]), w_uq=w_uq, w_uq_sw=w_uq_sw, mla_kv_norm=f(inp["mla_kv_norm"]),
        w_ukv=f(inp["mla_w_ukv"]), ret_decay_logit=f(inp["ret_decay_logit"]), ret_norm_w=f(inp["ret_norm_w"]),
        gwblk=gwblk, gbias=gbias, gla_norm_w=f(inp["gla_norm_w"]), w_branch=f(inp["w_branch"]), w_out=f(inp["w_out"]),
        ffn_w_gate=f(inp["ffn_w_gate"]), ffn_w_up=f(inp["ffn_w_up"]), ffn_w_down=f(inp["ffn_w_down"]),
        moe_router=f(inp["moe_router"]), moe_w_gate=f(inp["moe_w_gate"]), moe_w_up=f(inp["moe_w_up"]),
        moe_w_down=f(inp["moe_w_down"]), final_norm_w=f(inp["final_norm_w"]),
    )
    maps = []
    for core in range(NCORES):
        b, q = core // 4, core % 4
        m = dict(shared)
        m["xs_in"] = np.ascontiguousarray(np.concatenate([x[b, q * LAT:(q + 1) * LAT], ctx[b]], 0))
        m["cvecT"] = np.ascontiguousarray(np.stack([c[b], c_ctx], 1))
        m.update(_consts(q))
        maps.append(m)
    return maps


_PROG = {}


def kernel(**inputs):
    if "nc" not in _PROG:
        _PROG["nc"] = build_program()[0]
    nc = _PROG["nc"]
    maps = make_in_maps(inputs)
    res = run_bass_kernel_spmd(nc, maps, core_ids=list(range(NCORES)))
    outp = np.zeros((2, SEQ, D), np.float32)
    for core in range(NCORES):
        b, q = core // 4, core % 4
        outp[b, q * LAT:(q + 1) * LAT] = res.results[core]["out"]
    return outp
```
